# Optimizing a Trainium2 kernel written in Bass

```python
import math
import jax, jax.numpy as jnp
from jax import lax
import numpy as np

D_MODEL = 1024
BATCH = 16
SEQ = 4096
DEPTH = 4

HALF = D_MODEL // 2
DA_QK_DIM = 64
DA_V_DIM = 2 * DA_QK_DIM
DA_HEADS = HALF // DA_V_DIM
DA_BLOCK = 128
SG_CHUNK = 128
SG_GROUP_DIM = 128
SG_GROUPS = HALF // SG_GROUP_DIM
ML_HEAD_DIM = 128
ML_HEADS = HALF // ML_HEAD_DIM
ML_CHUNK = 64
ML_CONV = 4
RW_HEAD_DIM = 64
RW_HEADS = HALF // RW_HEAD_DIM
RW_DECAY_LORA = 64
RW_AAA_LORA = 64
RW_GATE_LORA = 128
RW_GN_EPS = 64e-5
_FF_RAW = -(-8 * D_MODEL // 3)
D_FF = -(-_FF_RAW // 256) * 256
NORM_EPS = 1e-6

EVEN_SPLITS = [HALF, HALF, HALF, HALF, HALF]
ML_SPLITS = [HALF, HALF, HALF, ML_HEADS, ML_HEADS, HALF]
RW_SPLITS = [HALF, HALF, HALF, RW_DECAY_LORA, RW_AAA_LORA, RW_GATE_LORA]
EVEN_IN = sum(EVEN_SPLITS)
ML_COLS = sum(ML_SPLITS)
RW_COLS = sum(RW_SPLITS)
ODD_IN = ML_COLS + RW_COLS

kernel_name = "hybrid_diffattn_gmlp_mlstm_rwkv7_trunk"

F32 = jnp.float32


def _split(x, sizes):
    bounds = np.cumsum(sizes)[:-1].tolist()
    return jnp.split(x, bounds, axis=-1)


def rms_norm(x, g, eps=NORM_EPS):
    xf = x.astype(F32)
    return xf * lax.rsqrt(jnp.mean(xf * xf, -1, keepdims=True) + eps) * g.astype(F32)


def _standardize(x, eps):
    xf = x.astype(F32)
    mu = jnp.mean(xf, -1, keepdims=True)
    var = jnp.mean(jnp.square(xf - mu), -1, keepdims=True)
    return (xf - mu) * lax.rsqrt(var + eps)


def causal_conv(x, w, b):
    K, S = w.shape[0], x.shape[1]
    xp = jnp.pad(x, ((0, 0), (K - 1, 0), (0, 0)))
    y = b.astype(F32)
    for j in range(K):
        y = y + xp[:, j:j + S] * w[j].astype(F32)
    return y


def token_shift(p, mu):
    prev = jnp.pad(p, ((0, 0), (1, 0), (0, 0)))[:, :-1]
    return p + (prev - p) * mu.astype(F32)


def diff_attention(q, k, v, q_g, k_g, lam_p, subln_g, lam_init):
    B, S, _ = q.shape
    q = rms_norm(q.reshape(B, S, DA_HEADS, 2, DA_QK_DIM), q_g)
    k = rms_norm(k.reshape(B, S, DA_HEADS, 2, DA_QK_DIM), k_g)
    q = q.transpose(0, 2, 3, 1, 4) * (DA_QK_DIM ** -0.5)
    k = k.transpose(0, 2, 3, 1, 4)
    v = v.astype(F32).reshape(B, S, DA_HEADS, DA_V_DIM).transpose(0, 2, 1, 3)
    lam_p = lam_p.astype(F32)
    lam = jnp.exp(jnp.dot(lam_p[0], lam_p[1])) - jnp.exp(jnp.dot(lam_p[2], lam_p[3])) + lam_init
    outs = []
    for blk in range(S // DA_BLOCK):
        q0 = blk * DA_BLOCK
        kend = q0 + DA_BLOCK
        s = jnp.einsum('bhcqd,bhckd->bhcqk', q[:, :, :, q0:kend], k[:, :, :, :kend])
        mask = jnp.arange(kend)[None, :] <= (q0 + jnp.arange(DA_BLOCK))[:, None]
        p = jax.nn.softmax(jnp.where(mask, s, -jnp.inf), axis=-1)
        a = p[:, :, 0] - lam * p[:, :, 1]
        outs.append(jnp.einsum('bhqk,bhkd->bhqd', a, v[:, :, :kend]))
    o = jnp.concatenate(outs, axis=2)
    o = rms_norm(o, subln_g) * (1.0 - lam_init)
    return o.transpose(0, 2, 1, 3).reshape(B, S, HALF)


def spatial_gating(u, v, ln_g, ln_b, w_s, b_s):
    B, S, _ = u.shape
    nC = S // SG_CHUNK
    u = jax.nn.gelu(u.astype(F32)).reshape(B, nC, SG_CHUNK, SG_GROUPS, SG_GROUP_DIM)
    v = jax.nn.gelu(v.astype(F32)).reshape(B, S, SG_GROUPS, SG_GROUP_DIM)
    v = _standardize(v, NORM_EPS) * ln_g.astype(F32) + ln_b.astype(F32)
    v = v.reshape(B, nC, SG_CHUNK, SG_GROUPS, SG_GROUP_DIM)
    w = jnp.tril(w_s.astype(F32))
    s = jnp.einsum('gts,bcsgd->bctgd', w, v) + b_s.astype(F32).T[None, None, :, :, None]
    return (u * s).reshape(B, S, HALF)


def _mlstm_chunkwise(q, k, v, ig, lf):
    B, H, S, D = q.shape
    L = ML_CHUNK
    nC = S // L

    def to_chunks(t):
        return jnp.moveaxis(t.reshape((B, H, nC, L) + t.shape[3:]), 2, 0)

    xs = (to_chunks(q), to_chunks(k), to_chunks(v), to_chunks(ig), to_chunks(lf))
    causal = jnp.tril(jnp.ones((L, L), bool))

    def step(carry, inp):
        C, n, m = carry
        qc, kc, vc, ic, fc = inp
        b = jnp.cumsum(fc, axis=-1)
        dmat = jnp.where(causal, b[..., :, None] - b[..., None, :] + ic[..., None, :], -jnp.inf)
        m_inter = b + m[..., None]
        m_t = jnp.maximum(m_inter, jnp.max(dmat, -1))
        p = jnp.einsum('bhtd,bhsd->bhts', qc, kc) * jnp.exp(dmat - m_t[..., None])
        scale = jnp.exp(m_inter - m_t)
        num = scale[..., None] * jnp.einsum('bhvk,bhtk->bhtv', C, qc) + jnp.einsum('bhts,bhsv->bhtv', p, vc)
        den = scale * jnp.einsum('bhk,bhtk->bht', n, qc) + jnp.sum(p, -1)
        h = num / jnp.maximum(jnp.abs(den), jnp.exp(-m_t))[..., None]
        bL = b[..., -1]
        g = bL[..., None] - b + ic
        m_new = jnp.maximum(bL + m, jnp.max(g, -1))
        wgt = jnp.exp(g - m_new[..., None])
        decay = jnp.exp(bL + m - m_new)
        C = decay[..., None, None] * C + jnp.einsum('bhsv,bhsk->bhvk', vc * wgt[..., None], kc)
        n = decay[..., None] * n + jnp.einsum('bhs,bhsk->bhk', wgt, kc)
        return (C, n, m_new), h

    init = (jnp.zeros((B, H, D, D), F32), jnp.zeros((B, H, D), F32), jnp.zeros((B, H), F32))
    _, hs = lax.scan(step, init, xs)
    return jnp.moveaxis(hs, 0, 2).reshape(B, H, S, D)


def mlstm(q, k, v, i_pre, f_pre, o_pre, conv_w, conv_b, gate_b, norm_g):
    B, S, _ = q.shape
    qk = jax.nn.silu(causal_conv(jnp.concatenate([q, k], -1).astype(F32), conv_w, conv_b))
    q, k = qk[..., :HALF], qk[..., HALF:]

    def heads(t):
        return t.astype(F32).reshape(B, S, ML_HEADS, ML_HEAD_DIM).transpose(0, 2, 1, 3)

    gate_b = gate_b.astype(F32)
    ig = (i_pre.astype(F32) + gate_b[:ML_HEADS]).transpose(0, 2, 1)
    lf = jax.nn.log_sigmoid(f_pre.astype(F32) + gate_b[ML_HEADS:]).transpose(0, 2, 1)
    h = _mlstm_chunkwise(heads(q), heads(k) * (ML_HEAD_DIM ** -0.5), heads(v), ig, lf)
    h = _standardize(h.transpose(0, 2, 1, 3), NORM_EPS) * norm_g.astype(F32).reshape(ML_HEADS, ML_HEAD_DIM)
    return h.reshape(B, S, HALF) * jax.nn.sigmoid(o_pre.astype(F32))


def rwkv7(r, k, v, wd, ad, gd, w0, w2, a0, a2, g2, k_k, k_a, r_k, ln_g, ln_b):
    B, S, _ = r.shape
    H, N = RW_HEADS, RW_HEAD_DIM
    w = -jax.nn.softplus(-(w0.astype(F32) + jnp.tanh(wd) @ w2.astype(F32))) - 0.5
    decay = jnp.exp(-jnp.exp(w))
    a = jax.nn.sigmoid(a0.astype(F32) + ad @ a2.astype(F32))
    g = jax.nn.sigmoid(gd) @ g2.astype(F32)
    kk = (k * k_k.astype(F32)).reshape(B, S, H, N)
    kk = kk / jnp.maximum(jnp.sqrt(jnp.sum(kk * kk, -1, keepdims=True)), 1e-12)
    k = k * (1.0 + (a - 1.0) * k_a.astype(F32))

    def hd(t):
        return t.reshape(B, S, H, N)

    r_h, k_h, v_h, a_h = hd(r), hd(k), hd(v), hd(a)
    b_h = kk * a_h

    def tm(t):
        return jnp.moveaxis(t, 1, 0)

    xs = (tm(r_h), tm(hd(decay)), tm(k_h), tm(v_h), tm(kk), tm(b_h))

    def step(st, inp):
        r_t, w_t, k_t, v_t, kk_t, b_t = inp
        sa = jnp.einsum('bhvk,bhk->bhv', st, -kk_t)
        st = st * w_t[:, :, None, :] + sa[..., None] * b_t[:, :, None, :] + v_t[..., None] * k_t[:, :, None, :]
        return st, jnp.einsum('bhvk,bhk->bhv', st, r_t)

    _, y = lax.scan(step, jnp.zeros((B, H, N, N), F32), xs)
    y = jnp.moveaxis(y, 0, 1)
    y = _standardize(y, RW_GN_EPS) * ln_g.astype(F32).reshape(H, N) + ln_b.astype(F32).reshape(H, N)
    y = y + jnp.sum(r_h * k_h * r_k.astype(F32), -1, keepdims=True) * v_h
    return y.reshape(B, S, HALF) * g


def even_mixer(h, w_in, w_out, q_g, k_g, lam_p, subln_g, ln_g, ln_b, w_s, b_s, lam_init):
    q, k, v, u, vg = _split(h @ w_in, EVEN_SPLITS)
    a_out = diff_attention(q, k, v, q_g, k_g, lam_p, subln_g, lam_init)
    b_out = spatial_gating(u, vg, ln_g, ln_b, w_s, b_s)
    return jnp.concatenate([a_out, b_out], -1).astype(h.dtype) @ w_out


def odd_mixer(h, w_in, w_out, conv_w, conv_b, gate_b, ml_norm_g, mu, w0, w2, a0, a2, g2,
              k_k, k_a, r_k, ln_g, ln_b):
    p = h @ w_in
    q, k, v, ip, fp, op = _split(p[..., :ML_COLS], ML_SPLITS)
    c_out = mlstm(q, k, v, ip, fp, op, conv_w, conv_b, gate_b, ml_norm_g)
    r, kr, vr, wd, ad, gd = _split(token_shift(p[..., ML_COLS:].astype(F32), mu), RW_SPLITS)
    d_out = rwkv7(r, kr, vr, wd, ad, gd, w0, w2, a0, a2, g2, k_k, k_a, r_k, ln_g, ln_b)
    return jnp.concatenate([c_out, d_out], -1).astype(h.dtype) @ w_out


def swiglu(h, w_gate, w_up, w_down):
    return (jax.nn.silu(h @ w_gate) * (h @ w_up)) @ w_down


def setup_inputs(seed: int = 0) -> dict:
    key = jax.random.key(seed)
    keys = list(jax.random.split(key, 40))
    NE, NO = (DEPTH + 1) // 2, DEPTH // 2

    def nrm(shape, scale):
        return jax.random.normal(keys.pop(), shape, F32) * scale

    def uni(shape, lo, hi):
        return jax.random.uniform(keys.pop(), shape, F32, lo, hi)

    return {
        "x": nrm((BATCH, SEQ, D_MODEL), 1.0),
        "norm_mix_g": 1.0 + nrm((DEPTH, D_MODEL), 0.02),
        "norm_ffn_g": 1.0 + nrm((DEPTH, D_MODEL), 0.02),
        "ev_w_in": nrm((NE, D_MODEL, EVEN_IN), D_MODEL ** -0.5),
        "ev_w_out": nrm((NE, 2 * HALF, D_MODEL), (2 * HALF) ** -0.5),
        "da_q_g": 1.0 + nrm((NE, DA_QK_DIM), 0.02),
        "da_k_g": 1.0 + nrm((NE, DA_QK_DIM), 0.02),
        "da_lambda": nrm((NE, 4, DA_QK_DIM), 0.1),
        "da_subln_g": 1.0 + nrm((NE, DA_V_DIM), 0.02),
        "sg_ln_g": 1.0 + nrm((NE, SG_GROUPS, SG_GROUP_DIM), 0.02),
        "sg_ln_b": nrm((NE, SG_GROUPS, SG_GROUP_DIM), 0.02),
        "sg_w": nrm((NE, SG_GROUPS, SG_CHUNK, SG_CHUNK), SG_CHUNK ** -0.5),
        "sg_b": 1.0 + nrm((NE, SG_GROUPS, SG_CHUNK), 0.1),
        "od_w_in": nrm((NO, D_MODEL, ODD_IN), D_MODEL ** -0.5),
        "od_w_out": nrm((NO, 2 * HALF, D_MODEL), (2 * HALF) ** -0.5),
        "ml_conv_w": nrm((NO, ML_CONV, 2 * HALF), ML_CONV ** -0.5),
        "ml_conv_b": nrm((NO, 2 * HALF), 0.02),
        "ml_gate_b": jnp.concatenate([nrm((NO, ML_HEADS), 0.1), uni((NO, ML_HEADS), 3.0, 6.0)], -1),
        "ml_norm_g": 1.0 + nrm((NO, HALF), 0.02),
        "rw_mu": uni((NO, RW_COLS), 0.0, 1.0),
        "rw_w0": uni((NO, HALF), -6.0, 0.0),
        "rw_w2": nrm((NO, RW_DECAY_LORA, HALF), 0.5 * RW_DECAY_LORA ** -0.5),
        "rw_a0": nrm((NO, HALF), 0.1),
        "rw_a2": nrm((NO, RW_AAA_LORA, HALF), 0.5 * RW_AAA_LORA ** -0.5),
        "rw_g2": nrm((NO, RW_GATE_LORA, HALF), RW_GATE_LORA ** -0.5),
        "rw_k_k": 0.85 + nrm((NO, HALF), 0.05),
        "rw_k_a": 1.0 + nrm((NO, HALF), 0.05),
        "rw_r_k": nrm((NO, RW_HEADS, RW_HEAD_DIM), 0.1),
        "rw_ln_g": 1.0 + nrm((NO, HALF), 0.02),
        "rw_ln_b": nrm((NO, HALF), 0.02),
        "ffn_w_gate": nrm((DEPTH, D_MODEL, D_FF), D_MODEL ** -0.5),
        "ffn_w_up": nrm((DEPTH, D_MODEL, D_FF), D_MODEL ** -0.5),
        "ffn_w_down": nrm((DEPTH, D_FF, D_MODEL), D_FF ** -0.5),
    }


def reference(x, norm_mix_g, norm_ffn_g, ev_w_in, ev_w_out, da_q_g, da_k_g, da_lambda, da_subln_g,
              sg_ln_g, sg_ln_b, sg_w, sg_b, od_w_in, od_w_out, ml_conv_w, ml_conv_b, ml_gate_b,
              ml_norm_g, rw_mu, rw_w0, rw_w2, rw_a0, rw_a2, rw_g2, rw_k_k, rw_k_a, rw_r_k,
              rw_ln_g, rw_ln_b, ffn_w_gate, ffn_w_up, ffn_w_down):
    for layer in range(DEPTH):
        h = rms_norm(x, norm_mix_g[layer]).astype(x.dtype)
        if layer % 2 == 0:
            e = layer // 2
            lam_init = 0.8 - 0.6 * math.exp(-0.3 * layer)
            mix = even_mixer(h, ev_w_in[e], ev_w_out[e], da_q_g[e], da_k_g[e], da_lambda[e],
                             da_subln_g[e], sg_ln_g[e], sg_ln_b[e], sg_w[e], sg_b[e], lam_init)
        else:
            o = layer // 2
            mix = odd_mixer(h, od_w_in[o], od_w_out[o], ml_conv_w[o], ml_conv_b[o], ml_gate_b[o],
                            ml_norm_g[o], rw_mu[o], rw_w0[o], rw_w2[o], rw_a0[o], rw_a2[o], rw_g2[o],
                            rw_k_k[o], rw_k_a[o], rw_r_k[o], rw_ln_g[o], rw_ln_b[o])
        x = x + mix.astype(x.dtype)
        h = rms_norm(x, norm_ffn_g[layer]).astype(x.dtype)
        x = x + swiglu(h, ffn_w_gate[layer], ffn_w_up[layer], ffn_w_down[layer]).astype(x.dtype)
    return x
```

```python
import math
from contextlib import ExitStack
import numpy as np
import ml_dtypes
import concourse.bass as bass
import concourse.mybir as mybir
from concourse.bass_utils import run_bass_kernel_spmd

F32 = mybir.dt.float32
BF16 = mybir.dt.bfloat16
AF = mybir.ActivationFunctionType
ALU = mybir.AluOpType
AX = mybir.AxisListType

D = 1024
DFF = 2816
NFF = DFF // 128
EPOCH = 30000
NSLOT = 24
EPS = 1e-6


class Prog:
    def __init__(self, nc):
        self.nc = nc
        self.E = {'pe': nc.tensor, 'act': nc.scalar, 'dve': nc.vector, 'pool': nc.gpsimd, 'sp': nc.sync}
        self.cnt = {e: 0 for e in ('pe', 'act', 'dve', 'pool')}
        self.csem = {e: [] for e in self.cnt}
        self.known = {e: {} for e in self.E}
        self.dsem = {}
        self.dgen = [0] * NSLOT
        self.dval = [0] * NSLOT
        for i in range(NSLOT):
            self.dsem[(i, 0)] = nc.alloc_semaphore(f"dq{i}_0")
        self.rr = 0
        self.W = {}
        self.R = {}
        self.uid = 0

    def uname(self, s):
        self.uid += 1
        return f"{s}_{self.uid}"

    def _wait(self, e, tok):
        if tok[0] == 'c':
            _, e2, seq = tok
            kk = ('c', e2)
            if self.known[e].get(kk, -1) >= seq:
                return
            ep = seq // EPOCH
            self.E[e].wait_ge(self.csem[e2][ep], seq - ep * EPOCH + 1)
            self.known[e][kk] = seq
        else:
            _, slot, gen, val = tok
            kk = ('d', slot, gen)
            if self.known[e].get(kk, 0) >= val:
                return
            self.E[e].wait_ge(self.dsem[(slot, gen)], val)
            self.known[e][kk] = val

    def _deps(self, e, reads, writes):
        for k in reads:
            t = self.W.get(k)
            if t is not None:
                if t[0] == 'c' and t[1] == e and e == 'pe':
                    continue
                self._wait(e, t)
        for k in writes:
            t = self.W.get(k)
            if t is not None and not (t[0] == 'c' and t[1] == e):
                self._wait(e, t)
            for t in self.R.get(k, {}).values():
                if t[0] == 'c' and t[1] == e:
                    continue
                self._wait(e, t)

    def _record(self, tok, src, reads, writes):
        for k in reads:
            self.R.setdefault(k, {})[src] = tok
        for k in writes:
            self.W[k] = tok
            self.R[k] = {}

    def op(self, e, fn, reads=(), writes=()):
        self._deps(e, reads, writes)
        ins = fn(self.E[e])
        seq = self.cnt[e]
        self.cnt[e] += 1
        ep = seq // EPOCH
        while len(self.csem[e]) <= ep:
            self.csem[e].append(self.nc.alloc_semaphore(f"c_{e}_{len(self.csem[e])}"))
        ins.then_inc(self.csem[e][ep], 1)
        self._record(('c', e, seq), ('c', e), reads, writes)
        return ins

    def dma(self, q, out, in_, reads=(), writes=(), **kw):
        slot = self.rr
        self.rr = (self.rr + 1) % NSLOT
        if self.dval[slot] > 60000:
            self._wait(q, ('d', slot, self.dgen[slot], self.dval[slot]))
            self.dgen[slot] += 1
            self.dval[slot] = 0
            self.dsem[(slot, self.dgen[slot])] = self.nc.alloc_semaphore(f"dq{slot}_{self.dgen[slot]}")
        gen = self.dgen[slot]
        if self.dval[slot] > 0:
            self._wait(q, ('d', slot, gen, self.dval[slot]))
        self._deps(q, reads, writes)
        ins = self.E[q].dma_start(out=out, in_=in_, **kw)
        self.dval[slot] += 16
        ins.then_inc(self.dsem[(slot, gen)], 16)
        self._record(('d', slot, gen, self.dval[slot]), ('d', slot), reads, writes)
        return ins

    def barrier(self):
        toks = [('c', e, self.cnt[e] - 1) for e in self.cnt if self.cnt[e] > 0]
        toks += [('d', s, self.dgen[s], self.dval[s]) for s in range(NSLOT) if self.dval[s] > 0]
        for e in self.E:
            for t in toks:
                self._wait(e, t)
        self.W.clear()
        self.R.clear()


class Ctx:
    def __init__(self, P):
        self.P = P
        self.st = ExitStack()

    def __enter__(self):
        self.st.__enter__()
        return self

    def __exit__(self, *a):
        self.P.barrier()
        return self.st.__exit__(*a)

    def sb(self, name, shape, dt):
        return self.st.enter_context(self.P.nc.sbuf_tensor(self.P.uname(name), list(shape), dt))

    def ps(self, name, shape, dt=F32):
        return self.st.enter_context(self.P.nc.psum_tensor(self.P.uname(name), list(shape), dt))


NCONST = 128 * 3 + 2048


def host_consts():
    cm = np.zeros((128, NCONST), np.float32)
    cm[:, 0:128] = np.eye(128)
    blk = np.zeros((128, 128), np.float32)
    blk[:64, :64] = 1
    blk[64:, 64:] = 1
    cm[:, 128:256] = blk
    k = np.arange(128)[:, None]
    cm[:, 256:384] = (k <= np.arange(128)[None, :])
    q = np.arange(512)[None, :]
    for r in range(4):
        cm[:, 384 + r * 512:384 + (r + 1) * 512] = (128 * r + k <= q)
    return cm


def load_consts(P, c, cd):
    K = {}
    cst = c.sb('cst', [128, 384], F32)
    P.dma('sp', cst[:], cd[:, 0:384], writes=['cst'])
    K['idf'] = cst[:, 0:128]
    K['triu'] = cst[:, 256:384]
    K['idb'] = c.sb('idb', [128, 128], BF16)
    K['blk64'] = c.sb('blk64', [128, 128], BF16)
    K['ones_bf'] = c.sb('ones_bf', [128, 128], BF16)
    K['cmask'] = c.sb('cmask', [128, 4, 512], BF16)
    K['ones_f'] = c.sb('ones_f', [128, 128], F32)
    P.op('dve', lambda e: e.tensor_copy(out=K['idb'][:], in_=cst[:, 0:128]), reads=['cst'], writes=['idb'])
    P.op('dve', lambda e: e.tensor_copy(out=K['blk64'][:], in_=cst[:, 128:256]), reads=['cst'], writes=['blk64'])
    with Ctx(P) as c2:
        cm = c2.sb('cm', [128, 2048], F32)
        P.dma('sp', cm[:], cd[:, 384:384 + 2048], writes=['cm'])
        P.op('dve', lambda e: e.tensor_copy(out=K['cmask'][:], in_=cm[:].rearrange("p (r q) -> p r q", r=4)),
             reads=['cm'], writes=['cmask'])
    P.op('dve', lambda e: e.memset(K['ones_bf'][:], 1.0), writes=['ones_bf'])
    P.op('dve', lambda e: e.memset(K['ones_f'][:], 1.0), writes=['ones_f'])
    P.barrier()
    K['blk64f'] = cst[:, 128:256]
    return K


def load_weight_bf(P, c, w_dram, rows, cols, dst, key, stg, gcol=None, col0=0, engs=('dve', 'pool'), piece=None):
    nch = rows // 128
    piece = piece or cols
    n = 0
    for ci in range(nch):
        for p0 in range(0, cols, piece):
            pc = min(piece, cols - p0)
            b = n % 2
            n += 1
            P.dma('sp', stg[b][:, :pc], w_dram[ci * 128:(ci + 1) * 128, col0 + p0:col0 + p0 + pc], writes=[('stg', id(stg), b)])
            en = engs[n % len(engs)]
            if gcol is not None:
                P.op(en, lambda e, ci=ci, b=b, p0=p0, pc=pc: e.tensor_scalar(out=dst[:, ci, p0:p0 + pc], in0=stg[b][:, :pc],
                                                                             scalar1=gcol[:, ci:ci + 1], scalar2=None, op0=ALU.mult),
                     reads=[('stg', id(stg), b), 'gcol'], writes=[key])
            else:
                P.op(en, lambda e, ci=ci, b=b, p0=p0, pc=pc: e.tensor_copy(out=dst[:, ci, p0:p0 + pc], in_=stg[b][:, :pc]),
                     reads=[('stg', id(stg), b)], writes=[key])


def norm_transpose(P, K, xt, xkey, hb, hkey, hT, hTkey, tp, tpkey, ss, rs, j, ncols, tcol0):
    junk = hb
    P.op('act', lambda e: e.activation(out=junk, in_=xt, func=AF.Square, accum_out=ss[:, j:j + 1]),
         reads=[xkey], writes=[hkey, ('ss', id(ss), j)])
    P.op('dve', lambda e: e.tensor_scalar(out=rs[:, j:j + 1], in0=ss[:, j:j + 1], scalar1=1.0 / ncols, scalar2=EPS,
                                          op0=ALU.mult, op1=ALU.add),
         reads=[('ss', id(ss), j)], writes=[('rs', id(rs), j)])
    P.op('act', lambda e: e.sqrt(out=rs[:, j:j + 1], in_=rs[:, j:j + 1]),
         reads=[('rs', id(rs), j)], writes=[('rs', id(rs), j)])
    P.op('dve', lambda e: e.reciprocal(out=rs[:, j:j + 1], in_=rs[:, j:j + 1]),
         reads=[('rs', id(rs), j)], writes=[('rs', id(rs), j)])
    P.op('act', lambda e: e.activation(out=hb, in_=xt, func=AF.Copy, scale=rs[:, j:j + 1]),
         reads=[xkey, ('rs', id(rs), j)], writes=[hkey])
    nch = ncols // 128
    for ci in range(nch):
        P.op('pe', lambda e, ci=ci: e.transpose(out=tp[:, ci, :], in_=hb[:, ci * 128:(ci + 1) * 128], identity=K['idb'][:]),
             reads=[hkey], writes=[tpkey])
    P.op('dve', lambda e: e.tensor_copy(out=hT[:, :, tcol0:tcol0 + 128], in_=tp[:, :nch, :]),
         reads=[tpkey], writes=[hTkey])


def stage_ffn(P, K, xin, xout, wg, wu, wd, gcol_d, T):
    GT = 256
    NT = GT // 128
    with Ctx(P) as c:
        wg_bf = c.sb('wg', [128, 8, DFF], BF16)
        wu_bf = c.sb('wu', [128, 8, DFF], BF16)
        wd_bf = c.sb('wd', [128, NFF, D], BF16)
        gcol = c.sb('gcol', [128, 8], F32)
        stg = [c.sb('stg', [128, 1408], F32) for _ in range(2)]
        P.dma('sp', gcol[:], gcol_d, writes=['gcol'])
        load_weight_bf(P, c, wg, D, DFF, wg_bf, 'wg', stg, gcol=gcol, piece=1408)
        load_weight_bf(P, c, wu, D, DFF, wu_bf, 'wu', stg, gcol=gcol, piece=1408)
        load_weight_bf(P, c, wd, DFF, D, wd_bf, 'wd', stg)
        xt = [c.sb('xt', [128, NT, D], F32) for _ in range(2)]
        hb = [c.sb('hb', [128, D], BF16) for _ in range(2)]
        hT = [c.sb('hT', [128, 8, GT], BF16) for _ in range(2)]
        act = [c.sb('act', [128, NFF, GT], BF16) for _ in range(1)]
        sg = [c.sb('sg', [128, GT], F32) for _ in range(2)]
        xo = [c.sb('xo', [128, D], F32) for _ in range(2)]
        ss = c.sb('ss', [128, 8], F32)
        rs = c.sb('rs', [128, 8], F32)
        tp = [c.ps('tp', [128, 8, 128], BF16) for _ in range(2)]
        pg = [c.ps('pg', [128, 512], F32) for _ in range(2)]
        pu = [c.ps('pu', [128, 512], F32) for _ in range(2)]
        po = [c.ps('po', [128, 512], F32) for _ in range(2)]
        ng = T // GT
        it = 0
        for g in range(ng):
            b = g % 2
            tok0 = g * GT
            for j in range(NT):
                P.dma('sp', xt[b][:, j, :], xin[tok0 + j * 128: tok0 + (j + 1) * 128, :], writes=[('xt', b, j)])
            for j in range(NT):
                hbb = (g * NT + j) % 2
                norm_transpose(P, K, xt[b][:, j, :], ('xt', b, j), hb[hbb][:], ('hb', hbb), hT[b], ('hT', b),
                               tp[hbb], ('tp', hbb), ss, rs, (g * NT + j) % 8, D, j * 128)
            for f in range(NFF):
                pb = f % 2
                for ci in range(8):
                    P.op('pe', lambda e, ci=ci, f=f, pb=pb: e.matmul(pg[pb][:, :GT], wg_bf[:, ci, f * 128:(f + 1) * 128],
                                                                     hT[b][:, ci, :], start=(ci == 0), stop=(ci == 7)),
                         reads=['wg', ('hT', b)], writes=[('pg', pb)])
                for ci in range(8):
                    P.op('pe', lambda e, ci=ci, f=f, pb=pb: e.matmul(pu[pb][:, :GT], wu_bf[:, ci, f * 128:(f + 1) * 128],
                                                                     hT[b][:, ci, :], start=(ci == 0), stop=(ci == 7)),
                         reads=['wu', ('hT', b)], writes=[('pu', pb)])
                P.op('act', lambda e, pb=pb: e.activation(out=sg[pb][:], in_=pg[pb][:, :GT], func=AF.Silu),
                     reads=[('pg', pb)], writes=[('sg', pb)])
                P.op('dve', lambda e, pb=pb, f=f: e.tensor_tensor(out=act[0][:, f, :], in0=sg[pb][:], in1=pu[pb][:, :GT],
                                                                  op=ALU.mult),
                     reads=[('sg', pb), ('pu', pb)], writes=[('act', f)])
            for j in range(NT):
                for dh in range(2):
                    ob = it % 2
                    it += 1
                    for f in range(NFF):
                        P.op('pe', lambda e, f=f, j=j, dh=dh, ob=ob: e.matmul(po[ob][:], act[0][:, f, j * 128:(j + 1) * 128],
                                                                              wd_bf[:, f, dh * 512:(dh + 1) * 512],
                                                                              start=(f == 0), stop=(f == NFF - 1)),
                             reads=['wd', ('act', f)], writes=[('po', ob)])
                    xob = (g * NT + j) % 2
                    P.op('dve', lambda e, j=j, dh=dh, ob=ob, xob=xob: e.tensor_tensor(
                        out=xo[xob][:, dh * 512:(dh + 1) * 512], in0=xt[b][:, j, dh * 512:(dh + 1) * 512], in1=po[ob][:],
                        op=ALU.add),
                        reads=[('po', ob), ('xt', b, j)], writes=[('xo', xob, dh)])
                P.dma('pool', xout[tok0 + j * 128: tok0 + (j + 1) * 128, :], xo[xob][:],
                      reads=[('xo', xob, 0), ('xo', xob, 1)])


GELU_C = 1.5957691216057308
GELU_A = 0.044715


def emit_gelu(P, src, skey, t1, t1key, out, okey, eng2='dve'):
    P.op('act', lambda e: e.activation(out=t1, in_=src, func=AF.Square), reads=[skey], writes=[t1key])
    P.op('dve', lambda e: e.tensor_scalar(out=t1, in0=t1, scalar1=GELU_A, scalar2=1.0, op0=ALU.mult, op1=ALU.add),
         reads=[t1key], writes=[t1key])
    P.op('dve', lambda e: e.tensor_tensor(out=t1, in0=t1, in1=src, op=ALU.mult), reads=[t1key, skey], writes=[t1key])
    P.op('act', lambda e: e.activation(out=t1, in_=t1, func=AF.Sigmoid, scale=GELU_C), reads=[t1key], writes=[t1key])
    P.op(eng2, lambda e: e.tensor_tensor(out=out, in0=t1, in1=src, op=ALU.mult), reads=[t1key, skey], writes=[okey])


def emit_rsqrt(P, dst, dkey, src, skey, mult, add):
    P.op('dve', lambda e: e.tensor_scalar(out=dst, in0=src, scalar1=mult, scalar2=add, op0=ALU.mult, op1=ALU.add),
         reads=[skey], writes=[dkey])
    P.op('act', lambda e: e.sqrt(out=dst, in_=dst), reads=[dkey], writes=[dkey])
    P.op('dve', lambda e: e.reciprocal(out=dst, in_=dst), reads=[dkey], writes=[dkey])


class InProj:
    def __init__(self, P, K, c, xin, w_d, N, gcol_d, T, GT=512):
        self.P, self.K, self.c, self.xin, self.T, self.GT = P, K, c, xin, T, GT
        self.NT = GT // 128
        self.w_bf = c.sb('win', [128, 8, N], BF16)
        self.gcol = c.sb('gcol', [128, 8], F32)
        stg = [c.sb('stg', [128, N], F32) for _ in range(2)]
        P.dma('sp', self.gcol[:], gcol_d, writes=['gcol'])
        load_weight_bf(P, c, w_d, D, N, self.w_bf, 'win', stg, gcol=self.gcol)
        self.xt = [c.sb('xt', [128, self.NT, D], F32) for _ in range(2)]
        self.hb = [c.sb('hb', [128, D], BF16) for _ in range(2)]
        self.hT = [c.sb('hT', [128, 8, GT], BF16) for _ in range(2)]
        self.ss = c.sb('ss', [128, 8], F32)
        self.rs = c.sb('rs', [128, 8], F32)
        self.tp = [c.ps('tp', [128, 8, 128], BF16) for _ in range(2)]
        self.pf = [c.ps('pf', [128, 512], F32) for _ in range(2)]
        self.pt = [c.ps('pt', [128, 512], F32) for _ in range(2)]
        self.nf = 0
        self.ntt = 0

    def run(self, fcols, tcols, f_epi, t_epi):
        P, K = self.P, self.K
        GT, NT = self.GT, self.NT
        for g in range(self.T // GT):
            b = g % 2
            tok0 = g * GT
            for j in range(NT):
                P.dma('sp', self.xt[b][:, j, :], self.xin[tok0 + j * 128: tok0 + (j + 1) * 128, :], writes=[('xt', b, j)])
            for j in range(NT):
                hbb = (g * NT + j) % 2
                norm_transpose(P, K, self.xt[b][:, j, :], ('xt', b, j), self.hb[hbb][:], ('hb', hbb), self.hT[b], ('hT', b),
                               self.tp[hbb], ('tp', hbb), self.ss, self.rs, (g * NT + j) % 8, D, j * 128)
            pend = None
            for idx, (col0, m) in enumerate(fcols):
                pb = self.nf % 2
                self.nf += 1
                for ci in range(8):
                    P.op('pe', lambda e, ci=ci, pb=pb, col0=col0, m=m: e.matmul(
                        self.pf[pb][:m, :GT], self.w_bf[:, ci, col0:col0 + m], self.hT[b][:, ci, :],
                        start=(ci == 0), stop=(ci == 7)), reads=['win', ('hT', b)], writes=[('pf', pb)])
                if pend is not None:
                    f_epi(*pend)
                pend = (idx, self.pf[pb], ('pf', pb), tok0, g)
            if pend is not None:
                f_epi(*pend)
            pend = None
            for j in range(NT):
                for idx, (col0, n) in enumerate(tcols):
                    pb = self.ntt % 2
                    self.ntt += 1
                    for ci in range(8):
                        P.op('pe', lambda e, ci=ci, pb=pb, col0=col0, n=n, j=j: e.matmul(
                            self.pt[pb][:, :n], self.hT[b][:, ci, j * 128:(j + 1) * 128], self.w_bf[:, ci, col0:col0 + n],
                            start=(ci == 0), stop=(ci == 7)), reads=['win', ('hT', b)], writes=[('pt', pb)])
                    if pend is not None:
                        t_epi(*pend)
                    pend = (idx, self.pt[pb], ('pt', pb), tok0 + j * 128, g * NT + j)
            if pend is not None:
                t_epi(*pend)


def stage_inproj_even(P, K, xin, w_d, gcol_d, prm, T, qkT, vA, uF, vG):
    with Ctx(P) as c:
        ip = InProj(P, K, c, xin, w_d, 2560, gcol_d, T)
        gq = c.sb('gq', [128, 2], F32)
        P.dma('sp', gq[:], prm['qk_g'], writes=['gq'])
        P.op('dve', lambda e: e.tensor_scalar(out=gq[:, 0:1], in0=gq[:, 0:1], scalar1=0.125, scalar2=None, op0=ALU.mult),
             reads=['gq'], writes=['gq'])
        lng = c.sb('lng', [128, 512], F32)
        lnb = c.sb('lnb', [128, 512], F32)
        P.dma('sp', lng[:], prm['sg_ln_g_rep'], writes=['lng'])
        P.dma('sp', lnb[:], prm['sg_ln_b_rep'], writes=['lnb'])
        sq = [c.sb('sq', [128, 512], BF16) for _ in range(2)]
        rr = [c.sb('rr', [128, 512], F32) for _ in range(2)]
        qo = [c.sb('qo', [128, 512], BF16) for _ in range(2)]
        t1 = [c.sb('t1', [128, 512], F32) for _ in range(2)]
        uo = [c.sb('uo', [128, 512], F32) for _ in range(2)]
        vo = [c.sb('vo', [128, 512], BF16) for _ in range(2)]
        gv = [c.sb('gv', [128, 512], F32) for _ in range(2)]
        st4 = c.sb('st4', [128, 8, 4], F32)
        pn = c.ps('pn', [128, 512], F32)
        cnt = {'f': 0, 't': 0}

        def f_epi(idx, ps, pkey, tok0, g):
            i = cnt['f'] % 2
            cnt['f'] += 1
            if idx < 8:
                P.op('act', lambda e: e.activation(out=sq[i][:], in_=ps[:], func=AF.Square), reads=[pkey], writes=[('sq', i)])
                P.op('pe', lambda e: e.matmul(pn[:], K['blk64'][:], sq[i][:], start=True, stop=True),
                     reads=[('sq', i), 'blk64'], writes=['pn'])
                emit_rsqrt(P, rr[i][:], ('rr', i), pn[:], 'pn', 1.0 / 64, EPS)
                gc = gq[:, 0:1] if idx < 4 else gq[:, 1:2]
                P.op('dve', lambda e: e.scalar_tensor_tensor(out=qo[i][:], in0=ps[:], scalar=gc, in1=rr[i][:],
                                                             op0=ALU.mult, op1=ALU.mult),
                     reads=[pkey, ('rr', i), 'gq'], writes=[('qo', i)])
                P.dma('pool', qkT[idx * 128:(idx + 1) * 128, tok0:tok0 + 512], qo[i][:], reads=[('qo', i)])
            else:
                emit_gelu(P, ps[:], pkey, t1[i][:], ('t1', i), uo[i][:], ('uo', i), eng2='pool' if False else 'dve')
                P.dma('pool', uF[(idx - 8) * 128:(idx - 7) * 128, tok0:tok0 + 512], uo[i][:], reads=[('uo', i)])

        def t_epi(idx, ps, pkey, tokj, jj):
            i = cnt['t'] % 2
            cnt['t'] += 1
            if idx == 0:
                P.op('act', lambda e: e.copy(out=vo[i][:], in_=ps[:]), reads=[pkey], writes=[('vo', i)])
                P.dma('pool', vA[tokj:tokj + 128, :], vo[i][:], reads=[('vo', i)])
            else:
                emit_gelu(P, ps[:], pkey, t1[i][:], ('t1', i), gv[i][:], ('gv', i))
                s = jj % 8
                gv3 = gv[i][:].rearrange("p (g d) -> p g d", g=4)
                t13 = t1[i][:].rearrange("p (g d) -> p g d", g=4)
                P.op('dve', lambda e: e.tensor_reduce(out=st4[:, s, :], in_=gv3, axis=AX.X, op=ALU.add),
                     reads=[('gv', i)], writes=[('st4', s)])
                P.op('act', lambda e: e.activation(out=t1[i][:], in_=gv[i][:], func=AF.Square), reads=[('gv', i)], writes=[('t1', i)])
                s2 = (jj + 4) % 8
                P.op('dve', lambda e: e.tensor_reduce(out=st4[:, s2, :], in_=t13, axis=AX.X, op=ALU.add),
                     reads=[('t1', i)], writes=[('st4', s2)])
                P.op('dve', lambda e: e.tensor_scalar(out=st4[:, s, :], in0=st4[:, s, :], scalar1=1.0 / 128, scalar2=None, op0=ALU.mult),
                     reads=[('st4', s)], writes=[('st4', s)])
                P.op('dve', lambda e: e.tensor_scalar(out=st4[:, s2, :], in0=st4[:, s2, :], scalar1=1.0 / 128, scalar2=EPS,
                                                      op0=ALU.mult, op1=ALU.add), reads=[('st4', s2)], writes=[('st4', s2)])
                m2 = t1[i][:, 0:4]
                P.op('dve', lambda e: e.tensor_tensor(out=m2, in0=st4[:, s, :], in1=st4[:, s, :], op=ALU.mult),
                     reads=[('st4', s)], writes=[('t1', i)])
                P.op('dve', lambda e: e.tensor_tensor(out=st4[:, s2, :], in0=st4[:, s2, :], in1=m2, op=ALU.subtract),
                     reads=[('st4', s2), ('t1', i)], writes=[('st4', s2)])
                P.op('act', lambda e: e.sqrt(out=st4[:, s2, :], in_=st4[:, s2, :]), reads=[('st4', s2)], writes=[('st4', s2)])
                P.op('dve', lambda e: e.reciprocal(out=st4[:, s2, :], in_=st4[:, s2, :]), reads=[('st4', s2)], writes=[('st4', s2)])
                for gi in range(4):
                    P.op('dve', lambda e, gi=gi: e.tensor_scalar(out=gv[i][:, gi * 128:(gi + 1) * 128], in0=gv[i][:, gi * 128:(gi + 1) * 128],
                                                                 scalar1=st4[:, s, gi:gi + 1], scalar2=st4[:, s2, gi:gi + 1],
                                                                 op0=ALU.subtract, op1=ALU.mult),
                         reads=[('gv', i), ('st4', s), ('st4', s2)], writes=[('gv', i)])
                P.op('pool', lambda e: e.tensor_tensor(out=gv[i][:], in0=gv[i][:], in1=lng[:], op=ALU.mult),
                     reads=[('gv', i), 'lng'], writes=[('gv', i)])
                P.op('pool', lambda e: e.tensor_tensor(out=vo[i][:], in0=gv[i][:], in1=lnb[:], op=ALU.add),
                     reads=[('gv', i), 'lnb'], writes=[('vo', i)])
                P.dma('pool', vG[tokj:tokj + 128, :], vo[i][:], reads=[('vo', i)])

        fcols = [(i * 128, 128) for i in range(8)] + [(1536 + i * 128, 128) for i in range(4)]
        tcols = [(1024, 512), (2048, 512)]
        ip.run(fcols, tcols, f_epi, t_epi)


def stage_attn(P, K, qkT, vA, mixT, prm, lam_init, S, nseq):
    NB = S // 128
    NG = S // 512
    with Ctx(P) as c:
        lam = c.sb('lam', [128, 256], F32)
        P.dma('sp', lam[:], prm['lam_rep'], writes=['lam'])
        prod = c.sb('prod', [128, 2, 64], F32)
        dots = c.sb('dots', [128, 4], F32)
        P.op('dve', lambda e: e.tensor_tensor(out=prod[:, 0, :], in0=lam[:, 0:64], in1=lam[:, 64:128], op=ALU.mult),
             reads=['lam'], writes=['prod'])
        P.op('dve', lambda e: e.tensor_tensor(out=prod[:, 1, :], in0=lam[:, 128:192], in1=lam[:, 192:256], op=ALU.mult),
             reads=['lam'], writes=['prod'])
        P.op('dve', lambda e: e.tensor_reduce(out=dots[:, 0:2], in_=prod[:], axis=AX.X, op=ALU.add), reads=['prod'], writes=['dots'])
        P.op('act', lambda e: e.activation(out=dots[:, 0:2], in_=dots[:, 0:2], func=AF.Exp), reads=['dots'], writes=['dots'])
        P.op('dve', lambda e: e.tensor_tensor(out=dots[:, 2:3], in0=dots[:, 1:2], in1=dots[:, 0:1], op=ALU.subtract),
             reads=['dots'], writes=['dots'])
        P.op('dve', lambda e: e.tensor_scalar(out=dots[:, 2:3], in0=dots[:, 2:3], scalar1=-float(lam_init), scalar2=None, op0=ALU.add),
             reads=['dots'], writes=['dots'])
        neglam = dots[:, 2:3]
        sgc = c.sb('sgc', [128, 1], F32)
        P.dma('sp', sgc[:], prm['subln_col'], writes=['sgc'])
        P.op('dve', lambda e: e.tensor_scalar(out=sgc[:], in0=sgc[:], scalar1=float(1.0 - lam_init), scalar2=None, op0=ALU.mult),
             reads=['sgc'], writes=['sgc'])
        kT = [c.sb('kT', [64, 2, S], BF16) for _ in range(2)]
        Vt = [c.sb('Vt', [128, NB, 128], BF16) for _ in range(2)]
        qT = [c.sb('qT', [64, 2, 512], BF16) for _ in range(2)]
        pT = [c.sb('pT', [128, 512], BF16) for _ in range(3)]
        e32 = [c.sb('e32', [128, 512], F32) for _ in range(2)]
        r0 = c.sb('r0', [128, 512], F32)
        r1 = c.sb('r1', [128, 512], F32)
        o0 = c.sb('o0', [128, 512], F32)
        o1 = c.sb('o1', [128, 512], F32)
        osq = c.sb('osq', [128, 512], BF16)
        ob = [c.sb('ob', [128, 512], BF16) for _ in range(2)]
        sT = [c.ps('sT', [128, 512], F32) for _ in range(3)]
        acc = [c.ps('acc', [128, 512], F32) for _ in range(2)]
        ls = [c.ps('ls', [128, 512], F32) for _ in range(2)]
        pn = c.ps('pn', [128, 512], F32)
        n_s = 0
        n_p = 0
        n_e = 0
        n_q = 0
        n_h = 0
        for s in range(nseq):
            for h in range(4):
                hb = n_h % 2
                n_h += 1
                for cm in range(2):
                    r = 512 + h * 128 + cm * 64
                    P.dma('sp', kT[hb][:, cm, :], qkT[r:r + 64, s * S:(s + 1) * S], writes=[('kT', hb)])
                P.dma('sp', Vt[hb][:], vA[s * S:(s + 1) * S, h * 128:(h + 1) * 128].rearrange("(j p) d -> p j d", p=128),
                      writes=[('Vt', hb)])
                for g in range(NG):
                    qb = n_q % 2
                    n_q += 1
                    t0 = s * S + g * 512
                    for cm in range(2):
                        r = h * 128 + cm * 64
                        P.dma('sp', qT[qb][:, cm, :], qkT[r:r + 64, t0:t0 + 512], writes=[('qT', qb)])
                    nkb = 4 * (g + 1)
                    items = [(j, cm) for j in range(nkb) for cm in range(2)]
                    sbs = {}

                    def emit_score(i):
                        nonlocal n_s
                        j, cm = items[i]
                        sb_ = n_s % 3
                        n_s += 1
                        sbs[i] = sb_
                        P.op('pe', lambda e: e.matmul(sT[sb_][:], kT[hb][:, cm, j * 128:(j + 1) * 128], qT[qb][:, cm, :], start=True, stop=True),
                             reads=[('kT', hb), ('qT', qb)], writes=[('sT', sb_)])

                    emit_score(0)
                    emit_score(1)
                    for i, (j, cm) in enumerate(items):
                        sb_ = sbs[i]
                        pb = n_p % 3
                        n_p += 1
                        if j < 4 * g:
                            P.op('act', lambda e: e.activation(out=pT[pb][:], in_=sT[sb_][:], func=AF.Exp),
                                 reads=[('sT', sb_)], writes=[('pT', pb)])
                        else:
                            eb = n_e % 2
                            n_e += 1
                            rr_ = j - 4 * g
                            P.op('act', lambda e: e.activation(out=e32[eb][:], in_=sT[sb_][:], func=AF.Exp),
                                 reads=[('sT', sb_)], writes=[('e32', eb)])
                            P.op('pool', lambda e: e.tensor_tensor(out=pT[pb][:], in0=e32[eb][:], in1=K['cmask'][:, rr_, :], op=ALU.mult),
                                 reads=[('e32', eb), 'cmask'], writes=[('pT', pb)])
                        if i + 2 < len(items):
                            emit_score(i + 2)
                        P.op('pe', lambda e: e.matmul(acc[cm][:], Vt[hb][:, j, :], pT[pb][:], start=(j == 0), stop=(j == nkb - 1)),
                             reads=[('Vt', hb), ('pT', pb)], writes=[('acc', cm)])
                        P.op('pe', lambda e: e.matmul(ls[cm][:], K['ones_bf'][:], pT[pb][:], start=(j == 0), stop=(j == nkb - 1)),
                             reads=['ones_bf', ('pT', pb)], writes=[('ls', cm)])
                    P.op('dve', lambda e: e.reciprocal(out=r0[:], in_=ls[0][:]), reads=[('ls', 0)], writes=['r0'])
                    P.op('dve', lambda e: e.reciprocal(out=r1[:], in_=ls[1][:]), reads=[('ls', 1)], writes=['r1'])
                    P.op('dve', lambda e: e.tensor_tensor(out=o0[:], in0=acc[0][:], in1=r0[:], op=ALU.mult),
                         reads=[('acc', 0), 'r0'], writes=['o0'])
                    P.op('dve', lambda e: e.tensor_tensor(out=o1[:], in0=acc[1][:], in1=r1[:], op=ALU.mult),
                         reads=[('acc', 1), 'r1'], writes=['o1'])
                    P.op('dve', lambda e: e.scalar_tensor_tensor(out=o0[:], in0=o1[:], scalar=neglam, in1=o0[:],
                                                                 op0=ALU.mult, op1=ALU.add),
                         reads=['o0', 'o1', 'dots'], writes=['o0'])
                    P.op('act', lambda e: e.activation(out=osq[:], in_=o0[:], func=AF.Square), reads=['o0'], writes=['osq'])
                    P.op('pe', lambda e: e.matmul(pn[:], K['ones_bf'][:], osq[:], start=True, stop=True),
                         reads=['ones_bf', 'osq'], writes=['pn'])
                    emit_rsqrt(P, r0[:], 'r0', pn[:], 'pn', 1.0 / 128, EPS)
                    obb = n_q % 2
                    P.op('dve', lambda e, obb=obb: e.scalar_tensor_tensor(out=ob[obb][:], in0=o0[:], scalar=sgc[:, 0:1], in1=r0[:],
                                                                          op0=ALU.mult, op1=ALU.mult),
                         reads=['o0', 'r0', 'sgc'], writes=[('ob', obb)])
                    P.dma('pool', mixT[h * 128:(h + 1) * 128, t0:t0 + 512], ob[obb][:], reads=[('ob', obb)])


def stage_sgu(P, K, vG, uF, mixT, prm, T):
    with Ctx(P) as c:
        wst = c.sb('wst', [128, 4, 128], F32)
        wtm = c.sb('wtm', [128, 4, 128], BF16)
        bias = c.sb('bias', [128, 4, 512], F32)
        P.dma('sp', wst[:], prm['sg_wT'].rearrange("g s t -> s g t"), writes=['wst'])
        P.dma('sp', bias[:], prm['sg_b_rep'].rearrange("p (g t) -> p g t", g=4), writes=['bias'])
        for g in range(4):
            P.op('dve', lambda e, g=g: e.tensor_tensor(out=wtm[:, g, :], in0=wst[:, g, :], in1=K['triu'][:], op=ALU.mult),
                 reads=['wst', 'triu'], writes=['wtm'])
        vt = [c.sb('vt', [128, 4, 512], BF16) for _ in range(2)]
        ut = [c.sb('ut', [128, 4, 512], F32) for _ in range(2)]
        tt = [c.sb('tt', [128, 512], F32) for _ in range(2)]
        ob = [c.sb('ob', [128, 4, 512], BF16) for _ in range(2)]
        ps = [c.ps('ps', [128, 512], F32) for _ in range(2)]
        n = 0
        for tg in range(T // 512):
            b = tg % 2
            tok0 = tg * 512
            P.dma('sp', vt[b][:], vG[tok0:tok0 + 512, :].rearrange("(c p) f -> p c f", p=128), writes=[('vt', b)])
            P.dma('sp', ut[b][:], uF[:, tok0:tok0 + 512].rearrange("(g d) t -> d g t", d=128), writes=[('ut', b)])
            for g in range(4):
                pb = n % 2
                n += 1
                for ch in range(4):
                    P.op('pe', lambda e, g=g, ch=ch, pb=pb: e.matmul(ps[pb][:, ch * 128:(ch + 1) * 128], vt[b][:, ch, g * 128:(g + 1) * 128],
                                                                     wtm[:, g, :], start=True, stop=True),
                         reads=[('vt', b), 'wtm'], writes=[('ps', pb)])
                P.op('dve', lambda e, g=g, pb=pb: e.tensor_tensor(out=tt[pb][:], in0=ps[pb][:], in1=bias[:, g, :], op=ALU.add),
                     reads=[('ps', pb), 'bias'], writes=[('tt', pb)])
                P.op('pool', lambda e, g=g, pb=pb: e.tensor_tensor(out=ob[b][:, g, :], in0=tt[pb][:], in1=ut[b][:, g, :], op=ALU.mult),
                     reads=[('tt', pb), ('ut', b)], writes=[('ob', b)])
            P.dma('pool', mixT[512:1024, tok0:tok0 + 512].rearrange("(g d) t -> d g t", d=128), ob[b][:], reads=[('ob', b)])


def stage_outproj(P, K, mixT, w_d, xin, xout, T):
    GT = 512
    with Ctx(P) as c:
        w_bf = c.sb('wo', [128, 8, D], BF16)
        stg = [c.sb('stg', [128, D], F32) for _ in range(2)]
        load_weight_bf(P, c, w_d, D, D, w_bf, 'wo', stg)
        mt = [c.sb('mt', [128, 8, GT], BF16) for _ in range(2)]
        xt = [c.sb('xt', [128, GT // 128, D], F32) for _ in range(2)]
        xo = [c.sb('xo', [128, D], F32) for _ in range(2)]
        po = [c.ps('po', [128, 512], F32) for _ in range(4)]
        n = 0
        for g in range(T // GT):
            b = g % 2
            tok0 = g * GT
            P.dma('sp', mt[b][:], mixT[:, tok0:tok0 + GT].rearrange("(c f) t -> f c t", f=128), writes=[('mt', b)])
            P.dma('sp', xt[b][:], xin[tok0:tok0 + GT, :].rearrange("(j p) d -> p j d", p=128), writes=[('xt', b)])
            for j in range(GT // 128):
                xob = (g * 4 + j) % 2
                for dh in range(2):
                    ob = n % 4
                    n += 1
                    for ci in range(8):
                        P.op('pe', lambda e, ci=ci, j=j, dh=dh, ob=ob: e.matmul(po[ob][:], mt[b][:, ci, j * 128:(j + 1) * 128],
                                                                                w_bf[:, ci, dh * 512:(dh + 1) * 512],
                                                                                start=(ci == 0), stop=(ci == 7)),
                             reads=['wo', ('mt', b)], writes=[('po', ob)])
                    P.op('dve', lambda e, j=j, dh=dh, ob=ob, xob=xob: e.tensor_tensor(
                        out=xo[xob][:, dh * 512:(dh + 1) * 512], in0=xt[b][:, j, dh * 512:(dh + 1) * 512], in1=po[ob][:], op=ALU.add),
                        reads=[('po', ob), ('xt', b)], writes=[('xo', xob, dh)])
                P.dma('pool', xout[tok0 + j * 128: tok0 + (j + 1) * 128, :], xo[xob][:], reads=[('xo', xob, 0), ('xo', xob, 1)])


def stage_inproj_odd(P, K, xin, w_d, gcol_d, T, mqk_raw, mif, rw_raw, v_aug, moA):
    with Ctx(P) as c:
        ip = InProj(P, K, c, xin, w_d, 3848, gcol_d, T)
        fo = [c.sb('fo', [128, 512], F32) for _ in range(3)]
        vs = [c.sb('vs', [128, 4, 129], BF16) for _ in range(2)]
        so = [c.sb('so', [128, 512], F32) for _ in range(2)]
        for i in range(2):
            P.op('dve', lambda e, i=i: e.memset(vs[i][:], 1.0), writes=[('vs', i)])
        cnt = {'f': 0, 't': 0}

        def f_epi(idx, ps, pkey, tok0, g):
            i = cnt['f'] % 3
            cnt['f'] += 1
            m = 8 if idx == 8 else 128
            en = 'act' if cnt['f'] % 2 else 'dve'
            if en == 'act':
                P.op('act', lambda e: e.copy(out=fo[i][:m, :], in_=ps[:m, :]), reads=[pkey], writes=[('fo', i)])
            else:
                P.op('dve', lambda e: e.tensor_copy(out=fo[i][:m, :], in_=ps[:m, :]), reads=[pkey], writes=[('fo', i)])
            if idx < 8:
                dst = mqk_raw[idx * 128:(idx + 1) * 128, tok0:tok0 + 512]
            elif idx == 8:
                dst = mif[0:8, tok0:tok0 + 512]
            else:
                dst = rw_raw[(idx - 9) * 128:(idx - 8) * 128, tok0:tok0 + 512]
            P.dma('pool', dst, fo[i][:m, :], reads=[('fo', i)])

        def t_epi(idx, ps, pkey, tokj, jj):
            i = cnt['t'] % 2
            cnt['t'] += 1
            if idx == 0:
                P.op('dve', lambda e: e.tensor_copy(out=vs[i][:, :, 0:128], in_=ps[:].rearrange("p (h d) -> p h d", h=4)),
                     reads=[pkey], writes=[('vs', i)])
                P.dma('pool', v_aug[tokj:tokj + 128, :], vs[i][:].rearrange("p h d -> p (h d)"), reads=[('vs', i)])
            else:
                P.op('act', lambda e: e.activation(out=so[i][:], in_=ps[:], func=AF.Sigmoid), reads=[pkey], writes=[('so', i)])
                P.dma('pool', moA[tokj:tokj + 128, :], so[i][:], reads=[('so', i)])

        fcols = [(i * 128, 128) for i in range(8)] + [(1536, 8)] + [(2056 + i * 128, 128) for i in range(14)]
        tcols = [(1024, 512), (1544, 512)]
        ip.run(fcols, tcols, f_epi, t_epi)


def stage_ml_prep(P, K, mqk_raw, prm, mqkT, mkA, S, nseq):
    with Ctx(P) as c:
        cw = c.sb('cw', [128, 8, 4], F32)
        cb = c.sb('cb', [128, 8], F32)
        P.dma('sp', cw[:], prm['ml_cw'], writes=['cw'])
        P.dma('sp', cb[:], prm['ml_cb'], writes=['cb'])
        buf = [c.sb('buf', [128, 515], F32) for _ in range(3)]
        acc = [c.sb('acc', [128, 512], F32) for _ in range(2)]
        qo = [c.sb('qo', [128, 512], BF16) for _ in range(2)]
        kt = [c.sb('kt', [128, 4, 128], BF16) for _ in range(2)]
        tp = [c.ps('tp', [128, 4, 128], BF16) for _ in range(2)]
        n = 0
        for s in range(nseq):
            for g in range(S // 512):
                t0 = s * S + g * 512
                for ch in range(8):
                    b3 = n % 3
                    b = n % 2
                    en = 'dve' if n % 2 == 0 else 'pool'
                    n += 1
                    if g == 0:
                        P.op('pool', lambda e, b3=b3: e.memset(buf[b3][:, 0:3], 0.0), writes=[('buf', b3)])
                        P.dma('sp', buf[b3][:, 3:515], mqk_raw[ch * 128:(ch + 1) * 128, t0:t0 + 512], writes=[('buf', b3)])
                    else:
                        P.dma('sp', buf[b3][:, :], mqk_raw[ch * 128:(ch + 1) * 128, t0 - 3:t0 + 512], writes=[('buf', b3)])
                    P.op(en, lambda e, b3=b3, b=b, ch=ch: e.tensor_scalar(out=acc[b][:], in0=buf[b3][:, 3:515], scalar1=cw[:, ch, 3:4],
                                                                         scalar2=cb[:, ch:ch + 1], op0=ALU.mult, op1=ALU.add),
                         reads=[('buf', b3), 'cw', 'cb'], writes=[('acc', b)])
                    for j in (2, 1, 0):
                        P.op('dve', lambda e, b3=b3, b=b, ch=ch, j=j: e.scalar_tensor_tensor(out=acc[b][:], in0=buf[b3][:, j:j + 512],
                                                                                           scalar=cw[:, ch, j:j + 1], in1=acc[b][:],
                                                                                           op0=ALU.mult, op1=ALU.add),
                             reads=[('buf', b3), ('acc', b), 'cw'], writes=[('acc', b)])
                    P.op('act', lambda e, b=b: e.activation(out=acc[b][:], in_=acc[b][:], func=AF.Silu), reads=[('acc', b)], writes=[('acc', b)])
                    sc = 1.0 if ch < 4 else float(128 ** -0.5)
                    P.op(en, lambda e, b=b, sc=sc: e.tensor_scalar(out=qo[b][:], in0=acc[b][:], scalar1=sc, scalar2=None, op0=ALU.mult),
                         reads=[('acc', b)], writes=[('qo', b)])
                    P.dma('pool', mqkT[ch * 128:(ch + 1) * 128, t0:t0 + 512], qo[b][:], reads=[('qo', b)])
                    if ch >= 4:
                        for j in range(4):
                            P.op('pe', lambda e, b=b, j=j: e.transpose(out=tp[b][:, j, :], in_=qo[b][:, j * 128:(j + 1) * 128], identity=K['idb'][:]),
                                 reads=[('qo', b)], writes=[('tp', b)])
                        P.op('act', lambda e, b=b: e.copy(out=kt[b][:], in_=tp[b][:]), reads=[('tp', b)], writes=[('kt', b)])
                        P.dma('pool', mkA[t0:t0 + 512, (ch - 4) * 128:(ch - 3) * 128].rearrange("(j p) d -> p j d", p=128), kt[b][:],
                              reads=[('kt', b)])


def stage_ml_gates(P, K, mif, prm, mgM, mcol, mend, S, nseq):
    NB = S // 128
    R8 = nseq * 4
    with Ctx(P) as c:
        it = c.sb('it', [R8, S], F32)
        ft = c.sb('ft', [R8, S], F32)
        Bt = c.sb('Bt', [R8, S], F32)
        Mt = c.sb('Mt', [R8, S], F32)
        ones = c.sb('ones', [R8, S], F32)
        gb = c.sb('gb', [R8, 2], F32)
        ngb = c.sb('ngb', [R8, 1], F32)
        P.dma('sp', gb[:], prm['ml_gb'][0:R8, :], writes=['gb'])
        for s in range(nseq):
            P.dma('sp', it[4 * s:4 * s + 4, :], mif[0:4, s * S:(s + 1) * S], writes=['it'])
            P.dma('sp', ft[4 * s:4 * s + 4, :], mif[4:8, s * S:(s + 1) * S], writes=['ft'])
        P.op('dve', lambda e: e.memset(ones[:], 1.0), writes=['ones'])
        P.op('dve', lambda e: e.tensor_scalar(out=ngb[:], in0=gb[:, 1:2], scalar1=-1.0, scalar2=None, op0=ALU.mult), reads=['gb'], writes=['ngb'])
        P.op('act', lambda e: e.activation(out=ft[:], in_=ft[:], func=AF.Exp, scale=-1.0, bias=ngb[:, 0:1]), reads=['ft', 'ngb'], writes=['ft'])
        P.op('dve', lambda e: e.tensor_scalar(out=ft[:], in0=ft[:], scalar1=1.0, scalar2=None, op0=ALU.add), reads=['ft'], writes=['ft'])
        P.op('act', lambda e: e.activation(out=ft[:], in_=ft[:], func=AF.Ln), reads=['ft'], writes=['ft'])
        P.op('dve', lambda e: e.tensor_tensor_scan(out=Bt[:], data0=ones[:], data1=ft[:], initial=0.0, op0=ALU.mult, op1=ALU.subtract),
             reads=['ones', 'ft'], writes=['Bt'])
        P.op('dve', lambda e: e.scalar_tensor_tensor(out=it[:], in0=it[:], scalar=gb[:, 0:1], in1=Bt[:], op0=ALU.add, op1=ALU.subtract),
             reads=['it', 'gb', 'Bt'], writes=['it'])
        P.op('dve', lambda e: e.tensor_tensor_scan(out=Mt[:], data0=it[:], data1=it[:], initial=0.0, op0=ALU.max, op1=ALU.max),
             reads=['it'], writes=['Mt'])
        P.op('dve', lambda e: e.tensor_tensor(out=Bt[:], in0=Bt[:], in1=Mt[:], op=ALU.add), reads=['Bt', 'Mt'], writes=['Bt'])
        P.op('act', lambda e: e.activation(out=Bt[:], in_=Bt[:], func=AF.Exp, scale=-1.0), reads=['Bt'], writes=['Bt'])
        P.dma('pool', mgM[0:R8, :], Mt[:], reads=['Mt'])
        P.dma('pool', mend.rearrange("(c r) -> r c", r=R8), Mt[:].rearrange("r (c k) -> r c k", k=128)[:, :, 127], reads=['Mt'], allow_slow_non_contiguous=True)
        pc = [c.ps('pc', [128, NB, R8], F32) for _ in range(2)]
        col = [c.sb('col', [128, NB, R8], F32) for _ in range(2)]
        for k, src in enumerate((it, Bt)):
            for cb_ in range(NB):
                P.op('pe', lambda e, k=k, cb_=cb_, src=src: e.transpose(out=pc[k][:, cb_, :], in_=src[:, cb_ * 128:(cb_ + 1) * 128],
                                                                       identity=K['idf'][0:R8, 0:R8]),
                     reads=['it', 'Bt', 'idf'], writes=[('pc', k)])
            P.op('dve', lambda e, k=k: e.tensor_copy(out=col[k][:], in_=pc[k][:]), reads=[('pc', k)], writes=[('col', k)])
            P.dma('pool', mcol[k], col[k][:].rearrange("p c r -> p (c r)"), reads=[('col', k)])


def stage_ml_core(P, K, mqkT, mkA, v_aug, moA, mgM, mcol, mend, prm, mixT, S, nseq):
    NB = S // 128
    R8 = nseq * 4
    with Ctx(P) as c:
        acol = c.sb('acol', [128, NB, R8], F32)
        encol = c.sb('encol', [128, NB, R8], F32)
        Mc = c.sb('Mc', [128, NB + 1, R8], F32)
        nMc = c.sb('nMc', [128, NB + 1, R8], F32)
        dcs = c.sb('dcs', [128, NB, R8], F32)
        wcs = c.sb('wcs', [128, NB, R8], F32)
        ngr = c.sb('ngr', [128, 512], F32)
        P.dma('sp', acol[:].rearrange("p c r -> p (c r)"), mcol[0], writes=['acol'])
        P.dma('sp', encol[:].rearrange("p c r -> p (c r)"), mcol[1], writes=['encol'])
        P.op('dve', lambda e: e.memset(Mc[:, 0, :], 0.0), writes=['Mc'])
        P.dma('sp', Mc[:, 1:, :].rearrange("p c r -> p (c r)"), mend.rearrange("(o n) -> o n", o=1).partition_broadcast(128), writes=['Mc'])
        P.dma('sp', ngr[:], prm['ml_ng_rep'], writes=['ngr'])
        P.op('dve', lambda e: e.tensor_scalar(out=nMc[:], in0=Mc[:], scalar1=-1.0, scalar2=None, op0=ALU.mult), reads=['Mc'], writes=['nMc'])
        P.op('dve', lambda e: e.tensor_tensor(out=dcs[:], in0=Mc[:, 0:NB, :], in1=Mc[:, 1:NB + 1, :], op=ALU.subtract), reads=['Mc'], writes=['dcs'])
        P.op('act', lambda e: e.activation(out=dcs[:], in_=dcs[:], func=AF.Exp), reads=['dcs'], writes=['dcs'])
        P.op('dve', lambda e: e.tensor_tensor(out=wcs[:], in0=acol[:], in1=Mc[:, 1:NB + 1, :], op=ALU.subtract), reads=['acol', 'Mc'], writes=['wcs'])
        P.op('act', lambda e: e.activation(out=wcs[:], in_=wcs[:], func=AF.Exp), reads=['wcs'], writes=['wcs'])
        Cst = [c.sb('Cst', [128, 129], F32) for _ in range(R8)]
        Cbf = [c.sb('Cbf', [128, 129], BF16) for _ in range(R8)]
        for r in range(R8):
            P.op('pool', lambda e, r=r: e.memset(Cst[r][:], 0.0), writes=[('Cst', r)])
            P.op('pool', lambda e, r=r: e.memset(Cbf[r][:], 0.0), writes=[('Cbf', r)])
        qT4 = [c.sb('qT4', [128, 4, 512], BF16) for _ in range(2)]
        kT4 = [c.sb('kT4', [128, 4, 512], BF16) for _ in range(2)]
        kA4 = [c.sb('kA4', [128, 4, 512], BF16) for _ in range(2)]
        vA4 = [c.sb('vA4', [128, 4, 516], BF16) for _ in range(2)]
        oA4 = [c.sb('oA4', [128, 4, 512], F32) for _ in range(2)]
        Mb4 = [c.sb('Mb4', [128, 4, 512], F32) for _ in range(2)]
        mo = [c.sb('mo', [128, 4, 512], BF16) for _ in range(2)]
        E = [c.sb('E', [128, 128], F32) for _ in range(2)]
        PT = [c.sb('PT', [128, 128], BF16) for _ in range(2)]
        er = [c.sb('er', [128, 128], F32) for _ in range(2)]
        qs = [c.sb('qs', [128, 128], BF16) for _ in range(2)]
        den = [c.sb('den', [128, 8], F32) for _ in range(2)]
        hs = [c.sb('hs', [128, 128], F32) for _ in range(2)]
        hj = [c.sb('hj', [128, 128], F32) for _ in range(2)]
        hf = [c.sb('hf', [128, 128], BF16) for _ in range(2)]
        vw = [c.sb('vw', [128, 129], BF16) for _ in range(2)]
        ps_st = [c.ps('pst', [128, 512], F32) for _ in range(2)]
        ps_o = [c.ps('pso', [128, 512], F32) for _ in range(2)]
        ps_u = [c.ps('psu', [128, 512], F32) for _ in range(2)]
        ps_t = [c.ps('pstt', [128, 8, 128], BF16) for _ in range(1)]
        n = 0
        for sg in range(S // 512):
            for s in range(nseq):
                lb = (sg * nseq + s) % 2
                t0 = s * S + sg * 512
                P.dma('sp', qT4[lb][:], mqkT[0:512, t0:t0 + 512].rearrange("(h d) t -> d h t", d=128), writes=[('qT4', lb)])
                P.dma('sp', kT4[lb][:], mqkT[512:1024, t0:t0 + 512].rearrange("(h d) t -> d h t", d=128), writes=[('kT4', lb)])
                P.dma('sp', kA4[lb][:], mkA[t0:t0 + 512, :].rearrange("(c p) f -> p c f", p=128), writes=[('kA4', lb)])
                P.dma('sp', vA4[lb][:], v_aug[t0:t0 + 512, :].rearrange("(c p) f -> p c f", p=128), writes=[('vA4', lb)])
                P.dma('sp', oA4[lb][:], moA[t0:t0 + 512, :].rearrange("(c p) f -> p c f", p=128), writes=[('oA4', lb)])
                for h in range(4):
                    P.dma('sp', Mb4[lb][:, h, :], mgM[s * 4 + h:s * 4 + h + 1, sg * 512:(sg + 1) * 512].partition_broadcast(128),
                          writes=[('Mb4', lb)])
                for c4 in range(4):
                    cg = sg * 4 + c4
                    cs = slice(c4 * 128, (c4 + 1) * 128)
                    for h in range(4):
                        sh = s * 4 + h
                        b = n % 2
                        n += 1
                        P.op('pe', lambda e, b=b, h=h, cs=cs: e.matmul(ps_st[b][:, 0:128], kT4[lb][:, h, cs], qT4[lb][:, h, cs], start=True, stop=True),
                             reads=[('kT4', lb), ('qT4', lb)], writes=[('pst', b)])
                        P.op('act', lambda e, b=b, h=h, cs=cs, cg=cg, sh=sh: e.activation(out=E[b][:], in_=Mb4[lb][:, h, cs], func=AF.Exp, scale=-1.0,
                                                                                         bias=acol[:, cg, sh:sh + 1]),
                             reads=[('Mb4', lb), 'acol'], writes=[('E', b)])
                        P.op('pool', lambda e, b=b: e.tensor_tensor(out=E[b][:], in0=E[b][:], in1=K['triu'], op=ALU.mult),
                             reads=[('E', b), 'triu'], writes=[('E', b)])
                        P.op('dve', lambda e, b=b: e.tensor_tensor(out=PT[b][:], in0=ps_st[b][:, 0:128], in1=E[b][:], op=ALU.mult),
                             reads=[('pst', b), ('E', b)], writes=[('PT', b)])
                        P.op('act', lambda e, b=b, h=h, cs=cs, cg=cg, sh=sh: e.activation(out=er[b][:], in_=Mb4[lb][:, h, cs], func=AF.Exp, scale=-1.0,
                                                                                         bias=Mc[:, cg, sh:sh + 1]),
                             reads=[('Mb4', lb), 'Mc'], writes=[('er', b)])
                        P.op('pool', lambda e, b=b, h=h, cs=cs: e.tensor_tensor(out=qs[b][:], in0=qT4[lb][:, h, cs], in1=er[b][:], op=ALU.mult),
                             reads=[('qT4', lb), ('er', b)], writes=[('qs', b)])
                        P.op('pe', lambda e, b=b, sh=sh: e.matmul(ps_o[b][:, 0:129], qs[b][:], Cbf[sh][:], start=True, stop=False),
                             reads=[('qs', b), ('Cbf', sh)], writes=[('pso', b)])
                        P.op('pe', lambda e, b=b, h=h, c4=c4: e.matmul(ps_o[b][:, 0:129], PT[b][:], vA4[lb][:, c4, h * 129:(h + 1) * 129], start=False, stop=True),
                             reads=[('PT', b), ('vA4', lb)], writes=[('pso', b)])
                        P.op('act', lambda e, b=b: e.activation(out=den[b][:, 0:1], in_=ps_o[b][:, 128:129], func=AF.Abs),
                             reads=[('pso', b)], writes=[('den', b)])
                        P.op('dve', lambda e, b=b, cg=cg, sh=sh: e.tensor_tensor(out=den[b][:, 0:1], in0=den[b][:, 0:1], in1=encol[:, cg, sh:sh + 1], op=ALU.max),
                             reads=[('den', b), 'encol'], writes=[('den', b)])
                        P.op('dve', lambda e, b=b: e.reciprocal(out=den[b][:, 0:1], in_=den[b][:, 0:1]), reads=[('den', b)], writes=[('den', b)])
                        P.op('act', lambda e, b=b: e.activation(out=hs[b][:], in_=ps_o[b][:, 0:128], func=AF.Copy, scale=den[b][:, 0:1],
                                                                accum_out=den[b][:, 1:2]),
                             reads=[('pso', b), ('den', b)], writes=[('hs', b), ('den', b)])
                        P.op('act', lambda e, b=b: e.activation(out=hj[b][:], in_=hs[b][:], func=AF.Square, accum_out=den[b][:, 2:3]),
                             reads=[('hs', b)], writes=[('hj', b), ('den', b)])
                        P.op('dve', lambda e, b=b: e.tensor_scalar(out=den[b][:, 3:4], in0=den[b][:, 1:2], scalar1=1.0 / 128, scalar2=None, op0=ALU.mult),
                             reads=[('den', b)], writes=[('den', b)])
                        P.op('dve', lambda e, b=b: e.tensor_tensor(out=den[b][:, 4:5], in0=den[b][:, 3:4], in1=den[b][:, 3:4], op=ALU.mult),
                             reads=[('den', b)], writes=[('den', b)])
                        P.op('dve', lambda e, b=b: e.tensor_scalar(out=den[b][:, 2:3], in0=den[b][:, 2:3], scalar1=1.0 / 128, scalar2=EPS, op0=ALU.mult, op1=ALU.add),
                             reads=[('den', b)], writes=[('den', b)])
                        P.op('dve', lambda e, b=b: e.tensor_tensor(out=den[b][:, 2:3], in0=den[b][:, 2:3], in1=den[b][:, 4:5], op=ALU.subtract),
                             reads=[('den', b)], writes=[('den', b)])
                        P.op('act', lambda e, b=b: e.sqrt(out=den[b][:, 2:3], in_=den[b][:, 2:3]), reads=[('den', b)], writes=[('den', b)])
                        P.op('dve', lambda e, b=b: e.reciprocal(out=den[b][:, 2:3], in_=den[b][:, 2:3]), reads=[('den', b)], writes=[('den', b)])
                        P.op('dve', lambda e, b=b: e.tensor_scalar(out=hs[b][:], in0=hs[b][:], scalar1=den[b][:, 3:4], scalar2=den[b][:, 2:3],
                                                                   op0=ALU.subtract, op1=ALU.mult),
                             reads=[('hs', b), ('den', b)], writes=[('hs', b)])
                        P.op('pool', lambda e, b=b, h=h: e.tensor_tensor(out=hs[b][:], in0=hs[b][:], in1=ngr[:, h * 128:(h + 1) * 128], op=ALU.mult),
                             reads=[('hs', b), 'ngr'], writes=[('hs', b)])
                        P.op('pool', lambda e, b=b, h=h, c4=c4: e.tensor_tensor(out=hf[b][:], in0=hs[b][:], in1=oA4[lb][:, c4, h * 128:(h + 1) * 128], op=ALU.mult),
                             reads=[('hs', b), ('oA4', lb)], writes=[('hf', b)])
                        tb = n % 8
                        P.op('pe', lambda e, b=b, tb=tb: e.transpose(out=ps_t[0][:, tb, :], in_=hf[b][:], identity=K['idb'][:]),
                             reads=[('hf', b)], writes=[('pstt', tb)])
                        P.op('act', lambda e, tb=tb, h=h, cs=cs: e.copy(out=mo[lb][:, h, cs], in_=ps_t[0][:, tb, :]),
                             reads=[('pstt', tb)], writes=[('mo', lb)])
                        P.op('dve', lambda e, b=b, h=h, c4=c4, cg=cg, sh=sh: e.tensor_scalar(out=vw[b][:], in0=vA4[lb][:, c4, h * 129:(h + 1) * 129],
                                                                                            scalar1=wcs[:, cg, sh:sh + 1], scalar2=None, op0=ALU.mult),
                             reads=[('vA4', lb), 'wcs'], writes=[('vw', b)])
                        P.op('pe', lambda e, b=b, h=h, c4=c4: e.matmul(ps_u[b][:, 0:129], kA4[lb][:, c4, h * 128:(h + 1) * 128], vw[b][:], start=True, stop=True),
                             reads=[('kA4', lb), ('vw', b)], writes=[('psu', b)])
                        P.op('dve', lambda e, b=b, cg=cg, sh=sh: e.scalar_tensor_tensor(out=Cst[sh][:], in0=Cst[sh][:], scalar=dcs[:, cg, sh:sh + 1],
                                                                                       in1=ps_u[b][:, 0:129], op0=ALU.mult, op1=ALU.add),
                             reads=[('Cst', sh), 'dcs', ('psu', b)], writes=[('Cst', sh)])
                        P.op('act', lambda e, sh=sh: e.copy(out=Cbf[sh][:], in_=Cst[sh][:]), reads=[('Cst', sh)], writes=[('Cbf', sh)])
                P.dma('pool', mixT[0:512, t0:t0 + 512].rearrange("(h d) t -> d h t", d=128), mo[lb][:], reads=[('mo', lb)])


RW_LD = -0.6065306597126334


def stage_rw_prep(P, K, rw_raw, prm, rwF, rwT, rwgL, rwbv, rwg, S, nseq):
    with Ctx(P) as c:
        mu = c.sb('mu', [128, 14], F32)
        pc = c.sb('pc', [128, 5, 4], F32)
        P.dma('sp', mu[:], prm['rw_mu'], writes=['mu'])
        P.dma('sp', pc[:], prm['rw_pc'], writes=['pc'])
        stg = c.sb('stg', [128, 3, 512], F32)
        P.op('dve', lambda e: e.memset(stg[:], 0.0), writes=['stg'])
        P.dma('sp', stg[0:64, 0, :], prm['rw_w2'], writes=['stg'])
        P.dma('sp', stg[64:128, 1, :], prm['rw_a2'], writes=['stg'])
        P.dma('sp', stg[:, 2, :], prm['rw_g2'], writes=['stg'])
        lw = c.sb('lw', [128, 3, 512], BF16)
        P.op('dve', lambda e: e.tensor_copy(out=lw[:], in_=stg[:]), reads=['stg'], writes=['lw'])
        blkf = K['blk64f']
        buf = [c.sb('buf', [128, 513], F32) for _ in range(3)]
        dd = [c.sb('dd', [128, 512], F32) for _ in range(2)]
        xs = c.sb('xs', [128, 14, 512], F32)
        wab = c.sb('wab', [128, 512], BF16)
        sgb = c.sb('sgb', [128, 512], BF16)
        ld2 = [c.sb('ld', [128, 512], F32) for _ in range(2)]
        av2 = [c.sb('av', [128, 512], F32) for _ in range(2)]
        gv2 = [c.sb('gv', [128, 512], F32) for _ in range(2)]
        kk2 = [c.sb('kk', [128, 512], F32) for _ in range(2)]
        km2 = [c.sb('km', [128, 512], F32) for _ in range(2)]
        bb2 = [c.sb('bb', [128, 512], F32) for _ in range(2)]
        t12 = [c.sb('t1', [128, 512], F32) for _ in range(2)]
        t22 = [c.sb('t2', [128, 512], F32) for _ in range(2)]
        lg2 = [c.sb('lg', [128, 512], F32) for _ in range(2)]
        eg2 = [c.sb('eg', [128, 512], F32) for _ in range(2)]
        egl2 = [c.sb('egl', [128, 512], F32) for _ in range(2)]
        gl2 = [c.sb('gl', [128, 8], F32) for _ in range(2)]
        of = [c.sb('of', [128, 4, 512], BF16) for _ in range(2)]
        o32 = [c.sb('o32', [128, 512], F32) for _ in range(2)]
        tb = [c.sb('tb', [128, 3, 512], BF16) for _ in range(2)]
        tt = [c.sb('tt', [128, 4, 128], BF16) for _ in range(2)]
        pz = [c.ps('pz', [128, 512], F32) for _ in range(2)]
        pb = [c.ps('pb', [128, 512], F32) for _ in range(2)]
        tp = [c.ps('tp', [128, 4, 128], BF16) for _ in range(2)]
        n = 0
        nt = 0
        no = 0
        for s in range(nseq):
            for g in range(S // 512):
                t0 = s * S + g * 512
                for ci in range(14):
                    b3 = n % 3
                    b = n % 2
                    en = 'dve' if n % 2 == 0 else 'pool'
                    n += 1
                    if g == 0:
                        P.op('pool', lambda e, b3=b3: e.memset(buf[b3][:, 0:1], 0.0), writes=[('buf', b3)])
                        P.dma('sp', buf[b3][:, 1:513], rw_raw[ci * 128:(ci + 1) * 128, t0:t0 + 512], writes=[('buf', b3)])
                    else:
                        P.dma('sp', buf[b3][:, :], rw_raw[ci * 128:(ci + 1) * 128, t0 - 1:t0 + 512], writes=[('buf', b3)])
                    P.op(en, lambda e, b3=b3, b=b: e.tensor_tensor(out=dd[b][:], in0=buf[b3][:, 0:512], in1=buf[b3][:, 1:513], op=ALU.subtract),
                         reads=[('buf', b3)], writes=[('dd', b)])
                    P.op('dve', lambda e, b3=b3, b=b, ci=ci: e.scalar_tensor_tensor(out=xs[:, ci, :], in0=dd[b][:], scalar=mu[:, ci:ci + 1],
                                                                                in1=buf[b3][:, 1:513], op0=ALU.mult, op1=ALU.add),
                         reads=[('dd', b), ('buf', b3), 'mu'], writes=[('xs', ci)])
                P.op('act', lambda e: e.activation(out=wab[0:64, :], in_=xs[0:64, 12, :], func=AF.Tanh), reads=[('xs', 12)], writes=['wab'])
                P.op('act', lambda e: e.copy(out=wab[64:128, :], in_=xs[64:128, 12, :]), reads=[('xs', 12)], writes=['wab'])
                P.op('act', lambda e: e.activation(out=sgb[:], in_=xs[:, 13, :], func=AF.Sigmoid), reads=[('xs', 13)], writes=['sgb'])
                ob = no % 2
                no += 1
                for fc in range(4):
                    fs = slice(fc * 128, (fc + 1) * 128)
                    zb = fc % 2
                    P.op('pe', lambda e, fs=fs, zb=zb: e.matmul(pz[zb][:], lw[:, 0, fs], wab[:], start=True, stop=True), reads=['lw', 'wab'], writes=[('pz', zb)])
                    P.op('act', lambda e, zb=zb, fc=fc: e.activation(out=ld2[fc % 2][:], in_=pz[zb][:], func=AF.Sigmoid, bias=pc[:, 0, fc:fc + 1]),
                         reads=[('pz', zb), 'pc'], writes=[('ld', fc % 2)])
                    P.op('dve', lambda e: e.tensor_scalar(out=ld2[fc % 2][:], in0=ld2[fc % 2][:], scalar1=RW_LD, scalar2=None, op0=ALU.mult), reads=[('ld', fc % 2)], writes=[('ld', fc % 2)])
                    P.op('pe', lambda e, fs=fs, zb=zb: e.matmul(pb[zb][:], lw[:, 1, fs], wab[:], start=True, stop=True), reads=['lw', 'wab'], writes=[('pb', zb)])
                    P.op('act', lambda e, zb=zb, fc=fc: e.activation(out=av2[fc % 2][:], in_=pb[zb][:], func=AF.Sigmoid, bias=pc[:, 1, fc:fc + 1]),
                         reads=[('pb', zb), 'pc'], writes=[('av', fc % 2)])
                    P.op('dve', lambda e, fc=fc: e.tensor_scalar(out=kk2[fc % 2][:], in0=xs[:, 4 + fc, :], scalar1=pc[:, 2, fc:fc + 1], scalar2=None, op0=ALU.mult),
                         reads=[('xs', 4 + fc), 'pc'], writes=[('kk', fc % 2)])
                    P.op('act', lambda e: e.activation(out=t12[fc % 2][:], in_=kk2[fc % 2][:], func=AF.Square), reads=[('kk', fc % 2)], writes=[('t1', fc % 2)])
                    P.op('pe', lambda e, zb=zb: e.matmul(pz[zb][:], blkf, t12[fc % 2][:], start=True, stop=True), reads=[('t1', fc % 2), 'blk64f'], writes=[('pz', zb)])
                    P.op('act', lambda e, zb=zb: e.sqrt(out=t12[fc % 2][:], in_=pz[zb][:]), reads=[('pz', zb)], writes=[('t1', fc % 2)])
                    P.op('dve', lambda e: e.tensor_scalar(out=t12[fc % 2][:], in0=t12[fc % 2][:], scalar1=1e-12, scalar2=None, op0=ALU.max), reads=[('t1', fc % 2)], writes=[('t1', fc % 2)])
                    P.op('dve', lambda e: e.reciprocal(out=t12[fc % 2][:], in_=t12[fc % 2][:]), reads=[('t1', fc % 2)], writes=[('t1', fc % 2)])
                    P.op('dve', lambda e: e.tensor_tensor(out=kk2[fc % 2][:], in0=kk2[fc % 2][:], in1=t12[fc % 2][:], op=ALU.mult), reads=[('kk', fc % 2), ('t1', fc % 2)], writes=[('kk', fc % 2)])
                    P.op('pool', lambda e, fc=fc: e.tensor_scalar(out=t22[fc % 2][:], in0=av2[fc % 2][:], scalar1=-1.0, scalar2=pc[:, 3, fc:fc + 1], op0=ALU.add, op1=ALU.mult),
                         reads=[('av', fc % 2), 'pc'], writes=[('t2', fc % 2)])
                    P.op('dve', lambda e, fc=fc: e.scalar_tensor_tensor(out=km2[fc % 2][:], in0=t22[fc % 2][:], scalar=1.0, in1=xs[:, 4 + fc, :], op0=ALU.add, op1=ALU.mult),
                         reads=[('t2', fc % 2), ('xs', 4 + fc)], writes=[('km', fc % 2)])
                    P.op('pool', lambda e: e.tensor_tensor(out=bb2[fc % 2][:], in0=kk2[fc % 2][:], in1=av2[fc % 2][:], op=ALU.mult), reads=[('kk', fc % 2), ('av', fc % 2)], writes=[('bb', fc % 2)])
                    P.op('pe', lambda e, fs=fs, zb=zb: e.matmul(pb[zb][:], lw[:, 2, fs], sgb[:], start=True, stop=True), reads=['lw', 'sgb'], writes=[('pb', zb)])
                    P.op('act', lambda e, zb=zb: e.copy(out=gv2[fc % 2][:], in_=pb[zb][:]), reads=[('pb', zb)], writes=[('gv', fc % 2)])
                    P.dma('pool', rwg[fs, t0:t0 + 512], gv2[fc % 2][:], reads=[('gv', fc % 2)])
                    P.op('dve', lambda e, fc=fc: e.scalar_tensor_tensor(out=t12[fc % 2][:], in0=xs[:, fc, :], scalar=pc[:, 4, fc:fc + 1], in1=km2[fc % 2][:], op0=ALU.mult, op1=ALU.mult),
                         reads=[('xs', fc), 'pc', ('km', fc % 2)], writes=[('t1', fc % 2)])
                    P.op('pe', lambda e, zb=zb: e.matmul(pz[zb][:], blkf, t12[fc % 2][:], start=True, stop=True), reads=[('t1', fc % 2), 'blk64f'], writes=[('pz', zb)])
                    P.op('dve', lambda e, zb=zb, fc=fc: e.tensor_tensor(out=t12[fc % 2][:], in0=pz[zb][:], in1=xs[:, 8 + fc, :], op=ALU.mult),
                         reads=[('pz', zb), ('xs', 8 + fc)], writes=[('t1', fc % 2)])
                    o3 = no % 2
                    P.op('dve', lambda e, o3=o3: e.tensor_tensor(out=o32[o3][:], in0=t12[fc % 2][:], in1=gv2[fc % 2][:], op=ALU.mult), reads=[('t1', fc % 2), ('gv', fc % 2)], writes=[('o32', o3)])
                    P.dma('pool', rwbv[fs, t0:t0 + 512], o32[o3][:], reads=[('o32', o3)])
                    for cc in range(8):
                        cs = slice(cc * 64, (cc + 1) * 64)
                        P.op('dve', lambda e, cs=cs: e.tensor_tensor_scan(out=lg2[fc % 2][:, cs], data0=K['ones_f'][:, 0:64], data1=ld2[fc % 2][:, cs], initial=0.0,
                                                                         op0=ALU.mult, op1=ALU.add),
                             reads=[('ld', fc % 2), 'ones_f'], writes=[('lg', fc % 2)])
                    lg3 = lg2[fc % 2][:].rearrange("p (c k) -> p c k", k=64)
                    P.op('act', lambda e: e.activation(out=eg2[fc % 2][:], in_=lg2[fc % 2][:], func=AF.Exp), reads=[('lg', fc % 2)], writes=[('eg', fc % 2)])
                    P.op('act', lambda e: e.copy(out=gl2[fc % 2][:], in_=eg2[fc % 2][:].rearrange("p (c k) -> p c k", k=64)[:, :, 63]), reads=[('eg', fc % 2)], writes=[('gl', fc % 2)])
                    P.dma('pool', rwgL[fs, t0 // 64:t0 // 64 + 8], gl2[fc % 2][:], reads=[('gl', fc % 2)])
                    P.op('dve', lambda e, fc=fc, ob=ob: e.tensor_tensor(out=of[ob][:, 1, :], in0=xs[:, fc, :], in1=eg2[fc % 2][:], op=ALU.mult),
                         reads=[('xs', fc), ('eg', fc % 2)], writes=[('of', ob)])
                    for cc in range(8):
                        cs = slice(cc * 64, (cc + 1) * 64)
                        P.op('act', lambda e, cs=cs, cc=cc: e.activation(out=egl2[fc % 2][:, cs], in_=lg2[fc % 2][:, cs], func=AF.Exp, scale=-1.0,
                                                                         bias=lg2[fc % 2][:, cc * 64 + 63:cc * 64 + 64]),
                             reads=[('lg', fc % 2)], writes=[('egl', fc % 2)])
                    tbb = nt % 2
                    P.op('pool', lambda e, tbb=tbb: e.tensor_tensor(out=tb[tbb][:, 1, :], in0=km2[fc % 2][:], in1=egl2[fc % 2][:], op=ALU.mult), reads=[('km', fc % 2), ('egl', fc % 2)], writes=[('tb', tbb)])
                    P.op('pool', lambda e, tbb=tbb: e.tensor_tensor(out=tb[tbb][:, 2, :], in0=bb2[fc % 2][:], in1=egl2[fc % 2][:], op=ALU.mult), reads=[('bb', fc % 2), ('egl', fc % 2)], writes=[('tb', tbb)])
                    P.op('act', lambda e, tbb=tbb, fc=fc: e.copy(out=tb[tbb][:, 0, :], in_=xs[:, 8 + fc, :]), reads=[('xs', 8 + fc)], writes=[('tb', tbb)])
                    P.op('act', lambda e: e.activation(out=eg2[fc % 2][:], in_=lg2[fc % 2][:], func=AF.Exp, scale=-1.0), reads=[('lg', fc % 2)], writes=[('eg', fc % 2)])
                    P.op('dve', lambda e, ob=ob: e.tensor_tensor(out=of[ob][:, 2, :], in0=bb2[fc % 2][:], in1=eg2[fc % 2][:], op=ALU.mult), reads=[('bb', fc % 2), ('eg', fc % 2)], writes=[('of', ob)])
                    P.op('dve', lambda e, ob=ob: e.tensor_tensor(out=of[ob][:, 3, :], in0=km2[fc % 2][:], in1=eg2[fc % 2][:], op=ALU.mult), reads=[('km', fc % 2), ('eg', fc % 2)], writes=[('of', ob)])
                    P.op('dve', lambda e: e.tensor_tensor(out=t22[fc % 2][:], in0=lg2[fc % 2][:], in1=ld2[fc % 2][:], op=ALU.subtract), reads=[('lg', fc % 2), ('ld', fc % 2)], writes=[('t2', fc % 2)])
                    P.op('act', lambda e: e.activation(out=t22[fc % 2][:], in_=t22[fc % 2][:], func=AF.Exp), reads=[('t2', fc % 2)], writes=[('t2', fc % 2)])
                    P.op('dve', lambda e, ob=ob: e.tensor_tensor(out=of[ob][:, 0, :], in0=kk2[fc % 2][:], in1=t22[fc % 2][:], op=ALU.mult), reads=[('kk', fc % 2), ('t2', fc % 2)], writes=[('of', ob)])
                    P.dma('pool', rwF[:, fs, t0:t0 + 512].rearrange("a p t -> p a t"), of[ob][:], reads=[('of', ob)])
                    for a3 in range(3):
                        tq = nt % 2
                        nt += 1
                        for j in range(4):
                            P.op('pe', lambda e, tq=tq, j=j, a3=a3, tbb=tbb: e.transpose(out=tp[tq][:, j, :], in_=tb[tbb][:, a3, j * 128:(j + 1) * 128], identity=K['idb'][:]),
                                 reads=[('tb', tbb)], writes=[('tp', tq)])
                        P.op('act', lambda e, tq=tq: e.copy(out=tt[tq][:], in_=tp[tq][:]), reads=[('tp', tq)], writes=[('tt', tq)])
                        P.dma('pool', rwT[a3, t0:t0 + 512, fs].rearrange("(j p) d -> p j d", p=128), tt[tq][:], reads=[('tt', tq)])
                    ob = no % 2
                    no += 1


def stage_rw_core(P, K, rwF, rwT, rwgL, rwbv, rwg, prm, mixT, S, nseq):
    GT = 256
    NCH = GT // 64
    with Ctx(P) as c:
        mk = c.sb('mk', [64, 4, 8, 64], F32)
        I8 = c.sb('I8', [64, 8, 64], F32)
        m0 = c.sb('m0', [64, 4, 64], F32)
        U = K['triu'][0:64, 0:64]
        Id = K['idf'][0:64, 0:64]
        P.op('dve', lambda e: e.tensor_tensor(out=m0[:, 2, :], in0=U, in1=Id, op=ALU.subtract), reads=['triu', 'idf'], writes=['m0'])
        P.op('dve', lambda e: e.tensor_scalar(out=m0[:, 0, :], in0=m0[:, 2, :], scalar1=-1.0, scalar2=None, op0=ALU.mult), reads=['m0'], writes=['m0'])
        P.op('dve', lambda e: e.tensor_copy(out=m0[:, 1, :], in_=U), reads=['triu'], writes=['m0'])
        P.op('dve', lambda e: e.tensor_scalar(out=m0[:, 3, :], in0=U, scalar1=-1.0, scalar2=None, op0=ALU.add), reads=['triu'], writes=['m0'])
        for h in range(8):
            P.op('dve', lambda e, h=h: e.tensor_copy(out=mk[:, :, h, :], in_=m0[:]), reads=['m0'], writes=['mk'])
            P.op('dve', lambda e, h=h: e.tensor_copy(out=I8[:, h, :], in_=Id), reads=['idf'], writes=['I8'])
        lnp = c.sb('lnp', [128, 2, 4], F32)
        P.dma('sp', lnp[:], prm['rw_ln'], writes=['lnp'])
        XF = [c.sb('XF', [64, 4, 8, GT], BF16) for _ in range(2)]
        XT = [c.sb('XT', [64, 3, NCH, 512], BF16) for _ in range(2)]
        gL = [c.sb('gL', [64, 8, NCH], F32) for _ in range(2)]
        bvt = [c.sb('bvt', [128, 4, GT], F32) for _ in range(2)]
        gt = [c.sb('gt', [128, 4, GT], F32) for _ in range(2)]
        yF = [c.sb('yF', [128, 4, GT], F32) for _ in range(2)]
        mo = [c.sb('mo', [128, 4, GT], BF16) for _ in range(2)]
        NT = [[c.sb('NT', [64, 8, 64], F32) for _ in range(2)] for _ in range(NCH)]
        NN = [[c.sb('NN', [64, 8, 64], F32) for _ in range(2)] for _ in range(NCH)]
        Wt = [c.sb('Wt', [64, 8, 64], F32) for _ in range(NCH)]
        CbT = [c.sb('CbT', [64, 8, 64], BF16) for _ in range(NCH)]
        BkT = [c.sb('BkT', [64, 8, 64], BF16) for _ in range(NCH)]
        CkT = [c.sb('CkT', [64, 8, 64], BF16) for _ in range(NCH)]
        raw = [c.sb('raw', [64, 8, 64], F32) for _ in range(3)]
        Hs = [c.sb('Hs', [64, 8, 64], F32) for _ in range(nseq)]
        Hb = [c.sb('Hb', [64, 8, 64], BF16) for _ in range(nseq)]
        Rr = c.sb('Rr', [64, 8, 64], F32)
        Un = c.sb('Un', [64, 8, 64], BF16)
        ysq = c.sb('ysq', [64, 8, 64], F32)
        yn = c.sb('yn', [64, 8, 64], F32)
        st = c.sb('st', [64, 4, 8], F32)
        for s in range(nseq):
            P.op('pool', lambda e, s=s: e.memset(Hs[s][:], 0.0), writes=[('Hs', s)])
            P.op('pool', lambda e, s=s: e.memset(Hb[s][:], 0.0), writes=[('Hb', s)])
        bank = [c.ps('bk', [128, 512], F32) for _ in range(8)]
        st_ = {'b': 0, 'r': 0}

        def nb():
            i = st_['b'] % 8
            st_['b'] += 1
            return i

        def bv(i):
            return bank[i][0:64, :].rearrange("p (h k) -> p h k", h=8)

        ng = 0
        for g in range(S // GT):
            for s in range(nseq):
                lb = ng % 2
                ng += 1
                t0 = s * S + g * GT
                for a in range(4):
                    P.dma('sp', XF[lb][:, a], rwF[a, :, t0:t0 + GT].rearrange("(h k) t -> k h t", k=64), writes=[('XF', lb)])
                for a in range(3):
                    P.dma('sp', XT[lb][:, a], rwT[a, t0:t0 + GT, :].rearrange("(c p) f -> p c f", p=64), writes=[('XT', lb)])
                P.dma('sp', gL[lb][:], rwgL[:, t0 // 64:t0 // 64 + NCH].rearrange("(h k) c -> k h c", k=64), writes=[('gL', lb)])
                P.dma('sp', bvt[lb][:], rwbv[:, t0:t0 + GT].rearrange("(f p) t -> p f t", p=128), writes=[('bvt', lb)])
                P.dma('sp', gt[lb][:], rwg[:, t0:t0 + GT].rearrange("(f p) t -> p f t", p=128), writes=[('gt', lb)])
                for ch in range(NCH):
                    cs = slice(ch * 64, (ch + 1) * 64)
                    specs = [
                        (2, 0, 0, NT[ch][0], 'f32'),
                        (0, 2, 3, NN[ch][0], 'f32'),
                        (2, 1, 1, CbT[ch], 'bf'),
                        (3, 0, 2, BkT[ch], 'bf'),
                        (3, 1, 1, CkT[ch], 'bf'),
                    ]
                    for (la, ra, mi, dst, kind) in specs:
                        bi = nb()
                        for h in range(8):
                            P.op('pe', lambda e, bi=bi, h=h, la=la, ra=ra, cs=cs: e.matmul(bv(bi)[:, h, :], XF[lb][:, la, h, cs], XF[lb][:, ra, h, cs],
                                                                                       start=True, stop=True),
                                 reads=[('XF', lb)], writes=[('bk', bi)])
                        if kind == 'f32':
                            P.op('dve', lambda e, bi=bi, mi=mi, dst=dst: e.tensor_tensor(out=dst[:], in0=bv(bi), in1=mk[:, mi], op=ALU.mult),
                                 reads=[('bk', bi), 'mk'], writes=[id(dst)])
                        else:
                            ri = st_['r'] % 3
                            st_['r'] += 1
                            P.op('act', lambda e, bi=bi, ri=ri: e.copy(out=raw[ri][:], in_=bv(bi)), reads=[('bk', bi)], writes=[('raw', ri)])
                            P.op('pool', lambda e, ri=ri, mi=mi, dst=dst: e.tensor_tensor(out=dst[:], in0=raw[ri][:], in1=mk[:, mi], op=ALU.mult),
                                 reads=[('raw', ri), 'mk'], writes=[id(dst)])
                    P.op('pool', lambda e, ch=ch: e.tensor_tensor(out=Wt[ch][:], in0=NT[ch][0][:], in1=I8[:], op=ALU.add),
                         reads=[id(NT[ch][0]), 'I8'], writes=[id(Wt[ch])])
                for j in range(1, 6):
                    o, nw = (j - 1) % 2, j % 2
                    b1s, b2s, b3s = {}, {}, {}
                    for ch in range(NCH):
                        b1 = b1s[ch] = nb()
                        for h in range(8):
                            P.op('pe', lambda e, h=h: e.matmul(bv(b1)[:, h, :], NT[ch][o][:, h, :], NN[ch][o][:, h, :], start=True, stop=True),
                                 reads=[id(NT[ch][o]), id(NN[ch][o])], writes=[('bk', b1)])
                    if j < 5:
                        for ch in range(NCH):
                            b2 = b2s[ch] = nb()
                            for h in range(8):
                                P.op('pe', lambda e, h=h: e.matmul(bv(b2)[:, h, :], NN[ch][o][:, h, :], NT[ch][o][:, h, :], start=True, stop=True),
                                     reads=[id(NT[ch][o]), id(NN[ch][o])], writes=[('bk', b2)])
                    for ch in range(NCH):
                        b1 = b1s[ch]
                        P.op('act', lambda e: e.copy(out=NN[ch][nw][:], in_=bv(b1)), reads=[('bk', b1)], writes=[id(NN[ch][nw])])
                        if j < 5:
                            b2 = b2s[ch]
                            P.op('dve', lambda e: e.tensor_copy(out=NT[ch][nw][:], in_=bv(b2)), reads=[('bk', b2)], writes=[id(NT[ch][nw])])
                    for ch in range(NCH):
                        b3 = b3s[ch] = nb()
                        for h in range(8):
                            P.op('pe', lambda e, h=h: e.matmul(bv(b3)[:, h, :], NN[ch][nw][:, h, :], Wt[ch][:, h, :], start=True, stop=True),
                                 reads=[id(NN[ch][nw]), id(Wt[ch])], writes=[('bk', b3)])
                    for ch in range(NCH):
                        b3 = b3s[ch]
                        P.op('dve', lambda e: e.tensor_tensor(out=Wt[ch][:], in0=Wt[ch][:], in1=bv(b3), op=ALU.add),
                             reads=[('bk', b3), id(Wt[ch])], writes=[id(Wt[ch])])
                for ch in range(NCH):
                    cs = slice(ch * 64, (ch + 1) * 64)
                    Vh = lambda h: XT[lb][:, 0, ch, h * 64:(h + 1) * 64]
                    bR = nb()
                    for h in range(8):
                        P.op('pe', lambda e, h=h: e.matmul(bv(bR)[:, h, :], XF[lb][:, 0, h, cs], Hb[s][:, h, :], start=True, stop=False),
                             reads=[('XF', lb), ('Hb', s)], writes=[('bk', bR)])
                        P.op('pe', lambda e, h=h: e.matmul(bv(bR)[:, h, :], BkT[ch][:, h, :], Vh(h), start=False, stop=True),
                             reads=[id(BkT[ch]), ('XT', lb)], writes=[('bk', bR)])
                    P.op('act', lambda e: e.copy(out=Rr[:], in_=bv(bR)), reads=[('bk', bR)], writes=['Rr'])
                    bU = nb()
                    for h in range(8):
                        P.op('pe', lambda e, h=h: e.matmul(bv(bU)[:, h, :], Wt[ch][:, h, :], Rr[:, h, :], start=True, stop=True),
                             reads=[id(Wt[ch]), 'Rr'], writes=[('bk', bU)])
                    P.op('dve', lambda e: e.tensor_scalar(out=Un[:], in0=bv(bU), scalar1=-1.0, scalar2=None, op0=ALU.mult), reads=[('bk', bU)], writes=['Un'])
                    bY = nb()
                    for h in range(8):
                        P.op('pe', lambda e, h=h: e.matmul(bv(bY)[:, h, :], XF[lb][:, 1, h, cs], Hb[s][:, h, :], start=True, stop=False),
                             reads=[('XF', lb), ('Hb', s)], writes=[('bk', bY)])
                        P.op('pe', lambda e, h=h: e.matmul(bv(bY)[:, h, :], CkT[ch][:, h, :], Vh(h), start=False, stop=False),
                             reads=[id(CkT[ch]), ('XT', lb)], writes=[('bk', bY)])
                        P.op('pe', lambda e, h=h: e.matmul(bv(bY)[:, h, :], CbT[ch][:, h, :], Un[:, h, :], start=False, stop=True),
                             reads=[id(CbT[ch]), 'Un'], writes=[('bk', bY)])
                    bH = nb()
                    for h in range(8):
                        P.op('pe', lambda e, h=h: e.matmul(bv(bH)[:, h, :], XT[lb][:, 1, ch, h * 64:(h + 1) * 64], Vh(h), start=True, stop=False),
                             reads=[('XT', lb)], writes=[('bk', bH)])
                        P.op('pe', lambda e, h=h: e.matmul(bv(bH)[:, h, :], XT[lb][:, 2, ch, h * 64:(h + 1) * 64], Un[:, h, :], start=False, stop=True),
                             reads=[('XT', lb), 'Un'], writes=[('bk', bH)])
                    for h in range(8):
                        P.op('dve', lambda e, h=h: e.scalar_tensor_tensor(out=Hs[s][:, h, :], in0=Hs[s][:, h, :], scalar=gL[lb][:, h, ch:ch + 1],
                                                                          in1=bv(bH)[:, h, :], op0=ALU.mult, op1=ALU.add),
                             reads=[('Hs', s), ('gL', lb), ('bk', bH)], writes=[('Hs', s)])
                    P.op('act', lambda e: e.copy(out=Hb[s][:], in_=Hs[s][:]), reads=[('Hs', s)], writes=[('Hb', s)])
                    P.op('dve', lambda e: e.tensor_reduce(out=st[:, 0, :], in_=bv(bY), axis=AX.X, op=ALU.add), reads=[('bk', bY)], writes=['st'])
                    P.op('act', lambda e: e.activation(out=ysq[:], in_=bv(bY), func=AF.Square), reads=[('bk', bY)], writes=['ysq'])
                    P.op('dve', lambda e: e.tensor_reduce(out=st[:, 1, :], in_=ysq[:], axis=AX.X, op=ALU.add), reads=['ysq'], writes=['st'])
                    P.op('dve', lambda e: e.tensor_scalar(out=st[:, 0, :], in0=st[:, 0, :], scalar1=1.0 / 64, scalar2=None, op0=ALU.mult), reads=['st'], writes=['st'])
                    P.op('dve', lambda e: e.tensor_tensor(out=st[:, 2, :], in0=st[:, 0, :], in1=st[:, 0, :], op=ALU.mult), reads=['st'], writes=['st'])
                    P.op('dve', lambda e: e.tensor_scalar(out=st[:, 1, :], in0=st[:, 1, :], scalar1=1.0 / 64, scalar2=64e-5, op0=ALU.mult, op1=ALU.add), reads=['st'], writes=['st'])
                    P.op('dve', lambda e: e.tensor_tensor(out=st[:, 1, :], in0=st[:, 1, :], in1=st[:, 2, :], op=ALU.subtract), reads=['st'], writes=['st'])
                    P.op('act', lambda e: e.sqrt(out=st[:, 1, :], in_=st[:, 1, :]), reads=['st'], writes=['st'])
                    P.op('dve', lambda e: e.reciprocal(out=st[:, 1, :], in_=st[:, 1, :]), reads=['st'], writes=['st'])
                    for h in range(8):
                        P.op('dve', lambda e, h=h: e.tensor_scalar(out=yn[:, h, :], in0=bv(bY)[:, h, :], scalar1=st[:, 0, h:h + 1], scalar2=st[:, 1, h:h + 1],
                                                                   op0=ALU.subtract, op1=ALU.mult),
                             reads=[('bk', bY), 'st'], writes=['yn'])
                    bT = nb()
                    for fc in range(4):
                        P.op('pe', lambda e, fc=fc: e.transpose(out=bank[bT][:, fc * 64:(fc + 1) * 64], in_=yn[:, 2 * fc:2 * fc + 2, :].rearrange("p a k -> p (a k)"), identity=Id),
                             reads=['yn', 'idf'], writes=[('bk', bT)])
                    P.op('act', lambda e: e.copy(out=yF[lb][:, :, cs], in_=bank[bT][:, 0:256].rearrange("p (f t) -> p f t", f=4)),
                         reads=[('bk', bT)], writes=[('yF', lb)])
                for fc in range(4):
                    P.op('dve', lambda e, fc=fc: e.tensor_scalar(out=yF[lb][:, fc, :], in0=yF[lb][:, fc, :], scalar1=lnp[:, 0, fc:fc + 1], scalar2=lnp[:, 1, fc:fc + 1],
                                                                 op0=ALU.mult, op1=ALU.add),
                         reads=[('yF', lb), 'lnp'], writes=[('yF', lb)])
                P.op('pool', lambda e: e.tensor_tensor(out=yF[lb][:], in0=yF[lb][:], in1=gt[lb][:], op=ALU.mult), reads=[('yF', lb), ('gt', lb)], writes=[('yF', lb)])
                P.op('pool', lambda e: e.tensor_tensor(out=mo[lb][:], in0=yF[lb][:], in1=bvt[lb][:], op=ALU.add), reads=[('yF', lb), ('bvt', lb)], writes=[('mo', lb)])
                P.dma('pool', mixT[512:1024, t0:t0 + GT].rearrange("(f p) t -> p f t", p=128), mo[lb][:], reads=[('mo', lb)])


def odd_params(inp, o):
    f = np.float32
    col = lambda v: np.ascontiguousarray(np.asarray(v, f).reshape(-1, 128).T)
    cw = np.ascontiguousarray(np.asarray(inp['ml_conv_w'][o], f).reshape(4, 8, 128).transpose(2, 1, 0))
    gbv = np.asarray(inp['ml_gate_b'][o], f)
    gb = np.stack([np.tile(gbv[:4], 2), np.tile(gbv[4:], 2)], 1)
    pc = np.stack([col(inp['rw_w0'][o]), col(inp['rw_a0'][o]), col(inp['rw_k_k'][o]), col(inp['rw_k_a'][o]),
                   col(inp['rw_r_k'][o].reshape(512))], 1)
    ln = np.stack([col(inp['rw_ln_g'][o]), col(inp['rw_ln_b'][o])], 1)
    return {
        'ml_cw': cw.astype(f), 'ml_cb': col(inp['ml_conv_b'][o]), 'ml_gb': np.ascontiguousarray(gb).astype(f),
        'ml_ng_rep': np.ascontiguousarray(np.broadcast_to(np.asarray(inp['ml_norm_g'][o], f).reshape(1, 512), (128, 512))),
        'rw_mu': col(inp['rw_mu'][o]), 'rw_pc': np.ascontiguousarray(pc).astype(f), 'rw_ln': np.ascontiguousarray(ln).astype(f),
        'rw_w2': np.ascontiguousarray(inp['rw_w2'][o]).astype(f), 'rw_a2': np.ascontiguousarray(inp['rw_a2'][o]).astype(f),
        'rw_g2': np.ascontiguousarray(inp['rw_g2'][o]).astype(f),
    }


ODD_SHAPES = {'ml_cw': [128, 8, 4], 'ml_cb': [128, 8], 'ml_gb': [8, 2], 'ml_ng_rep': [128, 512], 'rw_mu': [128, 14],
              'rw_pc': [128, 5, 4], 'rw_ln': [128, 2, 4], 'rw_w2': [64, 512], 'rw_a2': [64, 512], 'rw_g2': [128, 512]}


def odd_scratch(dt, S, nseq, pfx=""):
    T = S * nseq
    NB = S // 128
    R8 = nseq * 4
    return dict(
        mqk_raw=dt(pfx + "mqk_raw", [1024, T]), mif=dt(pfx + "mif", [8, T]), rw_raw=dt(pfx + "rw_raw", [1792, T]),
        v_aug=dt(pfx + "v_aug", [T, 516], BF16), moA=dt(pfx + "moA", [T, 512]),
        mqkT=dt(pfx + "mqkT", [1024, T], BF16), mkA=dt(pfx + "mkA", [T, 512], BF16),
        mgM=dt(pfx + "mgM", [R8, S]), mcol=dt(pfx + "mcol", [2, 128, NB * R8]), mend=dt(pfx + "mend", [NB * R8]),
        rwF=dt(pfx + "rwF", [4, 512, T], BF16), rwT=dt(pfx + "rwT", [3, T, 512], BF16), rwgL=dt(pfx + "rwgL", [512, T // 64]),
        rwbv=dt(pfx + "rwbv", [512, T]), rwg=dt(pfx + "rwg", [512, T]),
    )


def emit_odd_layer(P, K, xin, xout, w_in, w_out, gc, prm, sc, mixT, S, nseq, parts=('ml', 'rw')):
    T = S * nseq
    stage_inproj_odd(P, K, xin, w_in, gc, T, sc['mqk_raw'], sc['mif'], sc['rw_raw'], sc['v_aug'], sc['moA'])
    if 'ml' in parts:
        stage_ml_prep(P, K, sc['mqk_raw'], prm, sc['mqkT'], sc['mkA'], S, nseq)
        stage_ml_gates(P, K, sc['mif'], prm, sc['mgM'], sc['mcol'], sc['mend'], S, nseq)
        stage_ml_core(P, K, sc['mqkT'], sc['mkA'], sc['v_aug'], sc['moA'], sc['mgM'], sc['mcol'], sc['mend'], prm, mixT, S, nseq)
    if 'rw' in parts:
        stage_rw_prep(P, K, sc['rw_raw'], prm, sc['rwF'], sc['rwT'], sc['rwgL'], sc['rwbv'], sc['rwg'], S, nseq)
        stage_rw_core(P, K, sc['rwF'], sc['rwT'], sc['rwgL'], sc['rwbv'], sc['rwg'], prm, mixT, S, nseq)
    stage_outproj(P, K, mixT, w_out, xin, xout, T)


def build_test_odd(S, nseq, parts=('ml', 'rw')):
    T = S * nseq
    nc = bass.Bass("TRN2", target_bir_lowering=False)
    nc.allow_low_precision("bf16 matmul operands by design")
    dt = lambda name, shape, d=F32, kind="Internal": nc.dram_tensor(name, list(shape), d, kind=kind).ap()
    x = dt("x", [T, D], kind="ExternalInput")
    w_in = dt("w_in", [D, 3848], kind="ExternalInput")
    w_out = dt("w_out", [D, D], kind="ExternalInput")
    gc = dt("gc", [128, 8], kind="ExternalInput")
    cst = dt("consts", [128, NCONST], kind="ExternalInput")
    prm = {k: dt("p_" + k, v, kind="ExternalInput") for k, v in ODD_SHAPES.items()}
    y = dt("y", [T, D], kind="ExternalOutput")
    mixT = dt("mixT", [1024, T], BF16, kind="ExternalOutput")
    sc = odd_scratch(dt, S, nseq)
    P = Prog(nc)
    with Ctx(P) as c0:
        K = load_consts(P, c0, cst)
        emit_odd_layer(P, K, x, y, w_in, w_out, gc, prm, sc, mixT, S, nseq, parts)
    return nc

def even_params(inp, e):
    f = np.float32
    return {
        'qk_g': np.ascontiguousarray(np.stack([np.tile(inp['da_q_g'][e], 2), np.tile(inp['da_k_g'][e], 2)], 1)).astype(f),
        'sg_ln_g_rep': np.ascontiguousarray(np.broadcast_to(inp['sg_ln_g'][e].reshape(1, 512), (128, 512))).astype(f),
        'sg_ln_b_rep': np.ascontiguousarray(np.broadcast_to(inp['sg_ln_b'][e].reshape(1, 512), (128, 512))).astype(f),
        'lam_rep': np.ascontiguousarray(np.broadcast_to(inp['da_lambda'][e].reshape(1, 256), (128, 256))).astype(f),
        'subln_col': np.ascontiguousarray(inp['da_subln_g'][e].reshape(128, 1)).astype(f),
        'sg_wT': np.ascontiguousarray(inp['sg_w'][e].transpose(0, 2, 1)).astype(f),
        'sg_b_rep': np.ascontiguousarray(np.broadcast_to(inp['sg_b'][e][None, :, None, :], (128, 4, 4, 128)).reshape(128, 2048)).astype(f),
    }


EVEN_SHAPES = {'qk_g': [128, 2], 'sg_ln_g_rep': [128, 512], 'sg_ln_b_rep': [128, 512], 'lam_rep': [128, 256],
               'subln_col': [128, 1], 'sg_wT': [4, 128, 128], 'sg_b_rep': [128, 2048]}


def gcol_of(g):
    return np.ascontiguousarray(np.asarray(g, np.float32).reshape(8, 128).T)


def build_test_even(S, nseq, layer=0):
    T = S * nseq
    nc = bass.Bass("TRN2", target_bir_lowering=False)
    nc.allow_low_precision("bf16 matmul operands by design")
    dt = lambda name, shape, d=F32, kind="Internal": nc.dram_tensor(name, list(shape), d, kind=kind).ap()
    x = dt("x", [T, D], kind="ExternalInput")
    w_in = dt("w_in", [D, 2560], kind="ExternalInput")
    w_out = dt("w_out", [D, D], kind="ExternalInput")
    gc = dt("gc", [128, 8], kind="ExternalInput")
    cst = dt("consts", [128, NCONST], kind="ExternalInput")
    prm = {k: dt("p_" + k, v, kind="ExternalInput") for k, v in EVEN_SHAPES.items()}
    y = dt("y", [T, D], kind="ExternalOutput")
    qkT = dt("qkT", [1024, T], BF16)
    vA = dt("vA", [T, 512], BF16)
    uF = dt("uF", [512, T], F32)
    vG = dt("vG", [T, 512], BF16)
    mixT = dt("mixT", [1024, T], BF16)
    lam_init = 0.8 - 0.6 * math.exp(-0.3 * layer)
    P = Prog(nc)
    with Ctx(P) as c0:
        K = load_consts(P, c0, cst)
        stage_inproj_even(P, K, x, w_in, gc, prm, T, qkT, vA, uF, vG)
        stage_attn(P, K, qkT, vA, mixT, prm, lam_init, S, nseq)
        stage_sgu(P, K, vG, uF, mixT, prm, T)
        stage_outproj(P, K, mixT, w_out, x, y, T)
    return nc


def emit_even_layer(P, K, xin, xout, w_in, w_out, gc, prm, sc, mixT, S, nseq, layer):
    T = S * nseq
    lam_init = 0.8 - 0.6 * math.exp(-0.3 * layer)
    stage_inproj_even(P, K, xin, w_in, gc, prm, T, sc['qkT'], sc['vA'], sc['uF'], sc['vG'])
    stage_attn(P, K, sc['qkT'], sc['vA'], mixT, prm, lam_init, S, nseq)
    stage_sgu(P, K, sc['vG'], sc['uF'], mixT, prm, T)
    stage_outproj(P, K, mixT, w_out, xin, xout, T)


def build_full(S, nseq, depth=4):
    T = S * nseq
    nc = bass.Bass("TRN2", target_bir_lowering=False)
    nc.allow_low_precision("bf16 matmul operands by design")
    dt = lambda name, shape, d=F32, kind="Internal": nc.dram_tensor(name, list(shape), d, kind=kind).ap()
    ne, no = (depth + 1) // 2, depth // 2
    x = dt("x", [T, D], kind="ExternalInput")
    out = dt("out", [T, D], kind="ExternalOutput")
    cst = dt("consts", [128, NCONST], kind="ExternalInput")
    gmix = dt("gmix", [depth, 128, 8], kind="ExternalInput")
    gffn = dt("gffn", [depth, 128, 8], kind="ExternalInput")
    ev_w_in = dt("ev_w_in", [ne, D, 2560], kind="ExternalInput")
    ev_w_out = dt("ev_w_out", [ne, D, D], kind="ExternalInput")
    od_w_in = dt("od_w_in", [max(no, 1), D, 3848], kind="ExternalInput")
    od_w_out = dt("od_w_out", [max(no, 1), D, D], kind="ExternalInput")
    wg = dt("ffn_w_gate", [depth, D, DFF], kind="ExternalInput")
    wu = dt("ffn_w_up", [depth, D, DFF], kind="ExternalInput")
    wd = dt("ffn_w_down", [depth, DFF, D], kind="ExternalInput")
    eprm = [{k: dt(f"e{e}_{k}", v, kind="ExternalInput") for k, v in EVEN_SHAPES.items()} for e in range(ne)]
    oprm = [{k: dt(f"o{o}_{k}", v, kind="ExternalInput") for k, v in ODD_SHAPES.items()} for o in range(no)]
    xa = dt("xa", [T, D])
    xb = dt("xb", [T, D])
    mixT = dt("mixT", [1024, T], BF16)
    esc = dict(qkT=dt("qkT", [1024, T], BF16), vA=dt("vA", [T, 512], BF16), uF=dt("uF", [512, T]), vG=dt("vG", [T, 512], BF16))
    osc = odd_scratch(dt, S, nseq) if no else None
    P = Prog(nc)
    with Ctx(P) as c0:
        K = load_consts(P, c0, cst)
        xcur = x
        for l in range(depth):
            if l % 2 == 0:
                e = l // 2
                emit_even_layer(P, K, xcur, xa, ev_w_in[e], ev_w_out[e], gmix[l], eprm[e], esc, mixT, S, nseq, l)
            else:
                o = l // 2
                emit_odd_layer(P, K, xcur, xa, od_w_in[o], od_w_out[o], gmix[l], oprm[o], osc, mixT, S, nseq)
            xo = out if l == depth - 1 else xb
            stage_ffn(P, K, xa, xo, wg[l], wu[l], wd[l], gffn[l], T)
            xcur = xo
    return nc


def host_inputs(inputs, depth=4):
    f = np.float32
    ne, no = (depth + 1) // 2, depth // 2
    com = {
        "consts": host_consts(),
        "gmix": np.ascontiguousarray(np.stack([gcol_of(inputs['norm_mix_g'][l]) for l in range(depth)])),
        "gffn": np.ascontiguousarray(np.stack([gcol_of(inputs['norm_ffn_g'][l]) for l in range(depth)])),
        "ev_w_in": np.ascontiguousarray(inputs['ev_w_in'][:ne], f), "ev_w_out": np.ascontiguousarray(inputs['ev_w_out'][:ne], f),
        "od_w_in": np.ascontiguousarray(inputs['od_w_in'][:max(no, 1)], f), "od_w_out": np.ascontiguousarray(inputs['od_w_out'][:max(no, 1)], f),
        "ffn_w_gate": np.ascontiguousarray(inputs['ffn_w_gate'][:depth], f), "ffn_w_up": np.ascontiguousarray(inputs['ffn_w_up'][:depth], f),
        "ffn_w_down": np.ascontiguousarray(inputs['ffn_w_down'][:depth], f),
    }
    for e in range(ne):
        for k, v in even_params(inputs, e).items():
            com[f"e{e}_{k}"] = v
    for o in range(no):
        for k, v in odd_params(inputs, o).items():
            com[f"o{o}_{k}"] = v
    return com


_NC_CACHE = {}


def kernel(**inputs):
    inputs = {k: np.asarray(v) for k, v in inputs.items()}
    x = inputs['x']
    B, S, _ = x.shape
    ncores = 8
    nseq = B // ncores
    depth = inputs['norm_mix_g'].shape[0]
    key = (S, nseq, depth)
    if key not in _NC_CACHE:
        _NC_CACHE[key] = build_full(S, nseq, depth)
    nc = _NC_CACHE[key]
    com = host_inputs(inputs, depth)
    in_maps = []
    for i in range(ncores):
        m = dict(com)
        m["x"] = np.ascontiguousarray(x[i * nseq:(i + 1) * nseq].reshape(nseq * S, D), np.float32)
        in_maps.append(m)
    res = run_bass_kernel_spmd(nc, in_maps, core_ids=list(range(ncores)))
    outs = [np.asarray(r["out"]).reshape(nseq, S, D) for r in res.results]
    return np.concatenate(outs, 0).astype(np.float32)
```

```python
import math
from contextlib import ExitStack
import numpy as np
import ml_dtypes
import concourse.bass as bass
import concourse.mybir as mybir
from concourse.bass_utils import run_bass_kernel_spmd

F32 = mybir.dt.float32
BF16 = mybir.dt.bfloat16
AF = mybir.ActivationFunctionType
ALU = mybir.AluOpType
AX = mybir.AxisListType

D = 1024
DFF = 2816
NFF = DFF // 128
EPOCH = 30000
NSLOT = 24
EPS = 1e-6


class Prog:
    def __init__(self, nc):
        self.nc = nc
        self.E = {'pe': nc.tensor, 'act': nc.scalar, 'dve': nc.vector, 'pool': nc.gpsimd, 'sp': nc.sync}
        self.cnt = {e: 0 for e in ('pe', 'act', 'dve', 'pool')}
        self.csem = {e: [] for e in self.cnt}
        self.known = {e: {} for e in self.E}
        self.dsem = {}
        self.dgen = [0] * NSLOT
        self.dval = [0] * NSLOT
        for i in range(NSLOT):
            self.dsem[(i, 0)] = nc.alloc_semaphore(f"dq{i}_0")
        self.rr = 0
        self.W = {}
        self.R = {}
        self.uid = 0

    def uname(self, s):
        self.uid += 1
        return f"{s}_{self.uid}"

    def _wait(self, e, tok):
        if tok[0] == 'c':
            _, e2, seq = tok
            kk = ('c', e2)
            if self.known[e].get(kk, -1) >= seq:
                return
            ep = seq // EPOCH
            self.E[e].wait_ge(self.csem[e2][ep], seq - ep * EPOCH + 1)
            self.known[e][kk] = seq
        else:
            _, slot, gen, val = tok
            kk = ('d', slot, gen)
            if self.known[e].get(kk, 0) >= val:
                return
            self.E[e].wait_ge(self.dsem[(slot, gen)], val)
            self.known[e][kk] = val

    def _deps(self, e, reads, writes):
        for k in reads:
            t = self.W.get(k)
            if t is not None:
                if t[0] == 'c' and t[1] == e and e == 'pe':
                    continue
                self._wait(e, t)
        for k in writes:
            t = self.W.get(k)
            if t is not None and not (t[0] == 'c' and t[1] == e):
                self._wait(e, t)
            for t in self.R.get(k, {}).values():
                if t[0] == 'c' and t[1] == e:
                    continue
                self._wait(e, t)

    def _record(self, tok, src, reads, writes):
        for k in reads:
            self.R.setdefault(k, {})[src] = tok
        for k in writes:
            self.W[k] = tok
            self.R[k] = {}

    def op(self, e, fn, reads=(), writes=()):
        self._deps(e, reads, writes)
        ins = fn(self.E[e])
        seq = self.cnt[e]
        self.cnt[e] += 1
        ep = seq // EPOCH
        while len(self.csem[e]) <= ep:
            self.csem[e].append(self.nc.alloc_semaphore(f"c_{e}_{len(self.csem[e])}"))
        ins.then_inc(self.csem[e][ep], 1)
        self._record(('c', e, seq), ('c', e), reads, writes)
        return ins

    def dma(self, q, out, in_, reads=(), writes=(), **kw):
        slot = self.rr
        self.rr = (self.rr + 1) % NSLOT
        if self.dval[slot] > 60000:
            self._wait(q, ('d', slot, self.dgen[slot], self.dval[slot]))
            self.dgen[slot] += 1
            self.dval[slot] = 0
            self.dsem[(slot, self.dgen[slot])] = self.nc.alloc_semaphore(f"dq{slot}_{self.dgen[slot]}")
        gen = self.dgen[slot]
        if self.dval[slot] > 0:
            self._wait(q, ('d', slot, gen, self.dval[slot]))
        self._deps(q, reads, writes)
        ins = self.E[q].dma_start(out=out, in_=in_, **kw)
        self.dval[slot] += 16
        ins.then_inc(self.dsem[(slot, gen)], 16)
        self._record(('d', slot, gen, self.dval[slot]), ('d', slot), reads, writes)
        return ins

    def barrier(self):
        toks = [('c', e, self.cnt[e] - 1) for e in self.cnt if self.cnt[e] > 0]
        toks += [('d', s, self.dgen[s], self.dval[s]) for s in range(NSLOT) if self.dval[s] > 0]
        for e in self.E:
            for t in toks:
                self._wait(e, t)
        self.W.clear()
        self.R.clear()


class Ctx:
    def __init__(self, P):
        self.P = P
        self.st = ExitStack()

    def __enter__(self):
        self.st.__enter__()
        return self

    def __exit__(self, *a):
        self.P.barrier()
        return self.st.__exit__(*a)

    def sb(self, name, shape, dt):
        return self.st.enter_context(self.P.nc.sbuf_tensor(self.P.uname(name), list(shape), dt))

    def ps(self, name, shape, dt=F32):
        return self.st.enter_context(self.P.nc.psum_tensor(self.P.uname(name), list(shape), dt))


NCONST = 128 * 3 + 2048


def host_consts():
    cm = np.zeros((128, NCONST), np.float32)
    cm[:, 0:128] = np.eye(128)
    blk = np.zeros((128, 128), np.float32)
    blk[:64, :64] = 1
    blk[64:, 64:] = 1
    cm[:, 128:256] = blk
    k = np.arange(128)[:, None]
    cm[:, 256:384] = (k <= np.arange(128)[None, :])
    q = np.arange(512)[None, :]
    for r in range(4):
        cm[:, 384 + r * 512:384 + (r + 1) * 512] = (128 * r + k <= q)
    return cm


def load_consts(P, c, cd):
    K = {}
    cst = c.sb('cst', [128, 384], F32)
    P.dma('sp', cst[:], cd[:, 0:384], writes=['cst'])
    K['idf'] = cst[:, 0:128]
    K['triu'] = cst[:, 256:384]
    K['idb'] = c.sb('idb', [128, 128], BF16)
    K['blk64'] = c.sb('blk64', [128, 128], BF16)
    K['ones_bf'] = c.sb('ones_bf', [128, 128], BF16)
    K['cmask'] = c.sb('cmask', [128, 4, 512], BF16)
    K['ones_f'] = c.sb('ones_f', [128, 128], F32)
    P.op('dve', lambda e: e.tensor_copy(out=K['idb'][:], in_=cst[:, 0:128]), reads=['cst'], writes=['idb'])
    P.op('dve', lambda e: e.tensor_copy(out=K['blk64'][:], in_=cst[:, 128:256]), reads=['cst'], writes=['blk64'])
    with Ctx(P) as c2:
        cm = c2.sb('cm', [128, 2048], F32)
        P.dma('sp', cm[:], cd[:, 384:384 + 2048], writes=['cm'])
        P.op('dve', lambda e: e.tensor_copy(out=K['cmask'][:], in_=cm[:].rearrange("p (r q) -> p r q", r=4)),
             reads=['cm'], writes=['cmask'])
    P.op('dve', lambda e: e.memset(K['ones_bf'][:], 1.0), writes=['ones_bf'])
    P.op('dve', lambda e: e.memset(K['ones_f'][:], 1.0), writes=['ones_f'])
    P.barrier()
    K['blk64f'] = cst[:, 128:256]
    return K


def load_weight_bf(P, c, w_dram, rows, cols, dst, key, stg, gcol=None, col0=0, engs=('dve', 'pool'), piece=None):
    nch = rows // 128
    piece = piece or cols
    n = 0
    for ci in range(nch):
        for p0 in range(0, cols, piece):
            pc = min(piece, cols - p0)
            b = n % 2
            n += 1
            P.dma('sp', stg[b][:, :pc], w_dram[ci * 128:(ci + 1) * 128, col0 + p0:col0 + p0 + pc], writes=[('stg', id(stg), b)])
            en = engs[n % len(engs)]
            if gcol is not None:
                P.op(en, lambda e, ci=ci, b=b, p0=p0, pc=pc: e.tensor_scalar(out=dst[:, ci, p0:p0 + pc], in0=stg[b][:, :pc],
                                                                             scalar1=gcol[:, ci:ci + 1], scalar2=None, op0=ALU.mult),
                     reads=[('stg', id(stg), b), 'gcol'], writes=[key])
            else:
                P.op(en, lambda e, ci=ci, b=b, p0=p0, pc=pc: e.tensor_copy(out=dst[:, ci, p0:p0 + pc], in_=stg[b][:, :pc]),
                     reads=[('stg', id(stg), b)], writes=[key])


def norm_transpose(P, K, xt, xkey, hb, hkey, hT, hTkey, tp, tpkey, ss, rs, j, ncols, tcol0):
    junk = hb
    P.op('act', lambda e: e.activation(out=junk, in_=xt, func=AF.Square, accum_out=ss[:, j:j + 1]),
         reads=[xkey], writes=[hkey, ('ss', id(ss), j)])
    P.op('dve', lambda e: e.tensor_scalar(out=rs[:, j:j + 1], in0=ss[:, j:j + 1], scalar1=1.0 / ncols, scalar2=EPS,
                                          op0=ALU.mult, op1=ALU.add),
         reads=[('ss', id(ss), j)], writes=[('rs', id(rs), j)])
    P.op('act', lambda e: e.sqrt(out=rs[:, j:j + 1], in_=rs[:, j:j + 1]),
         reads=[('rs', id(rs), j)], writes=[('rs', id(rs), j)])
    P.op('dve', lambda e: e.reciprocal(out=rs[:, j:j + 1], in_=rs[:, j:j + 1]),
         reads=[('rs', id(rs), j)], writes=[('rs', id(rs), j)])
    P.op('act', lambda e: e.activation(out=hb, in_=xt, func=AF.Copy, scale=rs[:, j:j + 1]),
         reads=[xkey, ('rs', id(rs), j)], writes=[hkey])
    nch = ncols // 128
    for ci in range(nch):
        P.op('pe', lambda e, ci=ci: e.transpose(out=tp[:, ci, :], in_=hb[:, ci * 128:(ci + 1) * 128], identity=K['idb'][:]),
             reads=[hkey], writes=[tpkey])
    P.op('dve', lambda e: e.tensor_copy(out=hT[:, :, tcol0:tcol0 + 128], in_=tp[:, :nch, :]),
         reads=[tpkey], writes=[hTkey])


def stage_ffn(P, K, xin, xout, wg, wu, wd, gcol_d, T):
    GT = 256
    NT = GT // 128
    with Ctx(P) as c:
        wg_bf = c.sb('wg', [128, 8, DFF], BF16)
        wu_bf = c.sb('wu', [128, 8, DFF], BF16)
        wd_bf = c.sb('wd', [128, NFF, D], BF16)
        gcol = c.sb('gcol', [128, 8], F32)
        stg = [c.sb('stg', [128, 1408], F32) for _ in range(2)]
        P.dma('sp', gcol[:], gcol_d, writes=['gcol'])
        load_weight_bf(P, c, wg, D, DFF, wg_bf, 'wg', stg, gcol=gcol, piece=1408)
        load_weight_bf(P, c, wu, D, DFF, wu_bf, 'wu', stg, gcol=gcol, piece=1408)
        load_weight_bf(P, c, wd, DFF, D, wd_bf, 'wd', stg)
        xt = [c.sb('xt', [128, NT, D], F32) for _ in range(2)]
        hb = [c.sb('hb', [128, D], BF16) for _ in range(2)]
        hT = [c.sb('hT', [128, 8, GT], BF16) for _ in range(2)]
        act = [c.sb('act', [128, NFF, GT], BF16) for _ in range(1)]
        sg = [c.sb('sg', [128, GT], F32) for _ in range(4)]
        xo = [c.sb('xo', [128, D], F32) for _ in range(2)]
        ss = c.sb('ss', [128, 8], F32)
        rs = c.sb('rs', [128, 8], F32)
        tp = [c.ps('tp', [128, 8, 128], BF16) for _ in range(2)]
        pgu = [c.ps('pgu', [128, 512], F32) for _ in range(4)]
        pg = [t[:, 0:256] for t in pgu]
        pu = [t[:, 256:512] for t in pgu]
        po = [c.ps('po', [128, 512], F32) for _ in range(2)]
        ng = T // GT
        it = 0
        for g in range(ng):
            b = g % 2
            tok0 = g * GT
            for j in range(NT):
                P.dma('sp', xt[b][:, j, :], xin[tok0 + j * 128: tok0 + (j + 1) * 128, :], writes=[('xt', b, j)])
            for j in range(NT):
                hbb = (g * NT + j) % 2
                norm_transpose(P, K, xt[b][:, j, :], ('xt', b, j), hb[hbb][:], ('hb', hbb), hT[b], ('hT', b),
                               tp[hbb], ('tp', hbb), ss, rs, (g * NT + j) % 8, D, j * 128)
            for f in range(NFF):
                pb = (g * NFF + f) % 4
                for ci in range(8):
                    P.op('pe', lambda e, ci=ci, f=f, pb=pb: e.matmul(pg[pb][:, :GT], wg_bf[:, ci, f * 128:(f + 1) * 128],
                                                                     hT[b][:, ci, :], start=(ci == 0), stop=(ci == 7)),
                         reads=['wg', ('hT', b)], writes=[('pgu', pb)])
                for ci in range(8):
                    P.op('pe', lambda e, ci=ci, f=f, pb=pb: e.matmul(pu[pb][:, :GT], wu_bf[:, ci, f * 128:(f + 1) * 128],
                                                                     hT[b][:, ci, :], start=(ci == 0), stop=(ci == 7)),
                         reads=['wu', ('hT', b)], writes=[('pgu', pb)])
                P.op('act', lambda e, pb=pb: e.activation(out=sg[pb][:], in_=pg[pb][:, :GT], func=AF.Silu),
                     reads=[('pgu', pb)], writes=[('sg', pb)])
                P.op('dve', lambda e, pb=pb, f=f: e.tensor_tensor(out=act[0][:, f, :], in0=sg[pb][:], in1=pu[pb][:, :GT],
                                                                  op=ALU.mult),
                     reads=[('sg', pb), ('pgu', pb)], writes=[('act', f)])
            for j in range(NT):
                for dh in range(2):
                    ob = it % 2
                    it += 1
                    for f in range(NFF):
                        P.op('pe', lambda e, f=f, j=j, dh=dh, ob=ob: e.matmul(po[ob][:], act[0][:, f, j * 128:(j + 1) * 128],
                                                                              wd_bf[:, f, dh * 512:(dh + 1) * 512],
                                                                              start=(f == 0), stop=(f == NFF - 1)),
                             reads=['wd', ('act', f)], writes=[('po', ob)])
                    xob = (g * NT + j) % 2
                    P.op('dve', lambda e, j=j, dh=dh, ob=ob, xob=xob: e.tensor_tensor(
                        out=xo[xob][:, dh * 512:(dh + 1) * 512], in0=xt[b][:, j, dh * 512:(dh + 1) * 512], in1=po[ob][:],
                        op=ALU.add),
                        reads=[('po', ob), ('xt', b, j)], writes=[('xo', xob, dh)])
                P.dma('pool', xout[tok0 + j * 128: tok0 + (j + 1) * 128, :], xo[xob][:],
                      reads=[('xo', xob, 0), ('xo', xob, 1)])


GELU_C = 1.5957691216057308
GELU_A = 0.044715


def emit_gelu(P, src, skey, t1, t1key, out, okey, eng2='dve'):
    P.op('act', lambda e: e.activation(out=t1, in_=src, func=AF.Square), reads=[skey], writes=[t1key])
    P.op('dve', lambda e: e.tensor_scalar(out=t1, in0=t1, scalar1=GELU_A, scalar2=1.0, op0=ALU.mult, op1=ALU.add),
         reads=[t1key], writes=[t1key])
    P.op('dve', lambda e: e.tensor_tensor(out=t1, in0=t1, in1=src, op=ALU.mult), reads=[t1key, skey], writes=[t1key])
    P.op('act', lambda e: e.activation(out=t1, in_=t1, func=AF.Sigmoid, scale=GELU_C), reads=[t1key], writes=[t1key])
    P.op(eng2, lambda e: e.tensor_tensor(out=out, in0=t1, in1=src, op=ALU.mult), reads=[t1key, skey], writes=[okey])


def emit_rsqrt(P, dst, dkey, src, skey, mult, add):
    P.op('dve', lambda e: e.tensor_scalar(out=dst, in0=src, scalar1=mult, scalar2=add, op0=ALU.mult, op1=ALU.add),
         reads=[skey], writes=[dkey])
    P.op('act', lambda e: e.sqrt(out=dst, in_=dst), reads=[dkey], writes=[dkey])
    P.op('dve', lambda e: e.reciprocal(out=dst, in_=dst), reads=[dkey], writes=[dkey])


class InProj:
    def __init__(self, P, K, c, xin, w_d, N, gcol_d, T, GT=512):
        self.P, self.K, self.c, self.xin, self.T, self.GT = P, K, c, xin, T, GT
        self.NT = GT // 128
        self.w_bf = c.sb('win', [128, 8, N], BF16)
        self.gcol = c.sb('gcol', [128, 8], F32)
        stg = [c.sb('stg', [128, N], F32) for _ in range(2)]
        P.dma('sp', self.gcol[:], gcol_d, writes=['gcol'])
        load_weight_bf(P, c, w_d, D, N, self.w_bf, 'win', stg, gcol=self.gcol)
        self.xt = [c.sb('xt', [128, self.NT, D], F32) for _ in range(2)]
        self.hb = [c.sb('hb', [128, D], BF16) for _ in range(2)]
        self.hT = [c.sb('hT', [128, 8, GT], BF16) for _ in range(2)]
        self.ss = c.sb('ss', [128, 8], F32)
        self.rs = c.sb('rs', [128, 8], F32)
        self.tp = [c.ps('tp', [128, 8, 128], BF16) for _ in range(2)]
        self.pf = [c.ps('pf', [128, 512], F32) for _ in range(2)]
        self.pt = [c.ps('pt', [128, 512], F32) for _ in range(2)]
        self.nf = 0
        self.ntt = 0

    def run(self, fcols, tcols, f_epi, t_epi, copy_f=True, copy_t=(), nraw=4):
        P, K = self.P, self.K
        GT, NT = self.GT, self.NT
        raw = [self.c.sb('raw', [128, 512], F32) for _ in range(nraw)]
        nr = 0

        def copy_out(ps, pkey, m, n):
            nonlocal nr
            r = nr % nraw
            nr += 1
            if nr % 2:
                P.op('act', lambda e: e.copy(out=raw[r][:m, :n], in_=ps[:m, :n]), reads=[pkey], writes=[('raw', r)])
            else:
                P.op('dve', lambda e: e.tensor_copy(out=raw[r][:m, :n], in_=ps[:m, :n]), reads=[pkey], writes=[('raw', r)])
            return raw[r], ('raw', r)

        for g in range(self.T // GT):
            b = g % 2
            tok0 = g * GT
            for j in range(NT):
                P.dma('sp', self.xt[b][:, j, :], self.xin[tok0 + j * 128: tok0 + (j + 1) * 128, :], writes=[('xt', b, j)])
            for j in range(NT):
                hbb = (g * NT + j) % 2
                norm_transpose(P, K, self.xt[b][:, j, :], ('xt', b, j), self.hb[hbb][:], ('hb', hbb), self.hT[b], ('hT', b),
                               self.tp[hbb], ('tp', hbb), self.ss, self.rs, (g * NT + j) % 8, D, j * 128)
            for idx, (col0, m) in enumerate(fcols):
                pb = self.nf % 2
                self.nf += 1
                for ci in range(8):
                    P.op('pe', lambda e, ci=ci, pb=pb, col0=col0, m=m: e.matmul(
                        self.pf[pb][:m, :GT], self.w_bf[:, ci, col0:col0 + m], self.hT[b][:, ci, :],
                        start=(ci == 0), stop=(ci == 7)), reads=['win', ('hT', b)], writes=[('pf', pb)])
                if copy_f:
                    src, skey = copy_out(self.pf[pb], ('pf', pb), m, GT)
                else:
                    src, skey = self.pf[pb], ('pf', pb)
                f_epi(idx, src, skey, tok0, g)
            for j in range(NT):
                for idx, (col0, n) in enumerate(tcols):
                    pb = self.ntt % 2
                    self.ntt += 1
                    for ci in range(8):
                        P.op('pe', lambda e, ci=ci, pb=pb, col0=col0, n=n, j=j: e.matmul(
                            self.pt[pb][:, :n], self.hT[b][:, ci, j * 128:(j + 1) * 128], self.w_bf[:, ci, col0:col0 + n],
                            start=(ci == 0), stop=(ci == 7)), reads=['win', ('hT', b)], writes=[('pt', pb)])
                    if idx in copy_t:
                        src, skey = copy_out(self.pt[pb], ('pt', pb), 128, n)
                    else:
                        src, skey = self.pt[pb], ('pt', pb)
                    t_epi(idx, src, skey, tok0 + j * 128, g * NT + j)


def stage_inproj_even(P, K, xin, w_d, gcol_d, prm, T, qkT, vA, uF, vG):
    with Ctx(P) as c:
        ip = InProj(P, K, c, xin, w_d, 2560, gcol_d, T)
        gq = c.sb('gq', [128, 2], F32)
        P.dma('sp', gq[:], prm['qk_g'], writes=['gq'])
        P.op('dve', lambda e: e.tensor_scalar(out=gq[:, 0:1], in0=gq[:, 0:1], scalar1=0.125, scalar2=None, op0=ALU.mult),
             reads=['gq'], writes=['gq'])
        lng = c.sb('lng', [128, 512], F32)
        lnb = c.sb('lnb', [128, 512], F32)
        P.dma('sp', lng[:], prm['sg_ln_g_rep'], writes=['lng'])
        P.dma('sp', lnb[:], prm['sg_ln_b_rep'], writes=['lnb'])
        sq = [c.sb('sq', [128, 512], BF16) for _ in range(2)]
        rr = [c.sb('rr', [128, 512], F32) for _ in range(2)]
        qo = [c.sb('qo', [128, 512], BF16) for _ in range(2)]
        t1 = [c.sb('t1', [128, 512], F32) for _ in range(2)]
        uo = [c.sb('uo', [128, 512], F32) for _ in range(2)]
        vo = [c.sb('vo', [128, 512], BF16) for _ in range(2)]
        gv = [c.sb('gv', [128, 512], F32) for _ in range(2)]
        st4 = c.sb('st4', [128, 8, 4], F32)
        pn = c.ps('pn', [128, 512], F32)
        cnt = {'f': 0, 't': 0}

        def f_epi(idx, ps, pkey, tok0, g):
            i = cnt['f'] % 2
            cnt['f'] += 1
            if idx < 8:
                P.op('act', lambda e: e.activation(out=sq[i][:], in_=ps[:], func=AF.Square), reads=[pkey], writes=[('sq', i)])
                P.op('pe', lambda e: e.matmul(pn[:], K['blk64'][:], sq[i][:], start=True, stop=True),
                     reads=[('sq', i), 'blk64'], writes=['pn'])
                emit_rsqrt(P, rr[i][:], ('rr', i), pn[:], 'pn', 1.0 / 64, EPS)
                gc = gq[:, 0:1] if idx < 4 else gq[:, 1:2]
                P.op('dve', lambda e: e.scalar_tensor_tensor(out=qo[i][:], in0=ps[:], scalar=gc, in1=rr[i][:],
                                                             op0=ALU.mult, op1=ALU.mult),
                     reads=[pkey, ('rr', i), 'gq'], writes=[('qo', i)])
                P.dma('pool', qkT[idx * 128:(idx + 1) * 128, tok0:tok0 + 512], qo[i][:], reads=[('qo', i)])
            else:
                emit_gelu(P, ps[:], pkey, t1[i][:], ('t1', i), uo[i][:], ('uo', i), eng2='pool' if False else 'dve')
                P.dma('pool', uF[(idx - 8) * 128:(idx - 7) * 128, tok0:tok0 + 512], uo[i][:], reads=[('uo', i)])

        def t_epi(idx, ps, pkey, tokj, jj):
            i = cnt['t'] % 2
            cnt['t'] += 1
            if idx == 0:
                P.op('act', lambda e: e.copy(out=vo[i][:], in_=ps[:]), reads=[pkey], writes=[('vo', i)])
                P.dma('pool', vA[tokj:tokj + 128, :], vo[i][:], reads=[('vo', i)])
            else:
                emit_gelu(P, ps[:], pkey, t1[i][:], ('t1', i), gv[i][:], ('gv', i))
                s = jj % 8
                gv3 = gv[i][:].rearrange("p (g d) -> p g d", g=4)
                t13 = t1[i][:].rearrange("p (g d) -> p g d", g=4)
                P.op('dve', lambda e: e.tensor_reduce(out=st4[:, s, :], in_=gv3, axis=AX.X, op=ALU.add),
                     reads=[('gv', i)], writes=[('st4', s)])
                P.op('act', lambda e: e.activation(out=t1[i][:], in_=gv[i][:], func=AF.Square), reads=[('gv', i)], writes=[('t1', i)])
                s2 = (jj + 4) % 8
                P.op('dve', lambda e: e.tensor_reduce(out=st4[:, s2, :], in_=t13, axis=AX.X, op=ALU.add),
                     reads=[('t1', i)], writes=[('st4', s2)])
                P.op('dve', lambda e: e.tensor_scalar(out=st4[:, s, :], in0=st4[:, s, :], scalar1=1.0 / 128, scalar2=None, op0=ALU.mult),
                     reads=[('st4', s)], writes=[('st4', s)])
                P.op('dve', lambda e: e.tensor_scalar(out=st4[:, s2, :], in0=st4[:, s2, :], scalar1=1.0 / 128, scalar2=EPS,
                                                      op0=ALU.mult, op1=ALU.add), reads=[('st4', s2)], writes=[('st4', s2)])
                m2 = t1[i][:, 0:4]
                P.op('dve', lambda e: e.tensor_tensor(out=m2, in0=st4[:, s, :], in1=st4[:, s, :], op=ALU.mult),
                     reads=[('st4', s)], writes=[('t1', i)])
                P.op('dve', lambda e: e.tensor_tensor(out=st4[:, s2, :], in0=st4[:, s2, :], in1=m2, op=ALU.subtract),
                     reads=[('st4', s2), ('t1', i)], writes=[('st4', s2)])
                P.op('act', lambda e: e.sqrt(out=st4[:, s2, :], in_=st4[:, s2, :]), reads=[('st4', s2)], writes=[('st4', s2)])
                P.op('dve', lambda e: e.reciprocal(out=st4[:, s2, :], in_=st4[:, s2, :]), reads=[('st4', s2)], writes=[('st4', s2)])
                for gi in range(4):
                    P.op('dve', lambda e, gi=gi: e.tensor_scalar(out=gv[i][:, gi * 128:(gi + 1) * 128], in0=gv[i][:, gi * 128:(gi + 1) * 128],
                                                                 scalar1=st4[:, s, gi:gi + 1], scalar2=st4[:, s2, gi:gi + 1],
                                                                 op0=ALU.subtract, op1=ALU.mult),
                         reads=[('gv', i), ('st4', s), ('st4', s2)], writes=[('gv', i)])
                P.op('pool', lambda e: e.tensor_tensor(out=gv[i][:], in0=gv[i][:], in1=lng[:], op=ALU.mult),
                     reads=[('gv', i), 'lng'], writes=[('gv', i)])
                P.op('pool', lambda e: e.tensor_tensor(out=vo[i][:], in0=gv[i][:], in1=lnb[:], op=ALU.add),
                     reads=[('gv', i), 'lnb'], writes=[('vo', i)])
                P.dma('pool', vG[tokj:tokj + 128, :], vo[i][:], reads=[('vo', i)])

        fcols = [(i * 128, 128) for i in range(8)] + [(1536 + i * 128, 128) for i in range(4)]
        tcols = [(1024, 512), (2048, 512)]
        ip.run(fcols, tcols, f_epi, t_epi, copy_f=True, copy_t=(1,))


def stage_attn(P, K, qkT, vA, mixT, prm, lam_init, S, nseq):
    NB = S // 128
    NG = S // 512
    with Ctx(P) as c:
        lam = c.sb('lam', [128, 256], F32)
        P.dma('sp', lam[:], prm['lam_rep'], writes=['lam'])
        prod = c.sb('prod', [128, 2, 64], F32)
        dots = c.sb('dots', [128, 4], F32)
        P.op('dve', lambda e: e.tensor_tensor(out=prod[:, 0, :], in0=lam[:, 0:64], in1=lam[:, 64:128], op=ALU.mult),
             reads=['lam'], writes=['prod'])
        P.op('dve', lambda e: e.tensor_tensor(out=prod[:, 1, :], in0=lam[:, 128:192], in1=lam[:, 192:256], op=ALU.mult),
             reads=['lam'], writes=['prod'])
        P.op('dve', lambda e: e.tensor_reduce(out=dots[:, 0:2], in_=prod[:], axis=AX.X, op=ALU.add), reads=['prod'], writes=['dots'])
        P.op('act', lambda e: e.activation(out=dots[:, 0:2], in_=dots[:, 0:2], func=AF.Exp), reads=['dots'], writes=['dots'])
        P.op('dve', lambda e: e.tensor_tensor(out=dots[:, 2:3], in0=dots[:, 1:2], in1=dots[:, 0:1], op=ALU.subtract),
             reads=['dots'], writes=['dots'])
        P.op('dve', lambda e: e.tensor_scalar(out=dots[:, 2:3], in0=dots[:, 2:3], scalar1=-float(lam_init), scalar2=None, op0=ALU.add),
             reads=['dots'], writes=['dots'])
        neglam = dots[:, 2:3]
        sgc = c.sb('sgc', [128, 1], F32)
        P.dma('sp', sgc[:], prm['subln_col'], writes=['sgc'])
        P.op('dve', lambda e: e.tensor_scalar(out=sgc[:], in0=sgc[:], scalar1=float(1.0 - lam_init), scalar2=None, op0=ALU.mult),
             reads=['sgc'], writes=['sgc'])
        kT = [c.sb('kT', [64, 2, S], BF16) for _ in range(2)]
        Vt = [c.sb('Vt', [128, NB, 128], BF16) for _ in range(2)]
        qT = [c.sb('qT', [64, 2, 512], BF16) for _ in range(2)]
        pT = [c.sb('pT', [128, 512], BF16) for _ in range(4)]
        e32 = [c.sb('e32', [128, 512], F32) for _ in range(2)]
        r0 = c.sb('r0', [128, 512], F32)
        r1 = c.sb('r1', [128, 512], F32)
        o0 = c.sb('o0', [128, 512], F32)
        o1 = c.sb('o1', [128, 512], F32)
        osq = c.sb('osq', [128, 512], BF16)
        ob = [c.sb('ob', [128, 512], BF16) for _ in range(2)]
        sT = [c.ps('sT', [128, 512], F32) for _ in range(4)]
        acc = [c.ps('acc', [128, 512], F32) for _ in range(2)]
        pl = c.ps('pl', [128, 512], F32)
        pn = c.ps('pn', [128, 512], F32)
        lsum = [[c.sb('lsum', [128, 512], F32) for _ in range(2)] for _ in range(2)]
        n_g = 0
        n_s = 0
        n_p = 0
        n_e = 0
        n_q = 0
        n_h = 0
        for s in range(nseq):
            for h in range(4):
                hb = n_h % 2
                n_h += 1
                for cm in range(2):
                    r = 512 + h * 128 + cm * 64
                    P.dma('sp', kT[hb][:, cm, :], qkT[r:r + 64, s * S:(s + 1) * S], writes=[('kT', hb)])
                P.dma('sp', Vt[hb][:], vA[s * S:(s + 1) * S, h * 128:(h + 1) * 128].rearrange("(j p) d -> p j d", p=128),
                      writes=[('Vt', hb)])
                for g in range(NG):
                    qb = n_q % 2
                    n_q += 1
                    t0 = s * S + g * 512
                    for cm in range(2):
                        r = h * 128 + cm * 64
                        P.dma('sp', qT[qb][:, cm, :], qkT[r:r + 64, t0:t0 + 512], writes=[('qT', qb)])
                    nkb = 4 * (g + 1)
                    items = [(j, cm) for j in range(nkb) for cm in range(2)]
                    sbs = {}

                    def emit_score(i):
                        nonlocal n_s
                        j, cm = items[i]
                        sb_ = n_s % 4
                        n_s += 1
                        sbs[i] = sb_
                        P.op('pe', lambda e: e.matmul(sT[sb_][:], kT[hb][:, cm, j * 128:(j + 1) * 128], qT[qb][:, cm, :], start=True, stop=True),
                             reads=[('kT', hb), ('qT', qb)], writes=[('sT', sb_)])

                    gp = n_g % 2
                    n_g += 1
                    emit_score(0)
                    emit_score(1)
                    emit_score(2)
                    for i, (j, cm) in enumerate(items):
                        sb_ = sbs[i]
                        pb = n_p % 4
                        n_p += 1
                        if j < 4 * g:
                            P.op('act', lambda e: e.activation(out=pT[pb][:], in_=sT[sb_][:], func=AF.Exp),
                                 reads=[('sT', sb_)], writes=[('pT', pb)])
                        else:
                            eb = n_e % 2
                            n_e += 1
                            rr_ = j - 4 * g
                            P.op('act', lambda e: e.activation(out=e32[eb][:], in_=sT[sb_][:], func=AF.Exp),
                                 reads=[('sT', sb_)], writes=[('e32', eb)])
                            P.op('pool', lambda e: e.tensor_tensor(out=pT[pb][:], in0=e32[eb][:], in1=K['cmask'][:, rr_, :], op=ALU.mult),
                                 reads=[('e32', eb), 'cmask'], writes=[('pT', pb)])
                        if i + 3 < len(items):
                            emit_score(i + 3)
                        P.op('pe', lambda e: e.matmul(acc[cm][:], Vt[hb][:, j, :], pT[pb][:], start=(j == 0), stop=(j == nkb - 1)),
                             reads=[('Vt', hb), ('pT', pb)], writes=[('acc', cm)])
                        aen = 'dve' if cm == 0 else 'pool'
                        if j == 0:
                            P.op(aen, lambda e: e.tensor_copy(out=lsum[gp][cm][:], in_=pT[pb][:]), reads=[('pT', pb)], writes=[('lsum', gp, cm)])
                        else:
                            P.op(aen, lambda e: e.tensor_tensor(out=lsum[gp][cm][:], in0=lsum[gp][cm][:], in1=pT[pb][:], op=ALU.add),
                                 reads=[('pT', pb), ('lsum', gp, cm)], writes=[('lsum', gp, cm)])
                    P.op('pe', lambda e: e.matmul(pl[:], K['ones_f'][:], lsum[gp][0][:], start=True, stop=True), reads=[('lsum', gp, 0)], writes=['pl'])
                    P.op('dve', lambda e: e.reciprocal(out=r0[:], in_=pl[:]), reads=['pl'], writes=['r0'])
                    P.op('pe', lambda e: e.matmul(pl[:], K['ones_f'][:], lsum[gp][1][:], start=True, stop=True), reads=[('lsum', gp, 1)], writes=['pl'])
                    P.op('dve', lambda e: e.reciprocal(out=r1[:], in_=pl[:]), reads=['pl'], writes=['r1'])
                    P.op('dve', lambda e: e.tensor_tensor(out=o0[:], in0=acc[0][:], in1=r0[:], op=ALU.mult),
                         reads=[('acc', 0), 'r0'], writes=['o0'])
                    P.op('dve', lambda e: e.tensor_tensor(out=o1[:], in0=acc[1][:], in1=r1[:], op=ALU.mult),
                         reads=[('acc', 1), 'r1'], writes=['o1'])
                    P.op('dve', lambda e: e.scalar_tensor_tensor(out=o0[:], in0=o1[:], scalar=neglam, in1=o0[:],
                                                                 op0=ALU.mult, op1=ALU.add),
                         reads=['o0', 'o1', 'dots'], writes=['o0'])
                    P.op('act', lambda e: e.activation(out=osq[:], in_=o0[:], func=AF.Square), reads=['o0'], writes=['osq'])
                    P.op('pe', lambda e: e.matmul(pn[:], K['ones_bf'][:], osq[:], start=True, stop=True),
                         reads=['ones_bf', 'osq'], writes=['pn'])
                    emit_rsqrt(P, r0[:], 'r0', pn[:], 'pn', 1.0 / 128, EPS)
                    obb = n_q % 2
                    P.op('dve', lambda e, obb=obb: e.scalar_tensor_tensor(out=ob[obb][:], in0=o0[:], scalar=sgc[:, 0:1], in1=r0[:],
                                                                          op0=ALU.mult, op1=ALU.mult),
                         reads=['o0', 'r0', 'sgc'], writes=[('ob', obb)])
                    P.dma('pool', mixT[h * 128:(h + 1) * 128, t0:t0 + 512], ob[obb][:], reads=[('ob', obb)])


def stage_sgu(P, K, vG, uF, mixT, prm, T):
    with Ctx(P) as c:
        wst = c.sb('wst', [128, 4, 128], F32)
        wtm = c.sb('wtm', [128, 4, 128], BF16)
        bias = c.sb('bias', [128, 4, 512], F32)
        P.dma('sp', wst[:], prm['sg_wT'].rearrange("g s t -> s g t"), writes=['wst'])
        P.dma('sp', bias[:], prm['sg_b_rep'].rearrange("p (g t) -> p g t", g=4), writes=['bias'])
        for g in range(4):
            P.op('dve', lambda e, g=g: e.tensor_tensor(out=wtm[:, g, :], in0=wst[:, g, :], in1=K['triu'][:], op=ALU.mult),
                 reads=['wst', 'triu'], writes=['wtm'])
        vt = [c.sb('vt', [128, 4, 512], BF16) for _ in range(2)]
        ut = [c.sb('ut', [128, 4, 512], F32) for _ in range(2)]
        tt = [c.sb('tt', [128, 512], F32) for _ in range(2)]
        ob = [c.sb('ob', [128, 4, 512], BF16) for _ in range(2)]
        ps = [c.ps('ps', [128, 512], F32) for _ in range(2)]
        n = 0
        for tg in range(T // 512):
            b = tg % 2
            tok0 = tg * 512
            P.dma('sp', vt[b][:], vG[tok0:tok0 + 512, :].rearrange("(c p) f -> p c f", p=128), writes=[('vt', b)])
            P.dma('sp', ut[b][:], uF[:, tok0:tok0 + 512].rearrange("(g d) t -> d g t", d=128), writes=[('ut', b)])
            for g in range(4):
                pb = n % 2
                n += 1
                for ch in range(4):
                    P.op('pe', lambda e, g=g, ch=ch, pb=pb: e.matmul(ps[pb][:, ch * 128:(ch + 1) * 128], vt[b][:, ch, g * 128:(g + 1) * 128],
                                                                     wtm[:, g, :], start=True, stop=True),
                         reads=[('vt', b), 'wtm'], writes=[('ps', pb)])
                P.op('dve', lambda e, g=g, pb=pb: e.tensor_tensor(out=tt[pb][:], in0=ps[pb][:], in1=bias[:, g, :], op=ALU.add),
                     reads=[('ps', pb), 'bias'], writes=[('tt', pb)])
                P.op('pool', lambda e, g=g, pb=pb: e.tensor_tensor(out=ob[b][:, g, :], in0=tt[pb][:], in1=ut[b][:, g, :], op=ALU.mult),
                     reads=[('tt', pb), ('ut', b)], writes=[('ob', b)])
            P.dma('pool', mixT[512:1024, tok0:tok0 + 512].rearrange("(g d) t -> d g t", d=128), ob[b][:], reads=[('ob', b)])


def stage_outproj(P, K, mixT, w_d, xin, xout, T):
    GT = 512
    with Ctx(P) as c:
        w_bf = c.sb('wo', [128, 8, D], BF16)
        stg = [c.sb('stg', [128, D], F32) for _ in range(2)]
        load_weight_bf(P, c, w_d, D, D, w_bf, 'wo', stg)
        mt = [c.sb('mt', [128, 8, GT], BF16) for _ in range(2)]
        xt = [c.sb('xt', [128, GT // 128, D], F32) for _ in range(2)]
        xo = [c.sb('xo', [128, D], F32) for _ in range(2)]
        po = [c.ps('po', [128, 512], F32) for _ in range(4)]
        n = 0
        for g in range(T // GT):
            b = g % 2
            tok0 = g * GT
            P.dma('sp', mt[b][:], mixT[:, tok0:tok0 + GT].rearrange("(c f) t -> f c t", f=128), writes=[('mt', b)])
            P.dma('sp', xt[b][:], xin[tok0:tok0 + GT, :].rearrange("(j p) d -> p j d", p=128), writes=[('xt', b)])
            for j in range(GT // 128):
                xob = (g * 4 + j) % 2
                for dh in range(2):
                    ob = n % 4
                    n += 1
                    for ci in range(8):
                        P.op('pe', lambda e, ci=ci, j=j, dh=dh, ob=ob: e.matmul(po[ob][:], mt[b][:, ci, j * 128:(j + 1) * 128],
                                                                                w_bf[:, ci, dh * 512:(dh + 1) * 512],
                                                                                start=(ci == 0), stop=(ci == 7)),
                             reads=['wo', ('mt', b)], writes=[('po', ob)])
                    P.op('dve', lambda e, j=j, dh=dh, ob=ob, xob=xob: e.tensor_tensor(
                        out=xo[xob][:, dh * 512:(dh + 1) * 512], in0=xt[b][:, j, dh * 512:(dh + 1) * 512], in1=po[ob][:], op=ALU.add),
                        reads=[('po', ob), ('xt', b)], writes=[('xo', xob, dh)])
                P.dma('pool', xout[tok0 + j * 128: tok0 + (j + 1) * 128, :], xo[xob][:], reads=[('xo', xob, 0), ('xo', xob, 1)])


def stage_inproj_odd(P, K, xin, w_d, gcol_d, T, mqk_raw, mif, rw_raw, v_aug, moA):
    with Ctx(P) as c:
        ip = InProj(P, K, c, xin, w_d, 3848, gcol_d, T)
        vs = [c.sb('vs', [128, 4, 129], BF16) for _ in range(2)]
        so = [c.sb('so', [128, 512], F32) for _ in range(2)]
        for i in range(2):
            P.op('dve', lambda e, i=i: e.memset(vs[i][:], 1.0), writes=[('vs', i)])
        cnt = {'f': 0, 't': 0}

        def f_epi(idx, src, skey, tok0, g):
            m = 8 if idx == 8 else 128
            if idx < 8:
                dst = mqk_raw[idx * 128:(idx + 1) * 128, tok0:tok0 + 512]
            elif idx == 8:
                dst = mif[0:8, tok0:tok0 + 512]
            else:
                dst = rw_raw[(idx - 9) * 128:(idx - 8) * 128, tok0:tok0 + 512]
            P.dma('pool', dst, src[:m, :], reads=[skey])

        def t_epi(idx, ps, pkey, tokj, jj):
            i = cnt['t'] % 2
            cnt['t'] += 1
            if idx == 0:
                P.op('dve', lambda e: e.tensor_copy(out=vs[i][:, :, 0:128], in_=ps[:].rearrange("p (h d) -> p h d", h=4)),
                     reads=[pkey], writes=[('vs', i)])
                P.dma('pool', v_aug[tokj:tokj + 128, :], vs[i][:].rearrange("p h d -> p (h d)"), reads=[('vs', i)])
            else:
                P.op('act', lambda e: e.activation(out=so[i][:], in_=ps[:], func=AF.Sigmoid), reads=[pkey], writes=[('so', i)])
                P.dma('pool', moA[tokj:tokj + 128, :], so[i][:], reads=[('so', i)])

        fcols = [(i * 128, 128) for i in range(8)] + [(1536, 8)] + [(2056 + i * 128, 128) for i in range(14)]
        tcols = [(1024, 512), (1544, 512)]
        ip.run(fcols, tcols, f_epi, t_epi, copy_f=True, copy_t=(), nraw=6)


def stage_ml_prep(P, K, mqk_raw, prm, mqkT, mkA, S, nseq):
    with Ctx(P) as c:
        cw = c.sb('cw', [128, 8, 4], F32)
        cb = c.sb('cb', [128, 8], F32)
        P.dma('sp', cw[:], prm['ml_cw'], writes=['cw'])
        P.dma('sp', cb[:], prm['ml_cb'], writes=['cb'])
        buf = [c.sb('buf', [128, 515], F32) for _ in range(3)]
        acc = [c.sb('acc', [128, 512], F32) for _ in range(2)]
        qo = [c.sb('qo', [128, 512], BF16) for _ in range(2)]
        kt = [c.sb('kt', [128, 4, 128], BF16) for _ in range(2)]
        tp = [c.ps('tp', [128, 4, 128], BF16) for _ in range(2)]
        n = 0
        for s in range(nseq):
            for g in range(S // 512):
                t0 = s * S + g * 512
                for ch in range(8):
                    b3 = n % 3
                    b = n % 2
                    en = 'dve' if n % 2 == 0 else 'pool'
                    n += 1
                    if g == 0:
                        P.op('pool', lambda e, b3=b3: e.memset(buf[b3][:, 0:3], 0.0), writes=[('buf', b3)])
                        P.dma('sp', buf[b3][:, 3:515], mqk_raw[ch * 128:(ch + 1) * 128, t0:t0 + 512], writes=[('buf', b3)])
                    else:
                        P.dma('sp', buf[b3][:, :], mqk_raw[ch * 128:(ch + 1) * 128, t0 - 3:t0 + 512], writes=[('buf', b3)])
                    P.op(en, lambda e, b3=b3, b=b, ch=ch: e.tensor_scalar(out=acc[b][:], in0=buf[b3][:, 3:515], scalar1=cw[:, ch, 3:4],
                                                                         scalar2=cb[:, ch:ch + 1], op0=ALU.mult, op1=ALU.add),
                         reads=[('buf', b3), 'cw', 'cb'], writes=[('acc', b)])
                    for j in (2, 1, 0):
                        P.op('dve', lambda e, b3=b3, b=b, ch=ch, j=j: e.scalar_tensor_tensor(out=acc[b][:], in0=buf[b3][:, j:j + 512],
                                                                                           scalar=cw[:, ch, j:j + 1], in1=acc[b][:],
                                                                                           op0=ALU.mult, op1=ALU.add),
                             reads=[('buf', b3), ('acc', b), 'cw'], writes=[('acc', b)])
                    P.op('act', lambda e, b=b: e.activation(out=acc[b][:], in_=acc[b][:], func=AF.Silu), reads=[('acc', b)], writes=[('acc', b)])
                    sc = 1.0 if ch < 4 else float(128 ** -0.5)
                    P.op(en, lambda e, b=b, sc=sc: e.tensor_scalar(out=qo[b][:], in0=acc[b][:], scalar1=sc, scalar2=None, op0=ALU.mult),
                         reads=[('acc', b)], writes=[('qo', b)])
                    P.dma('pool', mqkT[ch * 128:(ch + 1) * 128, t0:t0 + 512], qo[b][:], reads=[('qo', b)])
                    if ch >= 4:
                        for j in range(4):
                            P.op('pe', lambda e, b=b, j=j: e.transpose(out=tp[b][:, j, :], in_=qo[b][:, j * 128:(j + 1) * 128], identity=K['idb'][:]),
                                 reads=[('qo', b)], writes=[('tp', b)])
                        P.op('act', lambda e, b=b: e.copy(out=kt[b][:], in_=tp[b][:]), reads=[('tp', b)], writes=[('kt', b)])
                        P.dma('pool', mkA[t0:t0 + 512, (ch - 4) * 128:(ch - 3) * 128].rearrange("(j p) d -> p j d", p=128), kt[b][:],
                              reads=[('kt', b)])


def stage_ml_gates(P, K, mif, prm, mgM, mcol, mend, S, nseq):
    NB = S // 128
    R8 = nseq * 4
    with Ctx(P) as c:
        it = c.sb('it', [R8, S], F32)
        ft = c.sb('ft', [R8, S], F32)
        Bt = c.sb('Bt', [R8, S], F32)
        Mt = c.sb('Mt', [R8, S], F32)
        ones = c.sb('ones', [R8, S], F32)
        gb = c.sb('gb', [R8, 2], F32)
        ngb = c.sb('ngb', [R8, 1], F32)
        P.dma('sp', gb[:], prm['ml_gb'][0:R8, :], writes=['gb'])
        for s in range(nseq):
            P.dma('sp', it[4 * s:4 * s + 4, :], mif[0:4, s * S:(s + 1) * S], writes=['it'])
            P.dma('sp', ft[4 * s:4 * s + 4, :], mif[4:8, s * S:(s + 1) * S], writes=['ft'])
        P.op('dve', lambda e: e.memset(ones[:], 1.0), writes=['ones'])
        P.op('dve', lambda e: e.tensor_scalar(out=ngb[:], in0=gb[:, 1:2], scalar1=-1.0, scalar2=None, op0=ALU.mult), reads=['gb'], writes=['ngb'])
        P.op('act', lambda e: e.activation(out=ft[:], in_=ft[:], func=AF.Exp, scale=-1.0, bias=ngb[:, 0:1]), reads=['ft', 'ngb'], writes=['ft'])
        P.op('dve', lambda e: e.tensor_scalar(out=ft[:], in0=ft[:], scalar1=1.0, scalar2=None, op0=ALU.add), reads=['ft'], writes=['ft'])
        P.op('act', lambda e: e.activation(out=ft[:], in_=ft[:], func=AF.Ln), reads=['ft'], writes=['ft'])
        P.op('dve', lambda e: e.tensor_tensor_scan(out=Bt[:], data0=ones[:], data1=ft[:], initial=0.0, op0=ALU.mult, op1=ALU.subtract),
             reads=['ones', 'ft'], writes=['Bt'])
        P.op('dve', lambda e: e.scalar_tensor_tensor(out=it[:], in0=it[:], scalar=gb[:, 0:1], in1=Bt[:], op0=ALU.add, op1=ALU.subtract),
             reads=['it', 'gb', 'Bt'], writes=['it'])
        P.op('dve', lambda e: e.tensor_tensor_scan(out=Mt[:], data0=it[:], data1=it[:], initial=0.0, op0=ALU.max, op1=ALU.max),
             reads=['it'], writes=['Mt'])
        P.op('dve', lambda e: e.tensor_tensor(out=Bt[:], in0=Bt[:], in1=Mt[:], op=ALU.add), reads=['Bt', 'Mt'], writes=['Bt'])
        P.op('act', lambda e: e.activation(out=Bt[:], in_=Bt[:], func=AF.Exp, scale=-1.0), reads=['Bt'], writes=['Bt'])
        P.dma('pool', mgM[0:R8, :], Mt[:], reads=['Mt'])
        P.dma('pool', mend.rearrange("(c r) -> r c", r=R8), Mt[:].rearrange("r (c k) -> r c k", k=128)[:, :, 127], reads=['Mt'], allow_slow_non_contiguous=True)
        pc = [c.ps('pc', [128, NB, R8], F32) for _ in range(2)]
        col = [c.sb('col', [128, NB, R8], F32) for _ in range(2)]
        for k, src in enumerate((it, Bt)):
            for cb_ in range(NB):
                P.op('pe', lambda e, k=k, cb_=cb_, src=src: e.transpose(out=pc[k][:, cb_, :], in_=src[:, cb_ * 128:(cb_ + 1) * 128],
                                                                       identity=K['idf'][0:R8, 0:R8]),
                     reads=['it', 'Bt', 'idf'], writes=[('pc', k)])
            P.op('dve', lambda e, k=k: e.tensor_copy(out=col[k][:], in_=pc[k][:]), reads=[('pc', k)], writes=[('col', k)])
            P.dma('pool', mcol[k], col[k][:].rearrange("p c r -> p (c r)"), reads=[('col', k)])


def stage_ml_core(P, K, mqkT, mkA, v_aug, moA, mgM, mcol, mend, prm, mixT, S, nseq):
    NB = S // 128
    R8 = nseq * 4
    with Ctx(P) as c:
        acol = c.sb('acol', [128, NB, R8], F32)
        encol = c.sb('encol', [128, NB, R8], F32)
        Mc = c.sb('Mc', [128, NB + 1, R8], F32)
        nMc = c.sb('nMc', [128, NB + 1, R8], F32)
        dcs = c.sb('dcs', [128, NB, R8], F32)
        wcs = c.sb('wcs', [128, NB, R8], F32)
        ngr = c.sb('ngr', [128, 512], F32)
        P.dma('sp', acol[:].rearrange("p c r -> p (c r)"), mcol[0], writes=['acol'])
        P.dma('sp', encol[:].rearrange("p c r -> p (c r)"), mcol[1], writes=['encol'])
        P.op('dve', lambda e: e.memset(Mc[:, 0, :], 0.0), writes=['Mc'])
        P.dma('sp', Mc[:, 1:, :].rearrange("p c r -> p (c r)"), mend.rearrange("(o n) -> o n", o=1).partition_broadcast(128), writes=['Mc'])
        P.dma('sp', ngr[:], prm['ml_ng_rep'], writes=['ngr'])
        P.op('dve', lambda e: e.tensor_scalar(out=nMc[:], in0=Mc[:], scalar1=-1.0, scalar2=None, op0=ALU.mult), reads=['Mc'], writes=['nMc'])
        P.op('dve', lambda e: e.tensor_tensor(out=dcs[:], in0=Mc[:, 0:NB, :], in1=Mc[:, 1:NB + 1, :], op=ALU.subtract), reads=['Mc'], writes=['dcs'])
        P.op('act', lambda e: e.activation(out=dcs[:], in_=dcs[:], func=AF.Exp), reads=['dcs'], writes=['dcs'])
        P.op('dve', lambda e: e.tensor_tensor(out=wcs[:], in0=acol[:], in1=Mc[:, 1:NB + 1, :], op=ALU.subtract), reads=['acol', 'Mc'], writes=['wcs'])
        P.op('act', lambda e: e.activation(out=wcs[:], in_=wcs[:], func=AF.Exp), reads=['wcs'], writes=['wcs'])
        Cst = [c.sb('Cst', [128, 129], F32) for _ in range(R8)]
        Cbf = [c.sb('Cbf', [128, 129], BF16) for _ in range(R8)]
        for r in range(R8):
            P.op('pool', lambda e, r=r: e.memset(Cst[r][:], 0.0), writes=[('Cst', r)])
            P.op('pool', lambda e, r=r: e.memset(Cbf[r][:], 0.0), writes=[('Cbf', r)])
        qT4 = [c.sb('qT4', [128, 4, 512], BF16) for _ in range(2)]
        kT4 = [c.sb('kT4', [128, 4, 512], BF16) for _ in range(2)]
        kA4 = [c.sb('kA4', [128, 4, 512], BF16) for _ in range(2)]
        vA4 = [c.sb('vA4', [128, 4, 516], BF16) for _ in range(2)]
        oA4 = [c.sb('oA4', [128, 4, 512], F32) for _ in range(2)]
        Mb4 = [c.sb('Mb4', [128, 4, 512], F32) for _ in range(2)]
        mo = [c.sb('mo', [128, 4, 512], BF16) for _ in range(2)]
        E = [c.sb('E', [128, 128], F32) for _ in range(4)]
        PT = [c.sb('PT', [128, 128], BF16) for _ in range(4)]
        er = [c.sb('er', [128, 128], F32) for _ in range(4)]
        qs = [c.sb('qs', [128, 128], BF16) for _ in range(4)]
        hs = [c.sb('hs', [128, 128], F32) for _ in range(4)]
        hj = [c.sb('hj', [128, 128], F32) for _ in range(2)]
        hf = [c.sb('hf', [128, 128], BF16) for _ in range(4)]
        vw = [c.sb('vw', [128, 129], BF16) for _ in range(4)]
        sts = [c.sb('sts', [128, 6, 4], F32) for _ in range(2)]
        ps_st = [c.ps('pst', [128, 512], F32) for _ in range(2)]
        ps_oa = [c.ps('psoa', [128, 512], F32) for _ in range(2)]
        ps_ob = [c.ps('psob', [128, 512], F32) for _ in range(1)]
        ps_u = [c.ps('psu', [128, 512], F32) for _ in range(2)]
        ps_t = [c.ps('pstt', [128, 4, 128], BF16) for _ in range(1)]
        n = 0

        def po(b, h):
            return ps_oa[b][:, h * 160:h * 160 + 129] if h < 3 else ps_ob[0][:, 0:129]

        def pok(b, h):
            return ('psoa', b) if h < 3 else ('psob', 0)

        def pu(h):
            return ps_u[0][:, h * 160:h * 160 + 129] if h < 3 else ps_u[1][:, 0:129]

        def puk(h):
            return ('psu', 0) if h < 3 else ('psu', 1)

        for sg in range(S // 512):
            for s in range(nseq):
                lb = (sg * nseq + s) % 2
                t0 = s * S + sg * 512
                P.dma('sp', qT4[lb][:], mqkT[0:512, t0:t0 + 512].rearrange("(h d) t -> d h t", d=128), writes=[('qT4', lb)])
                P.dma('sp', kT4[lb][:], mqkT[512:1024, t0:t0 + 512].rearrange("(h d) t -> d h t", d=128), writes=[('kT4', lb)])
                P.dma('sp', kA4[lb][:], mkA[t0:t0 + 512, :].rearrange("(c p) f -> p c f", p=128), writes=[('kA4', lb)])
                P.dma('sp', vA4[lb][:], v_aug[t0:t0 + 512, :].rearrange("(c p) f -> p c f", p=128), writes=[('vA4', lb)])
                P.dma('sp', oA4[lb][:], moA[t0:t0 + 512, :].rearrange("(c p) f -> p c f", p=128), writes=[('oA4', lb)])
                for h in range(4):
                    P.dma('sp', Mb4[lb][:, h, :], mgM[s * 4 + h:s * 4 + h + 1, sg * 512:(sg + 1) * 512].partition_broadcast(128),
                          writes=[('Mb4', lb)])
                for c4 in range(4):
                    cg = sg * 4 + c4
                    cs = slice(c4 * 128, (c4 + 1) * 128)
                    b = n % 2
                    n += 1
                    st = sts[b]
                    H4 = range(4)
                    for h in H4:
                        P.op('pe', lambda e: e.matmul(ps_st[b][:, h * 128:(h + 1) * 128], kT4[lb][:, h, cs], qT4[lb][:, h, cs], start=True, stop=True),
                             reads=[('kT4', lb), ('qT4', lb)], writes=[('pst', b)])
                    for h in H4:
                        sh = s * 4 + h
                        P.op('act', lambda e: e.activation(out=E[h][:], in_=Mb4[lb][:, h, cs], func=AF.Exp, scale=-1.0, bias=acol[:, cg, sh:sh + 1]),
                             reads=[('Mb4', lb), 'acol'], writes=[('E', h)])
                        P.op('act', lambda e: e.activation(out=er[h][:], in_=Mb4[lb][:, h, cs], func=AF.Exp, scale=-1.0, bias=Mc[:, cg, sh:sh + 1]),
                             reads=[('Mb4', lb), 'Mc'], writes=[('er', h)])
                    for h in H4:
                        P.op('pool', lambda e: e.tensor_tensor(out=E[h][:], in0=E[h][:], in1=K['triu'], op=ALU.mult), reads=[('E', h)], writes=[('E', h)])
                        P.op('pool', lambda e: e.tensor_tensor(out=qs[h][:], in0=qT4[lb][:, h, cs], in1=er[h][:], op=ALU.mult),
                             reads=[('qT4', lb), ('er', h)], writes=[('qs', h)])
                    for h in H4:
                        P.op('dve', lambda e: e.tensor_tensor(out=PT[h][:], in0=ps_st[b][:, h * 128:(h + 1) * 128], in1=E[h][:], op=ALU.mult),
                             reads=[('pst', b), ('E', h)], writes=[('PT', h)])
                    for h in H4:
                        sh = s * 4 + h
                        P.op('pe', lambda e: e.matmul(po(b, h), qs[h][:], Cbf[sh][:], start=True, stop=False),
                             reads=[('qs', h), ('Cbf', sh)], writes=[pok(b, h)])
                        P.op('pe', lambda e: e.matmul(po(b, h), PT[h][:], vA4[lb][:, c4, h * 129:(h + 1) * 129], start=False, stop=True),
                             reads=[('PT', h), ('vA4', lb)], writes=[pok(b, h)])
                    for h in H4:
                        P.op('act', lambda e: e.activation(out=st[:, 0, h:h + 1], in_=po(b, h)[:, 128:129], func=AF.Abs),
                             reads=[pok(b, h)], writes=[('sts', b, 0, h)])
                    for h in H4:
                        sh = s * 4 + h
                        P.op('dve', lambda e: e.tensor_tensor(out=st[:, 0, h:h + 1], in0=st[:, 0, h:h + 1], in1=encol[:, cg, sh:sh + 1], op=ALU.max),
                             reads=[('sts', b, 0, h), 'encol'], writes=[('sts', b, 0, h)])
                    P.op('dve', lambda e: e.reciprocal(out=st[:, 0, :], in_=st[:, 0, :]), reads=[('sts', b, 0, h) for h in H4], writes=[('sts', b, 0, h) for h in H4])
                    for h in H4:
                        P.op('act', lambda e: e.activation(out=hs[h][:], in_=po(b, h)[:, 0:128], func=AF.Copy, scale=st[:, 0, h:h + 1], accum_out=st[:, 1, h:h + 1]),
                             reads=[pok(b, h), ('sts', b, 0, h)], writes=[('hs', h), ('sts', b, 1, h)])
                        P.op('act', lambda e: e.activation(out=hj[h % 2][:], in_=hs[h][:], func=AF.Square, accum_out=st[:, 2, h:h + 1]),
                             reads=[('hs', h)], writes=[('hj', h % 2), ('sts', b, 2, h)])
                    k12 = [('sts', b, 1, h) for h in H4] + [('sts', b, 2, h) for h in H4]
                    P.op('dve', lambda e: e.tensor_scalar(out=st[:, 3, :], in0=st[:, 1, :], scalar1=1.0 / 128, scalar2=None, op0=ALU.mult), reads=k12, writes=[('sts', b, 3)])
                    P.op('dve', lambda e: e.tensor_tensor(out=st[:, 4, :], in0=st[:, 3, :], in1=st[:, 3, :], op=ALU.mult), reads=[('sts', b, 3)], writes=[('sts', b, 4)])
                    P.op('dve', lambda e: e.tensor_scalar(out=st[:, 2, :], in0=st[:, 2, :], scalar1=1.0 / 128, scalar2=EPS, op0=ALU.mult, op1=ALU.add), reads=k12, writes=k12)
                    P.op('dve', lambda e: e.tensor_tensor(out=st[:, 2, :], in0=st[:, 2, :], in1=st[:, 4, :], op=ALU.subtract), reads=k12 + [('sts', b, 4)], writes=k12)
                    P.op('act', lambda e: e.sqrt(out=st[:, 2, :], in_=st[:, 2, :]), reads=k12, writes=k12)
                    P.op('dve', lambda e: e.reciprocal(out=st[:, 2, :], in_=st[:, 2, :]), reads=k12, writes=k12)
                    for h in H4:
                        P.op('dve', lambda e: e.tensor_scalar(out=hs[h][:], in0=hs[h][:], scalar1=st[:, 3, h:h + 1], scalar2=st[:, 2, h:h + 1],
                                                              op0=ALU.subtract, op1=ALU.mult),
                             reads=[('hs', h), ('sts', b, 3)] + k12, writes=[('hs', h)])
                    for h in H4:
                        P.op('pool', lambda e: e.tensor_tensor(out=hs[h][:], in0=hs[h][:], in1=ngr[:, h * 128:(h + 1) * 128], op=ALU.mult),
                             reads=[('hs', h), 'ngr'], writes=[('hs', h)])
                        P.op('pool', lambda e: e.tensor_tensor(out=hf[h][:], in0=hs[h][:], in1=oA4[lb][:, c4, h * 128:(h + 1) * 128], op=ALU.mult),
                             reads=[('hs', h), ('oA4', lb)], writes=[('hf', h)])
                    for h in H4:
                        sh = s * 4 + h
                        P.op('dve', lambda e: e.tensor_scalar(out=vw[h][:], in0=vA4[lb][:, c4, h * 129:(h + 1) * 129], scalar1=wcs[:, cg, sh:sh + 1], scalar2=None, op0=ALU.mult),
                             reads=[('vA4', lb), 'wcs'], writes=[('vw', h)])
                    for h in H4:
                        P.op('pe', lambda e: e.matmul(pu(h), kA4[lb][:, c4, h * 128:(h + 1) * 128], vw[h][:], start=True, stop=True),
                             reads=[('kA4', lb), ('vw', h)], writes=[puk(h)])
                    for h in H4:
                        P.op('pe', lambda e: e.transpose(out=ps_t[0][:, h, :], in_=hf[h][:], identity=K['idb'][:]), reads=[('hf', h)], writes=['pstt'])
                    for h in H4:
                        sh = s * 4 + h
                        P.op('dve', lambda e: e.scalar_tensor_tensor(out=Cst[sh][:], in0=Cst[sh][:], scalar=dcs[:, cg, sh:sh + 1], in1=pu(h), op0=ALU.mult, op1=ALU.add),
                             reads=[('Cst', sh), 'dcs', puk(h)], writes=[('Cst', sh)])
                    for h in H4:
                        sh = s * 4 + h
                        P.op('act', lambda e: e.copy(out=Cbf[sh][:], in_=Cst[sh][:]), reads=[('Cst', sh)], writes=[('Cbf', sh)])
                    P.op('act', lambda e: e.copy(out=mo[lb][:, :, cs], in_=ps_t[0][:]), reads=['pstt'], writes=[('mo', lb)])
                P.dma('pool', mixT[0:512, t0:t0 + 512].rearrange("(h d) t -> d h t", d=128), mo[lb][:], reads=[('mo', lb)])


RW_LD = -0.6065306597126334


def stage_rw_prep(P, K, rw_raw, prm, rwF, rwT, rwgL, rwbv, rwg, S, nseq):
    with Ctx(P) as c:
        mu = c.sb('mu', [128, 14], F32)
        pc = c.sb('pc', [128, 5, 4], F32)
        P.dma('sp', mu[:], prm['rw_mu'], writes=['mu'])
        P.dma('sp', pc[:], prm['rw_pc'], writes=['pc'])
        stg = c.sb('stg', [128, 3, 512], F32)
        P.op('dve', lambda e: e.memset(stg[:], 0.0), writes=['stg'])
        P.dma('sp', stg[0:64, 0, :], prm['rw_w2'], writes=['stg'])
        P.dma('sp', stg[64:128, 1, :], prm['rw_a2'], writes=['stg'])
        P.dma('sp', stg[:, 2, :], prm['rw_g2'], writes=['stg'])
        lw = c.sb('lw', [128, 3, 512], BF16)
        P.op('dve', lambda e: e.tensor_copy(out=lw[:], in_=stg[:]), reads=['stg'], writes=['lw'])
        blkf = K['blk64f']
        buf = [c.sb('buf', [128, 513], F32) for _ in range(3)]
        dd = [c.sb('dd', [128, 512], F32) for _ in range(2)]
        xs = c.sb('xs', [128, 14, 512], F32)
        wab = c.sb('wab', [128, 512], BF16)
        sgb = c.sb('sgb', [128, 512], BF16)
        ld2 = [c.sb('ld', [128, 512], F32) for _ in range(2)]
        av2 = [c.sb('av', [128, 512], F32) for _ in range(2)]
        gv2 = [c.sb('gv', [128, 512], F32) for _ in range(2)]
        kk2 = [c.sb('kk', [128, 512], F32) for _ in range(2)]
        km2 = [c.sb('km', [128, 512], F32) for _ in range(2)]
        bb2 = [c.sb('bb', [128, 512], F32) for _ in range(2)]
        t12 = [c.sb('t1', [128, 512], F32) for _ in range(2)]
        t22 = [c.sb('t2', [128, 512], F32) for _ in range(2)]
        lg2 = [c.sb('lg', [128, 512], F32) for _ in range(2)]
        eg2 = [c.sb('eg', [128, 512], F32) for _ in range(2)]
        egl2 = [c.sb('egl', [128, 512], F32) for _ in range(2)]
        gl2 = [c.sb('gl', [128, 8], F32) for _ in range(2)]
        of = [c.sb('of', [128, 4, 512], BF16) for _ in range(2)]
        o32 = [c.sb('o32', [128, 512], F32) for _ in range(2)]
        tb = [c.sb('tb', [128, 3, 512], BF16) for _ in range(2)]
        tt = [c.sb('tt', [128, 4, 128], BF16) for _ in range(2)]
        pz = [c.ps('pz', [128, 512], F32) for _ in range(2)]
        pb = [c.ps('pb', [128, 512], F32) for _ in range(2)]
        tp = [c.ps('tp', [128, 4, 128], BF16) for _ in range(2)]
        n = 0
        nt = 0
        no = 0
        for s in range(nseq):
            for g in range(S // 512):
                t0 = s * S + g * 512
                for ci in range(14):
                    b3 = n % 3
                    b = n % 2
                    en = 'dve' if n % 2 == 0 else 'pool'
                    n += 1
                    if g == 0:
                        P.op('pool', lambda e, b3=b3: e.memset(buf[b3][:, 0:1], 0.0), writes=[('buf', b3)])
                        P.dma('sp', buf[b3][:, 1:513], rw_raw[ci * 128:(ci + 1) * 128, t0:t0 + 512], writes=[('buf', b3)])
                    else:
                        P.dma('sp', buf[b3][:, :], rw_raw[ci * 128:(ci + 1) * 128, t0 - 1:t0 + 512], writes=[('buf', b3)])
                    P.op(en, lambda e, b3=b3, b=b: e.tensor_tensor(out=dd[b][:], in0=buf[b3][:, 0:512], in1=buf[b3][:, 1:513], op=ALU.subtract),
                         reads=[('buf', b3)], writes=[('dd', b)])
                    P.op('dve', lambda e, b3=b3, b=b, ci=ci: e.scalar_tensor_tensor(out=xs[:, ci, :], in0=dd[b][:], scalar=mu[:, ci:ci + 1],
                                                                                in1=buf[b3][:, 1:513], op0=ALU.mult, op1=ALU.add),
                         reads=[('dd', b), ('buf', b3), 'mu'], writes=[('xs', ci)])
                P.op('act', lambda e: e.activation(out=wab[0:64, :], in_=xs[0:64, 12, :], func=AF.Tanh), reads=[('xs', 12)], writes=['wab'])
                P.op('act', lambda e: e.copy(out=wab[64:128, :], in_=xs[64:128, 12, :]), reads=[('xs', 12)], writes=['wab'])
                P.op('act', lambda e: e.activation(out=sgb[:], in_=xs[:, 13, :], func=AF.Sigmoid), reads=[('xs', 13)], writes=['sgb'])
                ob = no % 2
                no += 1
                for fc in range(4):
                    fs = slice(fc * 128, (fc + 1) * 128)
                    zb = fc % 2
                    P.op('pe', lambda e, fs=fs, zb=zb: e.matmul(pz[zb][:], lw[:, 0, fs], wab[:], start=True, stop=True), reads=['lw', 'wab'], writes=[('pz', zb)])
                    P.op('act', lambda e, zb=zb, fc=fc: e.activation(out=ld2[fc % 2][:], in_=pz[zb][:], func=AF.Sigmoid, bias=pc[:, 0, fc:fc + 1]),
                         reads=[('pz', zb), 'pc'], writes=[('ld', fc % 2)])
                    P.op('dve', lambda e: e.tensor_scalar(out=ld2[fc % 2][:], in0=ld2[fc % 2][:], scalar1=RW_LD, scalar2=None, op0=ALU.mult), reads=[('ld', fc % 2)], writes=[('ld', fc % 2)])
                    P.op('pe', lambda e, fs=fs, zb=zb: e.matmul(pb[zb][:], lw[:, 1, fs], wab[:], start=True, stop=True), reads=['lw', 'wab'], writes=[('pb', zb)])
                    P.op('act', lambda e, zb=zb, fc=fc: e.activation(out=av2[fc % 2][:], in_=pb[zb][:], func=AF.Sigmoid, bias=pc[:, 1, fc:fc + 1]),
                         reads=[('pb', zb), 'pc'], writes=[('av', fc % 2)])
                    P.op('dve', lambda e, fc=fc: e.tensor_scalar(out=kk2[fc % 2][:], in0=xs[:, 4 + fc, :], scalar1=pc[:, 2, fc:fc + 1], scalar2=None, op0=ALU.mult),
                         reads=[('xs', 4 + fc), 'pc'], writes=[('kk', fc % 2)])
                    P.op('act', lambda e: e.activation(out=t12[fc % 2][:], in_=kk2[fc % 2][:], func=AF.Square), reads=[('kk', fc % 2)], writes=[('t1', fc % 2)])
                    P.op('pe', lambda e, zb=zb: e.matmul(pz[zb][:], blkf, t12[fc % 2][:], start=True, stop=True), reads=[('t1', fc % 2), 'blk64f'], writes=[('pz', zb)])
                    P.op('act', lambda e, zb=zb: e.sqrt(out=t12[fc % 2][:], in_=pz[zb][:]), reads=[('pz', zb)], writes=[('t1', fc % 2)])
                    P.op('dve', lambda e: e.tensor_scalar(out=t12[fc % 2][:], in0=t12[fc % 2][:], scalar1=1e-12, scalar2=None, op0=ALU.max), reads=[('t1', fc % 2)], writes=[('t1', fc % 2)])
                    P.op('dve', lambda e: e.reciprocal(out=t12[fc % 2][:], in_=t12[fc % 2][:]), reads=[('t1', fc % 2)], writes=[('t1', fc % 2)])
                    P.op('dve', lambda e: e.tensor_tensor(out=kk2[fc % 2][:], in0=kk2[fc % 2][:], in1=t12[fc % 2][:], op=ALU.mult), reads=[('kk', fc % 2), ('t1', fc % 2)], writes=[('kk', fc % 2)])
                    P.op('pool', lambda e, fc=fc: e.tensor_scalar(out=t22[fc % 2][:], in0=av2[fc % 2][:], scalar1=-1.0, scalar2=pc[:, 3, fc:fc + 1], op0=ALU.add, op1=ALU.mult),
                         reads=[('av', fc % 2), 'pc'], writes=[('t2', fc % 2)])
                    P.op('dve', lambda e, fc=fc: e.scalar_tensor_tensor(out=km2[fc % 2][:], in0=t22[fc % 2][:], scalar=1.0, in1=xs[:, 4 + fc, :], op0=ALU.add, op1=ALU.mult),
                         reads=[('t2', fc % 2), ('xs', 4 + fc)], writes=[('km', fc % 2)])
                    P.op('pool', lambda e: e.tensor_tensor(out=bb2[fc % 2][:], in0=kk2[fc % 2][:], in1=av2[fc % 2][:], op=ALU.mult), reads=[('kk', fc % 2), ('av', fc % 2)], writes=[('bb', fc % 2)])
                    P.op('pe', lambda e, fs=fs, zb=zb: e.matmul(pb[zb][:], lw[:, 2, fs], sgb[:], start=True, stop=True), reads=['lw', 'sgb'], writes=[('pb', zb)])
                    P.op('act', lambda e, zb=zb: e.copy(out=gv2[fc % 2][:], in_=pb[zb][:]), reads=[('pb', zb)], writes=[('gv', fc % 2)])
                    P.dma('pool', rwg[fs, t0:t0 + 512], gv2[fc % 2][:], reads=[('gv', fc % 2)])
                    P.op('dve', lambda e, fc=fc: e.scalar_tensor_tensor(out=t12[fc % 2][:], in0=xs[:, fc, :], scalar=pc[:, 4, fc:fc + 1], in1=km2[fc % 2][:], op0=ALU.mult, op1=ALU.mult),
                         reads=[('xs', fc), 'pc', ('km', fc % 2)], writes=[('t1', fc % 2)])
                    P.op('pe', lambda e, zb=zb: e.matmul(pz[zb][:], blkf, t12[fc % 2][:], start=True, stop=True), reads=[('t1', fc % 2), 'blk64f'], writes=[('pz', zb)])
                    P.op('dve', lambda e, zb=zb, fc=fc: e.tensor_tensor(out=t12[fc % 2][:], in0=pz[zb][:], in1=xs[:, 8 + fc, :], op=ALU.mult),
                         reads=[('pz', zb), ('xs', 8 + fc)], writes=[('t1', fc % 2)])
                    o3 = no % 2
                    P.op('dve', lambda e, o3=o3: e.tensor_tensor(out=o32[o3][:], in0=t12[fc % 2][:], in1=gv2[fc % 2][:], op=ALU.mult), reads=[('t1', fc % 2), ('gv', fc % 2)], writes=[('o32', o3)])
                    P.dma('pool', rwbv[fs, t0:t0 + 512], o32[o3][:], reads=[('o32', o3)])
                    for cc in range(8):
                        cs = slice(cc * 64, (cc + 1) * 64)
                        P.op('dve', lambda e, cs=cs: e.tensor_tensor_scan(out=lg2[fc % 2][:, cs], data0=K['ones_f'][:, 0:64], data1=ld2[fc % 2][:, cs], initial=0.0,
                                                                         op0=ALU.mult, op1=ALU.add),
                             reads=[('ld', fc % 2), 'ones_f'], writes=[('lg', fc % 2)])
                    lg3 = lg2[fc % 2][:].rearrange("p (c k) -> p c k", k=64)
                    P.op('act', lambda e: e.activation(out=eg2[fc % 2][:], in_=lg2[fc % 2][:], func=AF.Exp), reads=[('lg', fc % 2)], writes=[('eg', fc % 2)])
                    P.op('act', lambda e: e.copy(out=gl2[fc % 2][:], in_=eg2[fc % 2][:].rearrange("p (c k) -> p c k", k=64)[:, :, 63]), reads=[('eg', fc % 2)], writes=[('gl', fc % 2)])
                    P.dma('pool', rwgL[fs, t0 // 64:t0 // 64 + 8], gl2[fc % 2][:], reads=[('gl', fc % 2)])
                    P.op('dve', lambda e, fc=fc, ob=ob: e.tensor_tensor(out=of[ob][:, 1, :], in0=xs[:, fc, :], in1=eg2[fc % 2][:], op=ALU.mult),
                         reads=[('xs', fc), ('eg', fc % 2)], writes=[('of', ob)])
                    for cc in range(8):
                        cs = slice(cc * 64, (cc + 1) * 64)
                        P.op('act', lambda e, cs=cs, cc=cc: e.activation(out=egl2[fc % 2][:, cs], in_=lg2[fc % 2][:, cs], func=AF.Exp, scale=-1.0,
                                                                         bias=lg2[fc % 2][:, cc * 64 + 63:cc * 64 + 64]),
                             reads=[('lg', fc % 2)], writes=[('egl', fc % 2)])
                    tbb = nt % 2
                    P.op('pool', lambda e, tbb=tbb: e.tensor_tensor(out=tb[tbb][:, 1, :], in0=km2[fc % 2][:], in1=egl2[fc % 2][:], op=ALU.mult), reads=[('km', fc % 2), ('egl', fc % 2)], writes=[('tb', tbb)])
                    P.op('pool', lambda e, tbb=tbb: e.tensor_tensor(out=tb[tbb][:, 2, :], in0=bb2[fc % 2][:], in1=egl2[fc % 2][:], op=ALU.mult), reads=[('bb', fc % 2), ('egl', fc % 2)], writes=[('tb', tbb)])
                    P.op('act', lambda e, tbb=tbb, fc=fc: e.copy(out=tb[tbb][:, 0, :], in_=xs[:, 8 + fc, :]), reads=[('xs', 8 + fc)], writes=[('tb', tbb)])
                    P.op('act', lambda e: e.activation(out=eg2[fc % 2][:], in_=lg2[fc % 2][:], func=AF.Exp, scale=-1.0), reads=[('lg', fc % 2)], writes=[('eg', fc % 2)])
                    P.op('dve', lambda e, ob=ob: e.tensor_tensor(out=of[ob][:, 2, :], in0=bb2[fc % 2][:], in1=eg2[fc % 2][:], op=ALU.mult), reads=[('bb', fc % 2), ('eg', fc % 2)], writes=[('of', ob)])
                    P.op('dve', lambda e, ob=ob: e.tensor_tensor(out=of[ob][:, 3, :], in0=km2[fc % 2][:], in1=eg2[fc % 2][:], op=ALU.mult), reads=[('km', fc % 2), ('eg', fc % 2)], writes=[('of', ob)])
                    P.op('dve', lambda e: e.tensor_tensor(out=t22[fc % 2][:], in0=lg2[fc % 2][:], in1=ld2[fc % 2][:], op=ALU.subtract), reads=[('lg', fc % 2), ('ld', fc % 2)], writes=[('t2', fc % 2)])
                    P.op('act', lambda e: e.activation(out=t22[fc % 2][:], in_=t22[fc % 2][:], func=AF.Exp), reads=[('t2', fc % 2)], writes=[('t2', fc % 2)])
                    P.op('dve', lambda e, ob=ob: e.tensor_tensor(out=of[ob][:, 0, :], in0=kk2[fc % 2][:], in1=t22[fc % 2][:], op=ALU.mult), reads=[('kk', fc % 2), ('t2', fc % 2)], writes=[('of', ob)])
                    P.dma('pool', rwF[:, fs, t0:t0 + 512].rearrange("a p t -> p a t"), of[ob][:], reads=[('of', ob)])
                    for a3 in range(3):
                        tq = nt % 2
                        nt += 1
                        for j in range(4):
                            P.op('pe', lambda e, tq=tq, j=j, a3=a3, tbb=tbb: e.transpose(out=tp[tq][:, j, :], in_=tb[tbb][:, a3, j * 128:(j + 1) * 128], identity=K['idb'][:]),
                                 reads=[('tb', tbb)], writes=[('tp', tq)])
                        P.op('act', lambda e, tq=tq: e.copy(out=tt[tq][:], in_=tp[tq][:]), reads=[('tp', tq)], writes=[('tt', tq)])
                        P.dma('pool', rwT[a3, t0:t0 + 512, fs].rearrange("(j p) d -> p j d", p=128), tt[tq][:], reads=[('tt', tq)])
                    ob = no % 2
                    no += 1


def stage_rw_core(P, K, rwF, rwT, rwgL, rwbv, rwg, prm, mixT, S, nseq):
    GT = 256
    NCH = GT // 64
    with Ctx(P) as c:
        mk = c.sb('mk', [64, 4, 8, 64], F32)
        I8 = c.sb('I8', [64, 8, 64], F32)
        m0 = c.sb('m0', [64, 4, 64], F32)
        U = K['triu'][0:64, 0:64]
        Id = K['idf'][0:64, 0:64]
        P.op('dve', lambda e: e.tensor_tensor(out=m0[:, 2, :], in0=U, in1=Id, op=ALU.subtract), reads=['triu', 'idf'], writes=['m0'])
        P.op('dve', lambda e: e.tensor_scalar(out=m0[:, 0, :], in0=m0[:, 2, :], scalar1=-1.0, scalar2=None, op0=ALU.mult), reads=['m0'], writes=['m0'])
        P.op('dve', lambda e: e.tensor_copy(out=m0[:, 1, :], in_=U), reads=['triu'], writes=['m0'])
        P.op('dve', lambda e: e.tensor_scalar(out=m0[:, 3, :], in0=U, scalar1=-1.0, scalar2=None, op0=ALU.add), reads=['triu'], writes=['m0'])
        for h in range(8):
            P.op('dve', lambda e, h=h: e.tensor_copy(out=mk[:, :, h, :], in_=m0[:]), reads=['m0'], writes=['mk'])
            P.op('dve', lambda e, h=h: e.tensor_copy(out=I8[:, h, :], in_=Id), reads=['idf'], writes=['I8'])
        lnp = c.sb('lnp', [128, 2, 4], F32)
        P.dma('sp', lnp[:], prm['rw_ln'], writes=['lnp'])
        XF = [c.sb('XF', [64, 4, 8, GT], BF16) for _ in range(2)]
        XT = [c.sb('XT', [64, 3, NCH, 512], BF16) for _ in range(2)]
        gL = [c.sb('gL', [64, 8, NCH], F32) for _ in range(2)]
        bvt = [c.sb('bvt', [128, 4, GT], F32) for _ in range(2)]
        gt = [c.sb('gt', [128, 4, GT], F32) for _ in range(2)]
        yF = [c.sb('yF', [128, 4, GT], F32) for _ in range(2)]
        mo = [c.sb('mo', [128, 4, GT], BF16) for _ in range(2)]
        NT = [[c.sb('NT', [64, 8, 64], F32) for _ in range(2)] for _ in range(NCH)]
        NN = [[c.sb('NN', [64, 8, 64], F32) for _ in range(2)] for _ in range(NCH)]
        Wt = [c.sb('Wt', [64, 8, 64], F32) for _ in range(NCH)]
        CbT = [c.sb('CbT', [64, 8, 64], BF16) for _ in range(NCH)]
        BkT = [c.sb('BkT', [64, 8, 64], BF16) for _ in range(NCH)]
        CkT = [c.sb('CkT', [64, 8, 64], BF16) for _ in range(NCH)]
        raw = [c.sb('raw', [64, 8, 64], F32) for _ in range(3)]
        Hs = [c.sb('Hs', [64, 8, 64], F32) for _ in range(nseq)]
        Hb = [c.sb('Hb', [64, 8, 64], BF16) for _ in range(nseq)]
        Rr = c.sb('Rr', [64, 8, 64], F32)
        Un = c.sb('Un', [64, 8, 64], BF16)
        ysq = c.sb('ysq', [64, 8, 64], F32)
        yn = c.sb('yn', [64, 8, 64], F32)
        st = c.sb('st', [64, 4, 8], F32)
        for s in range(nseq):
            P.op('pool', lambda e, s=s: e.memset(Hs[s][:], 0.0), writes=[('Hs', s)])
            P.op('pool', lambda e, s=s: e.memset(Hb[s][:], 0.0), writes=[('Hb', s)])
        bank = [c.ps('bk', [128, 512], F32) for _ in range(8)]
        st_ = {'b': 0, 'r': 0}

        def nb():
            i = st_['b'] % 8
            st_['b'] += 1
            return i

        def bv(i):
            return bank[i][0:64, :].rearrange("p (h k) -> p h k", h=8)

        ng = 0
        for g in range(S // GT):
            for s in range(nseq):
                lb = ng % 2
                ng += 1
                t0 = s * S + g * GT
                for a in range(4):
                    P.dma('sp', XF[lb][:, a], rwF[a, :, t0:t0 + GT].rearrange("(h k) t -> k h t", k=64), writes=[('XF', lb)])
                for a in range(3):
                    P.dma('sp', XT[lb][:, a], rwT[a, t0:t0 + GT, :].rearrange("(c p) f -> p c f", p=64), writes=[('XT', lb)])
                P.dma('sp', gL[lb][:], rwgL[:, t0 // 64:t0 // 64 + NCH].rearrange("(h k) c -> k h c", k=64), writes=[('gL', lb)])
                P.dma('sp', bvt[lb][:], rwbv[:, t0:t0 + GT].rearrange("(f p) t -> p f t", p=128), writes=[('bvt', lb)])
                P.dma('sp', gt[lb][:], rwg[:, t0:t0 + GT].rearrange("(f p) t -> p f t", p=128), writes=[('gt', lb)])
                for ch in range(NCH):
                    cs = slice(ch * 64, (ch + 1) * 64)
                    specs = [
                        (2, 0, 0, NT[ch][0], 'f32'),
                        (0, 2, 3, NN[ch][0], 'f32'),
                        (2, 1, 1, CbT[ch], 'bf'),
                        (3, 0, 2, BkT[ch], 'bf'),
                        (3, 1, 1, CkT[ch], 'bf'),
                    ]
                    for (la, ra, mi, dst, kind) in specs:
                        bi = nb()
                        for h in range(8):
                            P.op('pe', lambda e, bi=bi, h=h, la=la, ra=ra, cs=cs: e.matmul(bv(bi)[:, h, :], XF[lb][:, la, h, cs], XF[lb][:, ra, h, cs],
                                                                                       start=True, stop=True),
                                 reads=[('XF', lb)], writes=[('bk', bi)])
                        if kind == 'f32':
                            P.op('dve', lambda e, bi=bi, mi=mi, dst=dst: e.tensor_tensor(out=dst[:], in0=bv(bi), in1=mk[:, mi], op=ALU.mult),
                                 reads=[('bk', bi), 'mk'], writes=[id(dst)])
                        else:
                            ri = st_['r'] % 3
                            st_['r'] += 1
                            P.op('act', lambda e, bi=bi, ri=ri: e.copy(out=raw[ri][:], in_=bv(bi)), reads=[('bk', bi)], writes=[('raw', ri)])
                            P.op('pool', lambda e, ri=ri, mi=mi, dst=dst: e.tensor_tensor(out=dst[:], in0=raw[ri][:], in1=mk[:, mi], op=ALU.mult),
                                 reads=[('raw', ri), 'mk'], writes=[id(dst)])
                    P.op('pool', lambda e, ch=ch: e.tensor_tensor(out=Wt[ch][:], in0=NT[ch][0][:], in1=I8[:], op=ALU.add),
                         reads=[id(NT[ch][0]), 'I8'], writes=[id(Wt[ch])])
                for j in range(1, 6):
                    o, nw = (j - 1) % 2, j % 2
                    b1s, b2s, b3s = {}, {}, {}
                    for ch in range(NCH):
                        b1 = b1s[ch] = nb()
                        for h in range(8):
                            P.op('pe', lambda e, h=h: e.matmul(bv(b1)[:, h, :], NT[ch][o][:, h, :], NN[ch][o][:, h, :], start=True, stop=True),
                                 reads=[id(NT[ch][o]), id(NN[ch][o])], writes=[('bk', b1)])
                    if j < 5:
                        for ch in range(NCH):
                            b2 = b2s[ch] = nb()
                            for h in range(8):
                                P.op('pe', lambda e, h=h: e.matmul(bv(b2)[:, h, :], NN[ch][o][:, h, :], NT[ch][o][:, h, :], start=True, stop=True),
                                     reads=[id(NT[ch][o]), id(NN[ch][o])], writes=[('bk', b2)])
                    for ch in range(NCH):
                        b1 = b1s[ch]
                        P.op('act', lambda e: e.copy(out=NN[ch][nw][:], in_=bv(b1)), reads=[('bk', b1)], writes=[id(NN[ch][nw])])
                        if j < 5:
                            b2 = b2s[ch]
                            P.op('dve', lambda e: e.tensor_copy(out=NT[ch][nw][:], in_=bv(b2)), reads=[('bk', b2)], writes=[id(NT[ch][nw])])
                    for ch in range(NCH):
                        b3 = b3s[ch] = nb()
                        for h in range(8):
                            P.op('pe', lambda e, h=h: e.matmul(bv(b3)[:, h, :], NN[ch][nw][:, h, :], Wt[ch][:, h, :], start=True, stop=True),
                                 reads=[id(NN[ch][nw]), id(Wt[ch])], writes=[('bk', b3)])
                    for ch in range(NCH):
                        b3 = b3s[ch]
                        P.op('dve', lambda e: e.tensor_tensor(out=Wt[ch][:], in0=Wt[ch][:], in1=bv(b3), op=ALU.add),
                             reads=[('bk', b3), id(Wt[ch])], writes=[id(Wt[ch])])
                for ch in range(NCH):
                    cs = slice(ch * 64, (ch + 1) * 64)
                    Vh = lambda h: XT[lb][:, 0, ch, h * 64:(h + 1) * 64]
                    bR = nb()
                    for h in range(8):
                        P.op('pe', lambda e, h=h: e.matmul(bv(bR)[:, h, :], XF[lb][:, 0, h, cs], Hb[s][:, h, :], start=True, stop=False),
                             reads=[('XF', lb), ('Hb', s)], writes=[('bk', bR)])
                        P.op('pe', lambda e, h=h: e.matmul(bv(bR)[:, h, :], BkT[ch][:, h, :], Vh(h), start=False, stop=True),
                             reads=[id(BkT[ch]), ('XT', lb)], writes=[('bk', bR)])
                    P.op('act', lambda e: e.copy(out=Rr[:], in_=bv(bR)), reads=[('bk', bR)], writes=['Rr'])
                    bU = nb()
                    for h in range(8):
                        P.op('pe', lambda e, h=h: e.matmul(bv(bU)[:, h, :], Wt[ch][:, h, :], Rr[:, h, :], start=True, stop=True),
                             reads=[id(Wt[ch]), 'Rr'], writes=[('bk', bU)])
                    P.op('dve', lambda e: e.tensor_scalar(out=Un[:], in0=bv(bU), scalar1=-1.0, scalar2=None, op0=ALU.mult), reads=[('bk', bU)], writes=['Un'])
                    bY = nb()
                    for h in range(8):
                        P.op('pe', lambda e, h=h: e.matmul(bv(bY)[:, h, :], XF[lb][:, 1, h, cs], Hb[s][:, h, :], start=True, stop=False),
                             reads=[('XF', lb), ('Hb', s)], writes=[('bk', bY)])
                        P.op('pe', lambda e, h=h: e.matmul(bv(bY)[:, h, :], CkT[ch][:, h, :], Vh(h), start=False, stop=False),
                             reads=[id(CkT[ch]), ('XT', lb)], writes=[('bk', bY)])
                        P.op('pe', lambda e, h=h: e.matmul(bv(bY)[:, h, :], CbT[ch][:, h, :], Un[:, h, :], start=False, stop=True),
                             reads=[id(CbT[ch]), 'Un'], writes=[('bk', bY)])
                    bH = nb()
                    for h in range(8):
                        P.op('pe', lambda e, h=h: e.matmul(bv(bH)[:, h, :], XT[lb][:, 1, ch, h * 64:(h + 1) * 64], Vh(h), start=True, stop=False),
                             reads=[('XT', lb)], writes=[('bk', bH)])
                        P.op('pe', lambda e, h=h: e.matmul(bv(bH)[:, h, :], XT[lb][:, 2, ch, h * 64:(h + 1) * 64], Un[:, h, :], start=False, stop=True),
                             reads=[('XT', lb), 'Un'], writes=[('bk', bH)])
                    for h in range(8):
                        P.op('dve', lambda e, h=h: e.scalar_tensor_tensor(out=Hs[s][:, h, :], in0=Hs[s][:, h, :], scalar=gL[lb][:, h, ch:ch + 1],
                                                                          in1=bv(bH)[:, h, :], op0=ALU.mult, op1=ALU.add),
                             reads=[('Hs', s), ('gL', lb), ('bk', bH)], writes=[('Hs', s)])
                    P.op('act', lambda e: e.copy(out=Hb[s][:], in_=Hs[s][:]), reads=[('Hs', s)], writes=[('Hb', s)])
                    P.op('dve', lambda e: e.tensor_reduce(out=st[:, 0, :], in_=bv(bY), axis=AX.X, op=ALU.add), reads=[('bk', bY)], writes=['st'])
                    P.op('act', lambda e: e.activation(out=ysq[:], in_=bv(bY), func=AF.Square), reads=[('bk', bY)], writes=['ysq'])
                    P.op('dve', lambda e: e.tensor_reduce(out=st[:, 1, :], in_=ysq[:], axis=AX.X, op=ALU.add), reads=['ysq'], writes=['st'])
                    P.op('dve', lambda e: e.tensor_scalar(out=st[:, 0, :], in0=st[:, 0, :], scalar1=1.0 / 64, scalar2=None, op0=ALU.mult), reads=['st'], writes=['st'])
                    P.op('dve', lambda e: e.tensor_tensor(out=st[:, 2, :], in0=st[:, 0, :], in1=st[:, 0, :], op=ALU.mult), reads=['st'], writes=['st'])
                    P.op('dve', lambda e: e.tensor_scalar(out=st[:, 1, :], in0=st[:, 1, :], scalar1=1.0 / 64, scalar2=64e-5, op0=ALU.mult, op1=ALU.add), reads=['st'], writes=['st'])
                    P.op('dve', lambda e: e.tensor_tensor(out=st[:, 1, :], in0=st[:, 1, :], in1=st[:, 2, :], op=ALU.subtract), reads=['st'], writes=['st'])
                    P.op('act', lambda e: e.sqrt(out=st[:, 1, :], in_=st[:, 1, :]), reads=['st'], writes=['st'])
                    P.op('dve', lambda e: e.reciprocal(out=st[:, 1, :], in_=st[:, 1, :]), reads=['st'], writes=['st'])
                    for h in range(8):
                        P.op('dve', lambda e, h=h: e.tensor_scalar(out=yn[:, h, :], in0=bv(bY)[:, h, :], scalar1=st[:, 0, h:h + 1], scalar2=st[:, 1, h:h + 1],
                                                                   op0=ALU.subtract, op1=ALU.mult),
                             reads=[('bk', bY), 'st'], writes=['yn'])
                    bT = nb()
                    for fc in range(4):
                        P.op('pe', lambda e, fc=fc: e.transpose(out=bank[bT][:, fc * 64:(fc + 1) * 64], in_=yn[:, 2 * fc:2 * fc + 2, :].rearrange("p a k -> p (a k)"), identity=Id),
                             reads=['yn', 'idf'], writes=[('bk', bT)])
                    P.op('act', lambda e: e.copy(out=yF[lb][:, :, cs], in_=bank[bT][:, 0:256].rearrange("p (f t) -> p f t", f=4)),
                         reads=[('bk', bT)], writes=[('yF', lb)])
                for fc in range(4):
                    P.op('dve', lambda e, fc=fc: e.tensor_scalar(out=yF[lb][:, fc, :], in0=yF[lb][:, fc, :], scalar1=lnp[:, 0, fc:fc + 1], scalar2=lnp[:, 1, fc:fc + 1],
                                                                 op0=ALU.mult, op1=ALU.add),
                         reads=[('yF', lb), 'lnp'], writes=[('yF', lb)])
                P.op('pool', lambda e: e.tensor_tensor(out=yF[lb][:], in0=yF[lb][:], in1=gt[lb][:], op=ALU.mult), reads=[('yF', lb), ('gt', lb)], writes=[('yF', lb)])
                P.op('pool', lambda e: e.tensor_tensor(out=mo[lb][:], in0=yF[lb][:], in1=bvt[lb][:], op=ALU.add), reads=[('yF', lb), ('bvt', lb)], writes=[('mo', lb)])
                P.dma('pool', mixT[512:1024, t0:t0 + GT].rearrange("(f p) t -> p f t", p=128), mo[lb][:], reads=[('mo', lb)])


def odd_params(inp, o):
    f = np.float32
    col = lambda v: np.ascontiguousarray(np.asarray(v, f).reshape(-1, 128).T)
    cw = np.ascontiguousarray(np.asarray(inp['ml_conv_w'][o], f).reshape(4, 8, 128).transpose(2, 1, 0))
    gbv = np.asarray(inp['ml_gate_b'][o], f)
    gb = np.stack([np.tile(gbv[:4], 2), np.tile(gbv[4:], 2)], 1)
    pc = np.stack([col(inp['rw_w0'][o]), col(inp['rw_a0'][o]), col(inp['rw_k_k'][o]), col(inp['rw_k_a'][o]),
                   col(inp['rw_r_k'][o].reshape(512))], 1)
    ln = np.stack([col(inp['rw_ln_g'][o]), col(inp['rw_ln_b'][o])], 1)
    return {
        'ml_cw': cw.astype(f), 'ml_cb': col(inp['ml_conv_b'][o]), 'ml_gb': np.ascontiguousarray(gb).astype(f),
        'ml_ng_rep': np.ascontiguousarray(np.broadcast_to(np.asarray(inp['ml_norm_g'][o], f).reshape(1, 512), (128, 512))),
        'rw_mu': col(inp['rw_mu'][o]), 'rw_pc': np.ascontiguousarray(pc).astype(f), 'rw_ln': np.ascontiguousarray(ln).astype(f),
        'rw_w2': np.ascontiguousarray(inp['rw_w2'][o]).astype(f), 'rw_a2': np.ascontiguousarray(inp['rw_a2'][o]).astype(f),
        'rw_g2': np.ascontiguousarray(inp['rw_g2'][o]).astype(f),
    }


ODD_SHAPES = {'ml_cw': [128, 8, 4], 'ml_cb': [128, 8], 'ml_gb': [8, 2], 'ml_ng_rep': [128, 512], 'rw_mu': [128, 14],
              'rw_pc': [128, 5, 4], 'rw_ln': [128, 2, 4], 'rw_w2': [64, 512], 'rw_a2': [64, 512], 'rw_g2': [128, 512]}


def odd_scratch(dt, S, nseq, pfx=""):
    T = S * nseq
    NB = S // 128
    R8 = nseq * 4
    return dict(
        mqk_raw=dt(pfx + "mqk_raw", [1024, T]), mif=dt(pfx + "mif", [8, T]), rw_raw=dt(pfx + "rw_raw", [1792, T]),
        v_aug=dt(pfx + "v_aug", [T, 516], BF16), moA=dt(pfx + "moA", [T, 512]),
        mqkT=dt(pfx + "mqkT", [1024, T], BF16), mkA=dt(pfx + "mkA", [T, 512], BF16),
        mgM=dt(pfx + "mgM", [R8, S]), mcol=dt(pfx + "mcol", [2, 128, NB * R8]), mend=dt(pfx + "mend", [NB * R8]),
        rwF=dt(pfx + "rwF", [4, 512, T], BF16), rwT=dt(pfx + "rwT", [3, T, 512], BF16), rwgL=dt(pfx + "rwgL", [512, T // 64]),
        rwbv=dt(pfx + "rwbv", [512, T]), rwg=dt(pfx + "rwg", [512, T]),
    )


def emit_odd_layer(P, K, xin, xout, w_in, w_out, gc, prm, sc, mixT, S, nseq, parts=('ml', 'rw')):
    T = S * nseq
    stage_inproj_odd(P, K, xin, w_in, gc, T, sc['mqk_raw'], sc['mif'], sc['rw_raw'], sc['v_aug'], sc['moA'])
    if 'ml' in parts:
        stage_ml_prep(P, K, sc['mqk_raw'], prm, sc['mqkT'], sc['mkA'], S, nseq)
        stage_ml_gates(P, K, sc['mif'], prm, sc['mgM'], sc['mcol'], sc['mend'], S, nseq)
        stage_ml_core(P, K, sc['mqkT'], sc['mkA'], sc['v_aug'], sc['moA'], sc['mgM'], sc['mcol'], sc['mend'], prm, mixT, S, nseq)
    if 'rw' in parts:
        stage_rw_prep(P, K, sc['rw_raw'], prm, sc['rwF'], sc['rwT'], sc['rwgL'], sc['rwbv'], sc['rwg'], S, nseq)
        stage_rw_core(P, K, sc['rwF'], sc['rwT'], sc['rwgL'], sc['rwbv'], sc['rwg'], prm, mixT, S, nseq)
    stage_outproj(P, K, mixT, w_out, xin, xout, T)


def build_test_odd(S, nseq, parts=('ml', 'rw')):
    T = S * nseq
    nc = bass.Bass("TRN2", target_bir_lowering=False)
    nc.allow_low_precision("bf16 matmul operands by design")
    dt = lambda name, shape, d=F32, kind="Internal": nc.dram_tensor(name, list(shape), d, kind=kind).ap()
    x = dt("x", [T, D], kind="ExternalInput")
    w_in = dt("w_in", [D, 3848], kind="ExternalInput")
    w_out = dt("w_out", [D, D], kind="ExternalInput")
    gc = dt("gc", [128, 8], kind="ExternalInput")
    cst = dt("consts", [128, NCONST], kind="ExternalInput")
    prm = {k: dt("p_" + k, v, kind="ExternalInput") for k, v in ODD_SHAPES.items()}
    y = dt("y", [T, D], kind="ExternalOutput")
    mixT = dt("mixT", [1024, T], BF16, kind="ExternalOutput")
    sc = odd_scratch(dt, S, nseq)
    P = Prog(nc)
    with Ctx(P) as c0:
        K = load_consts(P, c0, cst)
        emit_odd_layer(P, K, x, y, w_in, w_out, gc, prm, sc, mixT, S, nseq, parts)
    return nc

def even_params(inp, e):
    f = np.float32
    return {
        'qk_g': np.ascontiguousarray(np.stack([np.tile(inp['da_q_g'][e], 2), np.tile(inp['da_k_g'][e], 2)], 1)).astype(f),
        'sg_ln_g_rep': np.ascontiguousarray(np.broadcast_to(inp['sg_ln_g'][e].reshape(1, 512), (128, 512))).astype(f),
        'sg_ln_b_rep': np.ascontiguousarray(np.broadcast_to(inp['sg_ln_b'][e].reshape(1, 512), (128, 512))).astype(f),
        'lam_rep': np.ascontiguousarray(np.broadcast_to(inp['da_lambda'][e].reshape(1, 256), (128, 256))).astype(f),
        'subln_col': np.ascontiguousarray(inp['da_subln_g'][e].reshape(128, 1)).astype(f),
        'sg_wT': np.ascontiguousarray(inp['sg_w'][e].transpose(0, 2, 1)).astype(f),
        'sg_b_rep': np.ascontiguousarray(np.broadcast_to(inp['sg_b'][e][None, :, None, :], (128, 4, 4, 128)).reshape(128, 2048)).astype(f),
    }


EVEN_SHAPES = {'qk_g': [128, 2], 'sg_ln_g_rep': [128, 512], 'sg_ln_b_rep': [128, 512], 'lam_rep': [128, 256],
               'subln_col': [128, 1], 'sg_wT': [4, 128, 128], 'sg_b_rep': [128, 2048]}


def gcol_of(g):
    return np.ascontiguousarray(np.asarray(g, np.float32).reshape(8, 128).T)


def build_test_even(S, nseq, layer=0):
    T = S * nseq
    nc = bass.Bass("TRN2", target_bir_lowering=False)
    nc.allow_low_precision("bf16 matmul operands by design")
    dt = lambda name, shape, d=F32, kind="Internal": nc.dram_tensor(name, list(shape), d, kind=kind).ap()
    x = dt("x", [T, D], kind="ExternalInput")
    w_in = dt("w_in", [D, 2560], kind="ExternalInput")
    w_out = dt("w_out", [D, D], kind="ExternalInput")
    gc = dt("gc", [128, 8], kind="ExternalInput")
    cst = dt("consts", [128, NCONST], kind="ExternalInput")
    prm = {k: dt("p_" + k, v, kind="ExternalInput") for k, v in EVEN_SHAPES.items()}
    y = dt("y", [T, D], kind="ExternalOutput")
    qkT = dt("qkT", [1024, T], BF16)
    vA = dt("vA", [T, 512], BF16)
    uF = dt("uF", [512, T], F32)
    vG = dt("vG", [T, 512], BF16)
    mixT = dt("mixT", [1024, T], BF16)
    lam_init = 0.8 - 0.6 * math.exp(-0.3 * layer)
    P = Prog(nc)
    with Ctx(P) as c0:
        K = load_consts(P, c0, cst)
        stage_inproj_even(P, K, x, w_in, gc, prm, T, qkT, vA, uF, vG)
        stage_attn(P, K, qkT, vA, mixT, prm, lam_init, S, nseq)
        stage_sgu(P, K, vG, uF, mixT, prm, T)
        stage_outproj(P, K, mixT, w_out, x, y, T)
    return nc


def emit_even_layer(P, K, xin, xout, w_in, w_out, gc, prm, sc, mixT, S, nseq, layer):
    T = S * nseq
    lam_init = 0.8 - 0.6 * math.exp(-0.3 * layer)
    stage_inproj_even(P, K, xin, w_in, gc, prm, T, sc['qkT'], sc['vA'], sc['uF'], sc['vG'])
    stage_attn(P, K, sc['qkT'], sc['vA'], mixT, prm, lam_init, S, nseq)
    stage_sgu(P, K, sc['vG'], sc['uF'], mixT, prm, T)
    stage_outproj(P, K, mixT, w_out, xin, xout, T)


def build_full(S, nseq, depth=4):
    T = S * nseq
    nc = bass.Bass("TRN2", target_bir_lowering=False)
    nc.allow_low_precision("bf16 matmul operands by design")
    dt = lambda name, shape, d=F32, kind="Internal": nc.dram_tensor(name, list(shape), d, kind=kind).ap()
    ne, no = (depth + 1) // 2, depth // 2
    x = dt("x", [T, D], kind="ExternalInput")
    out = dt("out", [T, D], kind="ExternalOutput")
    cst = dt("consts", [128, NCONST], kind="ExternalInput")
    gmix = dt("gmix", [depth, 128, 8], kind="ExternalInput")
    gffn = dt("gffn", [depth, 128, 8], kind="ExternalInput")
    ev_w_in = dt("ev_w_in", [ne, D, 2560], kind="ExternalInput")
    ev_w_out = dt("ev_w_out", [ne, D, D], kind="ExternalInput")
    od_w_in = dt("od_w_in", [max(no, 1), D, 3848], kind="ExternalInput")
    od_w_out = dt("od_w_out", [max(no, 1), D, D], kind="ExternalInput")
    wg = dt("ffn_w_gate", [depth, D, DFF], kind="ExternalInput")
    wu = dt("ffn_w_up", [depth, D, DFF], kind="ExternalInput")
    wd = dt("ffn_w_down", [depth, DFF, D], kind="ExternalInput")
    eprm = [{k: dt(f"e{e}_{k}", v, kind="ExternalInput") for k, v in EVEN_SHAPES.items()} for e in range(ne)]
    oprm = [{k: dt(f"o{o}_{k}", v, kind="ExternalInput") for k, v in ODD_SHAPES.items()} for o in range(no)]
    xa = dt("xa", [T, D])
    xb = dt("xb", [T, D])
    mixT = dt("mixT", [1024, T], BF16)
    esc = dict(qkT=dt("qkT", [1024, T], BF16), vA=dt("vA", [T, 512], BF16), uF=dt("uF", [512, T]), vG=dt("vG", [T, 512], BF16))
    osc = odd_scratch(dt, S, nseq) if no else None
    P = Prog(nc)
    with Ctx(P) as c0:
        K = load_consts(P, c0, cst)
        xcur = x
        for l in range(depth):
            if l % 2 == 0:
                e = l // 2
                emit_even_layer(P, K, xcur, xa, ev_w_in[e], ev_w_out[e], gmix[l], eprm[e], esc, mixT, S, nseq, l)
            else:
                o = l // 2
                emit_odd_layer(P, K, xcur, xa, od_w_in[o], od_w_out[o], gmix[l], oprm[o], osc, mixT, S, nseq)
            xo = out if l == depth - 1 else xb
            stage_ffn(P, K, xa, xo, wg[l], wu[l], wd[l], gffn[l], T)
            xcur = xo
    return nc


def host_inputs(inputs, depth=4):
    f = np.float32
    ne, no = (depth + 1) // 2, depth // 2
    com = {
        "consts": host_consts(),
        "gmix": np.ascontiguousarray(np.stack([gcol_of(inputs['norm_mix_g'][l]) for l in range(depth)])),
        "gffn": np.ascontiguousarray(np.stack([gcol_of(inputs['norm_ffn_g'][l]) for l in range(depth)])),
        "ev_w_in": np.ascontiguousarray(inputs['ev_w_in'][:ne], f), "ev_w_out": np.ascontiguousarray(inputs['ev_w_out'][:ne], f),
        "od_w_in": np.ascontiguousarray(inputs['od_w_in'][:max(no, 1)], f), "od_w_out": np.ascontiguousarray(inputs['od_w_out'][:max(no, 1)], f),
        "ffn_w_gate": np.ascontiguousarray(inputs['ffn_w_gate'][:depth], f), "ffn_w_up": np.ascontiguousarray(inputs['ffn_w_up'][:depth], f),
        "ffn_w_down": np.ascontiguousarray(inputs['ffn_w_down'][:depth], f),
    }
    for e in range(ne):
        for k, v in even_params(inputs, e).items():
            com[f"e{e}_{k}"] = v
    for o in range(no):
        for k, v in odd_params(inputs, o).items():
            com[f"o{o}_{k}"] = v
    return com


_NC_CACHE = {}


def kernel(**inputs):
    inputs = {k: np.asarray(v) for k, v in inputs.items()}
    x = inputs['x']
    B, S, _ = x.shape
    ncores = 8
    nseq = B // ncores
    depth = inputs['norm_mix_g'].shape[0]
    key = (S, nseq, depth)
    if key not in _NC_CACHE:
        _NC_CACHE[key] = build_full(S, nseq, depth)
    nc = _NC_CACHE[key]
    com = host_inputs(inputs, depth)
    in_maps = []
    for i in range(ncores):
        m = dict(com)
        m["x"] = np.ascontiguousarray(x[i * nseq:(i + 1) * nseq].reshape(nseq * S, D), np.float32)
        in_maps.append(m)
    res = run_bass_kernel_spmd(nc, in_maps, core_ids=list(range(ncores)))
    outs = [np.asarray(r["out"]).reshape(nseq, S, D) for r in res.results]
    return np.concatenate(outs, 0).astype(np.float32)
```

```python
import math
from contextlib import ExitStack
import numpy as np
import ml_dtypes
import concourse.bass as bass
import concourse.mybir as mybir
from concourse.bass_utils import run_bass_kernel_spmd

F32 = mybir.dt.float32
BF16 = mybir.dt.bfloat16
AF = mybir.ActivationFunctionType
ALU = mybir.AluOpType
AX = mybir.AxisListType

D = 1024
DFF = 2816
NFF = DFF // 128
EPOCH = 30000
NSLOT = 24
EPS = 1e-6


class Prog:
    def __init__(self, nc):
        self.nc = nc
        self.E = {'pe': nc.tensor, 'act': nc.scalar, 'dve': nc.vector, 'pool': nc.gpsimd, 'sp': nc.sync}
        self.cnt = {e: 0 for e in ('pe', 'act', 'dve', 'pool')}
        self.csem = {e: [] for e in self.cnt}
        self.known = {e: {} for e in self.E}
        self.dsem = {}
        self.dgen = [0] * NSLOT
        self.dval = [0] * NSLOT
        for i in range(NSLOT):
            self.dsem[(i, 0)] = nc.alloc_semaphore(f"dq{i}_0")
        self.rr = 0
        self.W = {}
        self.R = {}
        self.uid = 0

    def uname(self, s):
        self.uid += 1
        return f"{s}_{self.uid}"

    def _wait(self, e, tok):
        if tok[0] == 'c':
            _, e2, seq = tok
            kk = ('c', e2)
            if self.known[e].get(kk, -1) >= seq:
                return
            ep = seq // EPOCH
            self.E[e].wait_ge(self.csem[e2][ep], seq - ep * EPOCH + 1)
            self.known[e][kk] = seq
        else:
            _, slot, gen, val = tok
            kk = ('d', slot, gen)
            if self.known[e].get(kk, 0) >= val:
                return
            self.E[e].wait_ge(self.dsem[(slot, gen)], val)
            self.known[e][kk] = val

    def _deps(self, e, reads, writes):
        for k in reads:
            t = self.W.get(k)
            if t is not None:
                if t[0] == 'c' and t[1] == e and e == 'pe':
                    continue
                self._wait(e, t)
        for k in writes:
            t = self.W.get(k)
            if t is not None and not (t[0] == 'c' and t[1] == e):
                self._wait(e, t)
            for t in self.R.get(k, {}).values():
                if t[0] == 'c' and t[1] == e:
                    continue
                self._wait(e, t)

    def _record(self, tok, src, reads, writes):
        for k in reads:
            self.R.setdefault(k, {})[src] = tok
        for k in writes:
            self.W[k] = tok
            self.R[k] = {}

    def op(self, e, fn, reads=(), writes=()):
        self._deps(e, reads, writes)
        ins = fn(self.E[e])
        seq = self.cnt[e]
        self.cnt[e] += 1
        ep = seq // EPOCH
        while len(self.csem[e]) <= ep:
            self.csem[e].append(self.nc.alloc_semaphore(f"c_{e}_{len(self.csem[e])}"))
        ins.then_inc(self.csem[e][ep], 1)
        self._record(('c', e, seq), ('c', e), reads, writes)
        return ins

    def dma(self, q, out, in_, reads=(), writes=(), **kw):
        slot = self.rr
        self.rr = (self.rr + 1) % NSLOT
        if self.dval[slot] > 60000:
            self._wait(q, ('d', slot, self.dgen[slot], self.dval[slot]))
            self.dgen[slot] += 1
            self.dval[slot] = 0
            self.dsem[(slot, self.dgen[slot])] = self.nc.alloc_semaphore(f"dq{slot}_{self.dgen[slot]}")
        gen = self.dgen[slot]
        if self.dval[slot] > 0:
            self._wait(q, ('d', slot, gen, self.dval[slot]))
        self._deps(q, reads, writes)
        ins = self.E[q].dma_start(out=out, in_=in_, **kw)
        self.dval[slot] += 16
        ins.then_inc(self.dsem[(slot, gen)], 16)
        self._record(('d', slot, gen, self.dval[slot]), ('d', slot), reads, writes)
        return ins

    def barrier(self):
        toks = [('c', e, self.cnt[e] - 1) for e in self.cnt if self.cnt[e] > 0]
        toks += [('d', s, self.dgen[s], self.dval[s]) for s in range(NSLOT) if self.dval[s] > 0]
        for e in self.E:
            for t in toks:
                self._wait(e, t)
        self.W.clear()
        self.R.clear()


class Ctx:
    def __init__(self, P):
        self.P = P
        self.st = ExitStack()

    def __enter__(self):
        self.st.__enter__()
        return self

    def __exit__(self, *a):
        self.P.barrier()
        return self.st.__exit__(*a)

    def sb(self, name, shape, dt):
        return self.st.enter_context(self.P.nc.sbuf_tensor(self.P.uname(name), list(shape), dt))

    def ps(self, name, shape, dt=F32):
        return self.st.enter_context(self.P.nc.psum_tensor(self.P.uname(name), list(shape), dt))


NCONST = 128 * 3 + 2048


def host_consts():
    cm = np.zeros((128, NCONST), np.float32)
    cm[:, 0:128] = np.eye(128)
    blk = np.zeros((128, 128), np.float32)
    blk[:64, :64] = 1
    blk[64:, 64:] = 1
    cm[:, 128:256] = blk
    k = np.arange(128)[:, None]
    cm[:, 256:384] = (k <= np.arange(128)[None, :])
    q = np.arange(512)[None, :]
    for r in range(4):
        cm[:, 384 + r * 512:384 + (r + 1) * 512] = (128 * r + k <= q)
    return cm


def load_consts(P, c, cd):
    K = {}
    cst = c.sb('cst', [128, 384], F32)
    P.dma('sp', cst[:], cd[:, 0:384], writes=['cst'])
    K['idf'] = cst[:, 0:128]
    K['triu'] = cst[:, 256:384]
    K['idb'] = c.sb('idb', [128, 128], BF16)
    K['blk64'] = c.sb('blk64', [128, 128], BF16)
    K['ones_bf'] = c.sb('ones_bf', [128, 128], BF16)
    K['cmask'] = c.sb('cmask', [128, 4, 512], BF16)
    K['ones_f'] = c.sb('ones_f', [128, 128], F32)
    P.op('dve', lambda e: e.tensor_copy(out=K['idb'][:], in_=cst[:, 0:128]), reads=['cst'], writes=['idb'])
    P.op('dve', lambda e: e.tensor_copy(out=K['blk64'][:], in_=cst[:, 128:256]), reads=['cst'], writes=['blk64'])
    with Ctx(P) as c2:
        cm = c2.sb('cm', [128, 2048], F32)
        P.dma('sp', cm[:], cd[:, 384:384 + 2048], writes=['cm'])
        P.op('dve', lambda e: e.tensor_copy(out=K['cmask'][:], in_=cm[:].rearrange("p (r q) -> p r q", r=4)),
             reads=['cm'], writes=['cmask'])
    P.op('dve', lambda e: e.memset(K['ones_bf'][:], 1.0), writes=['ones_bf'])
    P.op('dve', lambda e: e.memset(K['ones_f'][:], 1.0), writes=['ones_f'])
    P.barrier()
    K['blk64f'] = cst[:, 128:256]
    return K


def load_weight_bf(P, c, w_dram, rows, cols, dst, key, stg, gcol=None, col0=0, engs=('dve', 'pool'), piece=None):
    nch = rows // 128
    piece = piece or cols
    n = 0
    for ci in range(nch):
        for p0 in range(0, cols, piece):
            pc = min(piece, cols - p0)
            b = n % 2
            n += 1
            P.dma('sp', stg[b][:, :pc], w_dram[ci * 128:(ci + 1) * 128, col0 + p0:col0 + p0 + pc], writes=[('stg', id(stg), b)])
            en = engs[n % len(engs)]
            if gcol is not None:
                P.op(en, lambda e, ci=ci, b=b, p0=p0, pc=pc: e.tensor_scalar(out=dst[:, ci, p0:p0 + pc], in0=stg[b][:, :pc],
                                                                             scalar1=gcol[:, ci:ci + 1], scalar2=None, op0=ALU.mult),
                     reads=[('stg', id(stg), b), 'gcol'], writes=[key])
            else:
                P.op(en, lambda e, ci=ci, b=b, p0=p0, pc=pc: e.tensor_copy(out=dst[:, ci, p0:p0 + pc], in_=stg[b][:, :pc]),
                     reads=[('stg', id(stg), b)], writes=[key])


def norm_transpose(P, K, xt, xkey, hb, hkey, hT, hTkey, tp, tpkey, ss, rs, j, ncols, tcol0):
    junk = hb
    P.op('act', lambda e: e.activation(out=junk, in_=xt, func=AF.Square, accum_out=ss[:, j:j + 1]),
         reads=[xkey], writes=[hkey, ('ss', id(ss), j)])
    P.op('dve', lambda e: e.tensor_scalar(out=rs[:, j:j + 1], in0=ss[:, j:j + 1], scalar1=1.0 / ncols, scalar2=EPS,
                                          op0=ALU.mult, op1=ALU.add),
         reads=[('ss', id(ss), j)], writes=[('rs', id(rs), j)])
    P.op('act', lambda e: e.sqrt(out=rs[:, j:j + 1], in_=rs[:, j:j + 1]),
         reads=[('rs', id(rs), j)], writes=[('rs', id(rs), j)])
    P.op('dve', lambda e: e.reciprocal(out=rs[:, j:j + 1], in_=rs[:, j:j + 1]),
         reads=[('rs', id(rs), j)], writes=[('rs', id(rs), j)])
    P.op('act', lambda e: e.activation(out=hb, in_=xt, func=AF.Copy, scale=rs[:, j:j + 1]),
         reads=[xkey, ('rs', id(rs), j)], writes=[hkey])
    nch = ncols // 128
    for ci in range(nch):
        P.op('pe', lambda e, ci=ci: e.transpose(out=tp[:, ci, :], in_=hb[:, ci * 128:(ci + 1) * 128], identity=K['idb'][:]),
             reads=[hkey], writes=[tpkey])
    P.op('dve', lambda e: e.tensor_copy(out=hT[:, :, tcol0:tcol0 + 128], in_=tp[:, :nch, :]),
         reads=[tpkey], writes=[hTkey])


def stage_ffn(P, K, xin, xout, wg, wu, wd, gcol_d, T):
    GT = 256
    NT = GT // 128
    with Ctx(P) as c:
        wg_bf = c.sb('wg', [128, 8, DFF], BF16)
        wu_bf = c.sb('wu', [128, 8, DFF], BF16)
        wd_bf = c.sb('wd', [128, NFF, D], BF16)
        gcol = c.sb('gcol', [128, 8], F32)
        stg = [c.sb('stg', [128, 1408], F32) for _ in range(2)]
        P.dma('sp', gcol[:], gcol_d, writes=['gcol'])
        load_weight_bf(P, c, wg, D, DFF, wg_bf, 'wg', stg, gcol=gcol, piece=1408)
        load_weight_bf(P, c, wu, D, DFF, wu_bf, 'wu', stg, gcol=gcol, piece=1408)
        load_weight_bf(P, c, wd, DFF, D, wd_bf, 'wd', stg)
        xt = [c.sb('xt', [128, NT, D], F32) for _ in range(2)]
        hb = [c.sb('hb', [128, D], BF16) for _ in range(2)]
        hT = [c.sb('hT', [128, 8, GT], BF16) for _ in range(2)]
        act = [c.sb('act', [128, NFF, GT], BF16) for _ in range(1)]
        sg = [c.sb('sg', [128, GT], F32) for _ in range(4)]
        xo = [c.sb('xo', [128, D], F32) for _ in range(2)]
        ss = c.sb('ss', [128, 8], F32)
        rs = c.sb('rs', [128, 8], F32)
        tp = [c.ps('tp', [128, 8, 128], BF16) for _ in range(2)]
        pgu = [c.ps('pgu', [128, 512], F32) for _ in range(4)]
        pg = [t[:, 0:256] for t in pgu]
        pu = [t[:, 256:512] for t in pgu]
        po = [c.ps('po', [128, 512], F32) for _ in range(2)]
        ng = T // GT
        it = 0
        for g in range(ng):
            b = g % 2
            tok0 = g * GT
            for j in range(NT):
                P.dma('sp', xt[b][:, j, :], xin[tok0 + j * 128: tok0 + (j + 1) * 128, :], writes=[('xt', b, j)])
            for j in range(NT):
                hbb = (g * NT + j) % 2
                norm_transpose(P, K, xt[b][:, j, :], ('xt', b, j), hb[hbb][:], ('hb', hbb), hT[b], ('hT', b),
                               tp[hbb], ('tp', hbb), ss, rs, (g * NT + j) % 8, D, j * 128)
            for f in range(NFF):
                pb = (g * NFF + f) % 4
                for ci in range(8):
                    P.op('pe', lambda e, ci=ci, f=f, pb=pb: e.matmul(pg[pb][:, :GT], wg_bf[:, ci, f * 128:(f + 1) * 128],
                                                                     hT[b][:, ci, :], start=(ci == 0), stop=(ci == 7)),
                         reads=['wg', ('hT', b)], writes=[('pgu', pb)])
                for ci in range(8):
                    P.op('pe', lambda e, ci=ci, f=f, pb=pb: e.matmul(pu[pb][:, :GT], wu_bf[:, ci, f * 128:(f + 1) * 128],
                                                                     hT[b][:, ci, :], start=(ci == 0), stop=(ci == 7)),
                         reads=['wu', ('hT', b)], writes=[('pgu', pb)])
                P.op('act', lambda e, pb=pb: e.activation(out=sg[pb][:], in_=pg[pb][:, :GT], func=AF.Silu),
                     reads=[('pgu', pb)], writes=[('sg', pb)])
                P.op('dve', lambda e, pb=pb, f=f: e.tensor_tensor(out=act[0][:, f, :], in0=sg[pb][:], in1=pu[pb][:, :GT],
                                                                  op=ALU.mult),
                     reads=[('sg', pb), ('pgu', pb)], writes=[('act', f)])
            for j in range(NT):
                for dh in range(2):
                    ob = it % 2
                    it += 1
                    for f in range(NFF):
                        P.op('pe', lambda e, f=f, j=j, dh=dh, ob=ob: e.matmul(po[ob][:], act[0][:, f, j * 128:(j + 1) * 128],
                                                                              wd_bf[:, f, dh * 512:(dh + 1) * 512],
                                                                              start=(f == 0), stop=(f == NFF - 1)),
                             reads=['wd', ('act', f)], writes=[('po', ob)])
                    xob = (g * NT + j) % 2
                    P.op('dve', lambda e, j=j, dh=dh, ob=ob, xob=xob: e.tensor_tensor(
                        out=xo[xob][:, dh * 512:(dh + 1) * 512], in0=xt[b][:, j, dh * 512:(dh + 1) * 512], in1=po[ob][:],
                        op=ALU.add),
                        reads=[('po', ob), ('xt', b, j)], writes=[('xo', xob, dh)])
                P.dma('pool', xout[tok0 + j * 128: tok0 + (j + 1) * 128, :], xo[xob][:],
                      reads=[('xo', xob, 0), ('xo', xob, 1)])


GELU_C = 1.5957691216057308
GELU_A = 0.044715


def emit_gelu(P, src, skey, t1, t1key, out, okey, eng2='dve'):
    P.op('act', lambda e: e.activation(out=t1, in_=src, func=AF.Square), reads=[skey], writes=[t1key])
    P.op('dve', lambda e: e.tensor_scalar(out=t1, in0=t1, scalar1=GELU_A, scalar2=1.0, op0=ALU.mult, op1=ALU.add),
         reads=[t1key], writes=[t1key])
    P.op('dve', lambda e: e.tensor_tensor(out=t1, in0=t1, in1=src, op=ALU.mult), reads=[t1key, skey], writes=[t1key])
    P.op('act', lambda e: e.activation(out=t1, in_=t1, func=AF.Sigmoid, scale=GELU_C), reads=[t1key], writes=[t1key])
    P.op(eng2, lambda e: e.tensor_tensor(out=out, in0=t1, in1=src, op=ALU.mult), reads=[t1key, skey], writes=[okey])


def emit_rsqrt(P, dst, dkey, src, skey, mult, add):
    P.op('dve', lambda e: e.tensor_scalar(out=dst, in0=src, scalar1=mult, scalar2=add, op0=ALU.mult, op1=ALU.add),
         reads=[skey], writes=[dkey])
    P.op('act', lambda e: e.sqrt(out=dst, in_=dst), reads=[dkey], writes=[dkey])
    P.op('dve', lambda e: e.reciprocal(out=dst, in_=dst), reads=[dkey], writes=[dkey])


class InProj:
    def __init__(self, P, K, c, xin, w_d, N, gcol_d, T, GT=512):
        self.P, self.K, self.c, self.xin, self.T, self.GT = P, K, c, xin, T, GT
        self.NT = GT // 128
        self.w_bf = c.sb('win', [128, 8, N], BF16)
        self.gcol = c.sb('gcol', [128, 8], F32)
        stg = [c.sb('stg', [128, N], F32) for _ in range(2)]
        P.dma('sp', self.gcol[:], gcol_d, writes=['gcol'])
        load_weight_bf(P, c, w_d, D, N, self.w_bf, 'win', stg, gcol=self.gcol)
        self.xt = [c.sb('xt', [128, self.NT, D], F32) for _ in range(2)]
        self.hb = [c.sb('hb', [128, D], BF16) for _ in range(2)]
        self.hT = [c.sb('hT', [128, 8, GT], BF16) for _ in range(2)]
        self.ss = c.sb('ss', [128, 8], F32)
        self.rs = c.sb('rs', [128, 8], F32)
        self.tp = [c.ps('tp', [128, 8, 128], BF16) for _ in range(2)]
        self.pf = [c.ps('pf', [128, 512], F32) for _ in range(2)]
        self.pt = [c.ps('pt', [128, 512], F32) for _ in range(2)]
        self.nf = 0
        self.ntt = 0

    def run(self, fcols, tcols, f_epi, t_epi, copy_f=True, copy_t=(), nraw=4):
        P, K = self.P, self.K
        GT, NT = self.GT, self.NT
        raw = [self.c.sb('raw', [128, 512], F32) for _ in range(nraw)]
        nr = 0

        def copy_out(ps, pkey, m, n):
            nonlocal nr
            r = nr % nraw
            nr += 1
            if nr % 2:
                P.op('act', lambda e: e.copy(out=raw[r][:m, :n], in_=ps[:m, :n]), reads=[pkey], writes=[('raw', r)])
            else:
                P.op('dve', lambda e: e.tensor_copy(out=raw[r][:m, :n], in_=ps[:m, :n]), reads=[pkey], writes=[('raw', r)])
            return raw[r], ('raw', r)

        for g in range(self.T // GT):
            b = g % 2
            tok0 = g * GT
            for j in range(NT):
                P.dma('sp', self.xt[b][:, j, :], self.xin[tok0 + j * 128: tok0 + (j + 1) * 128, :], writes=[('xt', b, j)])
            for j in range(NT):
                hbb = (g * NT + j) % 2
                norm_transpose(P, K, self.xt[b][:, j, :], ('xt', b, j), self.hb[hbb][:], ('hb', hbb), self.hT[b], ('hT', b),
                               self.tp[hbb], ('tp', hbb), self.ss, self.rs, (g * NT + j) % 8, D, j * 128)
            for idx, (col0, m) in enumerate(fcols):
                pb = self.nf % 2
                self.nf += 1
                for ci in range(8):
                    P.op('pe', lambda e, ci=ci, pb=pb, col0=col0, m=m: e.matmul(
                        self.pf[pb][:m, :GT], self.w_bf[:, ci, col0:col0 + m], self.hT[b][:, ci, :],
                        start=(ci == 0), stop=(ci == 7)), reads=['win', ('hT', b)], writes=[('pf', pb)])
                if copy_f:
                    src, skey = copy_out(self.pf[pb], ('pf', pb), m, GT)
                else:
                    src, skey = self.pf[pb], ('pf', pb)
                f_epi(idx, src, skey, tok0, g)
            for j in range(NT):
                for idx, (col0, n) in enumerate(tcols):
                    pb = self.ntt % 2
                    self.ntt += 1
                    for ci in range(8):
                        P.op('pe', lambda e, ci=ci, pb=pb, col0=col0, n=n, j=j: e.matmul(
                            self.pt[pb][:, :n], self.hT[b][:, ci, j * 128:(j + 1) * 128], self.w_bf[:, ci, col0:col0 + n],
                            start=(ci == 0), stop=(ci == 7)), reads=['win', ('hT', b)], writes=[('pt', pb)])
                    if idx in copy_t:
                        src, skey = copy_out(self.pt[pb], ('pt', pb), 128, n)
                    else:
                        src, skey = self.pt[pb], ('pt', pb)
                    t_epi(idx, src, skey, tok0 + j * 128, g * NT + j)


def stage_inproj_even(P, K, xin, w_d, gcol_d, prm, T, qkT, vA, uF, vG):
    with Ctx(P) as c:
        ip = InProj(P, K, c, xin, w_d, 2560, gcol_d, T)
        gq = c.sb('gq', [128, 2], F32)
        P.dma('sp', gq[:], prm['qk_g'], writes=['gq'])
        P.op('dve', lambda e: e.tensor_scalar(out=gq[:, 0:1], in0=gq[:, 0:1], scalar1=0.125, scalar2=None, op0=ALU.mult),
             reads=['gq'], writes=['gq'])
        lng = c.sb('lng', [128, 512], F32)
        lnb = c.sb('lnb', [128, 512], F32)
        P.dma('sp', lng[:], prm['sg_ln_g_rep'], writes=['lng'])
        P.dma('sp', lnb[:], prm['sg_ln_b_rep'], writes=['lnb'])
        sq = [c.sb('sq', [128, 512], BF16) for _ in range(2)]
        rr = [c.sb('rr', [128, 512], F32) for _ in range(2)]
        qo = [c.sb('qo', [128, 512], BF16) for _ in range(2)]
        t1 = [c.sb('t1', [128, 512], F32) for _ in range(2)]
        uo = [c.sb('uo', [128, 512], F32) for _ in range(2)]
        vo = [c.sb('vo', [128, 512], BF16) for _ in range(2)]
        gv = [c.sb('gv', [128, 512], F32) for _ in range(2)]
        st4 = c.sb('st4', [128, 8, 4], F32)
        pn = c.ps('pn', [128, 512], F32)
        cnt = {'f': 0, 't': 0}

        def f_epi(idx, ps, pkey, tok0, g):
            i = cnt['f'] % 2
            cnt['f'] += 1
            if idx < 8:
                P.op('act', lambda e: e.activation(out=sq[i][:], in_=ps[:], func=AF.Square), reads=[pkey], writes=[('sq', i)])
                P.op('pe', lambda e: e.matmul(pn[:], K['blk64'][:], sq[i][:], start=True, stop=True),
                     reads=[('sq', i), 'blk64'], writes=['pn'])
                emit_rsqrt(P, rr[i][:], ('rr', i), pn[:], 'pn', 1.0 / 64, EPS)
                gc = gq[:, 0:1] if idx < 4 else gq[:, 1:2]
                P.op('dve', lambda e: e.scalar_tensor_tensor(out=qo[i][:], in0=ps[:], scalar=gc, in1=rr[i][:],
                                                             op0=ALU.mult, op1=ALU.mult),
                     reads=[pkey, ('rr', i), 'gq'], writes=[('qo', i)])
                P.dma('pool', qkT[idx * 128:(idx + 1) * 128, tok0:tok0 + 512], qo[i][:], reads=[('qo', i)])
            else:
                emit_gelu(P, ps[:], pkey, t1[i][:], ('t1', i), uo[i][:], ('uo', i), eng2='pool' if False else 'dve')
                P.dma('pool', uF[(idx - 8) * 128:(idx - 7) * 128, tok0:tok0 + 512], uo[i][:], reads=[('uo', i)])

        def t_epi(idx, ps, pkey, tokj, jj):
            i = cnt['t'] % 2
            cnt['t'] += 1
            if idx == 0:
                P.op('act', lambda e: e.copy(out=vo[i][:], in_=ps[:]), reads=[pkey], writes=[('vo', i)])
                P.dma('pool', vA[tokj:tokj + 128, :], vo[i][:], reads=[('vo', i)])
            else:
                emit_gelu(P, ps[:], pkey, t1[i][:], ('t1', i), gv[i][:], ('gv', i))
                s = jj % 8
                gv3 = gv[i][:].rearrange("p (g d) -> p g d", g=4)
                t13 = t1[i][:].rearrange("p (g d) -> p g d", g=4)
                P.op('dve', lambda e: e.tensor_reduce(out=st4[:, s, :], in_=gv3, axis=AX.X, op=ALU.add),
                     reads=[('gv', i)], writes=[('st4', s)])
                P.op('act', lambda e: e.activation(out=t1[i][:], in_=gv[i][:], func=AF.Square), reads=[('gv', i)], writes=[('t1', i)])
                s2 = (jj + 4) % 8
                P.op('dve', lambda e: e.tensor_reduce(out=st4[:, s2, :], in_=t13, axis=AX.X, op=ALU.add),
                     reads=[('t1', i)], writes=[('st4', s2)])
                P.op('dve', lambda e: e.tensor_scalar(out=st4[:, s, :], in0=st4[:, s, :], scalar1=1.0 / 128, scalar2=None, op0=ALU.mult),
                     reads=[('st4', s)], writes=[('st4', s)])
                P.op('dve', lambda e: e.tensor_scalar(out=st4[:, s2, :], in0=st4[:, s2, :], scalar1=1.0 / 128, scalar2=EPS,
                                                      op0=ALU.mult, op1=ALU.add), reads=[('st4', s2)], writes=[('st4', s2)])
                m2 = t1[i][:, 0:4]
                P.op('dve', lambda e: e.tensor_tensor(out=m2, in0=st4[:, s, :], in1=st4[:, s, :], op=ALU.mult),
                     reads=[('st4', s)], writes=[('t1', i)])
                P.op('dve', lambda e: e.tensor_tensor(out=st4[:, s2, :], in0=st4[:, s2, :], in1=m2, op=ALU.subtract),
                     reads=[('st4', s2), ('t1', i)], writes=[('st4', s2)])
                P.op('act', lambda e: e.sqrt(out=st4[:, s2, :], in_=st4[:, s2, :]), reads=[('st4', s2)], writes=[('st4', s2)])
                P.op('dve', lambda e: e.reciprocal(out=st4[:, s2, :], in_=st4[:, s2, :]), reads=[('st4', s2)], writes=[('st4', s2)])
                for gi in range(4):
                    P.op('dve', lambda e, gi=gi: e.tensor_scalar(out=gv[i][:, gi * 128:(gi + 1) * 128], in0=gv[i][:, gi * 128:(gi + 1) * 128],
                                                                 scalar1=st4[:, s, gi:gi + 1], scalar2=st4[:, s2, gi:gi + 1],
                                                                 op0=ALU.subtract, op1=ALU.mult),
                         reads=[('gv', i), ('st4', s), ('st4', s2)], writes=[('gv', i)])
                P.op('pool', lambda e: e.tensor_tensor(out=gv[i][:], in0=gv[i][:], in1=lng[:], op=ALU.mult),
                     reads=[('gv', i), 'lng'], writes=[('gv', i)])
                P.op('pool', lambda e: e.tensor_tensor(out=vo[i][:], in0=gv[i][:], in1=lnb[:], op=ALU.add),
                     reads=[('gv', i), 'lnb'], writes=[('vo', i)])
                P.dma('pool', vG[tokj:tokj + 128, :], vo[i][:], reads=[('vo', i)])

        fcols = [(i * 128, 128) for i in range(8)] + [(1536 + i * 128, 128) for i in range(4)]
        tcols = [(1024, 512), (2048, 512)]
        ip.run(fcols, tcols, f_epi, t_epi, copy_f=True, copy_t=(1,))


def stage_attn(P, K, qkT, vA, mixT, prm, lam_init, S, nseq):
    NB = S // 128
    NG = S // 512
    with Ctx(P) as c:
        lam = c.sb('lam', [128, 256], F32)
        P.dma('sp', lam[:], prm['lam_rep'], writes=['lam'])
        prod = c.sb('prod', [128, 2, 64], F32)
        dots = c.sb('dots', [128, 4], F32)
        P.op('dve', lambda e: e.tensor_tensor(out=prod[:, 0, :], in0=lam[:, 0:64], in1=lam[:, 64:128], op=ALU.mult),
             reads=['lam'], writes=['prod'])
        P.op('dve', lambda e: e.tensor_tensor(out=prod[:, 1, :], in0=lam[:, 128:192], in1=lam[:, 192:256], op=ALU.mult),
             reads=['lam'], writes=['prod'])
        P.op('dve', lambda e: e.tensor_reduce(out=dots[:, 0:2], in_=prod[:], axis=AX.X, op=ALU.add), reads=['prod'], writes=['dots'])
        P.op('act', lambda e: e.activation(out=dots[:, 0:2], in_=dots[:, 0:2], func=AF.Exp), reads=['dots'], writes=['dots'])
        P.op('dve', lambda e: e.tensor_tensor(out=dots[:, 2:3], in0=dots[:, 1:2], in1=dots[:, 0:1], op=ALU.subtract),
             reads=['dots'], writes=['dots'])
        P.op('dve', lambda e: e.tensor_scalar(out=dots[:, 2:3], in0=dots[:, 2:3], scalar1=-float(lam_init), scalar2=None, op0=ALU.add),
             reads=['dots'], writes=['dots'])
        neglam = dots[:, 2:3]
        sgc = c.sb('sgc', [128, 1], F32)
        P.dma('sp', sgc[:], prm['subln_col'], writes=['sgc'])
        P.op('dve', lambda e: e.tensor_scalar(out=sgc[:], in0=sgc[:], scalar1=float(1.0 - lam_init), scalar2=None, op0=ALU.mult),
             reads=['sgc'], writes=['sgc'])
        kT = [c.sb('kT', [64, 2, S], BF16) for _ in range(2)]
        Vt = [c.sb('Vt', [128, NB, 128], BF16) for _ in range(2)]
        qT = [c.sb('qT', [64, 2, 512], BF16) for _ in range(2)]
        pT = [c.sb('pT', [128, 512], BF16) for _ in range(4)]
        e32 = [c.sb('e32', [128, 512], F32) for _ in range(2)]
        r0 = c.sb('r0', [128, 512], F32)
        r1 = c.sb('r1', [128, 512], F32)
        o0 = c.sb('o0', [128, 512], F32)
        o1 = c.sb('o1', [128, 512], F32)
        osq = c.sb('osq', [128, 512], BF16)
        ob = [c.sb('ob', [128, 512], BF16) for _ in range(2)]
        sT = [c.ps('sT', [128, 512], F32) for _ in range(4)]
        acc = [c.ps('acc', [128, 512], F32) for _ in range(2)]
        pl = c.ps('pl', [128, 512], F32)
        pn = c.ps('pn', [128, 512], F32)
        lsum = [[c.sb('lsum', [128, 512], F32) for _ in range(2)] for _ in range(2)]
        n_g = 0
        n_s = 0
        n_p = 0
        n_e = 0
        n_q = 0
        n_h = 0
        for s in range(nseq):
            for h in range(4):
                hb = n_h % 2
                n_h += 1
                for cm in range(2):
                    r = 512 + h * 128 + cm * 64
                    P.dma('sp', kT[hb][:, cm, :], qkT[r:r + 64, s * S:(s + 1) * S], writes=[('kT', hb)])
                P.dma('sp', Vt[hb][:], vA[s * S:(s + 1) * S, h * 128:(h + 1) * 128].rearrange("(j p) d -> p j d", p=128),
                      writes=[('Vt', hb)])
                for g in range(NG):
                    qb = n_q % 2
                    n_q += 1
                    t0 = s * S + g * 512
                    for cm in range(2):
                        r = h * 128 + cm * 64
                        P.dma('sp', qT[qb][:, cm, :], qkT[r:r + 64, t0:t0 + 512], writes=[('qT', qb)])
                    nkb = 4 * (g + 1)
                    items = [(j, cm) for j in range(nkb) for cm in range(2)]
                    sbs = {}

                    def emit_score(i):
                        nonlocal n_s
                        j, cm = items[i]
                        sb_ = n_s % 4
                        n_s += 1
                        sbs[i] = sb_
                        P.op('pe', lambda e: e.matmul(sT[sb_][:], kT[hb][:, cm, j * 128:(j + 1) * 128], qT[qb][:, cm, :], start=True, stop=True),
                             reads=[('kT', hb), ('qT', qb)], writes=[('sT', sb_)])

                    gp = n_g % 2
                    n_g += 1
                    emit_score(0)
                    emit_score(1)
                    emit_score(2)
                    for i, (j, cm) in enumerate(items):
                        sb_ = sbs[i]
                        pb = n_p % 4
                        n_p += 1
                        if j < 4 * g:
                            P.op('act', lambda e: e.activation(out=pT[pb][:], in_=sT[sb_][:], func=AF.Exp),
                                 reads=[('sT', sb_)], writes=[('pT', pb)])
                        else:
                            eb = n_e % 2
                            n_e += 1
                            rr_ = j - 4 * g
                            P.op('act', lambda e: e.activation(out=e32[eb][:], in_=sT[sb_][:], func=AF.Exp),
                                 reads=[('sT', sb_)], writes=[('e32', eb)])
                            P.op('pool', lambda e: e.tensor_tensor(out=pT[pb][:], in0=e32[eb][:], in1=K['cmask'][:, rr_, :], op=ALU.mult),
                                 reads=[('e32', eb), 'cmask'], writes=[('pT', pb)])
                        if i + 3 < len(items):
                            emit_score(i + 3)
                        P.op('pe', lambda e: e.matmul(acc[cm][:], Vt[hb][:, j, :], pT[pb][:], start=(j == 0), stop=(j == nkb - 1)),
                             reads=[('Vt', hb), ('pT', pb)], writes=[('acc', cm)])
                        aen = 'dve' if cm == 0 else 'pool'
                        if j == 0:
                            P.op(aen, lambda e: e.tensor_copy(out=lsum[gp][cm][:], in_=pT[pb][:]), reads=[('pT', pb)], writes=[('lsum', gp, cm)])
                        else:
                            P.op(aen, lambda e: e.tensor_tensor(out=lsum[gp][cm][:], in0=lsum[gp][cm][:], in1=pT[pb][:], op=ALU.add),
                                 reads=[('pT', pb), ('lsum', gp, cm)], writes=[('lsum', gp, cm)])
                    P.op('pe', lambda e: e.matmul(pl[:], K['ones_f'][:], lsum[gp][0][:], start=True, stop=True), reads=[('lsum', gp, 0)], writes=['pl'])
                    P.op('dve', lambda e: e.reciprocal(out=r0[:], in_=pl[:]), reads=['pl'], writes=['r0'])
                    P.op('pe', lambda e: e.matmul(pl[:], K['ones_f'][:], lsum[gp][1][:], start=True, stop=True), reads=[('lsum', gp, 1)], writes=['pl'])
                    P.op('dve', lambda e: e.reciprocal(out=r1[:], in_=pl[:]), reads=['pl'], writes=['r1'])
                    P.op('dve', lambda e: e.tensor_tensor(out=o0[:], in0=acc[0][:], in1=r0[:], op=ALU.mult),
                         reads=[('acc', 0), 'r0'], writes=['o0'])
                    P.op('dve', lambda e: e.tensor_tensor(out=o1[:], in0=acc[1][:], in1=r1[:], op=ALU.mult),
                         reads=[('acc', 1), 'r1'], writes=['o1'])
                    P.op('dve', lambda e: e.scalar_tensor_tensor(out=o0[:], in0=o1[:], scalar=neglam, in1=o0[:],
                                                                 op0=ALU.mult, op1=ALU.add),
                         reads=['o0', 'o1', 'dots'], writes=['o0'])
                    P.op('act', lambda e: e.activation(out=osq[:], in_=o0[:], func=AF.Square), reads=['o0'], writes=['osq'])
                    P.op('pe', lambda e: e.matmul(pn[:], K['ones_bf'][:], osq[:], start=True, stop=True),
                         reads=['ones_bf', 'osq'], writes=['pn'])
                    emit_rsqrt(P, r0[:], 'r0', pn[:], 'pn', 1.0 / 128, EPS)
                    obb = n_q % 2
                    P.op('dve', lambda e, obb=obb: e.scalar_tensor_tensor(out=ob[obb][:], in0=o0[:], scalar=sgc[:, 0:1], in1=r0[:],
                                                                          op0=ALU.mult, op1=ALU.mult),
                         reads=['o0', 'r0', 'sgc'], writes=[('ob', obb)])
                    P.dma('pool', mixT[h * 128:(h + 1) * 128, t0:t0 + 512], ob[obb][:], reads=[('ob', obb)])


def stage_sgu(P, K, vG, uF, mixT, prm, T):
    with Ctx(P) as c:
        wst = c.sb('wst', [128, 4, 128], F32)
        wtm = c.sb('wtm', [128, 4, 128], BF16)
        bias = c.sb('bias', [128, 4, 512], F32)
        P.dma('sp', wst[:], prm['sg_wT'].rearrange("g s t -> s g t"), writes=['wst'])
        P.dma('sp', bias[:], prm['sg_b_rep'].rearrange("p (g t) -> p g t", g=4), writes=['bias'])
        for g in range(4):
            P.op('dve', lambda e, g=g: e.tensor_tensor(out=wtm[:, g, :], in0=wst[:, g, :], in1=K['triu'][:], op=ALU.mult),
                 reads=['wst', 'triu'], writes=['wtm'])
        vt = [c.sb('vt', [128, 4, 512], BF16) for _ in range(2)]
        ut = [c.sb('ut', [128, 4, 512], F32) for _ in range(2)]
        tt = [c.sb('tt', [128, 512], F32) for _ in range(2)]
        ob = [c.sb('ob', [128, 4, 512], BF16) for _ in range(2)]
        ps = [c.ps('ps', [128, 512], F32) for _ in range(2)]
        n = 0
        for tg in range(T // 512):
            b = tg % 2
            tok0 = tg * 512
            P.dma('sp', vt[b][:], vG[tok0:tok0 + 512, :].rearrange("(c p) f -> p c f", p=128), writes=[('vt', b)])
            P.dma('sp', ut[b][:], uF[:, tok0:tok0 + 512].rearrange("(g d) t -> d g t", d=128), writes=[('ut', b)])
            for g in range(4):
                pb = n % 2
                n += 1
                for ch in range(4):
                    P.op('pe', lambda e, g=g, ch=ch, pb=pb: e.matmul(ps[pb][:, ch * 128:(ch + 1) * 128], vt[b][:, ch, g * 128:(g + 1) * 128],
                                                                     wtm[:, g, :], start=True, stop=True),
                         reads=[('vt', b), 'wtm'], writes=[('ps', pb)])
                P.op('dve', lambda e, g=g, pb=pb: e.tensor_tensor(out=tt[pb][:], in0=ps[pb][:], in1=bias[:, g, :], op=ALU.add),
                     reads=[('ps', pb), 'bias'], writes=[('tt', pb)])
                P.op('pool', lambda e, g=g, pb=pb: e.tensor_tensor(out=ob[b][:, g, :], in0=tt[pb][:], in1=ut[b][:, g, :], op=ALU.mult),
                     reads=[('tt', pb), ('ut', b)], writes=[('ob', b)])
            P.dma('pool', mixT[512:1024, tok0:tok0 + 512].rearrange("(g d) t -> d g t", d=128), ob[b][:], reads=[('ob', b)])


def stage_outproj(P, K, mixT, w_d, xin, xout, T):
    GT = 512
    with Ctx(P) as c:
        w_bf = c.sb('wo', [128, 8, D], BF16)
        stg = [c.sb('stg', [128, D], F32) for _ in range(2)]
        load_weight_bf(P, c, w_d, D, D, w_bf, 'wo', stg)
        mt = [c.sb('mt', [128, 8, GT], BF16) for _ in range(2)]
        xt = [c.sb('xt', [128, GT // 128, D], F32) for _ in range(2)]
        xo = [c.sb('xo', [128, D], F32) for _ in range(2)]
        po = [c.ps('po', [128, 512], F32) for _ in range(4)]
        n = 0
        for g in range(T // GT):
            b = g % 2
            tok0 = g * GT
            P.dma('sp', mt[b][:], mixT[:, tok0:tok0 + GT].rearrange("(c f) t -> f c t", f=128), writes=[('mt', b)])
            P.dma('sp', xt[b][:], xin[tok0:tok0 + GT, :].rearrange("(j p) d -> p j d", p=128), writes=[('xt', b)])
            for j in range(GT // 128):
                xob = (g * 4 + j) % 2
                for dh in range(2):
                    ob = n % 4
                    n += 1
                    for ci in range(8):
                        P.op('pe', lambda e, ci=ci, j=j, dh=dh, ob=ob: e.matmul(po[ob][:], mt[b][:, ci, j * 128:(j + 1) * 128],
                                                                                w_bf[:, ci, dh * 512:(dh + 1) * 512],
                                                                                start=(ci == 0), stop=(ci == 7)),
                             reads=['wo', ('mt', b)], writes=[('po', ob)])
                    P.op('dve', lambda e, j=j, dh=dh, ob=ob, xob=xob: e.tensor_tensor(
                        out=xo[xob][:, dh * 512:(dh + 1) * 512], in0=xt[b][:, j, dh * 512:(dh + 1) * 512], in1=po[ob][:], op=ALU.add),
                        reads=[('po', ob), ('xt', b)], writes=[('xo', xob, dh)])
                P.dma('pool', xout[tok0 + j * 128: tok0 + (j + 1) * 128, :], xo[xob][:], reads=[('xo', xob, 0), ('xo', xob, 1)])


def stage_inproj_odd(P, K, xin, w_d, gcol_d, T, mqk_raw, mif, rw_raw, v_aug, moA):
    with Ctx(P) as c:
        ip = InProj(P, K, c, xin, w_d, 3848, gcol_d, T)
        vs = [c.sb('vs', [128, 4, 129], BF16) for _ in range(2)]
        so = [c.sb('so', [128, 512], F32) for _ in range(2)]
        for i in range(2):
            P.op('dve', lambda e, i=i: e.memset(vs[i][:], 1.0), writes=[('vs', i)])
        cnt = {'f': 0, 't': 0}

        def f_epi(idx, src, skey, tok0, g):
            m = 8 if idx == 8 else 128
            if idx < 8:
                dst = mqk_raw[idx * 128:(idx + 1) * 128, tok0:tok0 + 512]
            elif idx == 8:
                dst = mif[0:8, tok0:tok0 + 512]
            else:
                dst = rw_raw[(idx - 9) * 128:(idx - 8) * 128, tok0:tok0 + 512]
            P.dma('pool', dst, src[:m, :], reads=[skey])

        def t_epi(idx, ps, pkey, tokj, jj):
            i = cnt['t'] % 2
            cnt['t'] += 1
            if idx == 0:
                P.op('dve', lambda e: e.tensor_copy(out=vs[i][:, :, 0:128], in_=ps[:].rearrange("p (h d) -> p h d", h=4)),
                     reads=[pkey], writes=[('vs', i)])
                P.dma('pool', v_aug[tokj:tokj + 128, :], vs[i][:].rearrange("p h d -> p (h d)"), reads=[('vs', i)])
            else:
                P.op('act', lambda e: e.activation(out=so[i][:], in_=ps[:], func=AF.Sigmoid), reads=[pkey], writes=[('so', i)])
                P.dma('pool', moA[tokj:tokj + 128, :], so[i][:], reads=[('so', i)])

        fcols = [(i * 128, 128) for i in range(8)] + [(1536, 8)] + [(2056 + i * 128, 128) for i in range(14)]
        tcols = [(1024, 512), (1544, 512)]
        ip.run(fcols, tcols, f_epi, t_epi, copy_f=True, copy_t=(), nraw=6)


def stage_ml_prep(P, K, mqk_raw, prm, mqkT, mkA, S, nseq):
    with Ctx(P) as c:
        cw = c.sb('cw', [128, 8, 4], F32)
        cb = c.sb('cb', [128, 8], F32)
        P.dma('sp', cw[:], prm['ml_cw'], writes=['cw'])
        P.dma('sp', cb[:], prm['ml_cb'], writes=['cb'])
        buf = [c.sb('buf', [128, 515], F32) for _ in range(3)]
        acc = [c.sb('acc', [128, 512], F32) for _ in range(2)]
        qo = [c.sb('qo', [128, 512], BF16) for _ in range(2)]
        kt = [c.sb('kt', [128, 4, 128], BF16) for _ in range(2)]
        tp = [c.ps('tp', [128, 4, 128], BF16) for _ in range(2)]
        n = 0
        for s in range(nseq):
            for g in range(S // 512):
                t0 = s * S + g * 512
                for ch in range(8):
                    b3 = n % 3
                    b = n % 2
                    en = 'dve' if n % 2 == 0 else 'pool'
                    n += 1
                    if g == 0:
                        P.op('pool', lambda e, b3=b3: e.memset(buf[b3][:, 0:3], 0.0), writes=[('buf', b3)])
                        P.dma('sp', buf[b3][:, 3:515], mqk_raw[ch * 128:(ch + 1) * 128, t0:t0 + 512], writes=[('buf', b3)])
                    else:
                        P.dma('sp', buf[b3][:, :], mqk_raw[ch * 128:(ch + 1) * 128, t0 - 3:t0 + 512], writes=[('buf', b3)])
                    P.op(en, lambda e, b3=b3, b=b, ch=ch: e.tensor_scalar(out=acc[b][:], in0=buf[b3][:, 3:515], scalar1=cw[:, ch, 3:4],
                                                                         scalar2=cb[:, ch:ch + 1], op0=ALU.mult, op1=ALU.add),
                         reads=[('buf', b3), 'cw', 'cb'], writes=[('acc', b)])
                    for j in (2, 1, 0):
                        P.op('dve', lambda e, b3=b3, b=b, ch=ch, j=j: e.scalar_tensor_tensor(out=acc[b][:], in0=buf[b3][:, j:j + 512],
                                                                                           scalar=cw[:, ch, j:j + 1], in1=acc[b][:],
                                                                                           op0=ALU.mult, op1=ALU.add),
                             reads=[('buf', b3), ('acc', b), 'cw'], writes=[('acc', b)])
                    P.op('act', lambda e, b=b: e.activation(out=acc[b][:], in_=acc[b][:], func=AF.Silu), reads=[('acc', b)], writes=[('acc', b)])
                    sc = 1.0 if ch < 4 else float(128 ** -0.5)
                    P.op(en, lambda e, b=b, sc=sc: e.tensor_scalar(out=qo[b][:], in0=acc[b][:], scalar1=sc, scalar2=None, op0=ALU.mult),
                         reads=[('acc', b)], writes=[('qo', b)])
                    P.dma('pool', mqkT[ch * 128:(ch + 1) * 128, t0:t0 + 512], qo[b][:], reads=[('qo', b)])
                    if ch >= 4:
                        for j in range(4):
                            P.op('pe', lambda e, b=b, j=j: e.transpose(out=tp[b][:, j, :], in_=qo[b][:, j * 128:(j + 1) * 128], identity=K['idb'][:]),
                                 reads=[('qo', b)], writes=[('tp', b)])
                        P.op('act', lambda e, b=b: e.copy(out=kt[b][:], in_=tp[b][:]), reads=[('tp', b)], writes=[('kt', b)])
                        P.dma('pool', mkA[t0:t0 + 512, (ch - 4) * 128:(ch - 3) * 128].rearrange("(j p) d -> p j d", p=128), kt[b][:],
                              reads=[('kt', b)])


def stage_ml_gates(P, K, mif, prm, mgM, mcol, mend, S, nseq):
    NB = S // 128
    R8 = nseq * 4
    with Ctx(P) as c:
        it = c.sb('it', [R8, S], F32)
        ft = c.sb('ft', [R8, S], F32)
        Bt = c.sb('Bt', [R8, S], F32)
        Mt = c.sb('Mt', [R8, S], F32)
        ones = c.sb('ones', [R8, S], F32)
        gb = c.sb('gb', [R8, 2], F32)
        ngb = c.sb('ngb', [R8, 1], F32)
        P.dma('sp', gb[:], prm['ml_gb'][0:R8, :], writes=['gb'])
        for s in range(nseq):
            P.dma('sp', it[4 * s:4 * s + 4, :], mif[0:4, s * S:(s + 1) * S], writes=['it'])
            P.dma('sp', ft[4 * s:4 * s + 4, :], mif[4:8, s * S:(s + 1) * S], writes=['ft'])
        P.op('dve', lambda e: e.memset(ones[:], 1.0), writes=['ones'])
        P.op('dve', lambda e: e.tensor_scalar(out=ngb[:], in0=gb[:, 1:2], scalar1=-1.0, scalar2=None, op0=ALU.mult), reads=['gb'], writes=['ngb'])
        P.op('act', lambda e: e.activation(out=ft[:], in_=ft[:], func=AF.Exp, scale=-1.0, bias=ngb[:, 0:1]), reads=['ft', 'ngb'], writes=['ft'])
        P.op('dve', lambda e: e.tensor_scalar(out=ft[:], in0=ft[:], scalar1=1.0, scalar2=None, op0=ALU.add), reads=['ft'], writes=['ft'])
        P.op('act', lambda e: e.activation(out=ft[:], in_=ft[:], func=AF.Ln), reads=['ft'], writes=['ft'])
        P.op('dve', lambda e: e.tensor_tensor_scan(out=Bt[:], data0=ones[:], data1=ft[:], initial=0.0, op0=ALU.mult, op1=ALU.subtract),
             reads=['ones', 'ft'], writes=['Bt'])
        P.op('dve', lambda e: e.scalar_tensor_tensor(out=it[:], in0=it[:], scalar=gb[:, 0:1], in1=Bt[:], op0=ALU.add, op1=ALU.subtract),
             reads=['it', 'gb', 'Bt'], writes=['it'])
        P.op('dve', lambda e: e.tensor_tensor_scan(out=Mt[:], data0=it[:], data1=it[:], initial=0.0, op0=ALU.max, op1=ALU.max),
             reads=['it'], writes=['Mt'])
        P.op('dve', lambda e: e.tensor_tensor(out=Bt[:], in0=Bt[:], in1=Mt[:], op=ALU.add), reads=['Bt', 'Mt'], writes=['Bt'])
        P.op('act', lambda e: e.activation(out=Bt[:], in_=Bt[:], func=AF.Exp, scale=-1.0), reads=['Bt'], writes=['Bt'])
        P.dma('pool', mgM[0:R8, :], Mt[:], reads=['Mt'])
        P.dma('pool', mend.rearrange("(c r) -> r c", r=R8), Mt[:].rearrange("r (c k) -> r c k", k=128)[:, :, 127], reads=['Mt'], allow_slow_non_contiguous=True)
        pc = [c.ps('pc', [128, NB, R8], F32) for _ in range(2)]
        col = [c.sb('col', [128, NB, R8], F32) for _ in range(2)]
        for k, src in enumerate((it, Bt)):
            for cb_ in range(NB):
                P.op('pe', lambda e, k=k, cb_=cb_, src=src: e.transpose(out=pc[k][:, cb_, :], in_=src[:, cb_ * 128:(cb_ + 1) * 128],
                                                                       identity=K['idf'][0:R8, 0:R8]),
                     reads=['it', 'Bt', 'idf'], writes=[('pc', k)])
            P.op('dve', lambda e, k=k: e.tensor_copy(out=col[k][:], in_=pc[k][:]), reads=[('pc', k)], writes=[('col', k)])
            P.dma('pool', mcol[k], col[k][:].rearrange("p c r -> p (c r)"), reads=[('col', k)])


def stage_ml_core(P, K, mqkT, mkA, v_aug, moA, mgM, mcol, mend, prm, mixT, S, nseq):
    NB = S // 128
    R8 = nseq * 4
    with Ctx(P) as c:
        acol = c.sb('acol', [128, NB, R8], F32)
        encol = c.sb('encol', [128, NB, R8], F32)
        Mc = c.sb('Mc', [128, NB + 1, R8], F32)
        nMc = c.sb('nMc', [128, NB + 1, R8], F32)
        dcs = c.sb('dcs', [128, NB, R8], F32)
        wcs = c.sb('wcs', [128, NB, R8], F32)
        ngr = c.sb('ngr', [128, 512], F32)
        P.dma('sp', acol[:].rearrange("p c r -> p (c r)"), mcol[0], writes=['acol'])
        P.dma('sp', encol[:].rearrange("p c r -> p (c r)"), mcol[1], writes=['encol'])
        P.op('dve', lambda e: e.memset(Mc[:, 0, :], 0.0), writes=['Mc'])
        P.dma('sp', Mc[:, 1:, :].rearrange("p c r -> p (c r)"), mend.rearrange("(o n) -> o n", o=1).partition_broadcast(128), writes=['Mc'])
        P.dma('sp', ngr[:], prm['ml_ng_rep'], writes=['ngr'])
        P.op('dve', lambda e: e.tensor_scalar(out=nMc[:], in0=Mc[:], scalar1=-1.0, scalar2=None, op0=ALU.mult), reads=['Mc'], writes=['nMc'])
        P.op('dve', lambda e: e.tensor_tensor(out=dcs[:], in0=Mc[:, 0:NB, :], in1=Mc[:, 1:NB + 1, :], op=ALU.subtract), reads=['Mc'], writes=['dcs'])
        P.op('act', lambda e: e.activation(out=dcs[:], in_=dcs[:], func=AF.Exp), reads=['dcs'], writes=['dcs'])
        P.op('dve', lambda e: e.tensor_tensor(out=wcs[:], in0=acol[:], in1=Mc[:, 1:NB + 1, :], op=ALU.subtract), reads=['acol', 'Mc'], writes=['wcs'])
        P.op('act', lambda e: e.activation(out=wcs[:], in_=wcs[:], func=AF.Exp), reads=['wcs'], writes=['wcs'])
        Cst = [c.sb('Cst', [128, 129], F32) for _ in range(R8)]
        Cbf = [c.sb('Cbf', [128, 129], BF16) for _ in range(R8)]
        for r in range(R8):
            P.op('pool', lambda e, r=r: e.memset(Cst[r][:], 0.0), writes=[('Cst', r)])
            P.op('pool', lambda e, r=r: e.memset(Cbf[r][:], 0.0), writes=[('Cbf', r)])
        qT4 = [c.sb('qT4', [128, 4, 512], BF16) for _ in range(2)]
        kT4 = [c.sb('kT4', [128, 4, 512], BF16) for _ in range(2)]
        kA4 = [c.sb('kA4', [128, 4, 512], BF16) for _ in range(2)]
        vA4 = [c.sb('vA4', [128, 4, 516], BF16) for _ in range(2)]
        oA4 = [c.sb('oA4', [128, 4, 512], F32) for _ in range(2)]
        Mb4 = [c.sb('Mb4', [128, 4, 512], F32) for _ in range(2)]
        mo = [c.sb('mo', [128, 4, 512], BF16) for _ in range(2)]
        E = [c.sb('E', [128, 128], F32) for _ in range(4)]
        PT = [c.sb('PT', [128, 128], BF16) for _ in range(4)]
        er = [c.sb('er', [128, 128], F32) for _ in range(4)]
        qs = [c.sb('qs', [128, 128], BF16) for _ in range(4)]
        hs = [c.sb('hs', [128, 128], F32) for _ in range(4)]
        hj = [c.sb('hj', [128, 128], F32) for _ in range(2)]
        hf = [c.sb('hf', [128, 128], BF16) for _ in range(4)]
        vw = [c.sb('vw', [128, 129], BF16) for _ in range(4)]
        sts = [c.sb('sts', [128, 6, 4], F32) for _ in range(2)]
        ps_st = [c.ps('pst', [128, 512], F32) for _ in range(2)]
        ps_oa = [c.ps('psoa', [128, 512], F32) for _ in range(2)]
        ps_ob = [c.ps('psob', [128, 512], F32) for _ in range(1)]
        ps_u = [c.ps('psu', [128, 512], F32) for _ in range(2)]
        ps_t = [c.ps('pstt', [128, 4, 128], BF16) for _ in range(1)]
        n = 0

        def po(b, h):
            return ps_oa[b][:, h * 160:h * 160 + 129] if h < 3 else ps_ob[0][:, 0:129]

        def pok(b, h):
            return ('psoa', b) if h < 3 else ('psob', 0)

        def pu(h):
            return ps_u[0][:, h * 160:h * 160 + 129] if h < 3 else ps_u[1][:, 0:129]

        def puk(h):
            return ('psu', 0) if h < 3 else ('psu', 1)

        for sg in range(S // 512):
            for s in range(nseq):
                lb = (sg * nseq + s) % 2
                t0 = s * S + sg * 512
                P.dma('sp', qT4[lb][:], mqkT[0:512, t0:t0 + 512].rearrange("(h d) t -> d h t", d=128), writes=[('qT4', lb)])
                P.dma('sp', kT4[lb][:], mqkT[512:1024, t0:t0 + 512].rearrange("(h d) t -> d h t", d=128), writes=[('kT4', lb)])
                P.dma('sp', kA4[lb][:], mkA[t0:t0 + 512, :].rearrange("(c p) f -> p c f", p=128), writes=[('kA4', lb)])
                P.dma('sp', vA4[lb][:], v_aug[t0:t0 + 512, :].rearrange("(c p) f -> p c f", p=128), writes=[('vA4', lb)])
                P.dma('sp', oA4[lb][:], moA[t0:t0 + 512, :].rearrange("(c p) f -> p c f", p=128), writes=[('oA4', lb)])
                for h in range(4):
                    P.dma('sp', Mb4[lb][:, h, :], mgM[s * 4 + h:s * 4 + h + 1, sg * 512:(sg + 1) * 512].partition_broadcast(128),
                          writes=[('Mb4', lb)])
                for c4 in range(4):
                    cg = sg * 4 + c4
                    cs = slice(c4 * 128, (c4 + 1) * 128)
                    b = n % 2
                    n += 1
                    st = sts[b]
                    H4 = range(4)
                    for h in H4:
                        P.op('pe', lambda e: e.matmul(ps_st[b][:, h * 128:(h + 1) * 128], kT4[lb][:, h, cs], qT4[lb][:, h, cs], start=True, stop=True),
                             reads=[('kT4', lb), ('qT4', lb)], writes=[('pst', b)])
                    for h in H4:
                        sh = s * 4 + h
                        P.op('act', lambda e: e.activation(out=E[h][:], in_=Mb4[lb][:, h, cs], func=AF.Exp, scale=-1.0, bias=acol[:, cg, sh:sh + 1]),
                             reads=[('Mb4', lb), 'acol'], writes=[('E', h)])
                        P.op('act', lambda e: e.activation(out=er[h][:], in_=Mb4[lb][:, h, cs], func=AF.Exp, scale=-1.0, bias=Mc[:, cg, sh:sh + 1]),
                             reads=[('Mb4', lb), 'Mc'], writes=[('er', h)])
                    for h in H4:
                        P.op('pool', lambda e: e.tensor_tensor(out=E[h][:], in0=E[h][:], in1=K['triu'], op=ALU.mult), reads=[('E', h)], writes=[('E', h)])
                        P.op('pool', lambda e: e.tensor_tensor(out=qs[h][:], in0=qT4[lb][:, h, cs], in1=er[h][:], op=ALU.mult),
                             reads=[('qT4', lb), ('er', h)], writes=[('qs', h)])
                    for h in H4:
                        P.op('dve', lambda e: e.tensor_tensor(out=PT[h][:], in0=ps_st[b][:, h * 128:(h + 1) * 128], in1=E[h][:], op=ALU.mult),
                             reads=[('pst', b), ('E', h)], writes=[('PT', h)])
                    for h in H4:
                        sh = s * 4 + h
                        P.op('pe', lambda e: e.matmul(po(b, h), qs[h][:], Cbf[sh][:], start=True, stop=False),
                             reads=[('qs', h), ('Cbf', sh)], writes=[pok(b, h)])
                        P.op('pe', lambda e: e.matmul(po(b, h), PT[h][:], vA4[lb][:, c4, h * 129:(h + 1) * 129], start=False, stop=True),
                             reads=[('PT', h), ('vA4', lb)], writes=[pok(b, h)])
                    for h in H4:
                        P.op('act', lambda e: e.activation(out=st[:, 0, h:h + 1], in_=po(b, h)[:, 128:129], func=AF.Abs),
                             reads=[pok(b, h)], writes=[('sts', b, 0, h)])
                    for h in H4:
                        sh = s * 4 + h
                        P.op('dve', lambda e: e.tensor_tensor(out=st[:, 0, h:h + 1], in0=st[:, 0, h:h + 1], in1=encol[:, cg, sh:sh + 1], op=ALU.max),
                             reads=[('sts', b, 0, h), 'encol'], writes=[('sts', b, 0, h)])
                    P.op('dve', lambda e: e.reciprocal(out=st[:, 0, :], in_=st[:, 0, :]), reads=[('sts', b, 0, h) for h in H4], writes=[('sts', b, 0, h) for h in H4])
                    for h in H4:
                        P.op('act', lambda e: e.activation(out=hs[h][:], in_=po(b, h)[:, 0:128], func=AF.Copy, scale=st[:, 0, h:h + 1], accum_out=st[:, 1, h:h + 1]),
                             reads=[pok(b, h), ('sts', b, 0, h)], writes=[('hs', h), ('sts', b, 1, h)])
                        P.op('act', lambda e: e.activation(out=hj[h % 2][:], in_=hs[h][:], func=AF.Square, accum_out=st[:, 2, h:h + 1]),
                             reads=[('hs', h)], writes=[('hj', h % 2), ('sts', b, 2, h)])
                    k12 = [('sts', b, 1, h) for h in H4] + [('sts', b, 2, h) for h in H4]
                    P.op('dve', lambda e: e.tensor_scalar(out=st[:, 3, :], in0=st[:, 1, :], scalar1=1.0 / 128, scalar2=None, op0=ALU.mult), reads=k12, writes=[('sts', b, 3)])
                    P.op('dve', lambda e: e.tensor_tensor(out=st[:, 4, :], in0=st[:, 3, :], in1=st[:, 3, :], op=ALU.mult), reads=[('sts', b, 3)], writes=[('sts', b, 4)])
                    P.op('dve', lambda e: e.tensor_scalar(out=st[:, 2, :], in0=st[:, 2, :], scalar1=1.0 / 128, scalar2=EPS, op0=ALU.mult, op1=ALU.add), reads=k12, writes=k12)
                    P.op('dve', lambda e: e.tensor_tensor(out=st[:, 2, :], in0=st[:, 2, :], in1=st[:, 4, :], op=ALU.subtract), reads=k12 + [('sts', b, 4)], writes=k12)
                    P.op('act', lambda e: e.sqrt(out=st[:, 2, :], in_=st[:, 2, :]), reads=k12, writes=k12)
                    P.op('dve', lambda e: e.reciprocal(out=st[:, 2, :], in_=st[:, 2, :]), reads=k12, writes=k12)
                    for h in H4:
                        P.op('dve', lambda e: e.tensor_scalar(out=hs[h][:], in0=hs[h][:], scalar1=st[:, 3, h:h + 1], scalar2=st[:, 2, h:h + 1],
                                                              op0=ALU.subtract, op1=ALU.mult),
                             reads=[('hs', h), ('sts', b, 3)] + k12, writes=[('hs', h)])
                    for h in H4:
                        P.op('pool', lambda e: e.tensor_tensor(out=hs[h][:], in0=hs[h][:], in1=ngr[:, h * 128:(h + 1) * 128], op=ALU.mult),
                             reads=[('hs', h), 'ngr'], writes=[('hs', h)])
                        P.op('pool', lambda e: e.tensor_tensor(out=hf[h][:], in0=hs[h][:], in1=oA4[lb][:, c4, h * 128:(h + 1) * 128], op=ALU.mult),
                             reads=[('hs', h), ('oA4', lb)], writes=[('hf', h)])
                    for h in H4:
                        sh = s * 4 + h
                        P.op('dve', lambda e: e.tensor_scalar(out=vw[h][:], in0=vA4[lb][:, c4, h * 129:(h + 1) * 129], scalar1=wcs[:, cg, sh:sh + 1], scalar2=None, op0=ALU.mult),
                             reads=[('vA4', lb), 'wcs'], writes=[('vw', h)])
                    for h in H4:
                        P.op('pe', lambda e: e.matmul(pu(h), kA4[lb][:, c4, h * 128:(h + 1) * 128], vw[h][:], start=True, stop=True),
                             reads=[('kA4', lb), ('vw', h)], writes=[puk(h)])
                    for h in H4:
                        P.op('pe', lambda e: e.transpose(out=ps_t[0][:, h, :], in_=hf[h][:], identity=K['idb'][:]), reads=[('hf', h)], writes=['pstt'])
                    for h in H4:
                        sh = s * 4 + h
                        P.op('dve', lambda e: e.scalar_tensor_tensor(out=Cst[sh][:], in0=Cst[sh][:], scalar=dcs[:, cg, sh:sh + 1], in1=pu(h), op0=ALU.mult, op1=ALU.add),
                             reads=[('Cst', sh), 'dcs', puk(h)], writes=[('Cst', sh)])
                    for h in H4:
                        sh = s * 4 + h
                        P.op('act', lambda e: e.copy(out=Cbf[sh][:], in_=Cst[sh][:]), reads=[('Cst', sh)], writes=[('Cbf', sh)])
                    P.op('act', lambda e: e.copy(out=mo[lb][:, :, cs], in_=ps_t[0][:]), reads=['pstt'], writes=[('mo', lb)])
                P.dma('pool', mixT[0:512, t0:t0 + 512].rearrange("(h d) t -> d h t", d=128), mo[lb][:], reads=[('mo', lb)])


RW_LD = -0.6065306597126334


def stage_rw_prep(P, K, rw_raw, prm, rwF, rwT, rwgL, rwbv, rwg, S, nseq):
    with Ctx(P) as c:
        mu = c.sb('mu', [128, 14], F32)
        pc = c.sb('pc', [128, 5, 4], F32)
        P.dma('sp', mu[:], prm['rw_mu'], writes=['mu'])
        P.dma('sp', pc[:], prm['rw_pc'], writes=['pc'])
        stg = c.sb('stg', [128, 3, 512], F32)
        P.op('dve', lambda e: e.memset(stg[:], 0.0), writes=['stg'])
        P.dma('sp', stg[0:64, 0, :], prm['rw_w2'], writes=['stg'])
        P.dma('sp', stg[64:128, 1, :], prm['rw_a2'], writes=['stg'])
        P.dma('sp', stg[:, 2, :], prm['rw_g2'], writes=['stg'])
        lw = c.sb('lw', [128, 3, 512], BF16)
        P.op('dve', lambda e: e.tensor_copy(out=lw[:], in_=stg[:]), reads=['stg'], writes=['lw'])
        blkf = K['blk64f']
        buf = [c.sb('buf', [128, 513], F32) for _ in range(3)]
        dd = [c.sb('dd', [128, 512], F32) for _ in range(2)]
        xs = c.sb('xs', [128, 14, 512], F32)
        wab = c.sb('wab', [128, 512], BF16)
        sgb = c.sb('sgb', [128, 512], BF16)
        ld2 = [c.sb('ld', [128, 512], F32) for _ in range(2)]
        av2 = [c.sb('av', [128, 512], F32) for _ in range(2)]
        gv2 = [c.sb('gv', [128, 512], F32) for _ in range(2)]
        kk2 = [c.sb('kk', [128, 512], F32) for _ in range(2)]
        km2 = [c.sb('km', [128, 512], F32) for _ in range(2)]
        bb2 = [c.sb('bb', [128, 512], F32) for _ in range(2)]
        t12 = [c.sb('t1', [128, 512], F32) for _ in range(2)]
        t22 = [c.sb('t2', [128, 512], F32) for _ in range(2)]
        lg2 = [c.sb('lg', [128, 512], F32) for _ in range(2)]
        eg2 = [c.sb('eg', [128, 512], F32) for _ in range(2)]
        egl2 = [c.sb('egl', [128, 512], F32) for _ in range(2)]
        gl2 = [c.sb('gl', [128, 8], F32) for _ in range(2)]
        of = [c.sb('of', [128, 4, 512], BF16) for _ in range(2)]
        o32 = [c.sb('o32', [128, 512], F32) for _ in range(2)]
        tb = [c.sb('tb', [128, 3, 512], BF16) for _ in range(2)]
        tt = [c.sb('tt', [128, 4, 128], BF16) for _ in range(2)]
        pz = [c.ps('pz', [128, 512], F32) for _ in range(2)]
        pb = [c.ps('pb', [128, 512], F32) for _ in range(2)]
        tp = [c.ps('tp', [128, 4, 128], BF16) for _ in range(2)]
        n = 0
        nt = 0
        no = 0
        for s in range(nseq):
            for g in range(S // 512):
                t0 = s * S + g * 512
                for ci in range(14):
                    b3 = n % 3
                    b = n % 2
                    en = 'dve' if n % 2 == 0 else 'pool'
                    n += 1
                    if g == 0:
                        P.op('pool', lambda e, b3=b3: e.memset(buf[b3][:, 0:1], 0.0), writes=[('buf', b3)])
                        P.dma('sp', buf[b3][:, 1:513], rw_raw[ci * 128:(ci + 1) * 128, t0:t0 + 512], writes=[('buf', b3)])
                    else:
                        P.dma('sp', buf[b3][:, :], rw_raw[ci * 128:(ci + 1) * 128, t0 - 1:t0 + 512], writes=[('buf', b3)])
                    P.op(en, lambda e, b3=b3, b=b: e.tensor_tensor(out=dd[b][:], in0=buf[b3][:, 0:512], in1=buf[b3][:, 1:513], op=ALU.subtract),
                         reads=[('buf', b3)], writes=[('dd', b)])
                    P.op('dve', lambda e, b3=b3, b=b, ci=ci: e.scalar_tensor_tensor(out=xs[:, ci, :], in0=dd[b][:], scalar=mu[:, ci:ci + 1],
                                                                                in1=buf[b3][:, 1:513], op0=ALU.mult, op1=ALU.add),
                         reads=[('dd', b), ('buf', b3), 'mu'], writes=[('xs', ci)])
                P.op('act', lambda e: e.activation(out=wab[0:64, :], in_=xs[0:64, 12, :], func=AF.Tanh), reads=[('xs', 12)], writes=['wab'])
                P.op('act', lambda e: e.copy(out=wab[64:128, :], in_=xs[64:128, 12, :]), reads=[('xs', 12)], writes=['wab'])
                P.op('act', lambda e: e.activation(out=sgb[:], in_=xs[:, 13, :], func=AF.Sigmoid), reads=[('xs', 13)], writes=['sgb'])
                ob = no % 2
                no += 1
                for fc in range(4):
                    fs = slice(fc * 128, (fc + 1) * 128)
                    zb = fc % 2
                    P.op('pe', lambda e, fs=fs, zb=zb: e.matmul(pz[zb][:], lw[:, 0, fs], wab[:], start=True, stop=True), reads=['lw', 'wab'], writes=[('pz', zb)])
                    P.op('act', lambda e, zb=zb, fc=fc: e.activation(out=ld2[fc % 2][:], in_=pz[zb][:], func=AF.Sigmoid, bias=pc[:, 0, fc:fc + 1]),
                         reads=[('pz', zb), 'pc'], writes=[('ld', fc % 2)])
                    P.op('dve', lambda e: e.tensor_scalar(out=ld2[fc % 2][:], in0=ld2[fc % 2][:], scalar1=RW_LD, scalar2=None, op0=ALU.mult), reads=[('ld', fc % 2)], writes=[('ld', fc % 2)])
                    P.op('pe', lambda e, fs=fs, zb=zb: e.matmul(pb[zb][:], lw[:, 1, fs], wab[:], start=True, stop=True), reads=['lw', 'wab'], writes=[('pb', zb)])
                    P.op('act', lambda e, zb=zb, fc=fc: e.activation(out=av2[fc % 2][:], in_=pb[zb][:], func=AF.Sigmoid, bias=pc[:, 1, fc:fc + 1]),
                         reads=[('pb', zb), 'pc'], writes=[('av', fc % 2)])
                    P.op('dve', lambda e, fc=fc: e.tensor_scalar(out=kk2[fc % 2][:], in0=xs[:, 4 + fc, :], scalar1=pc[:, 2, fc:fc + 1], scalar2=None, op0=ALU.mult),
                         reads=[('xs', 4 + fc), 'pc'], writes=[('kk', fc % 2)])
                    P.op('act', lambda e: e.activation(out=t12[fc % 2][:], in_=kk2[fc % 2][:], func=AF.Square), reads=[('kk', fc % 2)], writes=[('t1', fc % 2)])
                    P.op('pe', lambda e, zb=zb: e.matmul(pz[zb][:], blkf, t12[fc % 2][:], start=True, stop=True), reads=[('t1', fc % 2), 'blk64f'], writes=[('pz', zb)])
                    P.op('act', lambda e, zb=zb: e.sqrt(out=t12[fc % 2][:], in_=pz[zb][:]), reads=[('pz', zb)], writes=[('t1', fc % 2)])
                    P.op('dve', lambda e: e.tensor_scalar(out=t12[fc % 2][:], in0=t12[fc % 2][:], scalar1=1e-12, scalar2=None, op0=ALU.max), reads=[('t1', fc % 2)], writes=[('t1', fc % 2)])
                    P.op('dve', lambda e: e.reciprocal(out=t12[fc % 2][:], in_=t12[fc % 2][:]), reads=[('t1', fc % 2)], writes=[('t1', fc % 2)])
                    P.op('dve', lambda e: e.tensor_tensor(out=kk2[fc % 2][:], in0=kk2[fc % 2][:], in1=t12[fc % 2][:], op=ALU.mult), reads=[('kk', fc % 2), ('t1', fc % 2)], writes=[('kk', fc % 2)])
                    P.op('pool', lambda e, fc=fc: e.tensor_scalar(out=t22[fc % 2][:], in0=av2[fc % 2][:], scalar1=-1.0, scalar2=pc[:, 3, fc:fc + 1], op0=ALU.add, op1=ALU.mult),
                         reads=[('av', fc % 2), 'pc'], writes=[('t2', fc % 2)])
                    P.op('dve', lambda e, fc=fc: e.scalar_tensor_tensor(out=km2[fc % 2][:], in0=t22[fc % 2][:], scalar=1.0, in1=xs[:, 4 + fc, :], op0=ALU.add, op1=ALU.mult),
                         reads=[('t2', fc % 2), ('xs', 4 + fc)], writes=[('km', fc % 2)])
                    P.op('pool', lambda e: e.tensor_tensor(out=bb2[fc % 2][:], in0=kk2[fc % 2][:], in1=av2[fc % 2][:], op=ALU.mult), reads=[('kk', fc % 2), ('av', fc % 2)], writes=[('bb', fc % 2)])
                    P.op('pe', lambda e, fs=fs, zb=zb: e.matmul(pb[zb][:], lw[:, 2, fs], sgb[:], start=True, stop=True), reads=['lw', 'sgb'], writes=[('pb', zb)])
                    P.op('act', lambda e, zb=zb: e.copy(out=gv2[fc % 2][:], in_=pb[zb][:]), reads=[('pb', zb)], writes=[('gv', fc % 2)])
                    P.dma('pool', rwg[fs, t0:t0 + 512], gv2[fc % 2][:], reads=[('gv', fc % 2)])
                    P.op('dve', lambda e, fc=fc: e.scalar_tensor_tensor(out=t12[fc % 2][:], in0=xs[:, fc, :], scalar=pc[:, 4, fc:fc + 1], in1=km2[fc % 2][:], op0=ALU.mult, op1=ALU.mult),
                         reads=[('xs', fc), 'pc', ('km', fc % 2)], writes=[('t1', fc % 2)])
                    P.op('pe', lambda e, zb=zb: e.matmul(pz[zb][:], blkf, t12[fc % 2][:], start=True, stop=True), reads=[('t1', fc % 2), 'blk64f'], writes=[('pz', zb)])
                    P.op('dve', lambda e, zb=zb, fc=fc: e.tensor_tensor(out=t12[fc % 2][:], in0=pz[zb][:], in1=xs[:, 8 + fc, :], op=ALU.mult),
                         reads=[('pz', zb), ('xs', 8 + fc)], writes=[('t1', fc % 2)])
                    o3 = no % 2
                    P.op('dve', lambda e, o3=o3: e.tensor_tensor(out=o32[o3][:], in0=t12[fc % 2][:], in1=gv2[fc % 2][:], op=ALU.mult), reads=[('t1', fc % 2), ('gv', fc % 2)], writes=[('o32', o3)])
                    P.dma('pool', rwbv[fs, t0:t0 + 512], o32[o3][:], reads=[('o32', o3)])
                    for cc in range(8):
                        cs = slice(cc * 64, (cc + 1) * 64)
                        P.op('dve', lambda e, cs=cs: e.tensor_tensor_scan(out=lg2[fc % 2][:, cs], data0=K['ones_f'][:, 0:64], data1=ld2[fc % 2][:, cs], initial=0.0,
                                                                         op0=ALU.mult, op1=ALU.add),
                             reads=[('ld', fc % 2), 'ones_f'], writes=[('lg', fc % 2)])
                    lg3 = lg2[fc % 2][:].rearrange("p (c k) -> p c k", k=64)
                    P.op('act', lambda e: e.activation(out=eg2[fc % 2][:], in_=lg2[fc % 2][:], func=AF.Exp), reads=[('lg', fc % 2)], writes=[('eg', fc % 2)])
                    P.op('act', lambda e: e.copy(out=gl2[fc % 2][:], in_=eg2[fc % 2][:].rearrange("p (c k) -> p c k", k=64)[:, :, 63]), reads=[('eg', fc % 2)], writes=[('gl', fc % 2)])
                    P.dma('pool', rwgL[fs, t0 // 64:t0 // 64 + 8], gl2[fc % 2][:], reads=[('gl', fc % 2)])
                    P.op('dve', lambda e, fc=fc, ob=ob: e.tensor_tensor(out=of[ob][:, 1, :], in0=xs[:, fc, :], in1=eg2[fc % 2][:], op=ALU.mult),
                         reads=[('xs', fc), ('eg', fc % 2)], writes=[('of', ob)])
                    for cc in range(8):
                        cs = slice(cc * 64, (cc + 1) * 64)
                        P.op('act', lambda e, cs=cs, cc=cc: e.activation(out=egl2[fc % 2][:, cs], in_=lg2[fc % 2][:, cs], func=AF.Exp, scale=-1.0,
                                                                         bias=lg2[fc % 2][:, cc * 64 + 63:cc * 64 + 64]),
                             reads=[('lg', fc % 2)], writes=[('egl', fc % 2)])
                    tbb = nt % 2
                    P.op('pool', lambda e, tbb=tbb: e.tensor_tensor(out=tb[tbb][:, 1, :], in0=km2[fc % 2][:], in1=egl2[fc % 2][:], op=ALU.mult), reads=[('km', fc % 2), ('egl', fc % 2)], writes=[('tb', tbb)])
                    P.op('pool', lambda e, tbb=tbb: e.tensor_tensor(out=tb[tbb][:, 2, :], in0=bb2[fc % 2][:], in1=egl2[fc % 2][:], op=ALU.mult), reads=[('bb', fc % 2), ('egl', fc % 2)], writes=[('tb', tbb)])
                    P.op('act', lambda e, tbb=tbb, fc=fc: e.copy(out=tb[tbb][:, 0, :], in_=xs[:, 8 + fc, :]), reads=[('xs', 8 + fc)], writes=[('tb', tbb)])
                    P.op('act', lambda e: e.activation(out=eg2[fc % 2][:], in_=lg2[fc % 2][:], func=AF.Exp, scale=-1.0), reads=[('lg', fc % 2)], writes=[('eg', fc % 2)])
                    P.op('dve', lambda e, ob=ob: e.tensor_tensor(out=of[ob][:, 2, :], in0=bb2[fc % 2][:], in1=eg2[fc % 2][:], op=ALU.mult), reads=[('bb', fc % 2), ('eg', fc % 2)], writes=[('of', ob)])
                    P.op('dve', lambda e, ob=ob: e.tensor_tensor(out=of[ob][:, 3, :], in0=km2[fc % 2][:], in1=eg2[fc % 2][:], op=ALU.mult), reads=[('km', fc % 2), ('eg', fc % 2)], writes=[('of', ob)])
                    P.op('dve', lambda e: e.tensor_tensor(out=t22[fc % 2][:], in0=lg2[fc % 2][:], in1=ld2[fc % 2][:], op=ALU.subtract), reads=[('lg', fc % 2), ('ld', fc % 2)], writes=[('t2', fc % 2)])
                    P.op('act', lambda e: e.activation(out=t22[fc % 2][:], in_=t22[fc % 2][:], func=AF.Exp), reads=[('t2', fc % 2)], writes=[('t2', fc % 2)])
                    P.op('dve', lambda e, ob=ob: e.tensor_tensor(out=of[ob][:, 0, :], in0=kk2[fc % 2][:], in1=t22[fc % 2][:], op=ALU.mult), reads=[('kk', fc % 2), ('t2', fc % 2)], writes=[('of', ob)])
                    P.dma('pool', rwF[:, fs, t0:t0 + 512].rearrange("a p t -> p a t"), of[ob][:], reads=[('of', ob)])
                    for a3 in range(3):
                        tq = nt % 2
                        nt += 1
                        for j in range(4):
                            P.op('pe', lambda e, tq=tq, j=j, a3=a3, tbb=tbb: e.transpose(out=tp[tq][:, j, :], in_=tb[tbb][:, a3, j * 128:(j + 1) * 128], identity=K['idb'][:]),
                                 reads=[('tb', tbb)], writes=[('tp', tq)])
                        P.op('act', lambda e, tq=tq: e.copy(out=tt[tq][:], in_=tp[tq][:]), reads=[('tp', tq)], writes=[('tt', tq)])
                        P.dma('pool', rwT[a3, t0:t0 + 512, fs].rearrange("(j p) d -> p j d", p=128), tt[tq][:], reads=[('tt', tq)])
                    ob = no % 2
                    no += 1


def stage_rw_core(P, K, rwF, rwT, rwgL, rwbv, rwg, prm, mixT, S, nseq):
    GT = 256
    NCH = GT // 64
    with Ctx(P) as c:
        mk = c.sb('mk', [64, 4, 8, 64], F32)
        I8 = c.sb('I8', [64, 8, 64], F32)
        m0 = c.sb('m0', [64, 4, 64], F32)
        U = K['triu'][0:64, 0:64]
        Id = K['idf'][0:64, 0:64]
        P.op('dve', lambda e: e.tensor_tensor(out=m0[:, 2, :], in0=U, in1=Id, op=ALU.subtract), reads=['triu', 'idf'], writes=['m0'])
        P.op('dve', lambda e: e.tensor_scalar(out=m0[:, 0, :], in0=m0[:, 2, :], scalar1=-1.0, scalar2=None, op0=ALU.mult), reads=['m0'], writes=['m0'])
        P.op('dve', lambda e: e.tensor_copy(out=m0[:, 1, :], in_=U), reads=['triu'], writes=['m0'])
        P.op('dve', lambda e: e.tensor_scalar(out=m0[:, 3, :], in0=U, scalar1=-1.0, scalar2=None, op0=ALU.add), reads=['triu'], writes=['m0'])
        for h in range(8):
            P.op('dve', lambda e, h=h: e.tensor_copy(out=mk[:, :, h, :], in_=m0[:]), reads=['m0'], writes=['mk'])
            P.op('dve', lambda e, h=h: e.tensor_copy(out=I8[:, h, :], in_=Id), reads=['idf'], writes=['I8'])
        lnp = c.sb('lnp', [128, 2, 4], F32)
        P.dma('sp', lnp[:], prm['rw_ln'], writes=['lnp'])
        XF = [c.sb('XF', [64, 4, 8, GT], BF16) for _ in range(2)]
        XT = [c.sb('XT', [64, 3, NCH, 512], BF16) for _ in range(2)]
        gL = [c.sb('gL', [64, 8, NCH], F32) for _ in range(2)]
        bvt = [c.sb('bvt', [128, 4, GT], F32) for _ in range(2)]
        gt = [c.sb('gt', [128, 4, GT], F32) for _ in range(2)]
        yF = [c.sb('yF', [128, 4, GT], F32) for _ in range(2)]
        mo = [c.sb('mo', [128, 4, GT], BF16) for _ in range(2)]
        NT = [[c.sb('NT', [64, 8, 64], BF16) for _ in range(2)] for _ in range(NCH)]
        NN = [[c.sb('NN', [64, 8, 64], BF16) for _ in range(2)] for _ in range(NCH)]
        Wt = [c.sb('Wt', [64, 8, 64], BF16) for _ in range(NCH)]
        CbT = [c.sb('CbT', [64, 8, 64], BF16) for _ in range(NCH)]
        BkT = [c.sb('BkT', [64, 8, 64], BF16) for _ in range(NCH)]
        CkT = [c.sb('CkT', [64, 8, 64], BF16) for _ in range(NCH)]
        raw = [c.sb('raw', [64, 8, 64], F32) for _ in range(3)]
        Hs = [c.sb('Hs', [64, 8, 64], F32) for _ in range(nseq)]
        Hb = [c.sb('Hb', [64, 8, 64], BF16) for _ in range(nseq)]
        Rr = c.sb('Rr', [64, 8, 64], BF16)
        Un = c.sb('Un', [64, 8, 64], BF16)
        ysq = c.sb('ysq', [64, 8, 64], F32)
        yn = c.sb('yn', [64, 8, 64], F32)
        st = c.sb('st', [64, 4, 8], F32)
        for s in range(nseq):
            P.op('pool', lambda e, s=s: e.memset(Hs[s][:], 0.0), writes=[('Hs', s)])
            P.op('pool', lambda e, s=s: e.memset(Hb[s][:], 0.0), writes=[('Hb', s)])
        bank = [c.ps('bk', [128, 512], F32) for _ in range(8)]
        st_ = {'b': 0, 'r': 0}

        def nb():
            i = st_['b'] % 8
            st_['b'] += 1
            return i

        def bv(i):
            return bank[i][0:64, :].rearrange("p (h k) -> p h k", h=8)

        ng = 0
        for g in range(S // GT):
            for s in range(nseq):
                lb = ng % 2
                ng += 1
                t0 = s * S + g * GT
                for a in range(4):
                    P.dma('sp', XF[lb][:, a], rwF[a, :, t0:t0 + GT].rearrange("(h k) t -> k h t", k=64), writes=[('XF', lb)])
                for a in range(3):
                    P.dma('sp', XT[lb][:, a], rwT[a, t0:t0 + GT, :].rearrange("(c p) f -> p c f", p=64), writes=[('XT', lb)])
                P.dma('sp', gL[lb][:], rwgL[:, t0 // 64:t0 // 64 + NCH].rearrange("(h k) c -> k h c", k=64), writes=[('gL', lb)])
                P.dma('sp', bvt[lb][:], rwbv[:, t0:t0 + GT].rearrange("(f p) t -> p f t", p=128), writes=[('bvt', lb)])
                P.dma('sp', gt[lb][:], rwg[:, t0:t0 + GT].rearrange("(f p) t -> p f t", p=128), writes=[('gt', lb)])
                for ch in range(NCH):
                    cs = slice(ch * 64, (ch + 1) * 64)
                    specs = [
                        (2, 0, 0, NT[ch][0], 'f32'),
                        (0, 2, 3, NN[ch][0], 'f32'),
                        (2, 1, 1, CbT[ch], 'bf'),
                        (3, 0, 2, BkT[ch], 'bf'),
                        (3, 1, 1, CkT[ch], 'bf'),
                    ]
                    for (la, ra, mi, dst, kind) in specs:
                        bi = nb()
                        for h in range(8):
                            P.op('pe', lambda e, bi=bi, h=h, la=la, ra=ra, cs=cs: e.matmul(bv(bi)[:, h, :], XF[lb][:, la, h, cs], XF[lb][:, ra, h, cs],
                                                                                       start=True, stop=True),
                                 reads=[('XF', lb)], writes=[('bk', bi)])
                        if kind == 'f32':
                            P.op('dve', lambda e, bi=bi, mi=mi, dst=dst: e.tensor_tensor(out=dst[:], in0=bv(bi), in1=mk[:, mi], op=ALU.mult),
                                 reads=[('bk', bi), 'mk'], writes=[id(dst)])
                        else:
                            ri = st_['r'] % 3
                            st_['r'] += 1
                            P.op('act', lambda e, bi=bi, ri=ri: e.copy(out=raw[ri][:], in_=bv(bi)), reads=[('bk', bi)], writes=[('raw', ri)])
                            P.op('pool', lambda e, ri=ri, mi=mi, dst=dst: e.tensor_tensor(out=dst[:], in0=raw[ri][:], in1=mk[:, mi], op=ALU.mult),
                                 reads=[('raw', ri), 'mk'], writes=[id(dst)])
                    P.op('pool', lambda e, ch=ch: e.tensor_tensor(out=Wt[ch][:], in0=NT[ch][0][:], in1=I8[:], op=ALU.add),
                         reads=[id(NT[ch][0]), 'I8'], writes=[id(Wt[ch])])
                for j in range(1, 6):
                    o, nw = (j - 1) % 2, j % 2
                    b1s, b2s, b3s = {}, {}, {}
                    for ch in range(NCH):
                        b1 = b1s[ch] = nb()
                        for h in range(8):
                            P.op('pe', lambda e, h=h: e.matmul(bv(b1)[:, h, :], NT[ch][o][:, h, :], NN[ch][o][:, h, :], start=True, stop=True),
                                 reads=[id(NT[ch][o]), id(NN[ch][o])], writes=[('bk', b1)])
                    if j < 5:
                        for ch in range(NCH):
                            b2 = b2s[ch] = nb()
                            for h in range(8):
                                P.op('pe', lambda e, h=h: e.matmul(bv(b2)[:, h, :], NN[ch][o][:, h, :], NT[ch][o][:, h, :], start=True, stop=True),
                                     reads=[id(NT[ch][o]), id(NN[ch][o])], writes=[('bk', b2)])
                    for ch in range(NCH):
                        b1 = b1s[ch]
                        P.op('act', lambda e: e.copy(out=NN[ch][nw][:], in_=bv(b1)), reads=[('bk', b1)], writes=[id(NN[ch][nw])])
                        if j < 5:
                            b2 = b2s[ch]
                            P.op('dve', lambda e: e.tensor_copy(out=NT[ch][nw][:], in_=bv(b2)), reads=[('bk', b2)], writes=[id(NT[ch][nw])])
                    for ch in range(NCH):
                        b3 = b3s[ch] = nb()
                        for h in range(8):
                            P.op('pe', lambda e, h=h: e.matmul(bv(b3)[:, h, :], NN[ch][nw][:, h, :], Wt[ch][:, h, :], start=True, stop=True),
                                 reads=[id(NN[ch][nw]), id(Wt[ch])], writes=[('bk', b3)])
                    for ch in range(NCH):
                        b3 = b3s[ch]
                        P.op('dve', lambda e: e.tensor_tensor(out=Wt[ch][:], in0=Wt[ch][:], in1=bv(b3), op=ALU.add),
                             reads=[('bk', b3), id(Wt[ch])], writes=[id(Wt[ch])])
                for ch in range(NCH):
                    cs = slice(ch * 64, (ch + 1) * 64)
                    Vh = lambda h: XT[lb][:, 0, ch, h * 64:(h + 1) * 64]
                    bR = nb()
                    for h in range(8):
                        P.op('pe', lambda e, h=h: e.matmul(bv(bR)[:, h, :], XF[lb][:, 0, h, cs], Hb[s][:, h, :], start=True, stop=False),
                             reads=[('XF', lb), ('Hb', s)], writes=[('bk', bR)])
                        P.op('pe', lambda e, h=h: e.matmul(bv(bR)[:, h, :], BkT[ch][:, h, :], Vh(h), start=False, stop=True),
                             reads=[id(BkT[ch]), ('XT', lb)], writes=[('bk', bR)])
                    P.op('act', lambda e: e.copy(out=Rr[:], in_=bv(bR)), reads=[('bk', bR)], writes=['Rr'])
                    bU = nb()
                    for h in range(8):
                        P.op('pe', lambda e, h=h: e.matmul(bv(bU)[:, h, :], Wt[ch][:, h, :], Rr[:, h, :], start=True, stop=True),
                             reads=[id(Wt[ch]), 'Rr'], writes=[('bk', bU)])
                    P.op('dve', lambda e: e.tensor_scalar(out=Un[:], in0=bv(bU), scalar1=-1.0, scalar2=None, op0=ALU.mult), reads=[('bk', bU)], writes=['Un'])
                    bY = nb()
                    for h in range(8):
                        P.op('pe', lambda e, h=h: e.matmul(bv(bY)[:, h, :], XF[lb][:, 1, h, cs], Hb[s][:, h, :], start=True, stop=False),
                             reads=[('XF', lb), ('Hb', s)], writes=[('bk', bY)])
                        P.op('pe', lambda e, h=h: e.matmul(bv(bY)[:, h, :], CkT[ch][:, h, :], Vh(h), start=False, stop=False),
                             reads=[id(CkT[ch]), ('XT', lb)], writes=[('bk', bY)])
                        P.op('pe', lambda e, h=h: e.matmul(bv(bY)[:, h, :], CbT[ch][:, h, :], Un[:, h, :], start=False, stop=True),
                             reads=[id(CbT[ch]), 'Un'], writes=[('bk', bY)])
                    bH = nb()
                    for h in range(8):
                        P.op('pe', lambda e, h=h: e.matmul(bv(bH)[:, h, :], XT[lb][:, 1, ch, h * 64:(h + 1) * 64], Vh(h), start=True, stop=False),
                             reads=[('XT', lb)], writes=[('bk', bH)])
                        P.op('pe', lambda e, h=h: e.matmul(bv(bH)[:, h, :], XT[lb][:, 2, ch, h * 64:(h + 1) * 64], Un[:, h, :], start=False, stop=True),
                             reads=[('XT', lb), 'Un'], writes=[('bk', bH)])
                    for h in range(8):
                        P.op('dve', lambda e, h=h: e.scalar_tensor_tensor(out=Hs[s][:, h, :], in0=Hs[s][:, h, :], scalar=gL[lb][:, h, ch:ch + 1],
                                                                          in1=bv(bH)[:, h, :], op0=ALU.mult, op1=ALU.add),
                             reads=[('Hs', s), ('gL', lb), ('bk', bH)], writes=[('Hs', s)])
                    P.op('act', lambda e: e.copy(out=Hb[s][:], in_=Hs[s][:]), reads=[('Hs', s)], writes=[('Hb', s)])
                    P.op('dve', lambda e: e.tensor_reduce(out=st[:, 0, :], in_=bv(bY), axis=AX.X, op=ALU.add), reads=[('bk', bY)], writes=['st'])
                    P.op('act', lambda e: e.activation(out=ysq[:], in_=bv(bY), func=AF.Square), reads=[('bk', bY)], writes=['ysq'])
                    P.op('dve', lambda e: e.tensor_reduce(out=st[:, 1, :], in_=ysq[:], axis=AX.X, op=ALU.add), reads=['ysq'], writes=['st'])
                    P.op('dve', lambda e: e.tensor_scalar(out=st[:, 0, :], in0=st[:, 0, :], scalar1=1.0 / 64, scalar2=None, op0=ALU.mult), reads=['st'], writes=['st'])
                    P.op('dve', lambda e: e.tensor_tensor(out=st[:, 2, :], in0=st[:, 0, :], in1=st[:, 0, :], op=ALU.mult), reads=['st'], writes=['st'])
                    P.op('dve', lambda e: e.tensor_scalar(out=st[:, 1, :], in0=st[:, 1, :], scalar1=1.0 / 64, scalar2=64e-5, op0=ALU.mult, op1=ALU.add), reads=['st'], writes=['st'])
                    P.op('dve', lambda e: e.tensor_tensor(out=st[:, 1, :], in0=st[:, 1, :], in1=st[:, 2, :], op=ALU.subtract), reads=['st'], writes=['st'])
                    P.op('act', lambda e: e.sqrt(out=st[:, 1, :], in_=st[:, 1, :]), reads=['st'], writes=['st'])
                    P.op('dve', lambda e: e.reciprocal(out=st[:, 1, :], in_=st[:, 1, :]), reads=['st'], writes=['st'])
                    for h in range(8):
                        P.op('dve', lambda e, h=h: e.tensor_scalar(out=yn[:, h, :], in0=bv(bY)[:, h, :], scalar1=st[:, 0, h:h + 1], scalar2=st[:, 1, h:h + 1],
                                                                   op0=ALU.subtract, op1=ALU.mult),
                             reads=[('bk', bY), 'st'], writes=['yn'])
                    bT = nb()
                    for fc in range(4):
                        P.op('pe', lambda e, fc=fc: e.transpose(out=bank[bT][:, fc * 64:(fc + 1) * 64], in_=yn[:, 2 * fc:2 * fc + 2, :].rearrange("p a k -> p (a k)"), identity=Id),
                             reads=['yn', 'idf'], writes=[('bk', bT)])
                    P.op('act', lambda e: e.copy(out=yF[lb][:, :, cs], in_=bank[bT][:, 0:256].rearrange("p (f t) -> p f t", f=4)),
                         reads=[('bk', bT)], writes=[('yF', lb)])
                for fc in range(4):
                    P.op('dve', lambda e, fc=fc: e.tensor_scalar(out=yF[lb][:, fc, :], in0=yF[lb][:, fc, :], scalar1=lnp[:, 0, fc:fc + 1], scalar2=lnp[:, 1, fc:fc + 1],
                                                                 op0=ALU.mult, op1=ALU.add),
                         reads=[('yF', lb), 'lnp'], writes=[('yF', lb)])
                P.op('pool', lambda e: e.tensor_tensor(out=yF[lb][:], in0=yF[lb][:], in1=gt[lb][:], op=ALU.mult), reads=[('yF', lb), ('gt', lb)], writes=[('yF', lb)])
                P.op('pool', lambda e: e.tensor_tensor(out=mo[lb][:], in0=yF[lb][:], in1=bvt[lb][:], op=ALU.add), reads=[('yF', lb), ('bvt', lb)], writes=[('mo', lb)])
                P.dma('pool', mixT[512:1024, t0:t0 + GT].rearrange("(f p) t -> p f t", p=128), mo[lb][:], reads=[('mo', lb)])


def odd_params(inp, o):
    f = np.float32
    col = lambda v: np.ascontiguousarray(np.asarray(v, f).reshape(-1, 128).T)
    cw = np.ascontiguousarray(np.asarray(inp['ml_conv_w'][o], f).reshape(4, 8, 128).transpose(2, 1, 0))
    gbv = np.asarray(inp['ml_gate_b'][o], f)
    gb = np.stack([np.tile(gbv[:4], 2), np.tile(gbv[4:], 2)], 1)
    pc = np.stack([col(inp['rw_w0'][o]), col(inp['rw_a0'][o]), col(inp['rw_k_k'][o]), col(inp['rw_k_a'][o]),
                   col(inp['rw_r_k'][o].reshape(512))], 1)
    ln = np.stack([col(inp['rw_ln_g'][o]), col(inp['rw_ln_b'][o])], 1)
    return {
        'ml_cw': cw.astype(f), 'ml_cb': col(inp['ml_conv_b'][o]), 'ml_gb': np.ascontiguousarray(gb).astype(f),
        'ml_ng_rep': np.ascontiguousarray(np.broadcast_to(np.asarray(inp['ml_norm_g'][o], f).reshape(1, 512), (128, 512))),
        'rw_mu': col(inp['rw_mu'][o]), 'rw_pc': np.ascontiguousarray(pc).astype(f), 'rw_ln': np.ascontiguousarray(ln).astype(f),
        'rw_w2': np.ascontiguousarray(inp['rw_w2'][o]).astype(f), 'rw_a2': np.ascontiguousarray(inp['rw_a2'][o]).astype(f),
        'rw_g2': np.ascontiguousarray(inp['rw_g2'][o]).astype(f),
    }


ODD_SHAPES = {'ml_cw': [128, 8, 4], 'ml_cb': [128, 8], 'ml_gb': [8, 2], 'ml_ng_rep': [128, 512], 'rw_mu': [128, 14],
              'rw_pc': [128, 5, 4], 'rw_ln': [128, 2, 4], 'rw_w2': [64, 512], 'rw_a2': [64, 512], 'rw_g2': [128, 512]}


def odd_scratch(dt, S, nseq, pfx=""):
    T = S * nseq
    NB = S // 128
    R8 = nseq * 4
    return dict(
        mqk_raw=dt(pfx + "mqk_raw", [1024, T]), mif=dt(pfx + "mif", [8, T]), rw_raw=dt(pfx + "rw_raw", [1792, T]),
        v_aug=dt(pfx + "v_aug", [T, 516], BF16), moA=dt(pfx + "moA", [T, 512]),
        mqkT=dt(pfx + "mqkT", [1024, T], BF16), mkA=dt(pfx + "mkA", [T, 512], BF16),
        mgM=dt(pfx + "mgM", [R8, S]), mcol=dt(pfx + "mcol", [2, 128, NB * R8]), mend=dt(pfx + "mend", [NB * R8]),
        rwF=dt(pfx + "rwF", [4, 512, T], BF16), rwT=dt(pfx + "rwT", [3, T, 512], BF16), rwgL=dt(pfx + "rwgL", [512, T // 64]),
        rwbv=dt(pfx + "rwbv", [512, T]), rwg=dt(pfx + "rwg", [512, T]),
    )


def emit_odd_layer(P, K, xin, xout, w_in, w_out, gc, prm, sc, mixT, S, nseq, parts=('ml', 'rw')):
    T = S * nseq
    stage_inproj_odd(P, K, xin, w_in, gc, T, sc['mqk_raw'], sc['mif'], sc['rw_raw'], sc['v_aug'], sc['moA'])
    if 'ml' in parts:
        stage_ml_prep(P, K, sc['mqk_raw'], prm, sc['mqkT'], sc['mkA'], S, nseq)
        stage_ml_gates(P, K, sc['mif'], prm, sc['mgM'], sc['mcol'], sc['mend'], S, nseq)
        stage_ml_core(P, K, sc['mqkT'], sc['mkA'], sc['v_aug'], sc['moA'], sc['mgM'], sc['mcol'], sc['mend'], prm, mixT, S, nseq)
    if 'rw' in parts:
        stage_rw_prep(P, K, sc['rw_raw'], prm, sc['rwF'], sc['rwT'], sc['rwgL'], sc['rwbv'], sc['rwg'], S, nseq)
        stage_rw_core(P, K, sc['rwF'], sc['rwT'], sc['rwgL'], sc['rwbv'], sc['rwg'], prm, mixT, S, nseq)
    stage_outproj(P, K, mixT, w_out, xin, xout, T)


def build_test_odd(S, nseq, parts=('ml', 'rw')):
    T = S * nseq
    nc = bass.Bass("TRN2", target_bir_lowering=False)
    nc.allow_low_precision("bf16 matmul operands by design")
    dt = lambda name, shape, d=F32, kind="Internal": nc.dram_tensor(name, list(shape), d, kind=kind).ap()
    x = dt("x", [T, D], kind="ExternalInput")
    w_in = dt("w_in", [D, 3848], kind="ExternalInput")
    w_out = dt("w_out", [D, D], kind="ExternalInput")
    gc = dt("gc", [128, 8], kind="ExternalInput")
    cst = dt("consts", [128, NCONST], kind="ExternalInput")
    prm = {k: dt("p_" + k, v, kind="ExternalInput") for k, v in ODD_SHAPES.items()}
    y = dt("y", [T, D], kind="ExternalOutput")
    mixT = dt("mixT", [1024, T], BF16, kind="ExternalOutput")
    sc = odd_scratch(dt, S, nseq)
    P = Prog(nc)
    with Ctx(P) as c0:
        K = load_consts(P, c0, cst)
        emit_odd_layer(P, K, x, y, w_in, w_out, gc, prm, sc, mixT, S, nseq, parts)
    return nc

def even_params(inp, e):
    f = np.float32
    return {
        'qk_g': np.ascontiguousarray(np.stack([np.tile(inp['da_q_g'][e], 2), np.tile(inp['da_k_g'][e], 2)], 1)).astype(f),
        'sg_ln_g_rep': np.ascontiguousarray(np.broadcast_to(inp['sg_ln_g'][e].reshape(1, 512), (128, 512))).astype(f),
        'sg_ln_b_rep': np.ascontiguousarray(np.broadcast_to(inp['sg_ln_b'][e].reshape(1, 512), (128, 512))).astype(f),
        'lam_rep': np.ascontiguousarray(np.broadcast_to(inp['da_lambda'][e].reshape(1, 256), (128, 256))).astype(f),
        'subln_col': np.ascontiguousarray(inp['da_subln_g'][e].reshape(128, 1)).astype(f),
        'sg_wT': np.ascontiguousarray(inp['sg_w'][e].transpose(0, 2, 1)).astype(f),
        'sg_b_rep': np.ascontiguousarray(np.broadcast_to(inp['sg_b'][e][None, :, None, :], (128, 4, 4, 128)).reshape(128, 2048)).astype(f),
    }


EVEN_SHAPES = {'qk_g': [128, 2], 'sg_ln_g_rep': [128, 512], 'sg_ln_b_rep': [128, 512], 'lam_rep': [128, 256],
               'subln_col': [128, 1], 'sg_wT': [4, 128, 128], 'sg_b_rep': [128, 2048]}


def gcol_of(g):
    return np.ascontiguousarray(np.asarray(g, np.float32).reshape(8, 128).T)


def build_test_even(S, nseq, layer=0):
    T = S * nseq
    nc = bass.Bass("TRN2", target_bir_lowering=False)
    nc.allow_low_precision("bf16 matmul operands by design")
    dt = lambda name, shape, d=F32, kind="Internal": nc.dram_tensor(name, list(shape), d, kind=kind).ap()
    x = dt("x", [T, D], kind="ExternalInput")
    w_in = dt("w_in", [D, 2560], kind="ExternalInput")
    w_out = dt("w_out", [D, D], kind="ExternalInput")
    gc = dt("gc", [128, 8], kind="ExternalInput")
    cst = dt("consts", [128, NCONST], kind="ExternalInput")
    prm = {k: dt("p_" + k, v, kind="ExternalInput") for k, v in EVEN_SHAPES.items()}
    y = dt("y", [T, D], kind="ExternalOutput")
    qkT = dt("qkT", [1024, T], BF16)
    vA = dt("vA", [T, 512], BF16)
    uF = dt("uF", [512, T], F32)
    vG = dt("vG", [T, 512], BF16)
    mixT = dt("mixT", [1024, T], BF16)
    lam_init = 0.8 - 0.6 * math.exp(-0.3 * layer)
    P = Prog(nc)
    with Ctx(P) as c0:
        K = load_consts(P, c0, cst)
        stage_inproj_even(P, K, x, w_in, gc, prm, T, qkT, vA, uF, vG)
        stage_attn(P, K, qkT, vA, mixT, prm, lam_init, S, nseq)
        stage_sgu(P, K, vG, uF, mixT, prm, T)
        stage_outproj(P, K, mixT, w_out, x, y, T)
    return nc


def emit_even_layer(P, K, xin, xout, w_in, w_out, gc, prm, sc, mixT, S, nseq, layer):
    T = S * nseq
    lam_init = 0.8 - 0.6 * math.exp(-0.3 * layer)
    stage_inproj_even(P, K, xin, w_in, gc, prm, T, sc['qkT'], sc['vA'], sc['uF'], sc['vG'])
    stage_attn(P, K, sc['qkT'], sc['vA'], mixT, prm, lam_init, S, nseq)
    stage_sgu(P, K, sc['vG'], sc['uF'], mixT, prm, T)
    stage_outproj(P, K, mixT, w_out, xin, xout, T)


def build_full(S, nseq, depth=4):
    T = S * nseq
    nc = bass.Bass("TRN2", target_bir_lowering=False)
    nc.allow_low_precision("bf16 matmul operands by design")
    dt = lambda name, shape, d=F32, kind="Internal": nc.dram_tensor(name, list(shape), d, kind=kind).ap()
    ne, no = (depth + 1) // 2, depth // 2
    x = dt("x", [T, D], kind="ExternalInput")
    out = dt("out", [T, D], kind="ExternalOutput")
    cst = dt("consts", [128, NCONST], kind="ExternalInput")
    gmix = dt("gmix", [depth, 128, 8], kind="ExternalInput")
    gffn = dt("gffn", [depth, 128, 8], kind="ExternalInput")
    ev_w_in = dt("ev_w_in", [ne, D, 2560], kind="ExternalInput")
    ev_w_out = dt("ev_w_out", [ne, D, D], kind="ExternalInput")
    od_w_in = dt("od_w_in", [max(no, 1), D, 3848], kind="ExternalInput")
    od_w_out = dt("od_w_out", [max(no, 1), D, D], kind="ExternalInput")
    wg = dt("ffn_w_gate", [depth, D, DFF], kind="ExternalInput")
    wu = dt("ffn_w_up", [depth, D, DFF], kind="ExternalInput")
    wd = dt("ffn_w_down", [depth, DFF, D], kind="ExternalInput")
    eprm = [{k: dt(f"e{e}_{k}", v, kind="ExternalInput") for k, v in EVEN_SHAPES.items()} for e in range(ne)]
    oprm = [{k: dt(f"o{o}_{k}", v, kind="ExternalInput") for k, v in ODD_SHAPES.items()} for o in range(no)]
    xa = dt("xa", [T, D])
    xb = dt("xb", [T, D])
    mixT = dt("mixT", [1024, T], BF16)
    esc = dict(qkT=dt("qkT", [1024, T], BF16), vA=dt("vA", [T, 512], BF16), uF=dt("uF", [512, T]), vG=dt("vG", [T, 512], BF16))
    osc = odd_scratch(dt, S, nseq) if no else None
    P = Prog(nc)
    with Ctx(P) as c0:
        K = load_consts(P, c0, cst)
        xcur = x
        for l in range(depth):
            if l % 2 == 0:
                e = l // 2
                emit_even_layer(P, K, xcur, xa, ev_w_in[e], ev_w_out[e], gmix[l], eprm[e], esc, mixT, S, nseq, l)
            else:
                o = l // 2
                emit_odd_layer(P, K, xcur, xa, od_w_in[o], od_w_out[o], gmix[l], oprm[o], osc, mixT, S, nseq)
            xo = out if l == depth - 1 else xb
            stage_ffn(P, K, xa, xo, wg[l], wu[l], wd[l], gffn[l], T)
            xcur = xo
    return nc


def host_inputs(inputs, depth=4):
    f = np.float32
    ne, no = (depth + 1) // 2, depth // 2
    com = {
        "consts": host_consts(),
        "gmix": np.ascontiguousarray(np.stack([gcol_of(inputs['norm_mix_g'][l]) for l in range(depth)])),
        "gffn": np.ascontiguousarray(np.stack([gcol_of(inputs['norm_ffn_g'][l]) for l in range(depth)])),
        "ev_w_in": np.ascontiguousarray(inputs['ev_w_in'][:ne], f), "ev_w_out": np.ascontiguousarray(inputs['ev_w_out'][:ne], f),
        "od_w_in": np.ascontiguousarray(inputs['od_w_in'][:max(no, 1)], f), "od_w_out": np.ascontiguousarray(inputs['od_w_out'][:max(no, 1)], f),
        "ffn_w_gate": np.ascontiguousarray(inputs['ffn_w_gate'][:depth], f), "ffn_w_up": np.ascontiguousarray(inputs['ffn_w_up'][:depth], f),
        "ffn_w_down": np.ascontiguousarray(inputs['ffn_w_down'][:depth], f),
    }
    for e in range(ne):
        for k, v in even_params(inputs, e).items():
            com[f"e{e}_{k}"] = v
    for o in range(no):
        for k, v in odd_params(inputs, o).items():
            com[f"o{o}_{k}"] = v
    return com


_NC_CACHE = {}


def kernel(**inputs):
    inputs = {k: np.asarray(v) for k, v in inputs.items()}
    x = inputs['x']
    B, S, _ = x.shape
    ncores = 8
    nseq = B // ncores
    depth = inputs['norm_mix_g'].shape[0]
    key = (S, nseq, depth)
    if key not in _NC_CACHE:
        _NC_CACHE[key] = build_full(S, nseq, depth)
    nc = _NC_CACHE[key]
    com = host_inputs(inputs, depth)
    in_maps = []
    for i in range(ncores):
        m = dict(com)
        m["x"] = np.ascontiguousarray(x[i * nseq:(i + 1) * nseq].reshape(nseq * S, D), np.float32)
        in_maps.append(m)
    res = run_bass_kernel_spmd(nc, in_maps, core_ids=list(range(ncores)))
    outs = [np.asarray(r["out"]).reshape(nseq, S, D) for r in res.results]
    return np.concatenate(outs, 0).astype(np.float32)
```

```python
import math
from contextlib import ExitStack
import numpy as np
import ml_dtypes
import concourse.bass as bass
import concourse.mybir as mybir
from concourse.bass_utils import run_bass_kernel_spmd

F32 = mybir.dt.float32
BF16 = mybir.dt.bfloat16
AF = mybir.ActivationFunctionType
ALU = mybir.AluOpType
AX = mybir.AxisListType

D = 1024
DFF = 2816
NFF = DFF // 128
EPOCH = 30000
NSLOT = 24
EPS = 1e-6


class Prog:
    def __init__(self, nc):
        self.nc = nc
        self.E = {'pe': nc.tensor, 'act': nc.scalar, 'dve': nc.vector, 'pool': nc.gpsimd, 'sp': nc.sync}
        self.cnt = {e: 0 for e in ('pe', 'act', 'dve', 'pool')}
        self.csem = {e: [] for e in self.cnt}
        self.known = {e: {} for e in self.E}
        self.dsem = {}
        self.dgen = [0] * NSLOT
        self.dval = [0] * NSLOT
        for i in range(NSLOT):
            self.dsem[(i, 0)] = nc.alloc_semaphore(f"dq{i}_0")
        self.rr = 0
        self.W = {}
        self.R = {}
        self.uid = 0

    def uname(self, s):
        self.uid += 1
        return f"{s}_{self.uid}"

    def _wait(self, e, tok):
        if tok[0] == 'c':
            _, e2, seq = tok
            kk = ('c', e2)
            if self.known[e].get(kk, -1) >= seq:
                return
            ep = seq // EPOCH
            self.E[e].wait_ge(self.csem[e2][ep], seq - ep * EPOCH + 1)
            self.known[e][kk] = seq
        else:
            _, slot, gen, val = tok
            kk = ('d', slot, gen)
            if self.known[e].get(kk, 0) >= val:
                return
            self.E[e].wait_ge(self.dsem[(slot, gen)], val)
            self.known[e][kk] = val

    def _deps(self, e, reads, writes):
        for k in reads:
            t = self.W.get(k)
            if t is not None:
                if t[0] == 'c' and t[1] == e and e == 'pe':
                    continue
                self._wait(e, t)
        for k in writes:
            t = self.W.get(k)
            if t is not None and not (t[0] == 'c' and t[1] == e):
                self._wait(e, t)
            for t in self.R.get(k, {}).values():
                if t[0] == 'c' and t[1] == e:
                    continue
                self._wait(e, t)

    def _record(self, tok, src, reads, writes):
        for k in reads:
            self.R.setdefault(k, {})[src] = tok
        for k in writes:
            self.W[k] = tok
            self.R[k] = {}

    def op(self, e, fn, reads=(), writes=()):
        self._deps(e, reads, writes)
        ins = fn(self.E[e])
        seq = self.cnt[e]
        self.cnt[e] += 1
        ep = seq // EPOCH
        while len(self.csem[e]) <= ep:
            self.csem[e].append(self.nc.alloc_semaphore(f"c_{e}_{len(self.csem[e])}"))
        ins.then_inc(self.csem[e][ep], 1)
        self._record(('c', e, seq), ('c', e), reads, writes)
        return ins

    def dma(self, q, out, in_, reads=(), writes=(), **kw):
        slot = self.rr
        self.rr = (self.rr + 1) % NSLOT
        if self.dval[slot] > 60000:
            self._wait(q, ('d', slot, self.dgen[slot], self.dval[slot]))
            self.dgen[slot] += 1
            self.dval[slot] = 0
            self.dsem[(slot, self.dgen[slot])] = self.nc.alloc_semaphore(f"dq{slot}_{self.dgen[slot]}")
        gen = self.dgen[slot]
        if self.dval[slot] > 0:
            self._wait(q, ('d', slot, gen, self.dval[slot]))
        self._deps(q, reads, writes)
        ins = self.E[q].dma_start(out=out, in_=in_, **kw)
        self.dval[slot] += 16
        ins.then_inc(self.dsem[(slot, gen)], 16)
        self._record(('d', slot, gen, self.dval[slot]), ('d', slot), reads, writes)
        return ins

    def barrier(self):
        toks = [('c', e, self.cnt[e] - 1) for e in self.cnt if self.cnt[e] > 0]
        toks += [('d', s, self.dgen[s], self.dval[s]) for s in range(NSLOT) if self.dval[s] > 0]
        for e in self.E:
            for t in toks:
                self._wait(e, t)
        self.W.clear()
        self.R.clear()


class Ctx:
    def __init__(self, P):
        self.P = P
        self.st = ExitStack()

    def __enter__(self):
        self.st.__enter__()
        return self

    def __exit__(self, *a):
        self.P.barrier()
        return self.st.__exit__(*a)

    def sb(self, name, shape, dt):
        return self.st.enter_context(self.P.nc.sbuf_tensor(self.P.uname(name), list(shape), dt))

    def ps(self, name, shape, dt=F32):
        return self.st.enter_context(self.P.nc.psum_tensor(self.P.uname(name), list(shape), dt))


NCONST = 128 * 3 + 2048


def host_consts():
    cm = np.zeros((128, NCONST), np.float32)
    cm[:, 0:128] = np.eye(128)
    blk = np.zeros((128, 128), np.float32)
    blk[:64, :64] = 1
    blk[64:, 64:] = 1
    cm[:, 128:256] = blk
    k = np.arange(128)[:, None]
    cm[:, 256:384] = (k <= np.arange(128)[None, :])
    q = np.arange(512)[None, :]
    for r in range(4):
        cm[:, 384 + r * 512:384 + (r + 1) * 512] = (128 * r + k <= q)
    return cm


def load_consts(P, c, cd):
    K = {}
    cst = c.sb('cst', [128, 384], F32)
    P.dma('sp', cst[:], cd[:, 0:384], writes=['cst'])
    K['idf'] = cst[:, 0:128]
    K['triu'] = cst[:, 256:384]
    K['idb'] = c.sb('idb', [128, 128], BF16)
    K['blk64'] = c.sb('blk64', [128, 128], BF16)
    K['ones_bf'] = c.sb('ones_bf', [128, 128], BF16)
    K['cmask'] = c.sb('cmask', [128, 4, 512], BF16)
    K['ones_f'] = c.sb('ones_f', [128, 128], F32)
    P.op('dve', lambda e: e.tensor_copy(out=K['idb'][:], in_=cst[:, 0:128]), reads=['cst'], writes=['idb'])
    P.op('dve', lambda e: e.tensor_copy(out=K['blk64'][:], in_=cst[:, 128:256]), reads=['cst'], writes=['blk64'])
    with Ctx(P) as c2:
        cm = c2.sb('cm', [128, 2048], F32)
        P.dma('sp', cm[:], cd[:, 384:384 + 2048], writes=['cm'])
        P.op('dve', lambda e: e.tensor_copy(out=K['cmask'][:], in_=cm[:].rearrange("p (r q) -> p r q", r=4)),
             reads=['cm'], writes=['cmask'])
    P.op('dve', lambda e: e.memset(K['ones_bf'][:], 1.0), writes=['ones_bf'])
    P.op('dve', lambda e: e.memset(K['ones_f'][:], 1.0), writes=['ones_f'])
    P.barrier()
    K['blk64f'] = cst[:, 128:256]
    return K


def load_weight_bf(P, c, w_dram, rows, cols, dst, key, stg, gcol=None, col0=0, engs=('dve', 'pool'), piece=None):
    nch = rows // 128
    piece = piece or cols
    n = 0
    for ci in range(nch):
        for p0 in range(0, cols, piece):
            pc = min(piece, cols - p0)
            b = n % 2
            n += 1
            P.dma('sp', stg[b][:, :pc], w_dram[ci * 128:(ci + 1) * 128, col0 + p0:col0 + p0 + pc], writes=[('stg', id(stg), b)])
            en = engs[n % len(engs)]
            if gcol is not None:
                P.op(en, lambda e, ci=ci, b=b, p0=p0, pc=pc: e.tensor_scalar(out=dst[:, ci, p0:p0 + pc], in0=stg[b][:, :pc],
                                                                             scalar1=gcol[:, ci:ci + 1], scalar2=None, op0=ALU.mult),
                     reads=[('stg', id(stg), b), 'gcol'], writes=[key])
            else:
                P.op(en, lambda e, ci=ci, b=b, p0=p0, pc=pc: e.tensor_copy(out=dst[:, ci, p0:p0 + pc], in_=stg[b][:, :pc]),
                     reads=[('stg', id(stg), b)], writes=[key])


def norm_transpose(P, K, xt, xkey, hb, hkey, hT, hTkey, tp, tpkey, ss, rs, j, ncols, tcol0, phase=0):
    junk = hb
    if phase in (0, 1):
        _norm_stats(P, xt, xkey, hb, hkey, ss, rs, j, ncols, junk)
    if phase == 1:
        return
    nch = ncols // 128
    for ci in range(nch):
        P.op('pe', lambda e, ci=ci: e.transpose(out=tp[:, ci, :], in_=hb[:, ci * 128:(ci + 1) * 128], identity=K['idb'][:]),
             reads=[hkey], writes=[tpkey])
    P.op('dve', lambda e: e.tensor_copy(out=hT[:, :, tcol0:tcol0 + 128], in_=tp[:, :nch, :]),
         reads=[tpkey], writes=[hTkey])


def _norm_stats(P, xt, xkey, hb, hkey, ss, rs, j, ncols, junk):
    P.op('act', lambda e: e.activation(out=junk, in_=xt, func=AF.Square, accum_out=ss[:, j:j + 1]),
         reads=[xkey], writes=[hkey, ('ss', id(ss), j)])
    P.op('dve', lambda e: e.tensor_scalar(out=rs[:, j:j + 1], in0=ss[:, j:j + 1], scalar1=1.0 / ncols, scalar2=EPS,
                                          op0=ALU.mult, op1=ALU.add),
         reads=[('ss', id(ss), j)], writes=[('rs', id(rs), j)])
    P.op('act', lambda e: e.sqrt(out=rs[:, j:j + 1], in_=rs[:, j:j + 1]),
         reads=[('rs', id(rs), j)], writes=[('rs', id(rs), j)])
    P.op('dve', lambda e: e.reciprocal(out=rs[:, j:j + 1], in_=rs[:, j:j + 1]),
         reads=[('rs', id(rs), j)], writes=[('rs', id(rs), j)])
    P.op('act', lambda e: e.activation(out=hb, in_=xt, func=AF.Copy, scale=rs[:, j:j + 1]),
         reads=[xkey, ('rs', id(rs), j)], writes=[hkey])


def stage_ffn(P, K, xin, xout, wg, wu, wd, gcol_d, T):
    GT = 256
    NT = GT // 128
    with Ctx(P) as c:
        wg_bf = c.sb('wg', [128, 8, DFF], BF16)
        wu_bf = c.sb('wu', [128, 8, DFF], BF16)
        wd_bf = c.sb('wd', [128, NFF, D], BF16)
        gcol = c.sb('gcol', [128, 8], F32)
        stg = [c.sb('stg', [128, 1408], F32) for _ in range(2)]
        P.dma('sp', gcol[:], gcol_d, writes=['gcol'])
        load_weight_bf(P, c, wg, D, DFF, wg_bf, 'wg', stg, gcol=gcol, piece=1408)
        load_weight_bf(P, c, wu, D, DFF, wu_bf, 'wu', stg, gcol=gcol, piece=1408)
        load_weight_bf(P, c, wd, DFF, D, wd_bf, 'wd', stg)
        xt = [c.sb('xt', [128, NT, D], F32) for _ in range(2)]
        hb = [c.sb('hb', [128, D], BF16) for _ in range(2)]
        hT = [c.sb('hT', [128, 8, GT], BF16) for _ in range(2)]
        act = [c.sb('act', [128, NFF, GT], BF16) for _ in range(1)]
        sg = [c.sb('sg', [128, GT], F32) for _ in range(4)]
        xo = [c.sb('xo', [128, D], F32) for _ in range(2)]
        ss = c.sb('ss', [128, 8], F32)
        rs = c.sb('rs', [128, 8], F32)
        tp = [c.ps('tp', [128, 8, 128], BF16) for _ in range(2)]
        pgu = [c.ps('pgu', [128, 512], F32) for _ in range(4)]
        pg = [t[:, 0:256] for t in pgu]
        pu = [t[:, 256:512] for t in pgu]
        po = [c.ps('po', [128, 512], F32) for _ in range(2)]
        ng = T // GT
        it = 0

        def load_norm(g, phase):
            b = g % 2
            tok0 = g * GT
            if phase == 1:
                for j in range(NT):
                    P.dma('sp', xt[b][:, j, :], xin[tok0 + j * 128: tok0 + (j + 1) * 128, :], writes=[('xt', b, j)])
            for j in range(NT):
                hbb = (g * NT + j) % 2
                norm_transpose(P, K, xt[b][:, j, :], ('xt', b, j), hb[hbb][:], ('hb', hbb), hT[b], ('hT', b),
                               tp[hbb], ('tp', hbb), ss, rs, (g * NT + j) % 8, D, j * 128, phase=phase)

        load_norm(0, 1)
        load_norm(0, 2)
        for g in range(ng):
            b = g % 2
            tok0 = g * GT
            for f in range(NFF):
                pb = (g * NFF + f) % 4
                for ci in range(8):
                    P.op('pe', lambda e, ci=ci, f=f, pb=pb: e.matmul(pg[pb][:, :GT], wg_bf[:, ci, f * 128:(f + 1) * 128],
                                                                     hT[b][:, ci, :], start=(ci == 0), stop=(ci == 7)),
                         reads=['wg', ('hT', b)], writes=[('pgu', pb)])
                for ci in range(8):
                    P.op('pe', lambda e, ci=ci, f=f, pb=pb: e.matmul(pu[pb][:, :GT], wu_bf[:, ci, f * 128:(f + 1) * 128],
                                                                     hT[b][:, ci, :], start=(ci == 0), stop=(ci == 7)),
                         reads=['wu', ('hT', b)], writes=[('pgu', pb)])
                P.op('act', lambda e, pb=pb: e.activation(out=sg[pb][:], in_=pg[pb][:, :GT], func=AF.Silu),
                     reads=[('pgu', pb)], writes=[('sg', pb)])
                P.op('dve', lambda e, pb=pb, f=f: e.tensor_tensor(out=act[0][:, f, :], in0=sg[pb][:], in1=pu[pb][:, :GT],
                                                                  op=ALU.mult),
                     reads=[('sg', pb), ('pgu', pb)], writes=[('act', f)])
            if g + 1 < ng:
                load_norm(g + 1, 1)
            for j in range(NT):
                for dh in range(2):
                    ob = it % 2
                    it += 1
                    for f in range(NFF):
                        P.op('pe', lambda e, f=f, j=j, dh=dh, ob=ob: e.matmul(po[ob][:], act[0][:, f, j * 128:(j + 1) * 128],
                                                                              wd_bf[:, f, dh * 512:(dh + 1) * 512],
                                                                              start=(f == 0), stop=(f == NFF - 1)),
                             reads=['wd', ('act', f)], writes=[('po', ob)])
                    xob = (g * NT + j) % 2
                    P.op('dve', lambda e, j=j, dh=dh, ob=ob, xob=xob: e.tensor_tensor(
                        out=xo[xob][:, dh * 512:(dh + 1) * 512], in0=xt[b][:, j, dh * 512:(dh + 1) * 512], in1=po[ob][:],
                        op=ALU.add),
                        reads=[('po', ob), ('xt', b, j)], writes=[('xo', xob, dh)])
                P.dma('pool', xout[tok0 + j * 128: tok0 + (j + 1) * 128, :], xo[xob][:],
                      reads=[('xo', xob, 0), ('xo', xob, 1)])
            if g + 1 < ng:
                load_norm(g + 1, 2)


GELU_C = 1.5957691216057308
GELU_A = 0.044715


def emit_gelu(P, src, skey, t1, t1key, out, okey, eng2='dve'):
    P.op('act', lambda e: e.activation(out=t1, in_=src, func=AF.Square), reads=[skey], writes=[t1key])
    P.op('dve', lambda e: e.tensor_scalar(out=t1, in0=t1, scalar1=GELU_A, scalar2=1.0, op0=ALU.mult, op1=ALU.add),
         reads=[t1key], writes=[t1key])
    P.op('dve', lambda e: e.tensor_tensor(out=t1, in0=t1, in1=src, op=ALU.mult), reads=[t1key, skey], writes=[t1key])
    P.op('act', lambda e: e.activation(out=t1, in_=t1, func=AF.Sigmoid, scale=GELU_C), reads=[t1key], writes=[t1key])
    P.op(eng2, lambda e: e.tensor_tensor(out=out, in0=t1, in1=src, op=ALU.mult), reads=[t1key, skey], writes=[okey])


def emit_rsqrt(P, dst, dkey, src, skey, mult, add):
    P.op('dve', lambda e: e.tensor_scalar(out=dst, in0=src, scalar1=mult, scalar2=add, op0=ALU.mult, op1=ALU.add),
         reads=[skey], writes=[dkey])
    P.op('act', lambda e: e.sqrt(out=dst, in_=dst), reads=[dkey], writes=[dkey])
    P.op('dve', lambda e: e.reciprocal(out=dst, in_=dst), reads=[dkey], writes=[dkey])


class InProj:
    def __init__(self, P, K, c, xin, w_d, N, gcol_d, T, GT=512):
        self.P, self.K, self.c, self.xin, self.T, self.GT = P, K, c, xin, T, GT
        self.NT = GT // 128
        self.w_bf = c.sb('win', [128, 8, N], BF16)
        self.gcol = c.sb('gcol', [128, 8], F32)
        stg = [c.sb('stg', [128, N], F32) for _ in range(2)]
        P.dma('sp', self.gcol[:], gcol_d, writes=['gcol'])
        load_weight_bf(P, c, w_d, D, N, self.w_bf, 'win', stg, gcol=self.gcol)
        self.xt = [c.sb('xt', [128, self.NT, D], F32) for _ in range(2)]
        self.hb = [c.sb('hb', [128, D], BF16) for _ in range(2)]
        self.hT = [c.sb('hT', [128, 8, GT], BF16) for _ in range(2)]
        self.ss = c.sb('ss', [128, 8], F32)
        self.rs = c.sb('rs', [128, 8], F32)
        self.tp = [c.ps('tp', [128, 8, 128], BF16) for _ in range(2)]
        self.pf = [c.ps('pf', [128, 512], F32) for _ in range(2)]
        self.pt = [c.ps('pt', [128, 512], F32) for _ in range(2)]
        self.nf = 0
        self.ntt = 0

    def run(self, fcols, tcols, f_epi, t_epi, copy_f=True, copy_t=(), nraw=4):
        P, K = self.P, self.K
        GT, NT = self.GT, self.NT
        raw = [self.c.sb('raw', [128, 512], F32) for _ in range(nraw)]
        nr = 0

        def copy_out(ps, pkey, m, n):
            nonlocal nr
            r = nr % nraw
            nr += 1
            if nr % 2:
                P.op('act', lambda e: e.copy(out=raw[r][:m, :n], in_=ps[:m, :n]), reads=[pkey], writes=[('raw', r)])
            else:
                P.op('dve', lambda e: e.tensor_copy(out=raw[r][:m, :n], in_=ps[:m, :n]), reads=[pkey], writes=[('raw', r)])
            return raw[r], ('raw', r)

        for g in range(self.T // GT):
            b = g % 2
            tok0 = g * GT
            for j in range(NT):
                P.dma('sp', self.xt[b][:, j, :], self.xin[tok0 + j * 128: tok0 + (j + 1) * 128, :], writes=[('xt', b, j)])
            for j in range(NT):
                hbb = (g * NT + j) % 2
                norm_transpose(P, K, self.xt[b][:, j, :], ('xt', b, j), self.hb[hbb][:], ('hb', hbb), self.hT[b], ('hT', b),
                               self.tp[hbb], ('tp', hbb), self.ss, self.rs, (g * NT + j) % 8, D, j * 128)
            for idx, (col0, m) in enumerate(fcols):
                pb = self.nf % 2
                self.nf += 1
                for ci in range(8):
                    P.op('pe', lambda e, ci=ci, pb=pb, col0=col0, m=m: e.matmul(
                        self.pf[pb][:m, :GT], self.w_bf[:, ci, col0:col0 + m], self.hT[b][:, ci, :],
                        start=(ci == 0), stop=(ci == 7)), reads=['win', ('hT', b)], writes=[('pf', pb)])
                if copy_f:
                    src, skey = copy_out(self.pf[pb], ('pf', pb), m, GT)
                else:
                    src, skey = self.pf[pb], ('pf', pb)
                f_epi(idx, src, skey, tok0, g)
            for j in range(NT):
                for idx, (col0, n) in enumerate(tcols):
                    pb = self.ntt % 2
                    self.ntt += 1
                    for ci in range(8):
                        P.op('pe', lambda e, ci=ci, pb=pb, col0=col0, n=n, j=j: e.matmul(
                            self.pt[pb][:, :n], self.hT[b][:, ci, j * 128:(j + 1) * 128], self.w_bf[:, ci, col0:col0 + n],
                            start=(ci == 0), stop=(ci == 7)), reads=['win', ('hT', b)], writes=[('pt', pb)])
                    if idx in copy_t:
                        src, skey = copy_out(self.pt[pb], ('pt', pb), 128, n)
                    else:
                        src, skey = self.pt[pb], ('pt', pb)
                    t_epi(idx, src, skey, tok0 + j * 128, g * NT + j)


def stage_inproj_even(P, K, xin, w_d, gcol_d, prm, T, qkT, vA, uF, vG):
    with Ctx(P) as c:
        ip = InProj(P, K, c, xin, w_d, 2560, gcol_d, T)
        gq = c.sb('gq', [128, 2], F32)
        P.dma('sp', gq[:], prm['qk_g'], writes=['gq'])
        P.op('dve', lambda e: e.tensor_scalar(out=gq[:, 0:1], in0=gq[:, 0:1], scalar1=0.125, scalar2=None, op0=ALU.mult),
             reads=['gq'], writes=['gq'])
        lng = c.sb('lng', [128, 512], F32)
        lnb = c.sb('lnb', [128, 512], F32)
        P.dma('sp', lng[:], prm['sg_ln_g_rep'], writes=['lng'])
        P.dma('sp', lnb[:], prm['sg_ln_b_rep'], writes=['lnb'])
        sq = [c.sb('sq', [128, 512], BF16) for _ in range(2)]
        rr = [c.sb('rr', [128, 512], F32) for _ in range(2)]
        qo = [c.sb('qo', [128, 512], BF16) for _ in range(2)]
        t1 = [c.sb('t1', [128, 512], F32) for _ in range(2)]
        uo = [c.sb('uo', [128, 512], F32) for _ in range(2)]
        vo = [c.sb('vo', [128, 512], BF16) for _ in range(2)]
        gv = [c.sb('gv', [128, 512], F32) for _ in range(2)]
        st4 = c.sb('st4', [128, 8, 4], F32)
        pn = c.ps('pn', [128, 512], F32)
        cnt = {'f': 0, 't': 0}

        def f_epi(idx, ps, pkey, tok0, g):
            i = cnt['f'] % 2
            cnt['f'] += 1
            if idx < 8:
                P.op('act', lambda e: e.activation(out=sq[i][:], in_=ps[:], func=AF.Square), reads=[pkey], writes=[('sq', i)])
                P.op('pe', lambda e: e.matmul(pn[:], K['blk64'][:], sq[i][:], start=True, stop=True),
                     reads=[('sq', i), 'blk64'], writes=['pn'])
                emit_rsqrt(P, rr[i][:], ('rr', i), pn[:], 'pn', 1.0 / 64, EPS)
                gc = gq[:, 0:1] if idx < 4 else gq[:, 1:2]
                P.op('dve', lambda e: e.scalar_tensor_tensor(out=qo[i][:], in0=ps[:], scalar=gc, in1=rr[i][:],
                                                             op0=ALU.mult, op1=ALU.mult),
                     reads=[pkey, ('rr', i), 'gq'], writes=[('qo', i)])
                P.dma('pool', qkT[idx * 128:(idx + 1) * 128, tok0:tok0 + 512], qo[i][:], reads=[('qo', i)])
            else:
                emit_gelu(P, ps[:], pkey, t1[i][:], ('t1', i), uo[i][:], ('uo', i), eng2='pool' if False else 'dve')
                P.dma('pool', uF[(idx - 8) * 128:(idx - 7) * 128, tok0:tok0 + 512], uo[i][:], reads=[('uo', i)])

        def t_epi(idx, ps, pkey, tokj, jj):
            i = cnt['t'] % 2
            cnt['t'] += 1
            if idx == 0:
                P.op('act', lambda e: e.copy(out=vo[i][:], in_=ps[:]), reads=[pkey], writes=[('vo', i)])
                P.dma('pool', vA[tokj:tokj + 128, :], vo[i][:], reads=[('vo', i)])
            else:
                emit_gelu(P, ps[:], pkey, t1[i][:], ('t1', i), gv[i][:], ('gv', i))
                s = jj % 8
                gv3 = gv[i][:].rearrange("p (g d) -> p g d", g=4)
                t13 = t1[i][:].rearrange("p (g d) -> p g d", g=4)
                P.op('dve', lambda e: e.tensor_reduce(out=st4[:, s, :], in_=gv3, axis=AX.X, op=ALU.add),
                     reads=[('gv', i)], writes=[('st4', s)])
                P.op('act', lambda e: e.activation(out=t1[i][:], in_=gv[i][:], func=AF.Square), reads=[('gv', i)], writes=[('t1', i)])
                s2 = (jj + 4) % 8
                P.op('dve', lambda e: e.tensor_reduce(out=st4[:, s2, :], in_=t13, axis=AX.X, op=ALU.add),
                     reads=[('t1', i)], writes=[('st4', s2)])
                P.op('dve', lambda e: e.tensor_scalar(out=st4[:, s, :], in0=st4[:, s, :], scalar1=1.0 / 128, scalar2=None, op0=ALU.mult),
                     reads=[('st4', s)], writes=[('st4', s)])
                P.op('dve', lambda e: e.tensor_scalar(out=st4[:, s2, :], in0=st4[:, s2, :], scalar1=1.0 / 128, scalar2=EPS,
                                                      op0=ALU.mult, op1=ALU.add), reads=[('st4', s2)], writes=[('st4', s2)])
                m2 = t1[i][:, 0:4]
                P.op('dve', lambda e: e.tensor_tensor(out=m2, in0=st4[:, s, :], in1=st4[:, s, :], op=ALU.mult),
                     reads=[('st4', s)], writes=[('t1', i)])
                P.op('dve', lambda e: e.tensor_tensor(out=st4[:, s2, :], in0=st4[:, s2, :], in1=m2, op=ALU.subtract),
                     reads=[('st4', s2), ('t1', i)], writes=[('st4', s2)])
                P.op('act', lambda e: e.sqrt(out=st4[:, s2, :], in_=st4[:, s2, :]), reads=[('st4', s2)], writes=[('st4', s2)])
                P.op('dve', lambda e: e.reciprocal(out=st4[:, s2, :], in_=st4[:, s2, :]), reads=[('st4', s2)], writes=[('st4', s2)])
                for gi in range(4):
                    P.op('dve', lambda e, gi=gi: e.tensor_scalar(out=gv[i][:, gi * 128:(gi + 1) * 128], in0=gv[i][:, gi * 128:(gi + 1) * 128],
                                                                 scalar1=st4[:, s, gi:gi + 1], scalar2=st4[:, s2, gi:gi + 1],
                                                                 op0=ALU.subtract, op1=ALU.mult),
                         reads=[('gv', i), ('st4', s), ('st4', s2)], writes=[('gv', i)])
                P.op('pool', lambda e: e.tensor_tensor(out=gv[i][:], in0=gv[i][:], in1=lng[:], op=ALU.mult),
                     reads=[('gv', i), 'lng'], writes=[('gv', i)])
                P.op('pool', lambda e: e.tensor_tensor(out=vo[i][:], in0=gv[i][:], in1=lnb[:], op=ALU.add),
                     reads=[('gv', i), 'lnb'], writes=[('vo', i)])
                P.dma('pool', vG[tokj:tokj + 128, :], vo[i][:], reads=[('vo', i)])

        fcols = [(i * 128, 128) for i in range(8)] + [(1536 + i * 128, 128) for i in range(4)]
        tcols = [(1024, 512), (2048, 512)]
        ip.run(fcols, tcols, f_epi, t_epi, copy_f=True, copy_t=(1,))


def stage_attn(P, K, qkT, vA, mixT, prm, lam_init, S, nseq):
    NB = S // 128
    NG = S // 512
    with Ctx(P) as c:
        lam = c.sb('lam', [128, 256], F32)
        P.dma('sp', lam[:], prm['lam_rep'], writes=['lam'])
        prod = c.sb('prod', [128, 2, 64], F32)
        dots = c.sb('dots', [128, 4], F32)
        P.op('dve', lambda e: e.tensor_tensor(out=prod[:, 0, :], in0=lam[:, 0:64], in1=lam[:, 64:128], op=ALU.mult),
             reads=['lam'], writes=['prod'])
        P.op('dve', lambda e: e.tensor_tensor(out=prod[:, 1, :], in0=lam[:, 128:192], in1=lam[:, 192:256], op=ALU.mult),
             reads=['lam'], writes=['prod'])
        P.op('dve', lambda e: e.tensor_reduce(out=dots[:, 0:2], in_=prod[:], axis=AX.X, op=ALU.add), reads=['prod'], writes=['dots'])
        P.op('act', lambda e: e.activation(out=dots[:, 0:2], in_=dots[:, 0:2], func=AF.Exp), reads=['dots'], writes=['dots'])
        P.op('dve', lambda e: e.tensor_tensor(out=dots[:, 2:3], in0=dots[:, 1:2], in1=dots[:, 0:1], op=ALU.subtract),
             reads=['dots'], writes=['dots'])
        P.op('dve', lambda e: e.tensor_scalar(out=dots[:, 2:3], in0=dots[:, 2:3], scalar1=-float(lam_init), scalar2=None, op0=ALU.add),
             reads=['dots'], writes=['dots'])
        neglam = dots[:, 2:3]
        sgc = c.sb('sgc', [128, 1], F32)
        P.dma('sp', sgc[:], prm['subln_col'], writes=['sgc'])
        P.op('dve', lambda e: e.tensor_scalar(out=sgc[:], in0=sgc[:], scalar1=float(1.0 - lam_init), scalar2=None, op0=ALU.mult),
             reads=['sgc'], writes=['sgc'])
        kT = [c.sb('kT', [64, 2, S], BF16) for _ in range(2)]
        Vt = [c.sb('Vt', [128, NB, 128], BF16) for _ in range(2)]
        qT = [c.sb('qT', [64, 2, 512], BF16) for _ in range(2)]
        pT = [c.sb('pT', [128, 512], BF16) for _ in range(4)]
        e32 = [c.sb('e32', [128, 512], F32) for _ in range(2)]
        r0 = c.sb('r0', [128, 512], F32)
        r1 = c.sb('r1', [128, 512], F32)
        o0 = c.sb('o0', [128, 512], F32)
        o1 = c.sb('o1', [128, 512], F32)
        osq = c.sb('osq', [128, 512], BF16)
        ob = [c.sb('ob', [128, 512], BF16) for _ in range(2)]
        sT = [c.ps('sT', [128, 512], F32) for _ in range(4)]
        acc = [c.ps('acc', [128, 512], F32) for _ in range(2)]
        pl = c.ps('pl', [128, 512], F32)
        pn = c.ps('pn', [128, 512], F32)
        lsum = [[c.sb('lsum', [128, 512], F32) for _ in range(2)] for _ in range(2)]
        n_g = 0
        n_s = 0
        n_p = 0
        n_e = 0
        n_q = 0
        n_h = 0
        for s in range(nseq):
            for h in range(4):
                hb = n_h % 2
                n_h += 1
                for cm in range(2):
                    r = 512 + h * 128 + cm * 64
                    P.dma('sp', kT[hb][:, cm, :], qkT[r:r + 64, s * S:(s + 1) * S], writes=[('kT', hb)])
                P.dma('sp', Vt[hb][:], vA[s * S:(s + 1) * S, h * 128:(h + 1) * 128].rearrange("(j p) d -> p j d", p=128),
                      writes=[('Vt', hb)])
                for g in range(NG):
                    qb = n_q % 2
                    n_q += 1
                    t0 = s * S + g * 512
                    for cm in range(2):
                        r = h * 128 + cm * 64
                        P.dma('sp', qT[qb][:, cm, :], qkT[r:r + 64, t0:t0 + 512], writes=[('qT', qb)])
                    nkb = 4 * (g + 1)
                    items = [(j, cm) for j in range(nkb) for cm in range(2)]
                    sbs = {}

                    def emit_score(i):
                        nonlocal n_s
                        j, cm = items[i]
                        sb_ = n_s % 4
                        n_s += 1
                        sbs[i] = sb_
                        P.op('pe', lambda e: e.matmul(sT[sb_][:], kT[hb][:, cm, j * 128:(j + 1) * 128], qT[qb][:, cm, :], start=True, stop=True),
                             reads=[('kT', hb), ('qT', qb)], writes=[('sT', sb_)])

                    gp = n_g % 2
                    n_g += 1
                    emit_score(0)
                    emit_score(1)
                    emit_score(2)
                    for i, (j, cm) in enumerate(items):
                        sb_ = sbs[i]
                        pb = n_p % 4
                        n_p += 1
                        if j < 4 * g:
                            P.op('act', lambda e: e.activation(out=pT[pb][:], in_=sT[sb_][:], func=AF.Exp),
                                 reads=[('sT', sb_)], writes=[('pT', pb)])
                        else:
                            eb = n_e % 2
                            n_e += 1
                            rr_ = j - 4 * g
                            P.op('act', lambda e: e.activation(out=e32[eb][:], in_=sT[sb_][:], func=AF.Exp),
                                 reads=[('sT', sb_)], writes=[('e32', eb)])
                            P.op('pool', lambda e: e.tensor_tensor(out=pT[pb][:], in0=e32[eb][:], in1=K['cmask'][:, rr_, :], op=ALU.mult),
                                 reads=[('e32', eb), 'cmask'], writes=[('pT', pb)])
                        if i + 3 < len(items):
                            emit_score(i + 3)
                        P.op('pe', lambda e: e.matmul(acc[cm][:], Vt[hb][:, j, :], pT[pb][:], start=(j == 0), stop=(j == nkb - 1)),
                             reads=[('Vt', hb), ('pT', pb)], writes=[('acc', cm)])
                        aen = 'dve' if cm == 0 else 'pool'
                        if j == 0:
                            P.op(aen, lambda e: e.tensor_copy(out=lsum[gp][cm][:], in_=pT[pb][:]), reads=[('pT', pb)], writes=[('lsum', gp, cm)])
                        else:
                            P.op(aen, lambda e: e.tensor_tensor(out=lsum[gp][cm][:], in0=lsum[gp][cm][:], in1=pT[pb][:], op=ALU.add),
                                 reads=[('pT', pb), ('lsum', gp, cm)], writes=[('lsum', gp, cm)])
                    P.op('pe', lambda e: e.matmul(pl[:], K['ones_f'][:], lsum[gp][0][:], start=True, stop=True), reads=[('lsum', gp, 0)], writes=['pl'])
                    P.op('dve', lambda e: e.reciprocal(out=r0[:], in_=pl[:]), reads=['pl'], writes=['r0'])
                    P.op('pe', lambda e: e.matmul(pl[:], K['ones_f'][:], lsum[gp][1][:], start=True, stop=True), reads=[('lsum', gp, 1)], writes=['pl'])
                    P.op('dve', lambda e: e.reciprocal(out=r1[:], in_=pl[:]), reads=['pl'], writes=['r1'])
                    P.op('dve', lambda e: e.tensor_tensor(out=o0[:], in0=acc[0][:], in1=r0[:], op=ALU.mult),
                         reads=[('acc', 0), 'r0'], writes=['o0'])
                    P.op('dve', lambda e: e.tensor_tensor(out=o1[:], in0=acc[1][:], in1=r1[:], op=ALU.mult),
                         reads=[('acc', 1), 'r1'], writes=['o1'])
                    P.op('dve', lambda e: e.scalar_tensor_tensor(out=o0[:], in0=o1[:], scalar=neglam, in1=o0[:],
                                                                 op0=ALU.mult, op1=ALU.add),
                         reads=['o0', 'o1', 'dots'], writes=['o0'])
                    P.op('act', lambda e: e.activation(out=osq[:], in_=o0[:], func=AF.Square), reads=['o0'], writes=['osq'])
                    P.op('pe', lambda e: e.matmul(pn[:], K['ones_bf'][:], osq[:], start=True, stop=True),
                         reads=['ones_bf', 'osq'], writes=['pn'])
                    emit_rsqrt(P, r0[:], 'r0', pn[:], 'pn', 1.0 / 128, EPS)
                    obb = n_q % 2
                    P.op('dve', lambda e, obb=obb: e.scalar_tensor_tensor(out=ob[obb][:], in0=o0[:], scalar=sgc[:, 0:1], in1=r0[:],
                                                                          op0=ALU.mult, op1=ALU.mult),
                         reads=['o0', 'r0', 'sgc'], writes=[('ob', obb)])
                    P.dma('pool', mixT[h * 128:(h + 1) * 128, t0:t0 + 512], ob[obb][:], reads=[('ob', obb)])


def stage_sgu(P, K, vG, uF, mixT, prm, T):
    with Ctx(P) as c:
        wst = c.sb('wst', [128, 4, 128], F32)
        wtm = c.sb('wtm', [128, 4, 128], BF16)
        bias = c.sb('bias', [128, 4, 512], F32)
        P.dma('sp', wst[:], prm['sg_wT'].rearrange("g s t -> s g t"), writes=['wst'])
        P.dma('sp', bias[:], prm['sg_b_rep'].rearrange("p (g t) -> p g t", g=4), writes=['bias'])
        for g in range(4):
            P.op('dve', lambda e, g=g: e.tensor_tensor(out=wtm[:, g, :], in0=wst[:, g, :], in1=K['triu'][:], op=ALU.mult),
                 reads=['wst', 'triu'], writes=['wtm'])
        vt = [c.sb('vt', [128, 4, 512], BF16) for _ in range(2)]
        ut = [c.sb('ut', [128, 4, 512], F32) for _ in range(2)]
        tt = [c.sb('tt', [128, 512], F32) for _ in range(2)]
        ob = [c.sb('ob', [128, 4, 512], BF16) for _ in range(2)]
        ps = [c.ps('ps', [128, 512], F32) for _ in range(2)]
        n = 0
        for tg in range(T // 512):
            b = tg % 2
            tok0 = tg * 512
            P.dma('sp', vt[b][:], vG[tok0:tok0 + 512, :].rearrange("(c p) f -> p c f", p=128), writes=[('vt', b)])
            P.dma('sp', ut[b][:], uF[:, tok0:tok0 + 512].rearrange("(g d) t -> d g t", d=128), writes=[('ut', b)])
            for g in range(4):
                pb = n % 2
                n += 1
                for ch in range(4):
                    P.op('pe', lambda e, g=g, ch=ch, pb=pb: e.matmul(ps[pb][:, ch * 128:(ch + 1) * 128], vt[b][:, ch, g * 128:(g + 1) * 128],
                                                                     wtm[:, g, :], start=True, stop=True),
                         reads=[('vt', b), 'wtm'], writes=[('ps', pb)])
                P.op('dve', lambda e, g=g, pb=pb: e.tensor_tensor(out=tt[pb][:], in0=ps[pb][:], in1=bias[:, g, :], op=ALU.add),
                     reads=[('ps', pb), 'bias'], writes=[('tt', pb)])
                P.op('pool', lambda e, g=g, pb=pb: e.tensor_tensor(out=ob[b][:, g, :], in0=tt[pb][:], in1=ut[b][:, g, :], op=ALU.mult),
                     reads=[('tt', pb), ('ut', b)], writes=[('ob', b)])
            P.dma('pool', mixT[512:1024, tok0:tok0 + 512].rearrange("(g d) t -> d g t", d=128), ob[b][:], reads=[('ob', b)])


def stage_outproj(P, K, mixT, w_d, xin, xout, T):
    GT = 512
    with Ctx(P) as c:
        w_bf = c.sb('wo', [128, 8, D], BF16)
        stg = [c.sb('stg', [128, D], F32) for _ in range(2)]
        load_weight_bf(P, c, w_d, D, D, w_bf, 'wo', stg)
        mt = [c.sb('mt', [128, 8, GT], BF16) for _ in range(2)]
        xt = [c.sb('xt', [128, GT // 128, D], F32) for _ in range(2)]
        xo = [c.sb('xo', [128, D], F32) for _ in range(2)]
        po = [c.ps('po', [128, 512], F32) for _ in range(4)]
        n = 0
        for g in range(T // GT):
            b = g % 2
            tok0 = g * GT
            P.dma('sp', mt[b][:], mixT[:, tok0:tok0 + GT].rearrange("(c f) t -> f c t", f=128), writes=[('mt', b)])
            P.dma('sp', xt[b][:], xin[tok0:tok0 + GT, :].rearrange("(j p) d -> p j d", p=128), writes=[('xt', b)])
            for j in range(GT // 128):
                xob = (g * 4 + j) % 2
                for dh in range(2):
                    ob = n % 4
                    n += 1
                    for ci in range(8):
                        P.op('pe', lambda e, ci=ci, j=j, dh=dh, ob=ob: e.matmul(po[ob][:], mt[b][:, ci, j * 128:(j + 1) * 128],
                                                                                w_bf[:, ci, dh * 512:(dh + 1) * 512],
                                                                                start=(ci == 0), stop=(ci == 7)),
                             reads=['wo', ('mt', b)], writes=[('po', ob)])
                    P.op('dve', lambda e, j=j, dh=dh, ob=ob, xob=xob: e.tensor_tensor(
                        out=xo[xob][:, dh * 512:(dh + 1) * 512], in0=xt[b][:, j, dh * 512:(dh + 1) * 512], in1=po[ob][:], op=ALU.add),
                        reads=[('po', ob), ('xt', b)], writes=[('xo', xob, dh)])
                P.dma('pool', xout[tok0 + j * 128: tok0 + (j + 1) * 128, :], xo[xob][:], reads=[('xo', xob, 0), ('xo', xob, 1)])


def stage_inproj_odd(P, K, xin, w_d, gcol_d, T, mqk_raw, mif, rw_raw, v_aug, moA):
    with Ctx(P) as c:
        ip = InProj(P, K, c, xin, w_d, 3848, gcol_d, T)
        vs = [c.sb('vs', [128, 4, 129], BF16) for _ in range(2)]
        so = [c.sb('so', [128, 512], F32) for _ in range(2)]
        for i in range(2):
            P.op('dve', lambda e, i=i: e.memset(vs[i][:], 1.0), writes=[('vs', i)])
        cnt = {'f': 0, 't': 0}

        def f_epi(idx, src, skey, tok0, g):
            m = 8 if idx == 8 else 128
            if idx < 8:
                dst = mqk_raw[idx * 128:(idx + 1) * 128, tok0:tok0 + 512]
            elif idx == 8:
                dst = mif[0:8, tok0:tok0 + 512]
            else:
                dst = rw_raw[(idx - 9) * 128:(idx - 8) * 128, tok0:tok0 + 512]
            P.dma('pool', dst, src[:m, :], reads=[skey])

        def t_epi(idx, ps, pkey, tokj, jj):
            i = cnt['t'] % 2
            cnt['t'] += 1
            if idx == 0:
                P.op('dve', lambda e: e.tensor_copy(out=vs[i][:, :, 0:128], in_=ps[:].rearrange("p (h d) -> p h d", h=4)),
                     reads=[pkey], writes=[('vs', i)])
                P.dma('pool', v_aug[tokj:tokj + 128, :], vs[i][:].rearrange("p h d -> p (h d)"), reads=[('vs', i)])
            else:
                P.op('act', lambda e: e.activation(out=so[i][:], in_=ps[:], func=AF.Sigmoid), reads=[pkey], writes=[('so', i)])
                P.dma('pool', moA[tokj:tokj + 128, :], so[i][:], reads=[('so', i)])

        fcols = [(i * 128, 128) for i in range(8)] + [(1536, 8)] + [(2056 + i * 128, 128) for i in range(14)]
        tcols = [(1024, 512), (1544, 512)]
        ip.run(fcols, tcols, f_epi, t_epi, copy_f=True, copy_t=(), nraw=6)


def stage_ml_prep(P, K, mqk_raw, prm, mqkT, mkA, S, nseq):
    with Ctx(P) as c:
        cw = c.sb('cw', [128, 8, 4], F32)
        cb = c.sb('cb', [128, 8], F32)
        P.dma('sp', cw[:], prm['ml_cw'], writes=['cw'])
        P.dma('sp', cb[:], prm['ml_cb'], writes=['cb'])
        buf = [c.sb('buf', [128, 515], F32) for _ in range(3)]
        acc = [c.sb('acc', [128, 512], F32) for _ in range(2)]
        qo = [c.sb('qo', [128, 512], BF16) for _ in range(2)]
        kt = [c.sb('kt', [128, 4, 128], BF16) for _ in range(2)]
        tp = [c.ps('tp', [128, 4, 128], BF16) for _ in range(2)]
        n = 0
        for s in range(nseq):
            for g in range(S // 512):
                t0 = s * S + g * 512
                def unit(ch, nn):
                    b3 = nn % 3
                    b = nn % 2
                    en = 'dve' if nn % 2 == 0 else 'pool'
                    if g == 0:
                        P.op('pool', lambda e, b3=b3: e.memset(buf[b3][:, 0:3], 0.0), writes=[('buf', b3)])
                        yield
                        P.dma('sp', buf[b3][:, 3:515], mqk_raw[ch * 128:(ch + 1) * 128, t0:t0 + 512], writes=[('buf', b3)])
                        yield
                    else:
                        P.dma('sp', buf[b3][:, :], mqk_raw[ch * 128:(ch + 1) * 128, t0 - 3:t0 + 512], writes=[('buf', b3)])
                        yield
                    P.op(en, lambda e, b3=b3, b=b, ch=ch: e.tensor_scalar(out=acc[b][:], in0=buf[b3][:, 3:515], scalar1=cw[:, ch, 3:4],
                                                                         scalar2=cb[:, ch:ch + 1], op0=ALU.mult, op1=ALU.add),
                         reads=[('buf', b3), 'cw', 'cb'], writes=[('acc', b)])
                    yield
                    for j in (2, 1, 0):
                        P.op('dve', lambda e, b3=b3, b=b, ch=ch, j=j: e.scalar_tensor_tensor(out=acc[b][:], in0=buf[b3][:, j:j + 512],
                                                                                           scalar=cw[:, ch, j:j + 1], in1=acc[b][:],
                                                                                           op0=ALU.mult, op1=ALU.add),
                             reads=[('buf', b3), ('acc', b), 'cw'], writes=[('acc', b)])
                        yield
                    P.op('act', lambda e, b=b: e.activation(out=acc[b][:], in_=acc[b][:], func=AF.Silu), reads=[('acc', b)], writes=[('acc', b)])
                    yield
                    sc = 1.0 if ch < 4 else float(128 ** -0.5)
                    P.op(en, lambda e, b=b, sc=sc: e.tensor_scalar(out=qo[b][:], in0=acc[b][:], scalar1=sc, scalar2=None, op0=ALU.mult),
                         reads=[('acc', b)], writes=[('qo', b)])
                    yield
                    P.dma('pool', mqkT[ch * 128:(ch + 1) * 128, t0:t0 + 512], qo[b][:], reads=[('qo', b)])
                    yield
                    if ch >= 4:
                        for j in range(4):
                            P.op('pe', lambda e, b=b, j=j: e.transpose(out=tp[b][:, j, :], in_=qo[b][:, j * 128:(j + 1) * 128], identity=K['idb'][:]),
                                 reads=[('qo', b)], writes=[('tp', b)])
                            yield
                        P.op('act', lambda e, b=b: e.copy(out=kt[b][:], in_=tp[b][:]), reads=[('tp', b)], writes=[('kt', b)])
                        yield
                        P.dma('pool', mkA[t0:t0 + 512, (ch - 4) * 128:(ch - 3) * 128].rearrange("(j p) d -> p j d", p=128), kt[b][:],
                              reads=[('kt', b)])
                        yield

                for c0 in range(0, 8, 2):
                    gens = [unit(c0, n), unit(c0 + 1, n + 1)]
                    n += 2
                    while gens:
                        for gq in list(gens):
                            try:
                                next(gq)
                            except StopIteration:
                                gens.remove(gq)


def stage_ml_gates(P, K, mif, prm, mgM, mcol, mend, S, nseq):
    NB = S // 128
    R8 = nseq * 4
    with Ctx(P) as c:
        it = c.sb('it', [R8, S], F32)
        ft = c.sb('ft', [R8, S], F32)
        Bt = c.sb('Bt', [R8, S], F32)
        Mt = c.sb('Mt', [R8, S], F32)
        ones = c.sb('ones', [R8, S], F32)
        gb = c.sb('gb', [R8, 2], F32)
        ngb = c.sb('ngb', [R8, 1], F32)
        P.dma('sp', gb[:], prm['ml_gb'][0:R8, :], writes=['gb'])
        for s in range(nseq):
            P.dma('sp', it[4 * s:4 * s + 4, :], mif[0:4, s * S:(s + 1) * S], writes=['it'])
            P.dma('sp', ft[4 * s:4 * s + 4, :], mif[4:8, s * S:(s + 1) * S], writes=['ft'])
        P.op('dve', lambda e: e.memset(ones[:], 1.0), writes=['ones'])
        P.op('dve', lambda e: e.tensor_scalar(out=ngb[:], in0=gb[:, 1:2], scalar1=-1.0, scalar2=None, op0=ALU.mult), reads=['gb'], writes=['ngb'])
        P.op('act', lambda e: e.activation(out=ft[:], in_=ft[:], func=AF.Exp, scale=-1.0, bias=ngb[:, 0:1]), reads=['ft', 'ngb'], writes=['ft'])
        P.op('dve', lambda e: e.tensor_scalar(out=ft[:], in0=ft[:], scalar1=1.0, scalar2=None, op0=ALU.add), reads=['ft'], writes=['ft'])
        P.op('act', lambda e: e.activation(out=ft[:], in_=ft[:], func=AF.Ln), reads=['ft'], writes=['ft'])
        P.op('dve', lambda e: e.tensor_tensor_scan(out=Bt[:], data0=ones[:], data1=ft[:], initial=0.0, op0=ALU.mult, op1=ALU.subtract),
             reads=['ones', 'ft'], writes=['Bt'])
        P.op('dve', lambda e: e.scalar_tensor_tensor(out=it[:], in0=it[:], scalar=gb[:, 0:1], in1=Bt[:], op0=ALU.add, op1=ALU.subtract),
             reads=['it', 'gb', 'Bt'], writes=['it'])
        P.op('dve', lambda e: e.tensor_tensor_scan(out=Mt[:], data0=it[:], data1=it[:], initial=0.0, op0=ALU.max, op1=ALU.max),
             reads=['it'], writes=['Mt'])
        P.op('dve', lambda e: e.tensor_tensor(out=Bt[:], in0=Bt[:], in1=Mt[:], op=ALU.add), reads=['Bt', 'Mt'], writes=['Bt'])
        P.op('act', lambda e: e.activation(out=Bt[:], in_=Bt[:], func=AF.Exp, scale=-1.0), reads=['Bt'], writes=['Bt'])
        P.dma('pool', mgM[0:R8, :], Mt[:], reads=['Mt'])
        P.dma('pool', mend.rearrange("(c r) -> r c", r=R8), Mt[:].rearrange("r (c k) -> r c k", k=128)[:, :, 127], reads=['Mt'], allow_slow_non_contiguous=True)
        pc = [c.ps('pc', [128, NB, R8], F32) for _ in range(2)]
        col = [c.sb('col', [128, NB, R8], F32) for _ in range(2)]
        for k, src in enumerate((it, Bt)):
            for cb_ in range(NB):
                P.op('pe', lambda e, k=k, cb_=cb_, src=src: e.transpose(out=pc[k][:, cb_, :], in_=src[:, cb_ * 128:(cb_ + 1) * 128],
                                                                       identity=K['idf'][0:R8, 0:R8]),
                     reads=['it', 'Bt', 'idf'], writes=[('pc', k)])
            P.op('dve', lambda e, k=k: e.tensor_copy(out=col[k][:], in_=pc[k][:]), reads=[('pc', k)], writes=[('col', k)])
            P.dma('pool', mcol[k], col[k][:].rearrange("p c r -> p (c r)"), reads=[('col', k)])


def stage_ml_core(P, K, mqkT, mkA, v_aug, moA, mgM, mcol, mend, prm, mixT, S, nseq):
    NB = S // 128
    R8 = nseq * 4
    with Ctx(P) as c:
        acol = c.sb('acol', [128, NB, R8], F32)
        encol = c.sb('encol', [128, NB, R8], F32)
        Mc = c.sb('Mc', [128, NB + 1, R8], F32)
        nMc = c.sb('nMc', [128, NB + 1, R8], F32)
        dcs = c.sb('dcs', [128, NB, R8], F32)
        wcs = c.sb('wcs', [128, NB, R8], F32)
        ngr = c.sb('ngr', [128, 512], F32)
        P.dma('sp', acol[:].rearrange("p c r -> p (c r)"), mcol[0], writes=['acol'])
        P.dma('sp', encol[:].rearrange("p c r -> p (c r)"), mcol[1], writes=['encol'])
        P.op('dve', lambda e: e.memset(Mc[:, 0, :], 0.0), writes=['Mc'])
        P.dma('sp', Mc[:, 1:, :].rearrange("p c r -> p (c r)"), mend.rearrange("(o n) -> o n", o=1).partition_broadcast(128), writes=['Mc'])
        P.dma('sp', ngr[:], prm['ml_ng_rep'], writes=['ngr'])
        P.op('dve', lambda e: e.tensor_scalar(out=nMc[:], in0=Mc[:], scalar1=-1.0, scalar2=None, op0=ALU.mult), reads=['Mc'], writes=['nMc'])
        P.op('dve', lambda e: e.tensor_tensor(out=dcs[:], in0=Mc[:, 0:NB, :], in1=Mc[:, 1:NB + 1, :], op=ALU.subtract), reads=['Mc'], writes=['dcs'])
        P.op('act', lambda e: e.activation(out=dcs[:], in_=dcs[:], func=AF.Exp), reads=['dcs'], writes=['dcs'])
        P.op('dve', lambda e: e.tensor_tensor(out=wcs[:], in0=acol[:], in1=Mc[:, 1:NB + 1, :], op=ALU.subtract), reads=['acol', 'Mc'], writes=['wcs'])
        P.op('act', lambda e: e.activation(out=wcs[:], in_=wcs[:], func=AF.Exp), reads=['wcs'], writes=['wcs'])
        Cst = [c.sb('Cst', [128, 129], F32) for _ in range(R8)]
        Cbf = [c.sb('Cbf', [128, 129], BF16) for _ in range(R8)]
        for r in range(R8):
            P.op('pool', lambda e, r=r: e.memset(Cst[r][:], 0.0), writes=[('Cst', r)])
            P.op('pool', lambda e, r=r: e.memset(Cbf[r][:], 0.0), writes=[('Cbf', r)])
        qT4 = [c.sb('qT4', [128, 4, 512], BF16) for _ in range(2)]
        kT4 = [c.sb('kT4', [128, 4, 512], BF16) for _ in range(2)]
        kA4 = [c.sb('kA4', [128, 4, 512], BF16) for _ in range(2)]
        vA4 = [c.sb('vA4', [128, 4, 516], BF16) for _ in range(2)]
        oA4 = [c.sb('oA4', [128, 4, 512], F32) for _ in range(2)]
        Mb4 = [c.sb('Mb4', [128, 4, 512], F32) for _ in range(2)]
        mo = [c.sb('mo', [128, 4, 512], BF16) for _ in range(2)]
        E = [c.sb('E', [128, 128], F32) for _ in range(4)]
        PT = [c.sb('PT', [128, 128], BF16) for _ in range(4)]
        er = [c.sb('er', [128, 128], F32) for _ in range(4)]
        qs = [c.sb('qs', [128, 128], BF16) for _ in range(4)]
        hs = [c.sb('hs', [128, 128], F32) for _ in range(4)]
        hj = [c.sb('hj', [128, 128], F32) for _ in range(2)]
        hf = [c.sb('hf', [128, 128], BF16) for _ in range(4)]
        vw = [c.sb('vw', [128, 129], BF16) for _ in range(4)]
        sts = [c.sb('sts', [128, 6, 4], F32) for _ in range(2)]
        ps_st = [c.ps('pst', [128, 512], F32) for _ in range(2)]
        ps_oa = [c.ps('psoa', [128, 512], F32) for _ in range(2)]
        ps_ob = [c.ps('psob', [128, 512], F32) for _ in range(1)]
        ps_u = [c.ps('psu', [128, 512], F32) for _ in range(2)]
        ps_t = [c.ps('pstt', [128, 4, 128], BF16) for _ in range(1)]
        n = 0

        def po(b, h):
            return ps_oa[b][:, h * 160:h * 160 + 129] if h < 3 else ps_ob[0][:, 0:129]

        def pok(b, h):
            return ('psoa', b) if h < 3 else ('psob', 0)

        def pu(h):
            return ps_u[0][:, h * 160:h * 160 + 129] if h < 3 else ps_u[1][:, 0:129]

        def puk(h):
            return ('psu', 0) if h < 3 else ('psu', 1)

        for sg in range(S // 512):
            for s in range(nseq):
                lb = (sg * nseq + s) % 2
                t0 = s * S + sg * 512
                P.dma('sp', qT4[lb][:], mqkT[0:512, t0:t0 + 512].rearrange("(h d) t -> d h t", d=128), writes=[('qT4', lb)])
                P.dma('sp', kT4[lb][:], mqkT[512:1024, t0:t0 + 512].rearrange("(h d) t -> d h t", d=128), writes=[('kT4', lb)])
                P.dma('sp', kA4[lb][:], mkA[t0:t0 + 512, :].rearrange("(c p) f -> p c f", p=128), writes=[('kA4', lb)])
                P.dma('sp', vA4[lb][:], v_aug[t0:t0 + 512, :].rearrange("(c p) f -> p c f", p=128), writes=[('vA4', lb)])
                P.dma('sp', oA4[lb][:], moA[t0:t0 + 512, :].rearrange("(c p) f -> p c f", p=128), writes=[('oA4', lb)])
                for h in range(4):
                    P.dma('sp', Mb4[lb][:, h, :], mgM[s * 4 + h:s * 4 + h + 1, sg * 512:(sg + 1) * 512].partition_broadcast(128),
                          writes=[('Mb4', lb)])
                for c4 in range(4):
                    cg = sg * 4 + c4
                    cs = slice(c4 * 128, (c4 + 1) * 128)
                    b = n % 2
                    n += 1
                    st = sts[b]
                    H4 = range(4)
                    for h in H4:
                        P.op('pe', lambda e: e.matmul(ps_st[b][:, h * 128:(h + 1) * 128], kT4[lb][:, h, cs], qT4[lb][:, h, cs], start=True, stop=True),
                             reads=[('kT4', lb), ('qT4', lb)], writes=[('pst', b)])
                    for h in H4:
                        sh = s * 4 + h
                        P.op('act', lambda e: e.activation(out=E[h][:], in_=Mb4[lb][:, h, cs], func=AF.Exp, scale=-1.0, bias=acol[:, cg, sh:sh + 1]),
                             reads=[('Mb4', lb), 'acol'], writes=[('E', h)])
                        P.op('act', lambda e: e.activation(out=er[h][:], in_=Mb4[lb][:, h, cs], func=AF.Exp, scale=-1.0, bias=Mc[:, cg, sh:sh + 1]),
                             reads=[('Mb4', lb), 'Mc'], writes=[('er', h)])
                    for h in H4:
                        P.op('pool', lambda e: e.tensor_tensor(out=E[h][:], in0=E[h][:], in1=K['triu'], op=ALU.mult), reads=[('E', h)], writes=[('E', h)])
                        P.op('pool', lambda e: e.tensor_tensor(out=qs[h][:], in0=qT4[lb][:, h, cs], in1=er[h][:], op=ALU.mult),
                             reads=[('qT4', lb), ('er', h)], writes=[('qs', h)])
                    for h in H4:
                        P.op('dve', lambda e: e.tensor_tensor(out=PT[h][:], in0=ps_st[b][:, h * 128:(h + 1) * 128], in1=E[h][:], op=ALU.mult),
                             reads=[('pst', b), ('E', h)], writes=[('PT', h)])
                    for h in H4:
                        sh = s * 4 + h
                        P.op('pe', lambda e: e.matmul(po(b, h), qs[h][:], Cbf[sh][:], start=True, stop=False),
                             reads=[('qs', h), ('Cbf', sh)], writes=[pok(b, h)])
                        P.op('pe', lambda e: e.matmul(po(b, h), PT[h][:], vA4[lb][:, c4, h * 129:(h + 1) * 129], start=False, stop=True),
                             reads=[('PT', h), ('vA4', lb)], writes=[pok(b, h)])
                    for h in H4:
                        P.op('act', lambda e: e.activation(out=st[:, 0, h:h + 1], in_=po(b, h)[:, 128:129], func=AF.Abs),
                             reads=[pok(b, h)], writes=[('sts', b, 0, h)])
                    for h in H4:
                        sh = s * 4 + h
                        P.op('dve', lambda e: e.tensor_tensor(out=st[:, 0, h:h + 1], in0=st[:, 0, h:h + 1], in1=encol[:, cg, sh:sh + 1], op=ALU.max),
                             reads=[('sts', b, 0, h), 'encol'], writes=[('sts', b, 0, h)])
                    P.op('dve', lambda e: e.reciprocal(out=st[:, 0, :], in_=st[:, 0, :]), reads=[('sts', b, 0, h) for h in H4], writes=[('sts', b, 0, h) for h in H4])
                    for h in H4:
                        P.op('act', lambda e: e.activation(out=hs[h][:], in_=po(b, h)[:, 0:128], func=AF.Copy, scale=st[:, 0, h:h + 1], accum_out=st[:, 1, h:h + 1]),
                             reads=[pok(b, h), ('sts', b, 0, h)], writes=[('hs', h), ('sts', b, 1, h)])
                        P.op('act', lambda e: e.activation(out=hj[h % 2][:], in_=hs[h][:], func=AF.Square, accum_out=st[:, 2, h:h + 1]),
                             reads=[('hs', h)], writes=[('hj', h % 2), ('sts', b, 2, h)])
                    k12 = [('sts', b, 1, h) for h in H4] + [('sts', b, 2, h) for h in H4]
                    P.op('dve', lambda e: e.tensor_scalar(out=st[:, 3, :], in0=st[:, 1, :], scalar1=1.0 / 128, scalar2=None, op0=ALU.mult), reads=k12, writes=[('sts', b, 3)])
                    P.op('dve', lambda e: e.tensor_tensor(out=st[:, 4, :], in0=st[:, 3, :], in1=st[:, 3, :], op=ALU.mult), reads=[('sts', b, 3)], writes=[('sts', b, 4)])
                    P.op('dve', lambda e: e.tensor_scalar(out=st[:, 2, :], in0=st[:, 2, :], scalar1=1.0 / 128, scalar2=EPS, op0=ALU.mult, op1=ALU.add), reads=k12, writes=k12)
                    P.op('dve', lambda e: e.tensor_tensor(out=st[:, 2, :], in0=st[:, 2, :], in1=st[:, 4, :], op=ALU.subtract), reads=k12 + [('sts', b, 4)], writes=k12)
                    P.op('act', lambda e: e.sqrt(out=st[:, 2, :], in_=st[:, 2, :]), reads=k12, writes=k12)
                    P.op('dve', lambda e: e.reciprocal(out=st[:, 2, :], in_=st[:, 2, :]), reads=k12, writes=k12)
                    for h in H4:
                        P.op('dve', lambda e: e.tensor_scalar(out=hs[h][:], in0=hs[h][:], scalar1=st[:, 3, h:h + 1], scalar2=st[:, 2, h:h + 1],
                                                              op0=ALU.subtract, op1=ALU.mult),
                             reads=[('hs', h), ('sts', b, 3)] + k12, writes=[('hs', h)])
                    for h in H4:
                        P.op('pool', lambda e: e.tensor_tensor(out=hs[h][:], in0=hs[h][:], in1=ngr[:, h * 128:(h + 1) * 128], op=ALU.mult),
                             reads=[('hs', h), 'ngr'], writes=[('hs', h)])
                        P.op('pool', lambda e: e.tensor_tensor(out=hf[h][:], in0=hs[h][:], in1=oA4[lb][:, c4, h * 128:(h + 1) * 128], op=ALU.mult),
                             reads=[('hs', h), ('oA4', lb)], writes=[('hf', h)])
                    for h in H4:
                        sh = s * 4 + h
                        P.op('dve', lambda e: e.tensor_scalar(out=vw[h][:], in0=vA4[lb][:, c4, h * 129:(h + 1) * 129], scalar1=wcs[:, cg, sh:sh + 1], scalar2=None, op0=ALU.mult),
                             reads=[('vA4', lb), 'wcs'], writes=[('vw', h)])
                    for h in H4:
                        P.op('pe', lambda e: e.matmul(pu(h), kA4[lb][:, c4, h * 128:(h + 1) * 128], vw[h][:], start=True, stop=True),
                             reads=[('kA4', lb), ('vw', h)], writes=[puk(h)])
                    for h in H4:
                        P.op('pe', lambda e: e.transpose(out=ps_t[0][:, h, :], in_=hf[h][:], identity=K['idb'][:]), reads=[('hf', h)], writes=['pstt'])
                    for h in H4:
                        sh = s * 4 + h
                        P.op('dve', lambda e: e.scalar_tensor_tensor(out=Cst[sh][:], in0=Cst[sh][:], scalar=dcs[:, cg, sh:sh + 1], in1=pu(h), op0=ALU.mult, op1=ALU.add),
                             reads=[('Cst', sh), 'dcs', puk(h)], writes=[('Cst', sh)])
                    for h in H4:
                        sh = s * 4 + h
                        P.op('act', lambda e: e.copy(out=Cbf[sh][:], in_=Cst[sh][:]), reads=[('Cst', sh)], writes=[('Cbf', sh)])
                    P.op('act', lambda e: e.copy(out=mo[lb][:, :, cs], in_=ps_t[0][:]), reads=['pstt'], writes=[('mo', lb)])
                P.dma('pool', mixT[0:512, t0:t0 + 512].rearrange("(h d) t -> d h t", d=128), mo[lb][:], reads=[('mo', lb)])


RW_LD = -0.6065306597126334


def stage_rw_prep(P, K, rw_raw, prm, rwF, rwT, rwgL, rwbv, rwg, S, nseq):
    with Ctx(P) as c:
        mu = c.sb('mu', [128, 14], F32)
        pc = c.sb('pc', [128, 5, 4], F32)
        P.dma('sp', mu[:], prm['rw_mu'], writes=['mu'])
        P.dma('sp', pc[:], prm['rw_pc'], writes=['pc'])
        stg = c.sb('stg', [128, 3, 512], F32)
        P.op('dve', lambda e: e.memset(stg[:], 0.0), writes=['stg'])
        P.dma('sp', stg[0:64, 0, :], prm['rw_w2'], writes=['stg'])
        P.dma('sp', stg[64:128, 1, :], prm['rw_a2'], writes=['stg'])
        P.dma('sp', stg[:, 2, :], prm['rw_g2'], writes=['stg'])
        lw = c.sb('lw', [128, 3, 512], BF16)
        P.op('dve', lambda e: e.tensor_copy(out=lw[:], in_=stg[:]), reads=['stg'], writes=['lw'])
        blkf = K['blk64f']
        buf = [c.sb('buf', [128, 513], F32) for _ in range(3)]
        dd = [c.sb('dd', [128, 512], F32) for _ in range(2)]
        xs = c.sb('xs', [128, 14, 512], F32)
        wab = c.sb('wab', [128, 512], BF16)
        sgb = c.sb('sgb', [128, 512], BF16)
        ld2 = [c.sb('ld', [128, 512], F32) for _ in range(2)]
        av2 = [c.sb('av', [128, 512], F32) for _ in range(2)]
        gv2 = [c.sb('gv', [128, 512], F32) for _ in range(2)]
        kk2 = [c.sb('kk', [128, 512], F32) for _ in range(2)]
        km2 = [c.sb('km', [128, 512], F32) for _ in range(2)]
        bb2 = [c.sb('bb', [128, 512], F32) for _ in range(2)]
        t12 = [c.sb('t1', [128, 512], F32) for _ in range(2)]
        t22 = [c.sb('t2', [128, 512], F32) for _ in range(2)]
        lg2 = [c.sb('lg', [128, 512], F32) for _ in range(2)]
        eg2 = [c.sb('eg', [128, 512], F32) for _ in range(2)]
        egl2 = [c.sb('egl', [128, 512], F32) for _ in range(2)]
        gl2 = [c.sb('gl', [128, 8], F32) for _ in range(2)]
        of = [c.sb('of', [128, 4, 512], BF16) for _ in range(2)]
        o32 = [c.sb('o32', [128, 512], F32) for _ in range(2)]
        tb = [c.sb('tb', [128, 3, 512], BF16) for _ in range(2)]
        tt = [c.sb('tt', [128, 4, 128], BF16) for _ in range(2)]
        pz = [c.ps('pz', [128, 512], F32) for _ in range(2)]
        pb = [c.ps('pb', [128, 512], F32) for _ in range(2)]
        tp = [c.ps('tp', [128, 4, 128], BF16) for _ in range(2)]
        n = 0
        nt = 0
        no = 0
        for s in range(nseq):
            for g in range(S // 512):
                t0 = s * S + g * 512
                for ci in range(14):
                    b3 = n % 3
                    b = n % 2
                    en = 'dve' if n % 2 == 0 else 'pool'
                    n += 1
                    if g == 0:
                        P.op('pool', lambda e, b3=b3: e.memset(buf[b3][:, 0:1], 0.0), writes=[('buf', b3)])
                        P.dma('sp', buf[b3][:, 1:513], rw_raw[ci * 128:(ci + 1) * 128, t0:t0 + 512], writes=[('buf', b3)])
                    else:
                        P.dma('sp', buf[b3][:, :], rw_raw[ci * 128:(ci + 1) * 128, t0 - 1:t0 + 512], writes=[('buf', b3)])
                    P.op(en, lambda e, b3=b3, b=b: e.tensor_tensor(out=dd[b][:], in0=buf[b3][:, 0:512], in1=buf[b3][:, 1:513], op=ALU.subtract),
                         reads=[('buf', b3)], writes=[('dd', b)])
                    P.op('dve', lambda e, b3=b3, b=b, ci=ci: e.scalar_tensor_tensor(out=xs[:, ci, :], in0=dd[b][:], scalar=mu[:, ci:ci + 1],
                                                                                in1=buf[b3][:, 1:513], op0=ALU.mult, op1=ALU.add),
                         reads=[('dd', b), ('buf', b3), 'mu'], writes=[('xs', ci)])
                P.op('act', lambda e: e.activation(out=wab[0:64, :], in_=xs[0:64, 12, :], func=AF.Tanh), reads=[('xs', 12)], writes=['wab'])
                P.op('act', lambda e: e.copy(out=wab[64:128, :], in_=xs[64:128, 12, :]), reads=[('xs', 12)], writes=['wab'])
                P.op('act', lambda e: e.activation(out=sgb[:], in_=xs[:, 13, :], func=AF.Sigmoid), reads=[('xs', 13)], writes=['sgb'])
                def unit(fc):
                    ob = fc % 2
                    fs = slice(fc * 128, (fc + 1) * 128)
                    zb = fc % 2
                    P.op('pe', lambda e, fs=fs, zb=zb: e.matmul(pz[zb][:], lw[:, 0, fs], wab[:], start=True, stop=True), reads=['lw', 'wab'], writes=[('pz', zb)])
                    yield
                    P.op('act', lambda e, zb=zb, fc=fc: e.activation(out=ld2[fc % 2][:], in_=pz[zb][:], func=AF.Sigmoid, bias=pc[:, 0, fc:fc + 1]),
                         reads=[('pz', zb), 'pc'], writes=[('ld', fc % 2)])
                    yield
                    P.op('dve', lambda e: e.tensor_scalar(out=ld2[fc % 2][:], in0=ld2[fc % 2][:], scalar1=RW_LD, scalar2=None, op0=ALU.mult), reads=[('ld', fc % 2)], writes=[('ld', fc % 2)])
                    yield
                    P.op('pe', lambda e, fs=fs, zb=zb: e.matmul(pb[zb][:], lw[:, 1, fs], wab[:], start=True, stop=True), reads=['lw', 'wab'], writes=[('pb', zb)])
                    yield
                    P.op('act', lambda e, zb=zb, fc=fc: e.activation(out=av2[fc % 2][:], in_=pb[zb][:], func=AF.Sigmoid, bias=pc[:, 1, fc:fc + 1]),
                         reads=[('pb', zb), 'pc'], writes=[('av', fc % 2)])
                    yield
                    P.op('dve', lambda e, fc=fc: e.tensor_scalar(out=kk2[fc % 2][:], in0=xs[:, 4 + fc, :], scalar1=pc[:, 2, fc:fc + 1], scalar2=None, op0=ALU.mult),
                         reads=[('xs', 4 + fc), 'pc'], writes=[('kk', fc % 2)])
                    yield
                    P.op('act', lambda e: e.activation(out=t12[fc % 2][:], in_=kk2[fc % 2][:], func=AF.Square), reads=[('kk', fc % 2)], writes=[('t1', fc % 2)])
                    yield
                    P.op('pe', lambda e, zb=zb: e.matmul(pz[zb][:], blkf, t12[fc % 2][:], start=True, stop=True), reads=[('t1', fc % 2), 'blk64f'], writes=[('pz', zb)])
                    yield
                    P.op('act', lambda e, zb=zb: e.sqrt(out=t12[fc % 2][:], in_=pz[zb][:]), reads=[('pz', zb)], writes=[('t1', fc % 2)])
                    yield
                    P.op('dve', lambda e: e.tensor_scalar(out=t12[fc % 2][:], in0=t12[fc % 2][:], scalar1=1e-12, scalar2=None, op0=ALU.max), reads=[('t1', fc % 2)], writes=[('t1', fc % 2)])
                    yield
                    P.op('dve', lambda e: e.reciprocal(out=t12[fc % 2][:], in_=t12[fc % 2][:]), reads=[('t1', fc % 2)], writes=[('t1', fc % 2)])
                    yield
                    P.op('dve', lambda e: e.tensor_tensor(out=kk2[fc % 2][:], in0=kk2[fc % 2][:], in1=t12[fc % 2][:], op=ALU.mult), reads=[('kk', fc % 2), ('t1', fc % 2)], writes=[('kk', fc % 2)])
                    yield
                    P.op('pool', lambda e, fc=fc: e.tensor_scalar(out=t22[fc % 2][:], in0=av2[fc % 2][:], scalar1=-1.0, scalar2=pc[:, 3, fc:fc + 1], op0=ALU.add, op1=ALU.mult),
                         reads=[('av', fc % 2), 'pc'], writes=[('t2', fc % 2)])
                    yield
                    P.op('dve', lambda e, fc=fc: e.scalar_tensor_tensor(out=km2[fc % 2][:], in0=t22[fc % 2][:], scalar=1.0, in1=xs[:, 4 + fc, :], op0=ALU.add, op1=ALU.mult),
                         reads=[('t2', fc % 2), ('xs', 4 + fc)], writes=[('km', fc % 2)])
                    yield
                    P.op('pool', lambda e: e.tensor_tensor(out=bb2[fc % 2][:], in0=kk2[fc % 2][:], in1=av2[fc % 2][:], op=ALU.mult), reads=[('kk', fc % 2), ('av', fc % 2)], writes=[('bb', fc % 2)])
                    yield
                    P.op('pe', lambda e, fs=fs, zb=zb: e.matmul(pb[zb][:], lw[:, 2, fs], sgb[:], start=True, stop=True), reads=['lw', 'sgb'], writes=[('pb', zb)])
                    yield
                    P.op('act', lambda e, zb=zb: e.copy(out=gv2[fc % 2][:], in_=pb[zb][:]), reads=[('pb', zb)], writes=[('gv', fc % 2)])
                    yield
                    P.dma('pool', rwg[fs, t0:t0 + 512], gv2[fc % 2][:], reads=[('gv', fc % 2)])
                    yield
                    P.op('dve', lambda e, fc=fc: e.scalar_tensor_tensor(out=t12[fc % 2][:], in0=xs[:, fc, :], scalar=pc[:, 4, fc:fc + 1], in1=km2[fc % 2][:], op0=ALU.mult, op1=ALU.mult),
                         reads=[('xs', fc), 'pc', ('km', fc % 2)], writes=[('t1', fc % 2)])
                    yield
                    P.op('pe', lambda e, zb=zb: e.matmul(pz[zb][:], blkf, t12[fc % 2][:], start=True, stop=True), reads=[('t1', fc % 2), 'blk64f'], writes=[('pz', zb)])
                    yield
                    P.op('dve', lambda e, zb=zb, fc=fc: e.tensor_tensor(out=t12[fc % 2][:], in0=pz[zb][:], in1=xs[:, 8 + fc, :], op=ALU.mult),
                         reads=[('pz', zb), ('xs', 8 + fc)], writes=[('t1', fc % 2)])
                    yield
                    o3 = fc % 2
                    P.op('dve', lambda e, o3=o3: e.tensor_tensor(out=o32[o3][:], in0=t12[fc % 2][:], in1=gv2[fc % 2][:], op=ALU.mult), reads=[('t1', fc % 2), ('gv', fc % 2)], writes=[('o32', o3)])
                    yield
                    P.dma('pool', rwbv[fs, t0:t0 + 512], o32[o3][:], reads=[('o32', o3)])
                    yield
                    for cc in range(8):
                        cs = slice(cc * 64, (cc + 1) * 64)
                        P.op('dve', lambda e, cs=cs: e.tensor_tensor_scan(out=lg2[fc % 2][:, cs], data0=K['ones_f'][:, 0:64], data1=ld2[fc % 2][:, cs], initial=0.0,
                                                                         op0=ALU.mult, op1=ALU.add),
                             reads=[('ld', fc % 2), 'ones_f'], writes=[('lg', fc % 2)])
                        yield
                    lg3 = lg2[fc % 2][:].rearrange("p (c k) -> p c k", k=64)
                    P.op('act', lambda e: e.activation(out=eg2[fc % 2][:], in_=lg2[fc % 2][:], func=AF.Exp), reads=[('lg', fc % 2)], writes=[('eg', fc % 2)])
                    yield
                    P.op('act', lambda e: e.copy(out=gl2[fc % 2][:], in_=eg2[fc % 2][:].rearrange("p (c k) -> p c k", k=64)[:, :, 63]), reads=[('eg', fc % 2)], writes=[('gl', fc % 2)])
                    yield
                    P.dma('pool', rwgL[fs, t0 // 64:t0 // 64 + 8], gl2[fc % 2][:], reads=[('gl', fc % 2)])
                    yield
                    P.op('dve', lambda e, fc=fc, ob=ob: e.tensor_tensor(out=of[ob][:, 1, :], in0=xs[:, fc, :], in1=eg2[fc % 2][:], op=ALU.mult),
                         reads=[('xs', fc), ('eg', fc % 2)], writes=[('of', ob)])
                    yield
                    for cc in range(8):
                        cs = slice(cc * 64, (cc + 1) * 64)
                        P.op('act', lambda e, cs=cs, cc=cc: e.activation(out=egl2[fc % 2][:, cs], in_=lg2[fc % 2][:, cs], func=AF.Exp, scale=-1.0,
                                                                         bias=lg2[fc % 2][:, cc * 64 + 63:cc * 64 + 64]),
                             reads=[('lg', fc % 2)], writes=[('egl', fc % 2)])
                        yield
                    tbb = fc % 2
                    P.op('pool', lambda e, tbb=tbb: e.tensor_tensor(out=tb[tbb][:, 1, :], in0=km2[fc % 2][:], in1=egl2[fc % 2][:], op=ALU.mult), reads=[('km', fc % 2), ('egl', fc % 2)], writes=[('tb', tbb)])
                    yield
                    P.op('pool', lambda e, tbb=tbb: e.tensor_tensor(out=tb[tbb][:, 2, :], in0=bb2[fc % 2][:], in1=egl2[fc % 2][:], op=ALU.mult), reads=[('bb', fc % 2), ('egl', fc % 2)], writes=[('tb', tbb)])
                    yield
                    P.op('act', lambda e, tbb=tbb, fc=fc: e.copy(out=tb[tbb][:, 0, :], in_=xs[:, 8 + fc, :]), reads=[('xs', 8 + fc)], writes=[('tb', tbb)])
                    yield
                    P.op('act', lambda e: e.activation(out=eg2[fc % 2][:], in_=lg2[fc % 2][:], func=AF.Exp, scale=-1.0), reads=[('lg', fc % 2)], writes=[('eg', fc % 2)])
                    yield
                    P.op('dve', lambda e, ob=ob: e.tensor_tensor(out=of[ob][:, 2, :], in0=bb2[fc % 2][:], in1=eg2[fc % 2][:], op=ALU.mult), reads=[('bb', fc % 2), ('eg', fc % 2)], writes=[('of', ob)])
                    yield
                    P.op('dve', lambda e, ob=ob: e.tensor_tensor(out=of[ob][:, 3, :], in0=km2[fc % 2][:], in1=eg2[fc % 2][:], op=ALU.mult), reads=[('km', fc % 2), ('eg', fc % 2)], writes=[('of', ob)])
                    yield
                    P.op('dve', lambda e: e.tensor_tensor(out=t22[fc % 2][:], in0=lg2[fc % 2][:], in1=ld2[fc % 2][:], op=ALU.subtract), reads=[('lg', fc % 2), ('ld', fc % 2)], writes=[('t2', fc % 2)])
                    yield
                    P.op('act', lambda e: e.activation(out=t22[fc % 2][:], in_=t22[fc % 2][:], func=AF.Exp), reads=[('t2', fc % 2)], writes=[('t2', fc % 2)])
                    yield
                    P.op('dve', lambda e, ob=ob: e.tensor_tensor(out=of[ob][:, 0, :], in0=kk2[fc % 2][:], in1=t22[fc % 2][:], op=ALU.mult), reads=[('kk', fc % 2), ('t2', fc % 2)], writes=[('of', ob)])
                    yield
                    P.dma('pool', rwF[:, fs, t0:t0 + 512].rearrange("a p t -> p a t"), of[ob][:], reads=[('of', ob)])
                    yield
                    for a3 in range(3):
                        tq = fc % 2
                        for j in range(4):
                            P.op('pe', lambda e, tq=tq, j=j, a3=a3, tbb=tbb: e.transpose(out=tp[tq][:, j, :], in_=tb[tbb][:, a3, j * 128:(j + 1) * 128], identity=K['idb'][:]),
                                 reads=[('tb', tbb)], writes=[('tp', tq)])
                            yield
                        P.op('act', lambda e, tq=tq: e.copy(out=tt[tq][:], in_=tp[tq][:]), reads=[('tp', tq)], writes=[('tt', tq)])
                        yield
                        P.dma('pool', rwT[a3, t0:t0 + 512, fs].rearrange("(j p) d -> p j d", p=128), tt[tq][:], reads=[('tt', tq)])
                        yield

                for pair in ((0, 1), (2, 3)):
                    gens = [unit(f) for f in pair]
                    while gens:
                        for gq in list(gens):
                            try:
                                next(gq)
                            except StopIteration:
                                gens.remove(gq)


def stage_rw_core(P, K, rwF, rwT, rwgL, rwbv, rwg, prm, mixT, S, nseq):
    GT = 256
    NCH = GT // 64
    with Ctx(P) as c:
        mk = c.sb('mk', [64, 4, 8, 64], F32)
        I8 = c.sb('I8', [64, 8, 64], F32)
        m0 = c.sb('m0', [64, 4, 64], F32)
        U = K['triu'][0:64, 0:64]
        Id = K['idf'][0:64, 0:64]
        P.op('dve', lambda e: e.tensor_tensor(out=m0[:, 2, :], in0=U, in1=Id, op=ALU.subtract), reads=['triu', 'idf'], writes=['m0'])
        P.op('dve', lambda e: e.tensor_scalar(out=m0[:, 0, :], in0=m0[:, 2, :], scalar1=-1.0, scalar2=None, op0=ALU.mult), reads=['m0'], writes=['m0'])
        P.op('dve', lambda e: e.tensor_copy(out=m0[:, 1, :], in_=U), reads=['triu'], writes=['m0'])
        P.op('dve', lambda e: e.tensor_scalar(out=m0[:, 3, :], in0=U, scalar1=-1.0, scalar2=None, op0=ALU.add), reads=['triu'], writes=['m0'])
        for h in range(8):
            P.op('dve', lambda e, h=h: e.tensor_copy(out=mk[:, :, h, :], in_=m0[:]), reads=['m0'], writes=['mk'])
            P.op('dve', lambda e, h=h: e.tensor_copy(out=I8[:, h, :], in_=Id), reads=['idf'], writes=['I8'])
        lnp = c.sb('lnp', [128, 2, 4], F32)
        P.dma('sp', lnp[:], prm['rw_ln'], writes=['lnp'])
        XF = [c.sb('XF', [64, 4, 8, GT], BF16) for _ in range(2)]
        XT = [c.sb('XT', [64, 3, NCH, 512], BF16) for _ in range(2)]
        gL = [c.sb('gL', [64, 8, NCH], F32) for _ in range(2)]
        bvt = [c.sb('bvt', [128, 4, GT], F32) for _ in range(2)]
        gt = [c.sb('gt', [128, 4, GT], F32) for _ in range(2)]
        yF = [c.sb('yF', [128, 4, GT], F32) for _ in range(2)]
        mo = [c.sb('mo', [128, 4, GT], BF16) for _ in range(2)]
        NT = [[c.sb('NT', [64, 8, 64], BF16) for _ in range(2)] for _ in range(NCH)]
        NN = [[c.sb('NN', [64, 8, 64], BF16) for _ in range(2)] for _ in range(NCH)]
        Wt = [c.sb('Wt', [64, 8, 64], BF16) for _ in range(NCH)]
        CbT = [c.sb('CbT', [64, 8, 64], BF16) for _ in range(NCH)]
        BkT = [c.sb('BkT', [64, 8, 64], BF16) for _ in range(NCH)]
        CkT = [c.sb('CkT', [64, 8, 64], BF16) for _ in range(NCH)]
        raw = [c.sb('raw', [64, 8, 64], F32) for _ in range(3)]
        Hs = [c.sb('Hs', [64, 8, 64], F32) for _ in range(nseq)]
        Hb = [c.sb('Hb', [64, 8, 64], BF16) for _ in range(nseq)]
        Rr = c.sb('Rr', [64, 8, 64], BF16)
        Un = c.sb('Un', [64, 8, 64], BF16)
        ysq = c.sb('ysq', [64, 8, 64], F32)
        yn = c.sb('yn', [64, 8, 64], F32)
        st = c.sb('st', [64, 4, 8], F32)
        for s in range(nseq):
            P.op('pool', lambda e, s=s: e.memset(Hs[s][:], 0.0), writes=[('Hs', s)])
            P.op('pool', lambda e, s=s: e.memset(Hb[s][:], 0.0), writes=[('Hb', s)])
        bank = [c.ps('bk', [128, 512], F32) for _ in range(8)]
        st_ = {'b': 0, 'r': 0}

        def nb():
            i = st_['b'] % 8
            st_['b'] += 1
            return i

        def bv(i):
            return bank[i][0:64, :].rearrange("p (h k) -> p h k", h=8)

        ng = 0
        for g in range(S // GT):
            for s in range(nseq):
                lb = ng % 2
                ng += 1
                t0 = s * S + g * GT
                for a in range(4):
                    P.dma('sp', XF[lb][:, a], rwF[a, :, t0:t0 + GT].rearrange("(h k) t -> k h t", k=64), writes=[('XF', lb)])
                for a in range(3):
                    P.dma('sp', XT[lb][:, a], rwT[a, t0:t0 + GT, :].rearrange("(c p) f -> p c f", p=64), writes=[('XT', lb)])
                P.dma('sp', gL[lb][:], rwgL[:, t0 // 64:t0 // 64 + NCH].rearrange("(h k) c -> k h c", k=64), writes=[('gL', lb)])
                P.dma('sp', bvt[lb][:], rwbv[:, t0:t0 + GT].rearrange("(f p) t -> p f t", p=128), writes=[('bvt', lb)])
                P.dma('sp', gt[lb][:], rwg[:, t0:t0 + GT].rearrange("(f p) t -> p f t", p=128), writes=[('gt', lb)])
                for ch in range(NCH):
                    cs = slice(ch * 64, (ch + 1) * 64)
                    specs = [
                        (2, 0, 0, NT[ch][0], 'f32'),
                        (0, 2, 3, NN[ch][0], 'f32'),
                        (2, 1, 1, CbT[ch], 'bf'),
                        (3, 0, 2, BkT[ch], 'bf'),
                        (3, 1, 1, CkT[ch], 'bf'),
                    ]
                    for (la, ra, mi, dst, kind) in specs:
                        bi = nb()
                        for h in range(8):
                            P.op('pe', lambda e, bi=bi, h=h, la=la, ra=ra, cs=cs: e.matmul(bv(bi)[:, h, :], XF[lb][:, la, h, cs], XF[lb][:, ra, h, cs],
                                                                                       start=True, stop=True),
                                 reads=[('XF', lb)], writes=[('bk', bi)])
                        if kind == 'f32':
                            P.op('dve', lambda e, bi=bi, mi=mi, dst=dst: e.tensor_tensor(out=dst[:], in0=bv(bi), in1=mk[:, mi], op=ALU.mult),
                                 reads=[('bk', bi), 'mk'], writes=[id(dst)])
                        else:
                            ri = st_['r'] % 3
                            st_['r'] += 1
                            P.op('act', lambda e, bi=bi, ri=ri: e.copy(out=raw[ri][:], in_=bv(bi)), reads=[('bk', bi)], writes=[('raw', ri)])
                            P.op('pool', lambda e, ri=ri, mi=mi, dst=dst: e.tensor_tensor(out=dst[:], in0=raw[ri][:], in1=mk[:, mi], op=ALU.mult),
                                 reads=[('raw', ri), 'mk'], writes=[id(dst)])
                    P.op('pool', lambda e, ch=ch: e.tensor_tensor(out=Wt[ch][:], in0=NT[ch][0][:], in1=I8[:], op=ALU.add),
                         reads=[id(NT[ch][0]), 'I8'], writes=[id(Wt[ch])])
                for j in range(1, 6):
                    o, nw = (j - 1) % 2, j % 2
                    b1s, b2s, b3s = {}, {}, {}
                    for ch in range(NCH):
                        b1 = b1s[ch] = nb()
                        for h in range(8):
                            P.op('pe', lambda e, h=h: e.matmul(bv(b1)[:, h, :], NT[ch][o][:, h, :], NN[ch][o][:, h, :], start=True, stop=True),
                                 reads=[id(NT[ch][o]), id(NN[ch][o])], writes=[('bk', b1)])
                    if j < 5:
                        for ch in range(NCH):
                            b2 = b2s[ch] = nb()
                            for h in range(8):
                                P.op('pe', lambda e, h=h: e.matmul(bv(b2)[:, h, :], NN[ch][o][:, h, :], NT[ch][o][:, h, :], start=True, stop=True),
                                     reads=[id(NT[ch][o]), id(NN[ch][o])], writes=[('bk', b2)])
                    for ch in range(NCH):
                        b1 = b1s[ch]
                        P.op('act', lambda e: e.copy(out=NN[ch][nw][:], in_=bv(b1)), reads=[('bk', b1)], writes=[id(NN[ch][nw])])
                        if j < 5:
                            b2 = b2s[ch]
                            P.op('dve', lambda e: e.tensor_copy(out=NT[ch][nw][:], in_=bv(b2)), reads=[('bk', b2)], writes=[id(NT[ch][nw])])
                    for ch in range(NCH):
                        b3 = b3s[ch] = nb()
                        for h in range(8):
                            P.op('pe', lambda e, h=h: e.matmul(bv(b3)[:, h, :], NN[ch][nw][:, h, :], Wt[ch][:, h, :], start=True, stop=True),
                                 reads=[id(NN[ch][nw]), id(Wt[ch])], writes=[('bk', b3)])
                    for ch in range(NCH):
                        b3 = b3s[ch]
                        P.op('dve', lambda e: e.tensor_tensor(out=Wt[ch][:], in0=Wt[ch][:], in1=bv(b3), op=ALU.add),
                             reads=[('bk', b3), id(Wt[ch])], writes=[id(Wt[ch])])
                for ch in range(NCH):
                    cs = slice(ch * 64, (ch + 1) * 64)
                    Vh = lambda h: XT[lb][:, 0, ch, h * 64:(h + 1) * 64]
                    bR = nb()
                    for h in range(8):
                        P.op('pe', lambda e, h=h: e.matmul(bv(bR)[:, h, :], XF[lb][:, 0, h, cs], Hb[s][:, h, :], start=True, stop=False),
                             reads=[('XF', lb), ('Hb', s)], writes=[('bk', bR)])
                        P.op('pe', lambda e, h=h: e.matmul(bv(bR)[:, h, :], BkT[ch][:, h, :], Vh(h), start=False, stop=True),
                             reads=[id(BkT[ch]), ('XT', lb)], writes=[('bk', bR)])
                    P.op('act', lambda e: e.copy(out=Rr[:], in_=bv(bR)), reads=[('bk', bR)], writes=['Rr'])
                    bU = nb()
                    for h in range(8):
                        P.op('pe', lambda e, h=h: e.matmul(bv(bU)[:, h, :], Wt[ch][:, h, :], Rr[:, h, :], start=True, stop=True),
                             reads=[id(Wt[ch]), 'Rr'], writes=[('bk', bU)])
                    P.op('dve', lambda e: e.tensor_scalar(out=Un[:], in0=bv(bU), scalar1=-1.0, scalar2=None, op0=ALU.mult), reads=[('bk', bU)], writes=['Un'])
                    bY = nb()
                    for h in range(8):
                        P.op('pe', lambda e, h=h: e.matmul(bv(bY)[:, h, :], XF[lb][:, 1, h, cs], Hb[s][:, h, :], start=True, stop=False),
                             reads=[('XF', lb), ('Hb', s)], writes=[('bk', bY)])
                        P.op('pe', lambda e, h=h: e.matmul(bv(bY)[:, h, :], CkT[ch][:, h, :], Vh(h), start=False, stop=False),
                             reads=[id(CkT[ch]), ('XT', lb)], writes=[('bk', bY)])
                        P.op('pe', lambda e, h=h: e.matmul(bv(bY)[:, h, :], CbT[ch][:, h, :], Un[:, h, :], start=False, stop=True),
                             reads=[id(CbT[ch]), 'Un'], writes=[('bk', bY)])
                    bH = nb()
                    for h in range(8):
                        P.op('pe', lambda e, h=h: e.matmul(bv(bH)[:, h, :], XT[lb][:, 1, ch, h * 64:(h + 1) * 64], Vh(h), start=True, stop=False),
                             reads=[('XT', lb)], writes=[('bk', bH)])
                        P.op('pe', lambda e, h=h: e.matmul(bv(bH)[:, h, :], XT[lb][:, 2, ch, h * 64:(h + 1) * 64], Un[:, h, :], start=False, stop=True),
                             reads=[('XT', lb), 'Un'], writes=[('bk', bH)])
                    for h in range(8):
                        P.op('dve', lambda e, h=h: e.scalar_tensor_tensor(out=Hs[s][:, h, :], in0=Hs[s][:, h, :], scalar=gL[lb][:, h, ch:ch + 1],
                                                                          in1=bv(bH)[:, h, :], op0=ALU.mult, op1=ALU.add),
                             reads=[('Hs', s), ('gL', lb), ('bk', bH)], writes=[('Hs', s)])
                    P.op('act', lambda e: e.copy(out=Hb[s][:], in_=Hs[s][:]), reads=[('Hs', s)], writes=[('Hb', s)])
                    P.op('dve', lambda e: e.tensor_reduce(out=st[:, 0, :], in_=bv(bY), axis=AX.X, op=ALU.add), reads=[('bk', bY)], writes=['st'])
                    P.op('act', lambda e: e.activation(out=ysq[:], in_=bv(bY), func=AF.Square), reads=[('bk', bY)], writes=['ysq'])
                    P.op('dve', lambda e: e.tensor_reduce(out=st[:, 1, :], in_=ysq[:], axis=AX.X, op=ALU.add), reads=['ysq'], writes=['st'])
                    P.op('dve', lambda e: e.tensor_scalar(out=st[:, 0, :], in0=st[:, 0, :], scalar1=1.0 / 64, scalar2=None, op0=ALU.mult), reads=['st'], writes=['st'])
                    P.op('dve', lambda e: e.tensor_tensor(out=st[:, 2, :], in0=st[:, 0, :], in1=st[:, 0, :], op=ALU.mult), reads=['st'], writes=['st'])
                    P.op('dve', lambda e: e.tensor_scalar(out=st[:, 1, :], in0=st[:, 1, :], scalar1=1.0 / 64, scalar2=64e-5, op0=ALU.mult, op1=ALU.add), reads=['st'], writes=['st'])
                    P.op('dve', lambda e: e.tensor_tensor(out=st[:, 1, :], in0=st[:, 1, :], in1=st[:, 2, :], op=ALU.subtract), reads=['st'], writes=['st'])
                    P.op('act', lambda e: e.sqrt(out=st[:, 1, :], in_=st[:, 1, :]), reads=['st'], writes=['st'])
                    P.op('dve', lambda e: e.reciprocal(out=st[:, 1, :], in_=st[:, 1, :]), reads=['st'], writes=['st'])
                    for h in range(8):
                        P.op('dve', lambda e, h=h: e.tensor_scalar(out=yn[:, h, :], in0=bv(bY)[:, h, :], scalar1=st[:, 0, h:h + 1], scalar2=st[:, 1, h:h + 1],
                                                                   op0=ALU.subtract, op1=ALU.mult),
                             reads=[('bk', bY), 'st'], writes=['yn'])
                    bT = nb()
                    for fc in range(4):
                        P.op('pe', lambda e, fc=fc: e.transpose(out=bank[bT][:, fc * 64:(fc + 1) * 64], in_=yn[:, 2 * fc:2 * fc + 2, :].rearrange("p a k -> p (a k)"), identity=Id),
                             reads=['yn', 'idf'], writes=[('bk', bT)])
                    P.op('act', lambda e: e.copy(out=yF[lb][:, :, cs], in_=bank[bT][:, 0:256].rearrange("p (f t) -> p f t", f=4)),
                         reads=[('bk', bT)], writes=[('yF', lb)])
                for fc in range(4):
                    P.op('dve', lambda e, fc=fc: e.tensor_scalar(out=yF[lb][:, fc, :], in0=yF[lb][:, fc, :], scalar1=lnp[:, 0, fc:fc + 1], scalar2=lnp[:, 1, fc:fc + 1],
                                                                 op0=ALU.mult, op1=ALU.add),
                         reads=[('yF', lb), 'lnp'], writes=[('yF', lb)])
                P.op('pool', lambda e: e.tensor_tensor(out=yF[lb][:], in0=yF[lb][:], in1=gt[lb][:], op=ALU.mult), reads=[('yF', lb), ('gt', lb)], writes=[('yF', lb)])
                P.op('pool', lambda e: e.tensor_tensor(out=mo[lb][:], in0=yF[lb][:], in1=bvt[lb][:], op=ALU.add), reads=[('yF', lb), ('bvt', lb)], writes=[('mo', lb)])
                P.dma('pool', mixT[512:1024, t0:t0 + GT].rearrange("(f p) t -> p f t", p=128), mo[lb][:], reads=[('mo', lb)])


def odd_params(inp, o):
    f = np.float32
    col = lambda v: np.ascontiguousarray(np.asarray(v, f).reshape(-1, 128).T)
    cw = np.ascontiguousarray(np.asarray(inp['ml_conv_w'][o], f).reshape(4, 8, 128).transpose(2, 1, 0))
    gbv = np.asarray(inp['ml_gate_b'][o], f)
    gb = np.stack([np.tile(gbv[:4], 2), np.tile(gbv[4:], 2)], 1)
    pc = np.stack([col(inp['rw_w0'][o]), col(inp['rw_a0'][o]), col(inp['rw_k_k'][o]), col(inp['rw_k_a'][o]),
                   col(inp['rw_r_k'][o].reshape(512))], 1)
    ln = np.stack([col(inp['rw_ln_g'][o]), col(inp['rw_ln_b'][o])], 1)
    return {
        'ml_cw': cw.astype(f), 'ml_cb': col(inp['ml_conv_b'][o]), 'ml_gb': np.ascontiguousarray(gb).astype(f),
        'ml_ng_rep': np.ascontiguousarray(np.broadcast_to(np.asarray(inp['ml_norm_g'][o], f).reshape(1, 512), (128, 512))),
        'rw_mu': col(inp['rw_mu'][o]), 'rw_pc': np.ascontiguousarray(pc).astype(f), 'rw_ln': np.ascontiguousarray(ln).astype(f),
        'rw_w2': np.ascontiguousarray(inp['rw_w2'][o]).astype(f), 'rw_a2': np.ascontiguousarray(inp['rw_a2'][o]).astype(f),
        'rw_g2': np.ascontiguousarray(inp['rw_g2'][o]).astype(f),
    }


ODD_SHAPES = {'ml_cw': [128, 8, 4], 'ml_cb': [128, 8], 'ml_gb': [8, 2], 'ml_ng_rep': [128, 512], 'rw_mu': [128, 14],
              'rw_pc': [128, 5, 4], 'rw_ln': [128, 2, 4], 'rw_w2': [64, 512], 'rw_a2': [64, 512], 'rw_g2': [128, 512]}


def odd_scratch(dt, S, nseq, pfx=""):
    T = S * nseq
    NB = S // 128
    R8 = nseq * 4
    return dict(
        mqk_raw=dt(pfx + "mqk_raw", [1024, T]), mif=dt(pfx + "mif", [8, T]), rw_raw=dt(pfx + "rw_raw", [1792, T]),
        v_aug=dt(pfx + "v_aug", [T, 516], BF16), moA=dt(pfx + "moA", [T, 512]),
        mqkT=dt(pfx + "mqkT", [1024, T], BF16), mkA=dt(pfx + "mkA", [T, 512], BF16),
        mgM=dt(pfx + "mgM", [R8, S]), mcol=dt(pfx + "mcol", [2, 128, NB * R8]), mend=dt(pfx + "mend", [NB * R8]),
        rwF=dt(pfx + "rwF", [4, 512, T], BF16), rwT=dt(pfx + "rwT", [3, T, 512], BF16), rwgL=dt(pfx + "rwgL", [512, T // 64]),
        rwbv=dt(pfx + "rwbv", [512, T]), rwg=dt(pfx + "rwg", [512, T]),
    )


def emit_odd_layer(P, K, xin, xout, w_in, w_out, gc, prm, sc, mixT, S, nseq, parts=('ml', 'rw')):
    T = S * nseq
    stage_inproj_odd(P, K, xin, w_in, gc, T, sc['mqk_raw'], sc['mif'], sc['rw_raw'], sc['v_aug'], sc['moA'])
    if 'ml' in parts:
        stage_ml_prep(P, K, sc['mqk_raw'], prm, sc['mqkT'], sc['mkA'], S, nseq)
        stage_ml_gates(P, K, sc['mif'], prm, sc['mgM'], sc['mcol'], sc['mend'], S, nseq)
        stage_ml_core(P, K, sc['mqkT'], sc['mkA'], sc['v_aug'], sc['moA'], sc['mgM'], sc['mcol'], sc['mend'], prm, mixT, S, nseq)
    if 'rw' in parts:
        stage_rw_prep(P, K, sc['rw_raw'], prm, sc['rwF'], sc['rwT'], sc['rwgL'], sc['rwbv'], sc['rwg'], S, nseq)
        stage_rw_core(P, K, sc['rwF'], sc['rwT'], sc['rwgL'], sc['rwbv'], sc['rwg'], prm, mixT, S, nseq)
    stage_outproj(P, K, mixT, w_out, xin, xout, T)


def build_test_odd(S, nseq, parts=('ml', 'rw')):
    T = S * nseq
    nc = bass.Bass("TRN2", target_bir_lowering=False)
    nc.allow_low_precision("bf16 matmul operands by design")
    dt = lambda name, shape, d=F32, kind="Internal": nc.dram_tensor(name, list(shape), d, kind=kind).ap()
    x = dt("x", [T, D], kind="ExternalInput")
    w_in = dt("w_in", [D, 3848], kind="ExternalInput")
    w_out = dt("w_out", [D, D], kind="ExternalInput")
    gc = dt("gc", [128, 8], kind="ExternalInput")
    cst = dt("consts", [128, NCONST], kind="ExternalInput")
    prm = {k: dt("p_" + k, v, kind="ExternalInput") for k, v in ODD_SHAPES.items()}
    y = dt("y", [T, D], kind="ExternalOutput")
    mixT = dt("mixT", [1024, T], BF16, kind="ExternalOutput")
    sc = odd_scratch(dt, S, nseq)
    P = Prog(nc)
    with Ctx(P) as c0:
        K = load_consts(P, c0, cst)
        emit_odd_layer(P, K, x, y, w_in, w_out, gc, prm, sc, mixT, S, nseq, parts)
    return nc

def even_params(inp, e):
    f = np.float32
    return {
        'qk_g': np.ascontiguousarray(np.stack([np.tile(inp['da_q_g'][e], 2), np.tile(inp['da_k_g'][e], 2)], 1)).astype(f),
        'sg_ln_g_rep': np.ascontiguousarray(np.broadcast_to(inp['sg_ln_g'][e].reshape(1, 512), (128, 512))).astype(f),
        'sg_ln_b_rep': np.ascontiguousarray(np.broadcast_to(inp['sg_ln_b'][e].reshape(1, 512), (128, 512))).astype(f),
        'lam_rep': np.ascontiguousarray(np.broadcast_to(inp['da_lambda'][e].reshape(1, 256), (128, 256))).astype(f),
        'subln_col': np.ascontiguousarray(inp['da_subln_g'][e].reshape(128, 1)).astype(f),
        'sg_wT': np.ascontiguousarray(inp['sg_w'][e].transpose(0, 2, 1)).astype(f),
        'sg_b_rep': np.ascontiguousarray(np.broadcast_to(inp['sg_b'][e][None, :, None, :], (128, 4, 4, 128)).reshape(128, 2048)).astype(f),
    }


EVEN_SHAPES = {'qk_g': [128, 2], 'sg_ln_g_rep': [128, 512], 'sg_ln_b_rep': [128, 512], 'lam_rep': [128, 256],
               'subln_col': [128, 1], 'sg_wT': [4, 128, 128], 'sg_b_rep': [128, 2048]}


def gcol_of(g):
    return np.ascontiguousarray(np.asarray(g, np.float32).reshape(8, 128).T)


def build_test_even(S, nseq, layer=0):
    T = S * nseq
    nc = bass.Bass("TRN2", target_bir_lowering=False)
    nc.allow_low_precision("bf16 matmul operands by design")
    dt = lambda name, shape, d=F32, kind="Internal": nc.dram_tensor(name, list(shape), d, kind=kind).ap()
    x = dt("x", [T, D], kind="ExternalInput")
    w_in = dt("w_in", [D, 2560], kind="ExternalInput")
    w_out = dt("w_out", [D, D], kind="ExternalInput")
    gc = dt("gc", [128, 8], kind="ExternalInput")
    cst = dt("consts", [128, NCONST], kind="ExternalInput")
    prm = {k: dt("p_" + k, v, kind="ExternalInput") for k, v in EVEN_SHAPES.items()}
    y = dt("y", [T, D], kind="ExternalOutput")
    qkT = dt("qkT", [1024, T], BF16)
    vA = dt("vA", [T, 512], BF16)
    uF = dt("uF", [512, T], F32)
    vG = dt("vG", [T, 512], BF16)
    mixT = dt("mixT", [1024, T], BF16)
    lam_init = 0.8 - 0.6 * math.exp(-0.3 * layer)
    P = Prog(nc)
    with Ctx(P) as c0:
        K = load_consts(P, c0, cst)
        stage_inproj_even(P, K, x, w_in, gc, prm, T, qkT, vA, uF, vG)
        stage_attn(P, K, qkT, vA, mixT, prm, lam_init, S, nseq)
        stage_sgu(P, K, vG, uF, mixT, prm, T)
        stage_outproj(P, K, mixT, w_out, x, y, T)
    return nc


def emit_even_layer(P, K, xin, xout, w_in, w_out, gc, prm, sc, mixT, S, nseq, layer):
    T = S * nseq
    lam_init = 0.8 - 0.6 * math.exp(-0.3 * layer)
    stage_inproj_even(P, K, xin, w_in, gc, prm, T, sc['qkT'], sc['vA'], sc['uF'], sc['vG'])
    stage_attn(P, K, sc['qkT'], sc['vA'], mixT, prm, lam_init, S, nseq)
    stage_sgu(P, K, sc['vG'], sc['uF'], mixT, prm, T)
    stage_outproj(P, K, mixT, w_out, xin, xout, T)


def build_full(S, nseq, depth=4):
    T = S * nseq
    nc = bass.Bass("TRN2", target_bir_lowering=False)
    nc.allow_low_precision("bf16 matmul operands by design")
    dt = lambda name, shape, d=F32, kind="Internal": nc.dram_tensor(name, list(shape), d, kind=kind).ap()
    ne, no = (depth + 1) // 2, depth // 2
    x = dt("x", [T, D], kind="ExternalInput")
    out = dt("out", [T, D], kind="ExternalOutput")
    cst = dt("consts", [128, NCONST], kind="ExternalInput")
    gmix = dt("gmix", [depth, 128, 8], kind="ExternalInput")
    gffn = dt("gffn", [depth, 128, 8], kind="ExternalInput")
    ev_w_in = dt("ev_w_in", [ne, D, 2560], kind="ExternalInput")
    ev_w_out = dt("ev_w_out", [ne, D, D], kind="ExternalInput")
    od_w_in = dt("od_w_in", [max(no, 1), D, 3848], kind="ExternalInput")
    od_w_out = dt("od_w_out", [max(no, 1), D, D], kind="ExternalInput")
    wg = dt("ffn_w_gate", [depth, D, DFF], kind="ExternalInput")
    wu = dt("ffn_w_up", [depth, D, DFF], kind="ExternalInput")
    wd = dt("ffn_w_down", [depth, DFF, D], kind="ExternalInput")
    eprm = [{k: dt(f"e{e}_{k}", v, kind="ExternalInput") for k, v in EVEN_SHAPES.items()} for e in range(ne)]
    oprm = [{k: dt(f"o{o}_{k}", v, kind="ExternalInput") for k, v in ODD_SHAPES.items()} for o in range(no)]
    xa = dt("xa", [T, D])
    xb = dt("xb", [T, D])
    mixT = dt("mixT", [1024, T], BF16)
    esc = dict(qkT=dt("qkT", [1024, T], BF16), vA=dt("vA", [T, 512], BF16), uF=dt("uF", [512, T]), vG=dt("vG", [T, 512], BF16))
    osc = odd_scratch(dt, S, nseq) if no else None
    P = Prog(nc)
    with Ctx(P) as c0:
        K = load_consts(P, c0, cst)
        xcur = x
        for l in range(depth):
            if l % 2 == 0:
                e = l // 2
                emit_even_layer(P, K, xcur, xa, ev_w_in[e], ev_w_out[e], gmix[l], eprm[e], esc, mixT, S, nseq, l)
            else:
                o = l // 2
                emit_odd_layer(P, K, xcur, xa, od_w_in[o], od_w_out[o], gmix[l], oprm[o], osc, mixT, S, nseq)
            xo = out if l == depth - 1 else xb
            stage_ffn(P, K, xa, xo, wg[l], wu[l], wd[l], gffn[l], T)
            xcur = xo
    return nc


def host_inputs(inputs, depth=4):
    f = np.float32
    ne, no = (depth + 1) // 2, depth // 2
    com = {
        "consts": host_consts(),
        "gmix": np.ascontiguousarray(np.stack([gcol_of(inputs['norm_mix_g'][l]) for l in range(depth)])),
        "gffn": np.ascontiguousarray(np.stack([gcol_of(inputs['norm_ffn_g'][l]) for l in range(depth)])),
        "ev_w_in": np.ascontiguousarray(inputs['ev_w_in'][:ne], f), "ev_w_out": np.ascontiguousarray(inputs['ev_w_out'][:ne], f),
        "od_w_in": np.ascontiguousarray(inputs['od_w_in'][:max(no, 1)], f), "od_w_out": np.ascontiguousarray(inputs['od_w_out'][:max(no, 1)], f),
        "ffn_w_gate": np.ascontiguousarray(inputs['ffn_w_gate'][:depth], f), "ffn_w_up": np.ascontiguousarray(inputs['ffn_w_up'][:depth], f),
        "ffn_w_down": np.ascontiguousarray(inputs['ffn_w_down'][:depth], f),
    }
    for e in range(ne):
        for k, v in even_params(inputs, e).items():
            com[f"e{e}_{k}"] = v
    for o in range(no):
        for k, v in odd_params(inputs, o).items():
            com[f"o{o}_{k}"] = v
    return com


_NC_CACHE = {}


def kernel(**inputs):
    inputs = {k: np.asarray(v) for k, v in inputs.items()}
    x = inputs['x']
    B, S, _ = x.shape
    ncores = 8
    nseq = B // ncores
    depth = inputs['norm_mix_g'].shape[0]
    key = (S, nseq, depth)
    if key not in _NC_CACHE:
        _NC_CACHE[key] = build_full(S, nseq, depth)
    nc = _NC_CACHE[key]
    com = host_inputs(inputs, depth)
    in_maps = []
    for i in range(ncores):
        m = dict(com)
        m["x"] = np.ascontiguousarray(x[i * nseq:(i + 1) * nseq].reshape(nseq * S, D), np.float32)
        in_maps.append(m)
    res = run_bass_kernel_spmd(nc, in_maps, core_ids=list(range(ncores)))
    outs = [np.asarray(r["out"]).reshape(nseq, S, D) for r in res.results]
    return np.concatenate(outs, 0).astype(np.float32)
```
